# Optimizing a Trainium2 kernel written in Bass

```python
import math
import jax
import jax.numpy as jnp
from jax import lax
import numpy as np

D_MODEL = 2048
BATCH = 4
SEQ = 4096
DEPTH = 2

PLE_DIM = 256
NORM_EPS = 1e-6
ROPE_THETA = 500000.0
ROPE_FRACTION = 4
NEG_INF = -1e30

SSD_HEAD_DIM = 64
SSD_INNER = D_MODEL
SSD_HEADS = SSD_INNER // SSD_HEAD_DIM
SSD_GROUPS = 8
SSD_STATE = 128
SSD_CONV_WIDTH = 4
SSD_CONV_CH = SSD_INNER + 2 * SSD_GROUPS * SSD_STATE
SSD_CHUNK = 128
SC_CHANNELS = D_MODEL
SC_WIDTH = 3
EVEN_IN_SPLITS = (SSD_INNER, SSD_CONV_CH, SSD_HEADS, SC_CHANNELS, SC_CHANNELS, SC_CHANNELS)
EVEN_IN = sum(EVEN_IN_SPLITS)
EVEN_MIX = SSD_INNER + SC_CHANNELS

HEAD_DIM = 128
NSA_HEADS = 8
NSA_KV_HEADS = 2
NSA_GROUP = NSA_HEADS // NSA_KV_HEADS
CMP_BLOCK = 32
CMP_STRIDE = 16
SEL_BLOCK = 64
SEL_TOPK = 16
SEL_FORCE = 1e4
WINDOW = 512
ATTN_QBLOCK = 128
SEL_QBLOCK = 64
DIFF_HEADS = 4
DIFF_QK_DIM = 128
DIFF_V_DIM = 256
ODD_IN_SPLITS = (NSA_HEADS * HEAD_DIM,) + (NSA_KV_HEADS * HEAD_DIM,) * 6 + (
    3 * NSA_HEADS, DIFF_HEADS * 2 * DIFF_QK_DIM, DIFF_HEADS * 2 * DIFF_QK_DIM, DIFF_HEADS * DIFF_V_DIM)
ODD_IN = sum(ODD_IN_SPLITS)
ODD_MIX = NSA_HEADS * HEAD_DIM + DIFF_HEADS * DIFF_V_DIM

MOE_GROUPS = 4
MOE_EXPERTS_PER_GROUP = 8
MOE_EXPERTS = MOE_GROUPS * MOE_EXPERTS_PER_GROUP
MOE_TOP_K = 2
MOE_FF = D_MODEL // 2
MOE_BLOCK = 128

kernel_name = 'hybrid_ssd_conv_nsa_diffattn_hmoe'


def rms_norm(x, g):
    xf = x.astype(jnp.float32)
    y = xf * lax.rsqrt(jnp.mean(xf * xf, axis=-1, keepdims=True) + NORM_EPS)
    return (y * g.astype(jnp.float32)).astype(x.dtype)


def _split(x, sizes):
    return jnp.split(x, [int(v) for v in np.cumsum(sizes)[:-1]], axis=-1)


def partial_rope(x, pos):
    rot = x.shape[-1] // ROPE_FRACTION
    half = rot // 2
    inv_freq = ROPE_THETA ** (-jnp.arange(half, dtype=jnp.float32) / half)
    ang = pos.astype(jnp.float32)[:, None] * inv_freq[None, :]
    cos, sin = jnp.cos(ang)[:, None, :], jnp.sin(ang)[:, None, :]
    xf = x.astype(jnp.float32)
    x1, x2, rest = xf[..., :half], xf[..., half:rot], xf[..., rot:]
    return jnp.concatenate([x1 * cos - x2 * sin, x2 * cos + x1 * sin, rest], axis=-1).astype(x.dtype)


def causal_dwconv(x, w):
    k, c = w.shape
    return lax.conv_general_dilated(x, w[:, None, :].astype(x.dtype), window_strides=(1,),
                                    padding=[(k - 1, 0)], dimension_numbers=('NWC', 'WIO', 'NWC'),
                                    feature_group_count=c)


def ssd_chunked(xh, dt, a, bm, cm):
    b, s, h, p = xh.shape
    g, n = bm.shape[2], bm.shape[3]
    r = h // g
    q = SSD_CHUNK
    c = s // q
    xdt = (xh * dt[..., None]).reshape(b, c, q, g, r, p)
    a_cum = jnp.cumsum((dt * a).reshape(b, c, q, g, r), axis=2)
    bc = bm.reshape(b, c, q, g, n)
    cc = cm.reshape(b, c, q, g, n)
    causal = jnp.tril(jnp.ones((q, q), bool))[None, None, :, :, None, None]
    seg = a_cum[:, :, :, None] - a_cum[:, :, None, :]
    decay = jnp.exp(jnp.where(causal, seg, -jnp.inf))
    cb = jnp.einsum('bctgn,bcsgn->bctsg', cc, bc)
    y_diag = jnp.einsum('bctsgr,bcsgrp->bctgrp', cb[..., None] * decay, xdt)
    decay_end = jnp.exp(a_cum[:, :, -1:] - a_cum)
    chunk_states = jnp.einsum('bcsgn,bcsgrp->bcgrpn', bc, xdt * decay_end[..., None])
    chunk_decay = jnp.exp(a_cum[:, :, -1])

    def step(state, inp):
        st, dec = inp
        return state * dec[..., None, None] + st, state

    _, prev = lax.scan(step, jnp.zeros_like(chunk_states[:, 0]),
                       (jnp.moveaxis(chunk_states, 1, 0), jnp.moveaxis(chunk_decay, 1, 0)))
    prev = jnp.moveaxis(prev, 0, 1)
    y_off = jnp.einsum('bctgn,bcgrpn->bctgrp', cc, prev) * jnp.exp(a_cum)[..., None]
    return (y_diag + y_off).reshape(b, s, h, p)


def ssd_shortconv_mixer(hn, w_in, conv_w, conv_b, dt_bias, a_log, d_skip, gate_norm, sc_w, w_out):
    b, s, _ = hn.shape
    f32 = jnp.float32
    z, xbc, dt, sc_b, sc_c, sc_h = _split(hn @ w_in, EVEN_IN_SPLITS)
    xbc = jax.nn.silu(causal_dwconv(xbc, conv_w) + conv_b)
    xs, bm, cm = _split(xbc, (SSD_INNER, SSD_GROUPS * SSD_STATE, SSD_GROUPS * SSD_STATE))
    xh = xs.reshape(b, s, SSD_HEADS, SSD_HEAD_DIM).astype(f32)
    dt = jax.nn.softplus(dt.astype(f32) + dt_bias.astype(f32))
    a = -jnp.exp(a_log.astype(f32))
    y = ssd_chunked(xh, dt, a,
                    bm.reshape(b, s, SSD_GROUPS, SSD_STATE).astype(f32),
                    cm.reshape(b, s, SSD_GROUPS, SSD_STATE).astype(f32))
    y = y + d_skip.astype(f32)[:, None] * xh
    y = y.reshape(b, s, SSD_INNER) * jax.nn.silu(z.astype(f32))
    y = rms_norm(y.reshape(b, s, SSD_GROUPS, -1), gate_norm.reshape(SSD_GROUPS, -1))
    y = y.reshape(b, s, SSD_INNER).astype(hn.dtype)
    y_sc = sc_b * causal_dwconv(sc_c * sc_h, sc_w)
    return jnp.concatenate([y, y_sc], axis=-1) @ w_out


def compress_blocks(k, pos_emb, w1, w2):
    b, s, g, dh = k.shape
    span = CMP_BLOCK // CMP_STRIDE
    n_chunk = s // CMP_STRIDE
    n_cmp = n_chunk - span + 1
    kc = k.reshape(b, n_chunk, CMP_STRIDE, g, dh)
    blocks = jnp.concatenate([kc[:, j:j + n_cmp] for j in range(span)], axis=2)
    blocks = blocks + pos_emb[None, None, :, None, :]
    flat = jnp.moveaxis(blocks, 3, 2).reshape(b, n_cmp, g, CMP_BLOCK * dh)
    return jax.nn.silu(flat @ w1) @ w2


def nsa_diff_mixer(hn, w_in, cmp_pos, cmp_w1, cmp_w2, lam, subln, w_out, lambda_init):
    b, s, _ = hn.shape
    f32 = jnp.float32
    G, R, Dh = NSA_KV_HEADS, NSA_GROUP, HEAD_DIM
    pos = jnp.arange(s)
    (q, k_cmp, v_cmp, k_sel, v_sel, k_win, v_win, gates, dq, dk, dv) = _split(hn @ w_in, ODD_IN_SPLITS)

    def kv(t):
        return t.reshape(b, s, G, Dh)

    q = partial_rope(q.reshape(b, s, NSA_HEADS, Dh), pos).reshape(b, s, G, R, Dh)
    scale = Dh ** -0.5

    kc = compress_blocks(kv(k_cmp), cmp_pos[0], cmp_w1[0], cmp_w2[0])
    vc = compress_blocks(kv(v_cmp), cmp_pos[1], cmp_w1[1], cmp_w2[1])
    n_cmp = kc.shape[1]
    cmp_end = jnp.arange(n_cmp) * CMP_STRIDE + CMP_BLOCK - 1
    kc = partial_rope(kc, cmp_end)
    cmp_mask = cmp_end[None, :] <= pos[:, None]
    sc = jnp.einsum('bsgrd,bngd->bgrsn', q, kc).astype(f32) * scale
    p_cmp = jnp.where(cmp_mask, jax.nn.softmax(jnp.where(cmp_mask, sc, NEG_INF), axis=-1), 0.0)
    o_cmp = jnp.einsum('bgrsn,bngd->bsgrd', p_cmp.astype(vc.dtype), vc)

    n_sel = s // SEL_BLOCK
    per_sel = SEL_BLOCK // CMP_STRIDE
    per_cmp = CMP_BLOCK // CMP_STRIDE
    imp = jnp.pad(p_cmp.sum(axis=2), ((0, 0), (0, 0), (0, 0), (per_cmp - 1, n_sel * per_sel - n_cmp)))
    p_sel = imp[..., 0:per_sel * (n_sel - 1) + 1:per_sel]
    for o in range(1, per_sel + per_cmp - 1):
        p_sel = p_sel + imp[..., o:o + per_sel * (n_sel - 1) + 1:per_sel]
    blk = jnp.arange(n_sel)
    cur = pos // SEL_BLOCK
    forced = (blk[None] == 0) | (blk[None] == cur[:, None]) | (blk[None] == cur[:, None] - 1)
    valid = blk[None] * SEL_BLOCK <= pos[:, None]
    sel_score = jnp.where(forced, SEL_FORCE, jnp.where(valid, p_sel, NEG_INF))
    n_top = min(SEL_TOPK, n_sel)
    _, sel_idx = lax.top_k(sel_score, n_top)
    ks = partial_rope(kv(k_sel), pos).reshape(b, n_sel, SEL_BLOCK, G, Dh).transpose(0, 3, 1, 2, 4)
    vs = kv(v_sel).reshape(b, n_sel, SEL_BLOCK, G, Dh).transpose(0, 3, 1, 2, 4)
    bi = jnp.arange(b)[:, None, None, None]
    gi = jnp.arange(G)[None, :, None, None]
    in_blk = jnp.arange(SEL_BLOCK)

    def sel_attend(args):
        qb, ib, tb = args
        kg = ks[bi, gi, ib]
        vg = vs[bi, gi, ib]
        scs = jnp.einsum('btgrd,bgtkud->bgrtku', qb, kg).astype(f32) * scale
        kpos = ib[..., None] * SEL_BLOCK + in_blk
        m = (kpos <= tb[None, None, :, None, None])[:, :, None]
        scs = jnp.where(m, scs, NEG_INF)
        t_, k_ = ib.shape[2], ib.shape[3]
        pr = jax.nn.softmax(scs.reshape(b, G, R, t_, k_ * SEL_BLOCK), axis=-1).reshape(scs.shape)
        return jnp.einsum('bgrtku,bgtkud->btgrd', pr.astype(vg.dtype), vg)

    nq = s // SEL_QBLOCK
    o_sel = lax.map(sel_attend, (jnp.moveaxis(q.reshape(b, nq, SEL_QBLOCK, G, R, Dh), 1, 0),
                                 jnp.moveaxis(sel_idx.reshape(b, G, nq, SEL_QBLOCK, n_top), 2, 0),
                                 pos.reshape(nq, SEL_QBLOCK)))
    o_sel = jnp.moveaxis(o_sel, 0, 1).reshape(b, s, G, R, Dh)

    qbk = ATTN_QBLOCK
    nb = s // qbk
    nw = WINDOW // qbk

    def banded(t):
        tp = jnp.pad(t, ((0, 0), (WINDOW, 0), (0, 0), (0, 0))).reshape(b, nb + nw, qbk, G, Dh)
        return jnp.concatenate([tp[:, j:j + nb] for j in range(nw + 1)], axis=2)

    kwb = banded(partial_rope(kv(k_win), pos))
    vwb = banded(kv(v_win))
    qpos = pos.reshape(nb, qbk)
    kpos_w = jnp.arange(nb)[:, None] * qbk - WINDOW + jnp.arange((nw + 1) * qbk)[None, :]
    kq = kpos_w[:, None, :]
    tq = qpos[:, :, None]
    wmask = (kq <= tq) & (kq > tq - WINDOW) & (kq >= 0)
    scw = jnp.einsum('bnqgrd,bnkgd->bngrqk', q.reshape(b, nb, qbk, G, R, Dh), kwb).astype(f32) * scale
    pw = jax.nn.softmax(jnp.where(wmask[None, :, None, None], scw, NEG_INF), axis=-1)
    o_win = jnp.einsum('bngrqk,bnkgd->bnqgrd', pw.astype(vwb.dtype), vwb).reshape(b, s, G, R, Dh)

    g = jax.nn.sigmoid(gates.astype(f32)).reshape(b, s, G, R, 3)
    o_nsa = g[..., 0:1] * o_cmp + g[..., 1:2] * o_sel + g[..., 2:3] * o_win
    o_nsa = o_nsa.astype(hn.dtype).reshape(b, s, NSA_HEADS * Dh)

    Hd, Dd = DIFF_HEADS, DIFF_QK_DIM
    dq = partial_rope(dq.reshape(b, s, Hd * 2, Dd), pos).reshape(b, s, Hd, 2, Dd)
    dk = partial_rope(dk.reshape(b, s, Hd * 2, Dd), pos).reshape(b, s, Hd, 2, Dd)
    dv = dv.reshape(b, s, Hd, DIFF_V_DIM)
    lamf = lam.astype(f32)
    lam_full = jnp.exp(jnp.sum(lamf[0] * lamf[1])) - jnp.exp(jnp.sum(lamf[2] * lamf[3])) + lambda_init
    dscale = Dd ** -0.5

    def diff_attend(args):
        qb, tb = args
        scd = jnp.einsum('bthmd,bshmd->bhmts', qb, dk).astype(f32) * dscale
        scd = jnp.where(pos[None, :] <= tb[:, None], scd, NEG_INF)
        pr = jax.nn.softmax(scd, axis=-1)
        w = pr[:, :, 0] - lam_full * pr[:, :, 1]
        return jnp.einsum('bhts,bshe->bthe', w.astype(dv.dtype), dv)

    o_diff = lax.map(diff_attend, (jnp.moveaxis(dq.reshape(b, nb, qbk, Hd, 2, Dd), 1, 0), qpos))
    o_diff = jnp.moveaxis(o_diff, 0, 1).reshape(b, s, Hd, DIFF_V_DIM)
    o_diff = (rms_norm(o_diff, subln) * (1.0 - lambda_init)).astype(hn.dtype).reshape(b, s, Hd * DIFF_V_DIM)
    return jnp.concatenate([o_nsa, o_diff], axis=-1) @ w_out


def hier_moe(hn, w_group, b_group, w_expert, b_expert, w_gate, w_up, w_down):
    b, s, d = hn.shape
    f32 = jnp.float32
    xt = hn.reshape(b * s, d)
    n = xt.shape[0]
    g_logits = (xt @ w_group).astype(f32) + b_group.astype(f32)
    g_prob = jax.nn.softmax(g_logits, axis=-1)
    g_top = jnp.argmax(g_logits, axis=-1)
    e_logits = ((xt @ w_expert).astype(f32) + b_expert.astype(f32)).reshape(n, MOE_GROUPS, MOE_EXPERTS_PER_GROUP)
    e_logits = jnp.take_along_axis(e_logits, g_top[:, None, None], axis=1)[:, 0]
    top_p, top_i = lax.top_k(jax.nn.softmax(e_logits, axis=-1), MOE_TOP_K)
    gate_w = jnp.take_along_axis(g_prob, g_top[:, None], axis=1) * top_p / jnp.sum(top_p, axis=-1, keepdims=True)
    expert_id = (g_top[:, None] * MOE_EXPERTS_PER_GROUP + top_i).reshape(-1)
    gate_w = gate_w.reshape(-1)
    n_assign = n * MOE_TOP_K
    token_id = jnp.arange(n_assign) // MOE_TOP_K
    order = jnp.argsort(expert_id)
    e_sorted = expert_id[order]
    counts = jnp.bincount(expert_id, length=MOE_EXPERTS)
    padded = (counts + MOE_BLOCK - 1) // MOE_BLOCK * MOE_BLOCK
    pad_end = jnp.cumsum(padded)
    slot = (pad_end - padded)[e_sorted] + jnp.arange(n_assign) - (jnp.cumsum(counts) - counts)[e_sorted]
    n_blocks = -(-(n_assign + MOE_EXPERTS * (MOE_BLOCK - 1)) // MOE_BLOCK)
    cap = n_blocks * MOE_BLOCK
    slot_tok = jnp.zeros((cap,), jnp.int32).at[slot].set(token_id[order])
    slot_w = jnp.zeros((cap,), f32).at[slot].set(gate_w[order])
    blk_expert = jnp.minimum(jnp.searchsorted(pad_end, jnp.arange(n_blocks) * MOE_BLOCK, side='right'),
                             MOE_EXPERTS - 1)

    def run(args):
        xb, e = args
        return (jax.nn.silu(xb @ w_gate[e]) * (xb @ w_up[e])) @ w_down[e]

    yb = lax.map(run, (xt[slot_tok].reshape(n_blocks, MOE_BLOCK, d), blk_expert)).reshape(cap, d)
    y = jnp.zeros_like(xt).at[slot_tok].add(yb * slot_w[:, None].astype(yb.dtype))
    return y.reshape(b, s, d)


def setup_inputs(seed: int = 0) -> dict:
    key = jax.random.key(seed)
    keys = iter(jax.random.split(key, 64))
    f32 = jnp.float32

    def normal(shape, scale):
        return jax.random.normal(next(keys), shape, f32) * scale

    def gain(shape):
        return 1.0 + normal(shape, 0.01)

    ne, no = (DEPTH + 1) // 2, DEPTH // 2
    dt0 = jnp.exp(jax.random.uniform(next(keys), (ne, SSD_HEADS), f32, math.log(1e-3), math.log(1e-1)))
    a0 = jax.random.uniform(next(keys), (ne, SSD_HEADS), f32, 1.0, 16.0)
    return {
        'x': normal((BATCH, SEQ, D_MODEL), 1.0),
        'p': normal((DEPTH, BATCH, SEQ, PLE_DIM), 1.0),
        'norm_mix': gain((DEPTH, D_MODEL)),
        'norm_ffn': gain((DEPTH, D_MODEL)),
        'norm_ple': gain((DEPTH, D_MODEL)),
        'norm_final': gain((D_MODEL,)),
        'ev_w_in': normal((ne, D_MODEL, EVEN_IN), D_MODEL ** -0.5),
        'ev_conv_w': normal((ne, SSD_CONV_WIDTH, SSD_CONV_CH), SSD_CONV_WIDTH ** -0.5),
        'ev_conv_b': normal((ne, SSD_CONV_CH), 0.01),
        'ev_dt_bias': dt0 + jnp.log(-jnp.expm1(-dt0)),
        'ev_a_log': jnp.log(a0),
        'ev_d_skip': gain((ne, SSD_HEADS)),
        'ev_gate_norm': gain((ne, SSD_INNER)),
        'ev_sc_w': normal((ne, SC_WIDTH, SC_CHANNELS), SC_WIDTH ** -0.5),
        'ev_w_out': normal((ne, EVEN_MIX, D_MODEL), EVEN_MIX ** -0.5),
        'od_w_in': normal((no, D_MODEL, ODD_IN), D_MODEL ** -0.5),
        'od_cmp_pos': normal((no, 2, CMP_BLOCK, HEAD_DIM), 0.02),
        'od_cmp_w1': normal((no, 2, CMP_BLOCK * HEAD_DIM, HEAD_DIM), (CMP_BLOCK * HEAD_DIM) ** -0.5),
        'od_cmp_w2': normal((no, 2, HEAD_DIM, HEAD_DIM), HEAD_DIM ** -0.5),
        'od_lambda': normal((no, 4, DIFF_QK_DIM), 0.1),
        'od_subln': gain((no, DIFF_V_DIM)),
        'od_w_out': normal((no, ODD_MIX, D_MODEL), ODD_MIX ** -0.5),
        'moe_w_group': normal((DEPTH, D_MODEL, MOE_GROUPS), D_MODEL ** -0.5),
        'moe_b_group': normal((DEPTH, MOE_GROUPS), 0.01),
        'moe_w_expert': normal((DEPTH, D_MODEL, MOE_EXPERTS), D_MODEL ** -0.5),
        'moe_b_expert': normal((DEPTH, MOE_EXPERTS), 0.01),
        'moe_w_gate': normal((DEPTH, MOE_EXPERTS, D_MODEL, MOE_FF), D_MODEL ** -0.5),
        'moe_w_up': normal((DEPTH, MOE_EXPERTS, D_MODEL, MOE_FF), D_MODEL ** -0.5),
        'moe_w_down': normal((DEPTH, MOE_EXPERTS, MOE_FF, D_MODEL), MOE_FF ** -0.5),
        'ple_gate': normal((DEPTH, D_MODEL, D_MODEL), D_MODEL ** -0.5),
        'ple_proj': normal((DEPTH, PLE_DIM, D_MODEL), PLE_DIM ** -0.5),
    }


def reference(x, p, norm_mix, norm_ffn, norm_ple, norm_final,
              ev_w_in, ev_conv_w, ev_conv_b, ev_dt_bias, ev_a_log, ev_d_skip, ev_gate_norm, ev_sc_w, ev_w_out,
              od_w_in, od_cmp_pos, od_cmp_w1, od_cmp_w2, od_lambda, od_subln, od_w_out,
              moe_w_group, moe_b_group, moe_w_expert, moe_b_expert, moe_w_gate, moe_w_up, moe_w_down,
              ple_gate, ple_proj):
    h = x
    for i in range(DEPTH):
        j = i // 2
        hn = rms_norm(h, norm_mix[i])
        if i % 2 == 0:
            mix = ssd_shortconv_mixer(hn, ev_w_in[j], ev_conv_w[j], ev_conv_b[j], ev_dt_bias[j], ev_a_log[j],
                                      ev_d_skip[j], ev_gate_norm[j], ev_sc_w[j], ev_w_out[j])
        else:
            lambda_init = 0.8 - 0.6 * math.exp(-0.3 * i)
            mix = nsa_diff_mixer(hn, od_w_in[j], od_cmp_pos[j], od_cmp_w1[j], od_cmp_w2[j], od_lambda[j],
                                 od_subln[j], od_w_out[j], lambda_init)
        h = h + mix
        h = h + hier_moe(rms_norm(h, norm_ffn[i]), moe_w_group[i], moe_b_group[i], moe_w_expert[i],
                         moe_b_expert[i], moe_w_gate[i], moe_w_up[i], moe_w_down[i])
        gate = jax.nn.sigmoid(rms_norm(h, norm_ple[i]) @ ple_gate[i])
        h = h + gate * (p[i] @ ple_proj[i])
    return rms_norm(h, norm_final)
```

```python
import math
from contextlib import ExitStack
import numpy as np
import concourse.bass as bass
import concourse.mybir as mybir
from concourse.bass_utils import run_bass_kernel_spmd

F32 = mybir.dt.float32
BF16 = mybir.dt.bfloat16
I32 = mybir.dt.int32
AF = mybir.ActivationFunctionType
ALU = mybir.AluOpType
AX = mybir.AxisListType

D = 2048
NKC = D // 128
EPS = 1e-6
NDS = 48


class Stamp:
    __slots__ = ("sem", "val", "eng", "name")

    def __init__(self, sem, val, eng, name):
        self.sem, self.val, self.eng, self.name = sem, val, eng, name


class K:
    def __init__(self, nc):
        self.nc = nc
        self.stack = ExitStack()
        self.gstack = self.stack
        self.eng = {"pe": nc.tensor, "act": nc.scalar, "dve": nc.vector, "pool": nc.gpsimd, "sp": nc.sync}
        self.nsem = 0
        self.esem = {}
        self.ecnt = {}
        for e in self.eng:
            self._new_esem(e)
        self.waited = {e: {} for e in self.eng}
        self.dsem = [self._sem("d%d" % i) for i in range(NDS)]
        self.dcnt = [0] * NDS
        self.dlast = [None] * NDS
        self.dnext = 0
        self.trk = {}
        self.uid = 0
        self.ninst = 0

    def _sem(self, name):
        self.nsem += 1
        return (self.gstack.enter_context(self.nc.semaphore(name)), name)

    def _new_esem(self, e):
        self.esem[e] = self._sem("e_%s_%d" % (e, self.nsem))
        self.ecnt[e] = 0

    @staticmethod
    def keys(base, n):
        return ["%s_%d" % (base, i) for i in range(n)]

    def name(self, p):
        self.uid += 1
        return "%s_%d" % (p, self.uid)

    def sb(self, shape, dt, name="t"):
        return self.stack.enter_context(self.nc.sbuf_tensor(self.name(name), list(shape), dt))

    def wait(self, e, st):
        if st is None:
            return
        if self.waited[e].get(st.name, 0) >= st.val:
            return
        self.eng[e].wait_ge(st.sem, st.val)
        self.waited[e][st.name] = st.val
        self.ninst += 1

    def _deps(self, e, r, w):
        for k in r:
            t = self.trk.get(k)
            if t is not None and t[0] is not None:
                if not (e == "pe" and t[0].eng == "pe"):
                    self.wait(e, t[0])
        for k in w:
            t = self.trk.get(k)
            if t is not None:
                if t[0] is not None and not (e == "pe" and t[0].eng == "pe"):
                    self.wait(e, t[0])
                for st in t[1].values():
                    if not (e == "pe" and st.eng == "pe"):
                        self.wait(e, st)

    def _mark(self, st, r, w):
        for k in r:
            t = self.trk.setdefault(k, [None, {}])
            t[1][st.name] = st
        for k in w:
            self.trk[k] = [st, {}]

    def op(self, e, fn, r=(), w=()):
        self._deps(e, r, w)
        ins = fn(self.eng[e])
        if self.ecnt[e] >= 30000:
            self._new_esem(e)
        self.ecnt[e] += 1
        sem, name = self.esem[e]
        ins.then_inc(sem, 1)
        st = Stamp(sem, self.ecnt[e], e, name)
        self._mark(st, r, w)
        self.ninst += 1
        return st

    def dma(self, e, fn, r=(), w=()):
        self._deps(e, r, w)
        j = self.dnext
        self.dnext = (self.dnext + 1) % NDS
        self.wait(e, self.dlast[j])
        ins = fn(self.eng[e])
        sem, name = self.dsem[j]
        self.dcnt[j] += 16
        ins.then_inc(sem, 16)
        st = Stamp(sem, self.dcnt[j], "dma", name)
        self.dlast[j] = st
        self._mark(st, r, w)
        self.ninst += 1
        return st

    def barrier(self):
        lasts = []
        for e in self.eng:
            if self.ecnt[e] > 0:
                sem, name = self.esem[e]
                lasts.append(Stamp(sem, self.ecnt[e], e, name))
        for j in range(NDS):
            if self.dlast[j] is not None:
                lasts.append(self.dlast[j])
        for e in self.eng:
            for st in lasts:
                if st.eng == e:
                    continue
                self.wait(e, st)
        self.trk = {}

    def load(self, out, in_, r=(), w=(), e="sp"):
        return self.dma(e, lambda q: q.dma_start(out=out, in_=in_), r=r, w=w)

    def loadT(self, out, in_, r=(), w=(), e="sp"):
        return self.dma(e, lambda q: q.dma_start_transpose(out=out, in_=in_), r=r, w=w)

    def cast_load(self, out, in_, r=(), w=()):
        return self.dma("pool", lambda q: q.dma_start(out=out, in_=in_), r=r, w=w)

    def mm(self, out, lhsT, rhs, start, stop, r=(), w=()):
        return self.op("pe", lambda q: q.matmul(out, lhsT=lhsT, rhs=rhs, start=start, stop=stop), r=r, w=w)

    def act(self, out, in_, func, r=(), w=(), **kw):
        return self.op("act", lambda q: q.activation(out=out, in_=in_, func=func, **kw), r=r, w=w)

    def ts(self, out, in0, s1, s2, op0, op1=None, r=(), w=(), e="dve", **kw):
        if op1 is None:
            return self.op(e, lambda q: q.tensor_scalar(out=out, in0=in0, scalar1=s1, scalar2=None, op0=op0, **kw), r=r, w=w)
        return self.op(e, lambda q: q.tensor_scalar(out=out, in0=in0, scalar1=s1, scalar2=s2, op0=op0, op1=op1, **kw), r=r, w=w)

    def stt(self, out, in0, scalar, in1, op0, op1, r=(), w=(), e="dve"):
        return self.op(e, lambda q: q.scalar_tensor_tensor(out=out, in0=in0, scalar=scalar, in1=in1, op0=op0, op1=op1), r=r, w=w)

    def tt(self, out, in0, in1, op, r=(), w=(), e="dve"):
        return self.op(e, lambda q: q.tensor_tensor(out=out, in0=in0, in1=in1, op=op), r=r, w=w)

    def copy(self, out, in_, r=(), w=(), e="dve"):
        if e == "act":
            return self.op("act", lambda q: q.copy(out=out, in_=in_), r=r, w=w)
        return self.op(e, lambda q: q.tensor_copy(out=out, in_=in_), r=r, w=w)

    def memset(self, ap, v, w=(), e="dve"):
        return self.op(e, lambda q: q.memset(ap, v), w=w)


class Ctx:
    pass


def bcast_rows(ap1d_row, n):
    return ap1d_row.to_broadcast([128, n])


def make_consts(k, c):
    nc = k.nc
    c.ones_f = k.sb([128, 128], F32, "ones_f")
    c.ones_b = k.sb([128, 128], BF16, "ones_b")
    c.tri_incl_f = k.sb([128, 128], F32, "tri_incl")
    c.tri_strict_b = k.sb([128, 128], BF16, "tri_strict")
    c.mask_gt_f = k.sb([128, 128], F32, "mask_gt")
    c.ident_f = k.sb([128, 128], F32, "ident_f")
    c.iota_p = k.sb([128, 1], F32, "iota_p")
    k.memset(c.ones_f[:], 1.0, w=["ones_f"])
    k.memset(c.ones_b[:], 1.0, w=["ones_b"])
    k.op("pool", lambda q: q.affine_select(out=c.tri_incl_f[:], in_=c.ones_f[:], pattern=[[1, 128]],
                                            compare_op=ALU.is_ge, fill=0.0, base=0, channel_multiplier=-1),
         r=["ones_f"], w=["tri_incl"])
    k.op("pool", lambda q: q.affine_select(out=c.tri_strict_b[:], in_=c.ones_b[:], pattern=[[1, 128]],
                                            compare_op=ALU.is_gt, fill=0.0, base=0, channel_multiplier=-1),
         r=["ones_b"], w=["tri_strict"])
    k.op("pool", lambda q: q.affine_select(out=c.mask_gt_f[:], in_=c.ones_f[:], pattern=[[-1, 128]],
                                            compare_op=ALU.is_gt, fill=0.0, base=0, channel_multiplier=1),
         r=["ones_f"], w=["mask_gt"])
    k.op("pool", lambda q: q.affine_select(out=c.ident_f[:], in_=c.ones_f[:], pattern=[[-1, 128]],
                                            compare_op=ALU.is_equal, fill=0.0, base=0, channel_multiplier=1),
         r=["ones_f"], w=["ident_f"])
    k.op("pool", lambda q: q.iota(c.iota_p[:], pattern=[[0, 1]], base=0, channel_multiplier=1,
                                  allow_small_or_imprecise_dtypes=True), w=["iota_p"])


def rsqrt(k, out, in_, scale, eps, r, w):
    k.act(out, in_, AF.Sqrt, r=r, w=w, scale=scale, bias=eps)
    k.op("dve", lambda q: q.reciprocal(out=out, in_=out), r=w, w=w)


def rmsnorm_tile(k, xt, gbc, out, ss, rstd, junk, keys_r, keys_w, tag):
    k.act(junk, xt, AF.Square, r=keys_r, w=[tag + "junk", tag + "ss"], accum_out=ss)
    rsqrt(k, rstd, ss, 1.0 / D, EPS, [tag + "ss"], [tag + "rstd"])
    k.stt(out, xt, rstd, gbc, ALU.mult, ALU.mult, r=list(keys_r) + [tag + "rstd"], w=keys_w)


def norm_to_dram(k, c, src, g_row, dst_bf, T, tag, dst_f32=None):
    with ExitStack() as es:
        k.stack, old = es, k.stack
        gbc = k.sb([128, D], F32, "gbc")
        xt = [k.sb([128, D], F32, "nx%d" % i) for i in range(2)]
        ob = [k.sb([128, D], BF16, "no%d" % i) for i in range(2)]
        of = [k.sb([128, D], F32, "nf%d" % i) for i in range(2)] if dst_f32 is not None else None
        junk = k.sb([128, D], BF16, "njunk")
        ss = k.sb([128, 1], F32, "nss")
        rstd = k.sb([128, 1], F32, "nrstd")
        k.load(gbc[:], bcast_rows(g_row, D), w=[tag + "g"])
        for j in range(T // 128):
            b = j % 2
            k.load(xt[b][:], src[j * 128:(j + 1) * 128, :], w=[tag + "x%d" % b])
            if dst_f32 is not None:
                rmsnorm_tile(k, xt[b][:], gbc[:], of[b][:], ss[:], rstd[:], junk[:],
                             [tag + "x%d" % b, tag + "g"], [tag + "of%d" % b], tag)
                k.copy(ob[b][:], of[b][:], r=[tag + "of%d" % b], w=[tag + "o%d" % b], e="act")
                k.load(dst_f32[j * 128:(j + 1) * 128, :], of[b][:], r=[tag + "of%d" % b])
            else:
                rmsnorm_tile(k, xt[b][:], gbc[:], ob[b][:], ss[:], rstd[:], junk[:],
                             [tag + "x%d" % b, tag + "g"], [tag + "o%d" % b], tag)
            k.load(dst_bf[j * 128:(j + 1) * 128, :], ob[b][:], r=[tag + "o%d" % b])
        k.barrier()
        k.stack = old


def load_actT(k, dst_tile, src_tm, t0, nt, tag, r=()):
    for kc in range(NKC):
        for s0 in range(0, nt, 512):
            n = min(512, nt - s0)
            k.loadT(dst_tile[:, kc, s0:s0 + n], src_tm[t0 + s0:t0 + s0 + n, kc * 128:(kc + 1) * 128],
                    r=r, w=[tag])


def linear_stage(k, aT_src_tm, T, W, col_specs, tag, kdim=D):
    nkc = kdim // 128
    TS = min(T, 2048)
    with ExitStack() as es:
        k.stack, old = es, k.stack
        aT = k.sb([128, nkc, TS], BF16, "aT")
        wb = [k.sb([128, nkc, 512], BF16, "wb%d" % i) for i in range(2)]
        ot = [k.sb([128, 512], F32, "ot%d" % i) for i in range(2)]
        ob = [k.sb([128, 512], BF16, "ob%d" % i) for i in range(2)]
        ofm = [k.sb([128, TS], BF16, "ofm%d" % i) for i in range(2)]
        ps = [k.stack.enter_context(k.nc.psum_tensor(k.name("lps"), [128, 512], F32)) for _ in range(4)]
        blocks = []
        for (c0, ncols, mode, dst, doff, ddt) in col_specs:
            for b0 in range(0, ncols, 512):
                blocks.append((c0 + b0, min(512, ncols - b0), mode, dst, doff + b0, ddt))
        Wv = W.rearrange("(kc p) n -> p kc n", p=128)
        wi = 0
        pi = 0
        oi = 0
        fi = 0
        for st in range(T // TS):
            t0 = st * TS
            for kc in range(nkc):
                for s0 in range(0, TS, 512):
                    k.loadT(aT[:, kc, s0:s0 + 512], aT_src_tm[t0 + s0:t0 + s0 + 512, kc * 128:(kc + 1) * 128],
                            w=[tag + "aT_%d_%d" % (kc, s0 // 512)])
            for (c0, ncols, mode, dst, doff, ddt) in blocks:
                wbuf = wb[wi % 2]
                wkey = tag + "w%d" % (wi % 2)
                wi += 1
                k.cast_load(wbuf[:, :, 0:ncols], Wv[:, :, c0:c0 + ncols], w=[wkey])
                if mode == "tm":
                    for j in range(TS // 128):
                        p = ps[pi % 4]
                        pkey = tag + "ps%d" % (pi % 4)
                        pi += 1
                        for kc in range(nkc):
                            k.mm(p[:, 0:ncols], aT[:, kc, j * 128:(j + 1) * 128], wbuf[:, kc, 0:ncols],
                                 kc == 0, kc == nkc - 1, r=[tag + "aT_%d_%d" % (kc, j // 4), wkey], w=[pkey])
                        if ddt == F32:
                            o = ot[oi % 2]
                            okey = tag + "ot%d" % (oi % 2)
                        else:
                            o = ob[oi % 2]
                            okey = tag + "ob%d" % (oi % 2)
                        oi += 1
                        k.copy(o[:, 0:ncols], p[:, 0:ncols], r=[pkey], w=[okey], e="act")
                        k.load(dst[t0 + j * 128:t0 + (j + 1) * 128, doff:doff + ncols], o[:, 0:ncols], r=[okey])
                else:
                    for m0 in range(0, ncols, 128):
                        o = ofm[fi % 2]
                        okey = tag + "ofm%d" % (fi % 2)
                        fi += 1
                        for n0 in range(0, TS, 512):
                            p = ps[pi % 4]
                            pkey = tag + "ps%d" % (pi % 4)
                            pi += 1
                            for kc in range(nkc):
                                k.mm(p[:, :], wbuf[:, kc, m0:m0 + 128], aT[:, kc, n0:n0 + 512],
                                     kc == 0, kc == nkc - 1, r=[tag + "aT_%d_%d" % (kc, n0 // 512), wkey], w=[pkey])
                            eng = "act" if (n0 // 512) % 2 == 0 else "dve"
                            k.copy(o[:, n0:n0 + 512], p[:, :], r=[pkey], w=[okey], e=eng)
                        k.load(dst[doff + m0:doff + m0 + 128, t0:t0 + TS], o[:, :], r=[okey])
        k.barrier()
        k.stack = old


def conv_stage(k, c, T, xbcT, xbcT2, conv_w, conv_b, scT, sc_w, y_scT):
    with ExitStack() as es:
        k.stack, old = es, k.stack
        cw = k.sb([128, 128], F32, "cw")
        cb = k.sb([128, 32], F32, "cb")
        sw = k.sb([128, 48], F32, "sw")
        craw = k.sb([128, 128], F32, "craw")
        braw = k.sb([32, 128], F32, "braw")
        sraw = k.sb([48, 128], F32, "sraw")
        cps = k.stack.enter_context(k.nc.psum_tensor(k.name("cps"), [128, 512], F32))
        xin = [k.sb([128, 3 + T], BF16, "xin%d" % i) for i in range(2)]
        acc = [k.sb([128, T], F32, "acc%d" % i) for i in range(2)]
        ob = [k.sb([128, T], BF16, "cob%d" % i) for i in range(2)]
        tb = [k.sb([128, T], BF16, "ctb%d" % i) for i in range(2)]
        th = [k.sb([128, T], BF16, "cth%d" % i) for i in range(2)]
        k.load(craw[:], conv_w.rearrange("k (cc p) -> (k cc) p", p=128), w=["craw"])
        k.load(braw[:], conv_b.rearrange("o (cc p) -> (o cc) p", p=128), w=["braw"])
        k.load(sraw[:], sc_w.rearrange("k (cc p) -> (k cc) p", p=128), w=["sraw"])
        k.mm(cps[:, 0:128], craw[:], c.ident_f[:], True, True, r=["craw", "ident_f"], w=["cps"])
        k.mm(cps[:, 128:160], braw[:], c.ident_f[0:32, 0:32], True, True, r=["braw", "ident_f"], w=["cps"])
        k.mm(cps[:, 160:208], sraw[:], c.ident_f[0:48, 0:48], True, True, r=["sraw", "ident_f"], w=["cps"])
        k.copy(cw[:], cps[:, 0:128], r=["cps"], w=["cw"])
        k.copy(cb[:], cps[:, 128:160], r=["cps"], w=["cb"])
        k.copy(sw[:], cps[:, 160:208], r=["cps"], w=["sw"])
        for i in range(2):
            k.memset(xin[i][:, 0:3], 0.0, w=["xin%d" % i])
        for cc in range(32):
            b = cc % 2
            k.load(xin[b][:, 3:3 + T], xbcT[cc * 128:(cc + 1) * 128, :], w=["xin%d" % b])
            k.ts(acc[b][:], xin[b][:, 0:T], cw[:, cc:cc + 1], None, ALU.mult, r=["xin%d" % b, "cw"], w=["acc%d" % b])
            for kk in range(1, 4):
                k.stt(acc[b][:], xin[b][:, kk:kk + T], cw[:, kk * 32 + cc:kk * 32 + cc + 1], acc[b][:], ALU.mult, ALU.add,
                      r=["xin%d" % b, "cw", "acc%d" % b], w=["acc%d" % b])
            k.act(ob[b][:], acc[b][:], AF.Silu, r=["acc%d" % b, "cb"], w=["cob%d" % b], bias=cb[:, cc:cc + 1])
            k.load(xbcT2[cc * 128:(cc + 1) * 128, :], ob[b][:], r=["cob%d" % b])
        for cc in range(16):
            b = cc % 2
            k.load(tb[b][:], scT[cc * 128:(cc + 1) * 128, :], w=["tb%d" % b])
            k.load(xin[b][:, 3:3 + T], scT[2048 + cc * 128:2048 + (cc + 1) * 128, :], w=["xin%d" % b])
            k.load(th[b][:], scT[4096 + cc * 128:4096 + (cc + 1) * 128, :], w=["th%d" % b])
            k.tt(xin[b][:, 3:3 + T], xin[b][:, 3:3 + T], th[b][:], ALU.mult, r=["xin%d" % b, "th%d" % b], w=["xin%d" % b])
            k.ts(acc[b][:], xin[b][:, 1:1 + T], sw[:, cc:cc + 1], None, ALU.mult, r=["xin%d" % b, "sw"], w=["acc%d" % b])
            for kk in range(1, 3):
                k.stt(acc[b][:], xin[b][:, 1 + kk:1 + kk + T], sw[:, kk * 16 + cc:kk * 16 + cc + 1], acc[b][:], ALU.mult, ALU.add,
                      r=["xin%d" % b, "sw", "acc%d" % b], w=["acc%d" % b])
            k.tt(ob[b][:], acc[b][:], tb[b][:], ALU.mult, r=["acc%d" % b, "tb%d" % b], w=["cob%d" % b])
            k.load(y_scT[cc * 128:(cc + 1) * 128, :], ob[b][:], r=["cob%d" % b])
        k.barrier()
        k.stack = old


def ssd_stage(k, c, T, xbcT2, dt_d, z_d, y_tm, dt_bias, a_log, d_skip, gate_norm):
    NCH = T // 128
    with ExitStack() as es:
        k.stack, old = es, k.stack
        nc = k.nc
        sbt = k.sb
        dtb_bc = sbt([128, 32], F32, "dtb")
        a_bc = sbt([128, 32], F32, "abc")
        dsk_bc = sbt([128, 32], F32, "dsk")
        gn_bc = sbt([128, D], F32, "gnbc")
        k.load(dtb_bc[:], bcast_rows(dt_bias, 32), w=["dtb"])
        k.load(a_bc[:], bcast_rows(a_log, 32), w=["abc"])
        k.load(dsk_bc[:], bcast_rows(d_skip, 32), w=["dsk"])
        k.load(gn_bc[:], bcast_rows(gate_norm, D), w=["gnbc"])
        k.act(a_bc[:], a_bc[:], AF.Exp, r=["abc"], w=["abc"])
        k.ts(a_bc[:], a_bc[:], -1.0, None, ALU.mult, r=["abc"], w=["abc"])
        NB = 2
        xs = [sbt([128, D], BF16, "xs%d" % i) for i in range(NB)]
        Btm = [sbt([128, 1024], BF16, "Btm%d" % i) for i in range(NB)]
        BT = [sbt([128, 8, 128], BF16, "BT%d" % i) for i in range(NB)]
        CT = [sbt([128, 8, 128], BF16, "CT%d" % i) for i in range(NB)]
        dtr = [sbt([128, 32], F32, "dtr%d" % i) for i in range(NB)]
        zt = [sbt([128, D], BF16, "zt%d" % i) for i in range(NB)]
        dtp = sbt([128, 32], F32, "dtp")
        dA = sbt([128, 32], F32, "dA")
        cum = sbt([128, 64], F32, "cum")
        ea = sbt([128, 32], F32, "ea")
        cd = sbt([128, 32], F32, "cd")
        wgt = sbt([128, 32], F32, "wgt")
        G = sbt([128, 128], F32, "G")
        L = [sbt([128, 128], F32, "L%d" % i) for i in range(2)]
        E = [sbt([128, 128], F32, "E%d" % i) for i in range(2)]
        MT = [sbt([128, 128], BF16, "MT%d" % i) for i in range(2)]
        xw = [sbt([128, 256], BF16, "xw%d" % i) for i in range(2)]
        ydsb = [sbt([128, 256], F32, "ydsb%d" % i) for i in range(2)]
        tmp = sbt([128, 256], F32, "ytmp")
        yf = sbt([128, D], F32, "yf")
        sz = sbt([128, D], F32, "sz")
        sq = sbt([128, D], F32, "sq")
        ss8 = sbt([128, 8], F32, "ss8")
        yo = [sbt([128, D], BF16, "yo%d" % i) for i in range(2)]
        state_f = sbt([128, 8, 256], F32, "state_f")
        state_b = sbt([128, 8, 256], BF16, "state_b")
        k.memset(state_f[:], 0.0, w=["state_f%d" % g for g in range(8)])
        k.memset(state_b[:], 0.0, w=["state_b%d" % g for g in range(8)])
        P = lambda nm: k.stack.enter_context(nc.psum_tensor(k.name(nm), [128, 512], F32))
        ps_cum = P("ps_cum")
        ps_cb = [P("ps_cb0"), P("ps_cb1")]
        ps_seg = [P("ps_seg0"), P("ps_seg1")]
        ps_y = [P("ps_y0"), P("ps_y1")]
        ps_st = P("ps_st")

        def issue_loads(ch):
            b = ch % NB
            t0 = ch * 128
            for kc in range(16):
                k.loadT(xs[b][:, kc * 128:(kc + 1) * 128], xbcT2[kc * 128:(kc + 1) * 128, t0:t0 + 128], w=["xs%d_%d" % (b, kc)])
            for kc in range(8):
                k.loadT(Btm[b][:, kc * 128:(kc + 1) * 128], xbcT2[2048 + kc * 128:2048 + (kc + 1) * 128, t0:t0 + 128],
                        w=["Btm%d_%d" % (b, kc)])
            k.load(BT[b][:], xbcT2[2048:3072, t0:t0 + 128].rearrange("(g n) t -> n g t", n=128), w=["BT%d" % b])
            k.load(CT[b][:], xbcT2[3072:4096, t0:t0 + 128].rearrange("(g n) t -> n g t", n=128), w=["CT%d" % b])
            k.load(dtr[b][:], dt_d[t0:t0 + 128, :], w=["dtr%d" % b])
            k.load(zt[b][:], z_d[t0:t0 + 128, :], w=["zt%d" % b])

        issue_loads(0)
        hi = 0
        for ch in range(NCH):
            b = ch % NB
            t0 = ch * 128
            if ch + 1 < NCH:
                issue_loads(ch + 1)
            kBT, kCT = "BT%d" % b, "CT%d" % b
            k.tt(dtp[:], dtr[b][:], dtb_bc[:], ALU.add, r=["dtr%d" % b, "dtb"], w=["dtp"])
            k.act(dtp[:], dtp[:], AF.Exp, r=["dtp"], w=["dtp"])
            k.act(dtp[:], dtp[:], AF.Ln, r=["dtp"], w=["dtp"], bias=1.0)
            k.tt(dA[:], dtp[:], a_bc[:], ALU.mult, r=["dtp", "abc"], w=["dA"])
            k.mm(ps_cum[:, 0:32], c.tri_incl_f[:], dA[:], True, True, r=["tri_incl", "dA"], w=["ps_cum"])
            k.mm(ps_cum[:, 32:64], c.ones_f[:], dA[:], True, True, r=["ones_f", "dA"], w=["ps_cum"])
            k.copy(cum[:], ps_cum[:, 0:64], r=["ps_cum"], w=["cum"])
            k.act(ea[:], cum[:, 0:32], AF.Exp, r=["cum"], w=["ea"])
            k.act(cd[:], cum[:, 32:64], AF.Exp, r=["cum"], w=["cd"])
            k.tt(wgt[:], cum[:, 32:64], cum[:, 0:32], ALU.subtract, r=["cum"], w=["wgt"])
            k.act(wgt[:], wgt[:], AF.Exp, r=["wgt"], w=["wgt"])
            k.tt(wgt[:], wgt[:], dtp[:], ALU.mult, r=["wgt", "dtp"], w=["wgt"])
            for g in range(8):
                pcb = ps_cb[g % 2]
                kpcb = "ps_cb%d" % (g % 2)
                py = ps_y[g % 2]
                kpy = "ps_y%d" % (g % 2)
                k.mm(pcb[:, 0:128], BT[b][:, g, :], CT[b][:, g, :], True, True, r=[kBT, kCT], w=[kpcb])
                k.tt(G[:], pcb[:, 0:128], c.tri_incl_f[:], ALU.mult, r=[kpcb, "tri_incl"], w=["G"])
                for rr in range(4):
                    h = 4 * g + rr
                    i2 = hi % 2
                    hi += 1
                    k.ts(L[i2][:], c.mask_gt_f[:], dA[:, h:h + 1], None, ALU.mult, r=["mask_gt", "dA"], w=["L%d" % i2])
                    k.mm(ps_seg[i2][:, 0:128], L[i2][:], c.tri_incl_f[:], True, True, r=["L%d" % i2, "tri_incl"],
                         w=["ps_seg%d" % i2])
                    k.act(E[i2][:], ps_seg[i2][:, 0:128], AF.Exp, r=["ps_seg%d" % i2], w=["E%d" % i2])
                    k.stt(MT[i2][:], E[i2][:], dtp[:, h:h + 1], G[:], ALU.mult, ALU.mult, r=["E%d" % i2, "dtp", "G"],
                          w=["MT%d" % i2])
                    k.mm(py[:, rr * 64:(rr + 1) * 64], MT[i2][:], xs[b][:, h * 64:(h + 1) * 64], True, True,
                         r=["MT%d" % i2, "xs%d_%d" % (b, h // 2)], w=[kpy])
                    k.ts(xw[g % 2][:, rr * 64:(rr + 1) * 64], xs[b][:, h * 64:(h + 1) * 64], wgt[:, h:h + 1], None, ALU.mult,
                         r=["xs%d_%d" % (b, h // 2), "wgt"], w=["xw%d" % (g % 2)])
                k.mm(py[:, 256:512], CT[b][:, g, :], state_b[:, g, :], True, True, r=[kCT, "state_b%d" % g], w=[kpy])
                k.mm(ps_st[:, 0:256], Btm[b][:, g * 128:(g + 1) * 128], xw[g % 2][:], True, True,
                     r=["Btm%d_%d" % (b, g), "xw%d" % (g % 2)], w=["ps_st"])
                k.copy(ydsb[g % 2][:], py[:, 0:256], r=[kpy], w=["ydsb%d" % (g % 2)], e="act")
                for rr in range(4):
                    h = 4 * g + rr
                    k.stt(tmp[:, rr * 64:(rr + 1) * 64], py[:, 256 + rr * 64:256 + (rr + 1) * 64], ea[:, h:h + 1],
                          ydsb[g % 2][:, rr * 64:(rr + 1) * 64], ALU.mult, ALU.add,
                          r=[kpy, "ea", "ydsb%d" % (g % 2)], w=["ytmp"])
                    k.stt(yf[:, h * 64:(h + 1) * 64], xs[b][:, h * 64:(h + 1) * 64], dsk_bc[:, h:h + 1],
                          tmp[:, rr * 64:(rr + 1) * 64], ALU.mult, ALU.add, r=["xs%d_%d" % (b, h // 2), "dsk", "ytmp"], w=["yf"])
                    k.stt(state_f[:, g, rr * 64:(rr + 1) * 64], state_f[:, g, rr * 64:(rr + 1) * 64], cd[:, h:h + 1],
                          ps_st[:, rr * 64:(rr + 1) * 64], ALU.mult, ALU.add,
                          r=["state_f%d" % g, "cd", "ps_st"], w=["state_f%d" % g])
                k.copy(state_b[:, g, :], state_f[:, g, :], r=["state_f%d" % g], w=["state_b%d" % g], e="act")
            k.act(sz[:], zt[b][:], AF.Silu, r=["zt%d" % b], w=["sz"])
            k.tt(yf[:], yf[:], sz[:], ALU.mult, r=["yf", "sz"], w=["yf"])
            k.tt(sq[:], yf[:], yf[:], ALU.mult, r=["yf"], w=["sq"])
            k.op("dve", lambda q: q.tensor_reduce(out=ss8[:], in_=sq[:].rearrange("p (g e) -> p g e", g=8),
                                                  axis=AX.X, op=ALU.add), r=["sq"], w=["ss8"])
            rsqrt(k, ss8[:], ss8[:], 1.0 / 256, EPS, ["ss8"], ["ss8"])
            o = yo[ch % 2]
            for g in range(8):
                k.stt(o[:, g * 256:(g + 1) * 256], yf[:, g * 256:(g + 1) * 256], ss8[:, g:g + 1],
                      gn_bc[:, g * 256:(g + 1) * 256], ALU.mult, ALU.mult, r=["yf", "ss8", "gnbc"], w=["yo%d" % (ch % 2)])
            k.load(y_tm[t0:t0 + 128, :], o[:], r=["yo%d" % (ch % 2)])
        k.barrier()
        k.stack = old


def outproj_stage(k, c, T, kin_specs, W, resid, dst, tag):
    nkc = W.shape[0] // 128
    with ExitStack() as es:
        k.stack, old = es, k.stack
        wb = [k.sb([128, nkc, 512], BF16, "owb%d" % i) for i in range(2)]
        mt = [k.sb([128, nkc, 128], BF16, "omt%d" % i) for i in range(2)]
        rt = [k.sb([128, 512], F32, "ort%d" % i) for i in range(2)]
        ot = [k.sb([128, 512], F32, "oot%d" % i) for i in range(2)]
        ps = [k.stack.enter_context(k.nc.psum_tensor(k.name("ops"), [128, 512], F32)) for _ in range(2)]
        Wv = W.rearrange("(kc p) n -> p kc n", p=128)
        it = 0
        for cb in range(D // 512):
            wbuf = wb[cb % 2]
            wkey = tag + "w%d" % (cb % 2)
            k.cast_load(wbuf[:], Wv[:, :, cb * 512:(cb + 1) * 512], w=[wkey])
            for j in range(T // 128):
                b = it % 2
                it += 1
                t0 = j * 128
                kc = 0
                for (mode, src) in kin_specs:
                    if mode == "tm":
                        for q in range(src.shape[1] // 128):
                            k.loadT(mt[b][:, kc, :], src[t0:t0 + 128, q * 128:(q + 1) * 128], w=[tag + "mt%d_%d" % (b, kc)])
                            kc += 1
                    else:
                        n = src.shape[0] // 128
                        k.load(mt[b][:, kc:kc + n, :], src[:, t0:t0 + 128].rearrange("(q p) t -> p q t", p=128),
                               w=[tag + "mt%d_%d" % (b, q2) for q2 in range(kc, kc + n)])
                        kc += n
                k.load(rt[b][:], resid[t0:t0 + 128, cb * 512:(cb + 1) * 512], w=[tag + "rt%d" % b])
                for kc in range(nkc):
                    k.mm(ps[b][:], mt[b][:, kc, :], wbuf[:, kc, :], kc == 0, kc == nkc - 1,
                         r=[tag + "mt%d_%d" % (b, kc), wkey], w=[tag + "ps%d" % b])
                k.tt(ot[b][:], ps[b][:], rt[b][:], ALU.add, r=[tag + "ps%d" % b, tag + "rt%d" % b], w=[tag + "ot%d" % b])
                k.load(dst[t0:t0 + 128, cb * 512:(cb + 1) * 512], ot[b][:], r=[tag + "ot%d" % b])
        k.barrier()
        k.stack = old


BLK = 256
_FREED = {}


def free_pool_tmps(k, n0):
    import re
    nc = k.nc
    freed = _FREED.setdefault(id(nc), set())
    for i in nc.main_func.blocks[-1].instructions[n0:]:
        for nm in set(re.findall(r"(Pool_tmp[A-Za-z0-9_]*|Pool_Pool_[A-Za-z0-9_]*_snap_[0-9]+)", str(i))):
            if nm not in freed:
                freed.add(nm)
                nc.gpsimd.free_register(bass.RegisterHandle(nm, mybir.EngineType.Pool))


def moe_stage(k, c, T, li, h_in, h_out, I, S):
    NT = T // 128
    NSLOT = ((2 * T + 32 * (BLK - 1)) + BLK - 1) // BLK * BLK
    NBLK = NSLOT // BLK
    assert NBLK <= 128
    nc = k.nc
    with ExitStack() as es0:
        k.stack, old0 = es0, k.stack
        R = k.sb([128, NT, 32], F32, "mR")
        OH1 = k.sb([128, NT, 32], F32, "mOH1")
        OH2 = k.sb([128, NT, 32], F32, "mOH2")
        W12 = k.sb([128, NT, 2], F32, "mW12")
        SI = k.sb([128, NT, 2], I32, "mSI")
        cnt = k.sb([128, 32], F32, "mcnt")
        with ExitStack() as es:
            k.stack = es
            gbc = k.sb([128, D], F32, "gbc")
            wr = k.sb([128, NKC, 36], F32, "wr")
            bias = k.sb([128, 36], F32, "rbias")
            xt = [k.sb([128, D], F32, "mx%d" % i) for i in range(2)]
            of = [k.sb([128, D], F32, "mof%d" % i) for i in range(2)]
            ob = [k.sb([128, D], BF16, "mob%d" % i) for i in range(2)]
            hT = [k.sb([128, NKC, 128], F32, "mhT%d" % i) for i in range(2)]
            junk = k.sb([128, D], BF16, "mjunk")
            ss = k.sb([128, 1], F32, "mss")
            rstd = k.sb([128, 1], F32, "mrstd")
            lg = k.sb([128, 36], F32, "mlg")
            gmx = k.sb([128, 4], F32, "mgmx")
            goh = k.sb([128, 4], F32, "mgoh")
            pen = k.sb([128, 4], F32, "mpen")
            ejk = k.sb([128, 4], F32, "mejk")
            elm = k.sb([128, 32], F32, "melm")
            top8 = k.sb([128, 8], F32, "mtop8")
            sc = k.sb([128, 4], F32, "msc")
            A = k.sb([128, 32], BF16, "mA")
            pst = [k.stack.enter_context(nc.psum_tensor(k.name("mpst"), [128, 512], F32)) for _ in range(4)]
            psl = k.stack.enter_context(nc.psum_tensor(k.name("mpsl"), [128, 512], F32))
            psr = k.stack.enter_context(nc.psum_tensor(k.name("mpsr"), [128, 512], F32))
            k.load(gbc[:], bcast_rows(I["norm_ffn"][li:li + 1, :], D), w=["mg"])
            with nc.allow_non_contiguous_dma(reason="router weights are tiny"):
                k.load(wr[:, :, 0:4], I["moe_w_group"][li].rearrange("(kc p) n -> p kc n", p=128), w=["wr"])
                k.load(wr[:, :, 4:36], I["moe_w_expert"][li].rearrange("(kc p) n -> p kc n", p=128), w=["wr"])
            k.load(bias[:, 0:4], bcast_rows(I["moe_b_group"][li:li + 1, :], 4), w=["rbias"])
            k.load(bias[:, 4:36], bcast_rows(I["moe_b_expert"][li:li + 1, :], 32), w=["rbias"])
            k.memset(cnt[:], 0.0, w=["mcnt"])
            for j in range(NT):
                b = j % 2
                k.load(xt[b][:], h_in[j * 128:(j + 1) * 128, :], w=["mx%d" % b])
                rmsnorm_tile(k, xt[b][:], gbc[:], of[b][:], ss[:], rstd[:], junk[:], ["mx%d" % b, "mg"], ["mof%d" % b], "m")
                k.copy(ob[b][:], of[b][:], r=["mof%d" % b], w=["mob%d" % b], e="act")
                k.load(S.hn[j * 128:(j + 1) * 128, :], ob[b][:], r=["mob%d" % b], w=["hn_%d" % j])
                for q in range(4):
                    for u in range(4):
                        kc = q * 4 + u
                        k.mm(pst[q][:, u * 128:(u + 1) * 128], of[b][:, kc * 128:(kc + 1) * 128], c.ident_f[:], True, True,
                             r=["mof%d" % b, "ident_f"], w=["mpst%d" % q])
                    k.copy(hT[b][:, q * 4:(q + 1) * 4, :], pst[q][:, :].rearrange("p (u t) -> p u t", u=4),
                           r=["mpst%d" % q], w=["mhT%d_%d" % (b, q)], e=("act" if q % 2 == 0 else "dve"))
                for kc in range(NKC):
                    k.mm(psl[:, 0:36], hT[b][:, kc, :], wr[:, kc, :], kc == 0, kc == NKC - 1,
                         r=["mhT%d_%d" % (b, kc // 4), "wr"], w=["mpsl"])
                k.tt(lg[:], psl[:, 0:36], bias[:], ALU.add, r=["mpsl", "rbias"], w=["mlg"])
                k.op("dve", lambda q_: q_.reduce_max(out=gmx[:, 0:1], in_=lg[:, 0:4], axis=AX.X), r=["mlg"], w=["mgmx"])
                k.ts(goh[:], lg[:, 0:4], gmx[:, 0:1], None, ALU.is_equal, r=["mlg", "mgmx"], w=["mgoh"])
                k.ts(gmx[:, 1:2], gmx[:, 0:1], -1.0, None, ALU.mult, r=["mgmx"], w=["mgmx"])
                k.act(ejk[:], lg[:, 0:4], AF.Exp, r=["mlg", "mgmx"], w=["mejk", "mgsum"], bias=gmx[:, 1:2], accum_out=gmx[:, 2:3])
                k.op("dve", lambda q_: q_.reciprocal(out=gmx[:, 3:4], in_=gmx[:, 2:3]), r=["mgsum"], w=["mgprob"])
                k.ts(pen[:], goh[:], 1e30, -1e30, ALU.mult, ALU.add, r=["mgoh"], w=["mpen"])
                for g in range(4):
                    k.ts(elm[:, g * 8:(g + 1) * 8], lg[:, 4 + g * 8:4 + (g + 1) * 8], pen[:, g:g + 1], None, ALU.add,
                         r=["mlg", "mpen"], w=["melm"])
                k.op("dve", lambda q_: q_.max(out=top8[:], in_=elm[:]), r=["melm"], w=["mtop8"])
                k.ts(OH1[:, j, :], elm[:], top8[:, 0:1], None, ALU.is_equal, r=["melm", "mtop8"], w=["mOH1_%d" % j])
                k.ts(OH2[:, j, :], elm[:], top8[:, 1:2], None, ALU.is_equal, r=["melm", "mtop8"], w=["mOH2_%d" % j])
                k.tt(sc[:, 0:1], top8[:, 1:2], top8[:, 0:1], ALU.subtract, r=["mtop8"], w=["msc"])
                k.act(sc[:, 1:2], sc[:, 0:1], AF.Exp, r=["msc"], w=["msc"])
                k.ts(sc[:, 1:2], sc[:, 1:2], 1.0, None, ALU.add, r=["msc"], w=["msc"])
                k.op("dve", lambda q_: q_.reciprocal(out=sc[:, 2:3], in_=sc[:, 1:2]), r=["msc"], w=["msc"])
                k.tt(W12[:, j, 0:1], gmx[:, 3:4], sc[:, 2:3], ALU.mult, r=["mgprob", "msc"], w=["mW12_%d" % j])
                k.tt(W12[:, j, 1:2], gmx[:, 3:4], W12[:, j, 0:1], ALU.subtract, r=["mgprob", "mW12_%d" % j], w=["mW12_%d" % j])
                k.tt(A[:], OH1[:, j, :], OH2[:, j, :], ALU.add, r=["mOH1_%d" % j, "mOH2_%d" % j], w=["mA"])
                k.mm(psr[:, 0:32], c.tri_strict_b[:], A[:], True, True, r=["tri_strict", "mA"], w=["mpsr"])
                k.mm(psr[:, 32:64], c.ones_b[:], A[:], True, True, r=["ones_b", "mA"], w=["mpsr"])
                k.tt(R[:, j, :], psr[:, 0:32], cnt[:], ALU.add, r=["mpsr", "mcnt"], w=["mR_%d" % j])
                k.tt(cnt[:], psr[:, 32:64], cnt[:], ALU.add, r=["mpsr", "mcnt"], w=["mcnt"])
            nb = k.sb([128, 32], F32, "mnb")
            cs = [k.sb([128, 32], F32, "mcs%d" % i) for i in range(2)]
            pstart = k.sb([128, 32], F32, "mpstart")
            tmp = k.sb([128, 32], F32, "mtmp")
            SF = k.sb([128, NT, 2], F32, "mSF")
            be = k.sb([128, 4], F32, "mbe")
            bei = k.sb([128, 1], I32, "mbei")
            k.memset(nb[:], 0.0, w=["mnb"])
            for m in range((T + BLK - 1) // BLK):
                k.stt(nb[:], cnt[:], float(m * BLK), nb[:], ALU.is_gt, ALU.add, r=["mcnt", "mnb"], w=["mnb"])
            k.ts(cs[0][:], nb[:], float(BLK), None, ALU.mult, r=["mnb"], w=["mcs0"])
            k.copy(nb[:], cs[0][:], r=["mcs0"], w=["mnb"])
            cur = 0
            for sh in (1, 2, 4, 8, 16):
                nx = 1 - cur
                k.copy(cs[nx][:, 0:sh], cs[cur][:, 0:sh], r=["mcs%d" % cur], w=["mcs%d" % nx])
                k.tt(cs[nx][:, sh:32], cs[cur][:, sh:32], cs[cur][:, 0:32 - sh], ALU.add, r=["mcs%d" % cur], w=["mcs%d" % nx])
                cur = nx
            pend = cs[cur]
            k.tt(pstart[:], pend[:], nb[:], ALU.subtract, r=["mcs%d" % cur, "mnb"], w=["mpstart"])
            for j in range(NT):
                k.tt(tmp[:], R[:, j, :], pstart[:], ALU.add, r=["mR_%d" % j, "mpstart"], w=["mtmp"])
                k.tt(elm[:], tmp[:], OH1[:, j, :], ALU.mult, r=["mtmp", "mOH1_%d" % j], w=["melm"])
                k.op("dve", lambda q_: q_.reduce_sum(out=SF[:, j, 0:1], in_=elm[:], axis=AX.X), r=["melm"], w=["mSF"])
                k.tt(elm[:], tmp[:], OH2[:, j, :], ALU.mult, r=["mtmp", "mOH2_%d" % j], w=["melm"])
                k.op("dve", lambda q_: q_.reduce_sum(out=SF[:, j, 1:2], in_=elm[:], axis=AX.X), r=["melm"], w=["mSF"])
            k.copy(SI[:], SF[:], r=["mSF"], w=["mSI"])
            k.ts(be[:, 0:1], c.iota_p[:], float(BLK), None, ALU.mult, r=["iota_p"], w=["mbe"])
            k.ts(elm[:], pend[:], be[:, 0:1], None, ALU.is_le, r=["mcs%d" % cur, "mbe"], w=["melm"])
            k.op("dve", lambda q_: q_.reduce_sum(out=be[:, 1:2], in_=elm[:], axis=AX.X), r=["melm"], w=["mbe"])
            k.ts(be[:, 1:2], be[:, 1:2], 31.0, None, ALU.min, r=["mbe"], w=["mbe"])
            k.copy(bei[:], be[:, 1:2], r=["mbe"], w=["mbei"])
            k.load(S.blk_e[:, :], bei[:], r=["mbei"])
            for j in range(NT):
                b = j % 2
                k.load(ob[b][:], S.hn[j * 128:(j + 1) * 128, :], r=["hn_%d" % j], w=["mob%d" % b])
                for kk in range(2):
                    k.dma("pool", lambda q_, j=j, kk=kk, b=b: q_.indirect_dma_start(
                        out=S.xg, out_offset=bass.IndirectOffsetOnAxis(ap=SI[:, j, kk:kk + 1], axis=0),
                        in_=ob[b][:], in_offset=None), r=["mob%d" % b, "mSI"])
            k.barrier()
            k.stack = es0
        with ExitStack() as es:
            k.stack = es
            xgT = [k.sb([128, NKC, BLK], BF16, "xgT%d" % i) for i in range(2)]
            wgu = [k.sb([128, NKC, 512], BF16, "wgu%d" % i) for i in range(4)]
            wdn = [k.sb([128, 8, 512], BF16, "wdn%d" % i) for i in range(3)]
            hTt = [k.sb([128, 8, BLK], BF16, "hTt%d" % i) for i in range(2)]
            sg = [k.sb([128, BLK], F32, "sg%d" % i) for i in range(2)]
            yt = [k.sb([128, D], F32, "yt%d" % i) for i in range(4)]
            psg = [k.stack.enter_context(nc.psum_tensor(k.name("psg"), [128, 512], F32)) for _ in range(2)]
            psu = [k.stack.enter_context(nc.psum_tensor(k.name("psu"), [128, 512], F32)) for _ in range(2)]
            psy = [k.stack.enter_context(nc.psum_tensor(k.name("psy"), [128, 512], F32)) for _ in range(2)]
            ereg = nc.gpsimd.alloc_register(k.name("ereg"))
            obase = nc.gpsimd.alloc_register(k.name("obase"))
            oreg = [nc.gpsimd.alloc_register(k.name("oreg")) for _ in range(8)]
            Hg, Hu, Hd = I["_h_moe_w_gate%d" % li], I["_h_moe_w_up%d" % li], I["_h_moe_w_down%d" % li]
            PAT_GU = [[1024, 128], [128 * 1024, NKC], [1, 512]]
            PAT_D = [[2048, 128], [128 * 2048, 8], [1, 512]]

            def load_xg(bk):
                bb = bk % 2
                for kc in range(NKC):
                    k.loadT(xgT[bb][:, kc, :], S.xg[bk * BLK:(bk + 1) * BLK, kc * 128:(kc + 1) * 128], w=["xgT%d_%d" % (bb, kc)])

            load_xg(0)
            gi = 0
            di = 0
            mi = 0
            yi = 0
            for bk in range(NBLK):
                bb = bk % 2
                if bk + 1 < NBLK:
                    load_xg(bk + 1)
                n_ins0 = len(nc.main_func.blocks[-1].instructions)
                nc.gpsimd.reg_load(ereg, S.blk_e[bk:bk + 1, 0:1])
                nc.gpsimd.reg_mul(obase, ereg, 2048 * 1024)
                ori = 0
                for hq in range(2):
                    gb, ub = wgu[gi % 4], wgu[(gi + 1) % 4]
                    gk, uk = "wgu%d" % (gi % 4), "wgu%d" % ((gi + 1) % 4)
                    gi += 2
                    nc.gpsimd.reg_add(oreg[ori], obase, hq * 512)
                    k.cast_load(gb[:], bass.AP(Hg, oreg[ori], PAT_GU), w=[gk])
                    k.cast_load(ub[:], bass.AP(Hu, oreg[ori], PAT_GU), w=[uk])
                    ori += 1
                    for m in range(4):
                        ffc = hq * 4 + m
                        pi = mi % 2
                        mi += 1
                        for kc in range(NKC):
                            k.mm(psg[pi][:, 0:BLK], gb[:, kc, m * 128:(m + 1) * 128], xgT[bb][:, kc, :], kc == 0, kc == NKC - 1,
                                 r=[gk, "xgT%d_%d" % (bb, kc)], w=["psg%d" % pi])
                        for kc in range(NKC):
                            k.mm(psu[pi][:, 0:BLK], ub[:, kc, m * 128:(m + 1) * 128], xgT[bb][:, kc, :], kc == 0, kc == NKC - 1,
                                 r=[uk, "xgT%d_%d" % (bb, kc)], w=["psu%d" % pi])
                        k.act(sg[pi][:], psg[pi][:, 0:BLK], AF.Silu, r=["psg%d" % pi], w=["sg%d" % pi])
                        k.tt(hTt[bb][:, ffc, :], sg[pi][:], psu[pi][:, 0:BLK], ALU.mult, r=["sg%d" % pi, "psu%d" % pi],
                             w=["hTt%d_%d" % (bb, ffc)])
                yts = [yt[(yi + s_) % 4] for s_ in range(BLK // 128)]
                ytk = ["yt%d" % ((yi + s_) % 4) for s_ in range(BLK // 128)]
                yi += BLK // 128
                for cb in range(4):
                    db = wdn[di % 3]
                    dk = "wdn%d" % (di % 3)
                    di += 1
                    nc.gpsimd.reg_add(oreg[ori], obase, cb * 512)
                    k.cast_load(db[:], bass.AP(Hd, oreg[ori], PAT_D), w=[dk])
                    ori += 1
                    for s_ in range(BLK // 128):
                        pi = mi % 2
                        mi += 1
                        for ffc in range(8):
                            k.mm(psy[pi][:], hTt[bb][:, ffc, s_ * 128:(s_ + 1) * 128], db[:, ffc, :], ffc == 0, ffc == 7,
                                 r=["hTt%d_%d" % (bb, ffc), dk], w=["psy%d" % pi])
                        k.copy(yts[s_][:, cb * 512:(cb + 1) * 512], psy[pi][:], r=["psy%d" % pi], w=[ytk[s_]],
                               e=("act" if s_ % 2 == 0 else "dve"))
                for s_ in range(BLK // 128):
                    k.load(S.yslot[bk * BLK + s_ * 128:bk * BLK + (s_ + 1) * 128, :], yts[s_][:], r=[ytk[s_]])
                free_pool_tmps(k, n_ins0)
            k.barrier()
            k.stack = es0
        with ExitStack() as es:
            k.stack = es
            ht = [k.sb([128, D], F32, "ch%d" % i) for i in range(2)]
            y1 = [k.sb([128, D], F32, "cy1%d" % i) for i in range(2)]
            y2 = [k.sb([128, D], F32, "cy2%d" % i) for i in range(2)]
            for j in range(NT):
                b = j % 2
                k.load(ht[b][:], h_in[j * 128:(j + 1) * 128, :], w=["ch%d" % b])
                k.dma("pool", lambda q_, j=j, b=b: q_.indirect_dma_start(
                    out=y1[b][:], out_offset=None, in_=S.yslot,
                    in_offset=bass.IndirectOffsetOnAxis(ap=SI[:, j, 0:1], axis=0)), w=["cy1%d" % b])
                k.dma("pool", lambda q_, j=j, b=b: q_.indirect_dma_start(
                    out=y2[b][:], out_offset=None, in_=S.yslot,
                    in_offset=bass.IndirectOffsetOnAxis(ap=SI[:, j, 1:2], axis=0)), w=["cy2%d" % b])
                k.stt(ht[b][:], y1[b][:], W12[:, j, 0:1], ht[b][:], ALU.mult, ALU.add, r=["cy1%d" % b, "ch%d" % b], w=["ch%d" % b])
                k.stt(ht[b][:], y2[b][:], W12[:, j, 1:2], ht[b][:], ALU.mult, ALU.add, r=["cy2%d" % b, "ch%d" % b], w=["ch%d" % b])
                k.load(h_out[j * 128:(j + 1) * 128, :], ht[b][:], r=["ch%d" % b])
            k.barrier()
            k.stack = es0
        k.stack = old0


def zero_dram(k, dst, rows, cols, dt):
    with ExitStack() as es:
        k.stack, old = es, k.stack
        z = k.sb([128, cols], dt, "zero")
        k.memset(z[:], 0.0, w=["zero"])
        for r0 in range(0, rows, 128):
            k.load(dst[r0:r0 + 128, :], z[:], r=["zero"])
        k.barrier()
        k.stack = old


def ple_stage(k, c, T, li, h_in, h_out, I, S, final_g=None, final_out=None):
    nc = k.nc
    norm_to_dram(k, c, h_in, I["norm_ple"][li:li + 1, :], S.hn, T, "pl")
    with ExitStack() as es:
        k.stack, old = es, k.stack
        wg = k.sb([128, NKC, D], BF16, "plwg")
        wp = k.sb([128, 2, D], BF16, "plwp")
        aT = [k.sb([128, NKC, 128], BF16, "plaT%d" % i) for i in range(2)]
        pf = [k.sb([128, 256], F32, "plpf%d" % i) for i in range(2)]
        pb = [k.sb([128, 256], BF16, "plpb%d" % i) for i in range(2)]
        pT = [k.sb([128, 2, 128], BF16, "plpT%d" % i) for i in range(2)]
        ht = [k.sb([128, D], F32, "plh%d" % i) for i in range(2)]
        gt = [k.sb([128, 512], F32, "plg%d" % i) for i in range(2)]
        psa = [k.stack.enter_context(nc.psum_tensor(k.name("plpsa"), [128, 512], F32)) for _ in range(2)]
        psb = [k.stack.enter_context(nc.psum_tensor(k.name("plpsb"), [128, 512], F32)) for _ in range(2)]
        if final_g is not None:
            fg = k.sb([128, D], F32, "plfg")
            fo = [k.sb([128, D], F32, "plfo%d" % i) for i in range(2)]
            junk = k.sb([128, D], BF16, "pljunk")
            ss = k.sb([128, 1], F32, "plss")
            rstd = k.sb([128, 1], F32, "plrstd")
            k.load(fg[:], bcast_rows(final_g, D), w=["plfg"])
        k.cast_load(wg[:], I["ple_gate"][li].rearrange("(kc p) n -> p kc n", p=128), w=["plwg"])
        k.cast_load(wp[:], I["ple_proj"][li].rearrange("(kc p) n -> p kc n", p=128), w=["plwp"])
        for j in range(T // 128):
            b = j % 2
            k.load(pf[b][:], I["p"][li, j * 128:(j + 1) * 128, :], w=["plpf%d" % b])
            k.copy(pb[b][:], pf[b][:], r=["plpf%d" % b], w=["plpb%d" % b])
            k.load(S.pbf[j * 128:(j + 1) * 128, :], pb[b][:], r=["plpb%d" % b], w=["pbf_%d" % j])
        mi = 0
        for j in range(T // 128):
            b = j % 2
            t0 = j * 128
            for kc in range(NKC):
                k.loadT(aT[b][:, kc, :], S.hn[t0:t0 + 128, kc * 128:(kc + 1) * 128], w=["plaT%d_%d" % (b, kc)])
            for q in range(2):
                k.loadT(pT[b][:, q, :], S.pbf[t0:t0 + 128, q * 128:(q + 1) * 128], r=["pbf_%d" % j], w=["plpT%d_%d" % (b, q)])
            k.load(ht[b][:], h_in[t0:t0 + 128, :], w=["plh%d" % b])
            for cb in range(4):
                pi = mi % 2
                mi += 1
                for kc in range(NKC):
                    k.mm(psa[pi][:], aT[b][:, kc, :], wg[:, kc, cb * 512:(cb + 1) * 512], kc == 0, kc == NKC - 1,
                         r=["plaT%d_%d" % (b, kc), "plwg"], w=["plpsa%d" % pi])
                for q in range(2):
                    k.mm(psb[pi][:], pT[b][:, q, :], wp[:, q, cb * 512:(cb + 1) * 512], q == 0, q == 1,
                         r=["plpT%d_%d" % (b, q), "plwp"], w=["plpsb%d" % pi])
                k.act(gt[pi][:], psa[pi][:], AF.Sigmoid, r=["plpsa%d" % pi], w=["plg%d" % pi])
                k.tt(gt[pi][:], gt[pi][:], psb[pi][:], ALU.mult, r=["plg%d" % pi, "plpsb%d" % pi], w=["plg%d" % pi])
                k.tt(ht[b][:, cb * 512:(cb + 1) * 512], ht[b][:, cb * 512:(cb + 1) * 512], gt[pi][:], ALU.add,
                     r=["plg%d" % pi, "plh%d" % b], w=["plh%d" % b])
            if final_g is None:
                k.load(h_out[t0:t0 + 128, :], ht[b][:], r=["plh%d" % b])
            else:
                rmsnorm_tile(k, ht[b][:], fg[:], fo[b][:], ss[:], rstd[:], junk[:], ["plh%d" % b, "plfg"], ["plfo%d" % b], "plf")
                k.load(final_out[t0:t0 + 128, :], fo[b][:], r=["plfo%d" % b])
        k.barrier()
        k.stack = old


SCALE = 128.0 ** -0.5
NEGB = -30000.0


def rope_stage(k, c, T, items, ropeC, ropeS, cmp_items=()):
    nc = k.nc
    with ExitStack() as es:
        k.stack, old = es, k.stack
        Cs = k.sb([32, T], F32, "ropeC")
        Ss = k.sb([32, T], F32, "ropeS")
        pa = k.sb([32, 32], F32, "rpa")
        pb = k.sb([32, 32], F32, "rpb")
        Pm = k.sb([32, 32], BF16, "rPm")
        xt = [k.sb([32, T], BF16, "rx%d" % i) for i in range(2)]
        t1 = [k.sb([32, 512], F32, "rt1%d" % i) for i in range(2)]
        t2 = [k.sb([32, 512], F32, "rt2%d" % i) for i in range(2)]
        ot = [k.sb([32, T], BF16, "ro%d" % i) for i in range(2)]
        ps = [k.stack.enter_context(nc.psum_tensor(k.name("rps"), [128, 512], F32)) for _ in range(2)]
        k.load(Cs[:], ropeC, w=["ropeC"])
        k.load(Ss[:], ropeS, w=["ropeS"])
        k.op("pool", lambda q: q.affine_select(out=pa[:], in_=c.ones_f[0:32, 0:32], pattern=[[1, 32]], compare_op=ALU.is_equal,
                                               fill=0.0, base=-16, channel_multiplier=-1), r=["ones_f"], w=["rpa"])
        k.op("pool", lambda q: q.affine_select(out=pb[:], in_=c.ones_f[0:32, 0:32], pattern=[[-1, 32]], compare_op=ALU.is_equal,
                                               fill=0.0, base=-16, channel_multiplier=1), r=["ones_f"], w=["rpb"])
        k.tt(Pm[:], pa[:], pb[:], ALU.subtract, r=["rpa", "rpb"], w=["rPm"])
        it = 0
        pi = 0
        for (ten, row0, Tn, cstep, c0) in [(a, b, T, 1, 0) for (a, b) in items] + [(a, b, n, 16, 31) for (a, b, n) in cmp_items]:
            b = it % 2
            it += 1
            k.load(xt[b][:, 0:Tn], ten[row0:row0 + 32, 0:Tn], w=["rx%d" % b])
            for n0 in range(0, Tn, 512):
                n = min(512, Tn - n0)
                p = pi % 2
                pi += 1
                k.mm(ps[p][0:32, 0:n], Pm[:], xt[b][:, n0:n0 + n], True, True, r=["rPm", "rx%d" % b], w=["rps%d" % p])
                if cstep == 1:
                    cc, sc_ = Cs[:, n0:n0 + n], Ss[:, n0:n0 + n]
                else:
                    cc = Cs[:, c0 + cstep * n0:c0 + cstep * (n0 + n - 1) + 1:cstep]
                    sc_ = Ss[:, c0 + cstep * n0:c0 + cstep * (n0 + n - 1) + 1:cstep]
                k.tt(t1[p][:, 0:n], xt[b][:, n0:n0 + n], cc, ALU.mult, r=["rx%d" % b, "ropeC"], w=["rt1%d" % p])
                k.tt(t2[p][:, 0:n], ps[p][0:32, 0:n], sc_, ALU.mult, r=["rps%d" % p, "ropeS"], w=["rt2%d" % p])
                k.tt(ot[b][:, n0:n0 + n], t1[p][:, 0:n], t2[p][:, 0:n], ALU.add, r=["rt1%d" % p, "rt2%d" % p], w=["ro%d" % b])
            k.load(ten[row0:row0 + 32, 0:Tn], ot[b][:, 0:Tn], r=["ro%d" % b])
        k.barrier()
        k.stack = old


def compress_stage(k, c, T, S, I):
    nc = k.nc
    NC = T // 16 - 1
    with ExitStack() as es:
        k.stack, old = es, k.stack
        w1 = k.sb([128, 32, 128], BF16, "cw1")
        w2 = k.sb([128, 128], BF16, "cw2")
        posr = k.sb([32, 128], F32, "cposr")
        posT = k.sb([128, 32], BF16, "cposT")
        cb = k.sb([128, 1], F32, "ccb")
        xT = k.sb([128, T], BF16, "cxT")
        h1 = k.sb([128, 256], BF16, "ch1")
        okc = k.sb([128, 256], BF16, "cokc")
        ovc = k.sb([128, 2, 128], BF16, "covc")
        ps1 = k.stack.enter_context(nc.psum_tensor(k.name("cps1"), [128, 512], F32))
        ps2 = k.stack.enter_context(nc.psum_tensor(k.name("cps2"), [128, 512], F32))
        ps3 = k.stack.enter_context(nc.psum_tensor(k.name("cps3"), [128, 512], F32))
        k.memset(okc[:], 0.0, w=["cokc"])
        for kv in range(2):
            k.cast_load(w1[:], I["od_cmp_w1"][kv].rearrange("(j d) o -> d j o", d=128), w=["cw1"])
            k.cast_load(w2[:], I["od_cmp_w2"][kv], w=["cw2"])
            k.load(posr[:], I["od_cmp_pos"][kv], w=["cposr"])
            k.mm(ps3[:, 0:32], posr[:], c.ident_f[0:32, 0:32], True, True, r=["cposr", "ident_f"], w=["cps3"])
            k.copy(posT[:], ps3[:, 0:32], r=["cps3"], w=["cposT"])
            for j in range(32):
                k.mm(ps3[:, 64:65], w1[:, j, :], posT[:, j:j + 1], j == 0, j == 31, r=["cw1", "cposT"], w=["cps3"])
            k.copy(cb[:], ps3[:, 64:65], r=["cps3"], w=["ccb"])
            src = S.kcmpT if kv == 0 else S.vcmpT
            for g in range(2):
                k.load(xT[:], src[g * 128:(g + 1) * 128, :], w=["cxT"])
                for j in range(32):
                    k.mm(ps1[:, 0:NC], w1[:, j, :], xT[:, j:j + 16 * (NC - 1) + 1:16], j == 0, j == 31, r=["cw1", "cxT"], w=["cps1"])
                k.act(h1[:, 0:NC], ps1[:, 0:NC], AF.Silu, r=["cps1", "ccb"], w=["ch1"], bias=cb[:, 0:1])
                if kv == 0:
                    k.mm(ps2[:, 0:NC], w2[:], h1[:, 0:NC], True, True, r=["cw2", "ch1"], w=["cps2"])
                    k.copy(okc[:, 0:NC], ps2[:, 0:NC], r=["cps2"], w=["cokc"])
                    k.load(S.kcT[g * 128:(g + 1) * 128, :], okc[:], r=["cokc"])
                else:
                    for h in range(2):
                        n = min(128, NC - h * 128)
                        if n <= 0:
                            continue
                        k.mm(ps2[0:n, h * 128:(h + 1) * 128], h1[:, h * 128:h * 128 + n], w2[:], True, True, r=["cw2", "ch1"], w=["cps2"])
                    k.memset(ovc[:], 0.0, w=["covc"])
                    for h in range(2):
                        n = min(128, NC - h * 128)
                        if n <= 0:
                            continue
                        k.copy(ovc[0:n, h, :], ps2[0:n, h * 128:(h + 1) * 128], r=["cps2"], w=["covc"])
                    k.load(S.vc[:, g * 128:(g + 1) * 128].rearrange("(h p) d -> p h d", p=128), ovc[:], r=["covc"])
        k.barrier()
        k.stack = old


class Attn:
    def __init__(self, k, NV):
        nc = k.nc
        self.k = k
        self.NV = NV
        self.pst = [k.stack.enter_context(nc.psum_tensor(k.name("aps"), [128, 512], F32)) for _ in range(2)]
        self.acc = [k.stack.enter_context(nc.psum_tensor(k.name("aacc"), [128, 512], F32)) for _ in range(4)]
        self.pT = [k.sb([128, 512], BF16, "apT%d" % i) for i in range(3)]
        self.si = 0
        self.pi = 0

    def run(self, qT_ap, rq, ktiles):
        k = self.k
        NV = self.NV
        first = [None] * 4
        last = [None] * 4
        for ti, t in enumerate(ktiles):
            for a in range(t["subs"][0], t["subs"][1]):
                if first[a] is None:
                    first[a] = ti
                last[a] = ti
        for ti, t in enumerate(ktiles):
            a0, a1 = t["subs"]
            ps = self.pst[self.si % 2]
            pk = "aps%d" % (self.si % 2)
            self.si += 1
            pT = self.pT[self.pi % 3]
            tk = "apT%d" % (self.pi % 3)
            self.pi += 1
            c0, c1 = a0 * 128, a1 * 128
            k.mm(ps[:, c0:c1], t["kT"], qT_ap[:, c0:c1], True, t.get("bias") is None, r=list(t["rk"]) + list(rq), w=[pk])
            if t.get("bias") is not None:
                bl, br, bkeys = t["bias"]
                k.mm(ps[:, c0:c1], bl, br[:, c0:c1], False, True, r=list(bkeys), w=[pk])
            k.act(pT[:, c0:c1], ps[:, c0:c1], AF.Exp, r=[pk], w=[tk], scale=SCALE)
            if t.get("mask") is not None:
                k.tt(pT[:, c0:c1], pT[:, c0:c1], t["mask"][:, c0:c1], ALU.mult, r=[tk] + list(t["rm"]), w=[tk],
                     e=t.get("meng", "dve"))
            for a in range(a0, a1):
                k.mm(self.acc[a][:, 0:NV], pT[:, a * 128:(a + 1) * 128], t["V"], first[a] == ti, last[a] == ti,
                     r=[tk] + list(t["rv"]), w=["aacc%d" % a])


def make_attn_masks(k, c, m):
    m.caus = k.sb([128, 4, 512], BF16, "mcaus")
    m.win = k.sb([128, 8, 512], BF16, "mwin")
    ones = k.sb([128, 512], BF16, "mones")
    tmp = k.sb([128, 512], BF16, "mtmp")
    k.memset(ones[:], 1.0, w=["mones"])
    m.ones = ones
    for b in range(4):
        k.op("pool", lambda q, b=b: q.affine_select(out=m.caus[:, b, :], in_=ones[:], pattern=[[1, 512]], compare_op=ALU.is_ge,
                                                    fill=0.0, base=-128 * b, channel_multiplier=-1), r=["mones"], w=["mcaus"])
    for cc in range(8):
        k.op("pool", lambda q, cc=cc: q.affine_select(out=tmp[:], in_=ones[:], pattern=[[1, 512]], compare_op=ALU.is_ge,
                                                      fill=0.0, base=-128 * (cc - 4), channel_multiplier=-1), r=["mones"], w=["mtmp"])
        k.op("pool", lambda q, cc=cc: q.affine_select(out=m.win[:, cc, :], in_=tmp[:], pattern=[[-1, 512]], compare_op=ALU.is_ge,
                                                      fill=0.0, base=128 * (cc - 4) + 511, channel_multiplier=1), r=["mtmp"], w=["mwin"])


def nsa_stage(k, c, T, S, I):
    nc = k.nc
    NT = T // 128
    NQB = T // 512
    NC = T // 16 - 1
    NSEL = T // 64
    NTOP = min(16, NSEL)
    with ExitStack() as es0:
        k.stack, old0 = es0, k.stack
        m = Ctx()
        make_attn_masks(k, c, m)
        Eb = k.sb([128, NT, 128], BF16, "Eb")
        onesE = k.sb([128, NT, 128], BF16, "onesE")
        k.memset(onesE[:], 1.0, w=["onesE"])
        tmpE = k.sb([128, NT, 128], BF16, "tmpE")
        k.op("pool", lambda q: q.affine_select(out=tmpE[:], in_=onesE[:], pattern=[[128, NT], [1, 128]], compare_op=ALU.is_ge,
                                               fill=0.0, base=0, channel_multiplier=-64), r=["onesE"], w=["tmpE"])
        k.op("pool", lambda q: q.affine_select(out=Eb[:], in_=tmpE[:], pattern=[[-128, NT], [-1, 128]], compare_op=ALU.is_ge,
                                               fill=0.0, base=63, channel_multiplier=64), r=["tmpE"], w=["Eb"])
        Am = k.sb([128, 2, 64], BF16, "Am")
        tmpA = k.sb([128, 2, 64], BF16, "tmpA")
        k.op("pool", lambda q: q.affine_select(out=tmpA[:], in_=onesE[:, 0, :].rearrange("p (h b) -> p h b", h=2), pattern=[[128, 2], [-4, 64]],
                                               compare_op=ALU.is_ge, fill=0.0, base=1, channel_multiplier=1), r=["onesE"], w=["tmpA"])
        k.op("pool", lambda q: q.affine_select(out=Am[:], in_=tmpA[:], pattern=[[-128, 2], [4, 64]],
                                               compare_op=ALU.is_ge, fill=0.0, base=3, channel_multiplier=-1), r=["tmpA"], w=["Am"])
        selbT = k.sb([64, T], BF16, "selbT")
        gates = k.sb([128, NT, 24], F32, "gates")
        k.load(gates[:], S.gates.rearrange("(j p) n -> p j n", p=128), w=["gates"])
        k.act(gates[:], gates[:], AF.Sigmoid, r=["gates"], w=["gates"])
        for g in range(2):
            with ExitStack() as es:
                k.stack = es
                at = Attn(k, 193)
                kcT = k.sb([128, 256], BF16, "kcT")
                V1 = k.sb([128, 2, 193], BF16, "cV1")
                qT = [k.sb([128, T], BF16, "cqT%d" % i) for i in range(2)]
                cmask = [k.sb([128, 512], BF16, "cmask%d" % i) for i in range(4)]
                psel = k.sb([128, NT, 64], F32, "psel")
                oc = [k.sb([128, 4, 128], F32, "coc%d" % i) for i in range(2)]
                den = k.sb([128, 4], F32, "cden")
                usb = k.sb([128, 64], F32, "cusb")
                k.load(kcT[:], S.kcT[g * 128:(g + 1) * 128, :], w=["kcT"])
                k.memset(V1[:], 0.0, w=["cV1"])
                k.load(V1[:, :, 0:128], S.vc[:, g * 128:(g + 1) * 128].rearrange("(h p) d -> p h d", p=128), w=["cV1"])
                k.memset(V1[:, :, 128:129], 1.0, w=["cV1"])
                k.copy(V1[:, :, 129:193], Am[:], r=["Am", "cV1"], w=["cV1"])
                k.memset(psel[:], 0.0, w=["psel"])
                ci = 0
                for r_ in range(4):
                    h = 4 * g + r_
                    qb = qT[r_ % 2]
                    qk = "cqT%d" % (r_ % 2)
                    k.load(qb[:], S.qT[h * 128:(h + 1) * 128, :], w=[qk])
                    for Q in range(NQB):
                        kts = []
                        for nt in range(2):
                            if 16 * (nt * 128) + 31 > Q * 512 + 511:
                                continue
                            cm = cmask[ci % 4]
                            ck = "cmask%d" % (ci % 4)
                            ci += 1
                            k.op("pool", lambda q, Q=Q, nt=nt, cm=cm: q.affine_select(
                                out=cm[:], in_=m.ones[:], pattern=[[1, 512]], compare_op=ALU.is_ge, fill=0.0,
                                base=512 * Q - 16 * 128 * nt - 31, channel_multiplier=-16), r=["mones"], w=[ck])
                            kts.append(dict(kT=kcT[:, nt * 128:(nt + 1) * 128], rk=["kcT"], V=V1[:, nt, :], rv=["cV1"],
                                            subs=(0, 4), mask=cm, rm=[ck]))
                        at.run(qb[:, Q * 512:(Q + 1) * 512], [qk], kts)
                        for a in range(4):
                            j = Q * 4 + a
                            ob = oc[j % 2]
                            okk = "coc%d" % (j % 2)
                            k.ts(den[:, 0:1], at.acc[a][:, 128:129], 1e-30, None, ALU.max, r=["aacc%d" % a], w=["cden"])
                            k.op("dve", lambda q_: q_.reciprocal(out=den[:, 1:2], in_=den[:, 0:1]), r=["cden"], w=["cden"])
                            k.ts(ob[:, r_, :], at.acc[a][:, 0:128], den[:, 1:2], None, ALU.mult, r=["aacc%d" % a, "cden"], w=[okk])
                            k.stt(psel[:, j, :], at.acc[a][:, 129:193], den[:, 1:2], psel[:, j, :], ALU.mult, ALU.add,
                                  r=["aacc%d" % a, "cden", "psel"], w=["psel"])
                            k.load(S.ocmp[j * 128:(j + 1) * 128, h * 128:(h + 1) * 128], ob[:, r_, :], r=[okk])
                vm = k.sb([128, 64], F32, "svm")
                fm_ = k.sb([128, 64], F32, "sfm")
                f2 = k.sb([128, 64], F32, "sf2")
                sc = k.sb([128, 64], F32, "ssc")
                sc2 = k.sb([128, 64], F32, "ssc2")
                t8 = k.sb([128, 16], F32, "st8")
                selb = k.sb([128, 64], F32, "sselb")
                pss = k.stack.enter_context(nc.psum_tensor(k.name("spss"), [128, 512], F32)) if False else at.pst[0]
                for j in range(NT):
                    q0 = j * 128
                    k.op("pool", lambda q, q0=q0: q.affine_select(out=vm[:, 0:NSEL], in_=c.ones_f[:, 0:NSEL], pattern=[[-64, NSEL]], compare_op=ALU.is_ge,
                                                                  fill=0.0, base=q0, channel_multiplier=1), r=["ones_f"], w=["svm"])
                    k.op("pool", lambda q, q0=q0: q.affine_select(out=f2[:, 0:NSEL], in_=vm[:, 0:NSEL], pattern=[[64, NSEL]], compare_op=ALU.is_ge,
                                                                  fill=0.0, base=127 - q0, channel_multiplier=-1), r=["svm"], w=["sf2"])
                    k.memset(f2[:, 0:1], 1.0, w=["sf2"], e="pool")
                    k.tt(sc[:, 0:NSEL], psel[:, j, 0:NSEL], vm[:, 0:NSEL], ALU.mult, r=["psel", "svm"], w=["ssc"])
                    k.ts(fm_[:, 0:NSEL], vm[:, 0:NSEL], 1e30, -1e30, ALU.mult, ALU.add, r=["svm"], w=["sfm"])
                    k.tt(sc[:, 0:NSEL], sc[:, 0:NSEL], fm_[:, 0:NSEL], ALU.add, r=["ssc", "sfm"], w=["ssc"])
                    k.stt(sc[:, 0:NSEL], f2[:, 0:NSEL], 1e4, sc[:, 0:NSEL], ALU.mult, ALU.max, r=["sf2", "ssc"], w=["ssc"])
                    if NSEL > NTOP:
                        k.op("dve", lambda q_: q_.max(out=t8[:, 0:8], in_=sc[:, 0:NSEL]), r=["ssc"], w=["st8"])
                        k.op("dve", lambda q_: q_.match_replace(out=sc2[:, 0:NSEL], in_to_replace=t8[:, 0:8], in_values=sc[:, 0:NSEL],
                                                                imm_value=-3e38), r=["ssc", "st8"], w=["ssc2"])
                        k.op("dve", lambda q_: q_.max(out=t8[:, 8:16], in_=sc2[:, 0:NSEL]), r=["ssc2"], w=["st8"])
                        k.ts(selb[:, 0:NSEL], sc[:, 0:NSEL], t8[:, 15:16], None, ALU.is_ge, r=["ssc", "st8"], w=["sselb"])
                        k.ts(selb[:, 0:NSEL], selb[:, 0:NSEL], -NEGB, NEGB, ALU.mult, ALU.add, r=["sselb"], w=["sselb"])
                    else:
                        k.memset(selb[:, 0:NSEL], 0.0, w=["sselb"])
                    k.mm(pss[0:NSEL, 0:128], selb[:, 0:NSEL], c.ident_f[:], True, True, r=["sselb", "ident_f"], w=["aps0"])
                    k.copy(selbT[0:NSEL, q0:q0 + 128], pss[0:NSEL, 0:128], r=["aps0"], w=["selbT"])
                k.barrier()
                k.stack = es0
            with ExitStack() as es:
                k.stack = es
                at = Attn(k, 129)
                ksT = k.sb([128, T], BF16, "ksT")
                kwT = k.sb([128, T], BF16, "kwT")
                Vs = k.sb([128, NT, 129], BF16, "Vs")
                Vw = k.sb([128, NT, 129], BF16, "Vw")
                qT = [k.sb([128, T], BF16, "sqT%d" % i) for i in range(2)]
                osel = [k.sb([128, 128], F32, "osel%d" % i) for i in range(4)]
                ocm = [k.sb([128, 128], F32, "ocm%d" % i) for i in range(2)]
                oo = [k.sb([128, 128], BF16, "oo%d" % i) for i in range(2)]
                den = k.sb([128, 4], F32, "sden")
                k.load(ksT[:], S.kselT[g * 128:(g + 1) * 128, :], w=["ksT"])
                k.load(kwT[:], S.kwinT[g * 128:(g + 1) * 128, :], w=["kwT"])
                k.load(Vs[:, :, 0:128], S.vsel[:, g * 128:(g + 1) * 128].rearrange("(j p) d -> p j d", p=128), w=["Vs"])
                k.load(Vw[:, :, 0:128], S.vwin[:, g * 128:(g + 1) * 128].rearrange("(j p) d -> p j d", p=128), w=["Vw"])
                k.memset(Vs[:, :, 128:129], 1.0, w=["Vs"])
                k.memset(Vw[:, :, 128:129], 1.0, w=["Vw"])
                oi = 0
                for r_ in range(4):
                    h = 4 * g + r_
                    qb = qT[r_ % 2]
                    qk = "sqT%d" % (r_ % 2)
                    k.load(qb[:], S.qT[h * 128:(h + 1) * 128, :], w=[qk])
                    for Q in range(NQB):
                        kts = []
                        for jt in range(4 * Q + 4):
                            b = jt - 4 * Q
                            d_ = dict(kT=ksT[:, jt * 128:(jt + 1) * 128], rk=["ksT"], V=Vs[:, jt, :], rv=["Vs"],
                                      subs=(max(b, 0), 4), bias=(Eb[0:NSEL, jt, :], selbT[0:NSEL, Q * 512:(Q + 1) * 512], ["Eb", "selbT"]))
                            if b >= 0:
                                d_["mask"] = m.caus[:, b, :]
                                d_["rm"] = ["mcaus"]
                            kts.append(d_)
                        at.run(qb[:, Q * 512:(Q + 1) * 512], [qk], kts)
                        res = []
                        for a in range(4):
                            ob = osel[a]
                            okk = "osel%d" % a
                            j = Q * 4 + a
                            gi = (4 * g + r_) * 3
                            k.op("dve", lambda q_, a=a: q_.reciprocal(out=den[:, 0:1], in_=at.acc[a][:, 128:129]), r=["aacc%d" % a], w=["sden"])
                            k.tt(den[:, 0:1], den[:, 0:1], gates[:, j, gi + 1:gi + 2], ALU.mult, r=["sden", "gates"], w=["sden"])
                            k.ts(ob[:], at.acc[a][:, 0:128], den[:, 0:1], None, ALU.mult, r=["aacc%d" % a, "sden"], w=[okk])
                            res.append((ob, okk))
                        kts = []
                        for cc in range(8):
                            jt = 4 * Q - 4 + cc
                            if jt < 0:
                                continue
                            a0 = max(cc - 4, 0)
                            a1 = min(cc + 1, 4)
                            kts.append(dict(kT=kwT[:, jt * 128:(jt + 1) * 128], rk=["kwT"], V=Vw[:, jt, :], rv=["Vw"], subs=(a0, a1),
                                            mask=m.win[:, cc, :], rm=["mwin"]))
                        at.run(qb[:, Q * 512:(Q + 1) * 512], [qk], kts)
                        for a in range(4):
                            j = Q * 4 + a
                            gi = (4 * g + r_) * 3
                            ob, okk = res[a]
                            cmb = ocm[a % 2]
                            ckk = "ocm%d" % (a % 2)
                            k.load(cmb[:], S.ocmp[j * 128:(j + 1) * 128, h * 128:(h + 1) * 128], w=[ckk])
                            k.op("dve", lambda q_, a=a: q_.reciprocal(out=den[:, 1:2], in_=at.acc[a][:, 128:129]), r=["aacc%d" % a], w=["sden"])
                            k.tt(den[:, 1:2], den[:, 1:2], gates[:, j, gi + 2:gi + 3], ALU.mult, r=["sden", "gates"], w=["sden"])
                            k.stt(ob[:], at.acc[a][:, 0:128], den[:, 1:2], ob[:], ALU.mult, ALU.add, r=["aacc%d" % a, "sden", okk], w=[okk])
                            k.stt(oo[a % 2][:], cmb[:], gates[:, j, gi:gi + 1], ob[:], ALU.mult, ALU.add, r=[ckk, "gates", okk], w=["oo%d" % (a % 2)])
                            k.load(S.o_tm[j * 128:(j + 1) * 128, h * 128:(h + 1) * 128], oo[a % 2][:], r=["oo%d" % (a % 2)])
                k.barrier()
                k.stack = es0
        k.stack = old0


def diff_stage(k, c, T, S, I, lambda_init):
    nc = k.nc
    NT = T // 128
    NQB = T // 512
    with ExitStack() as es0:
        k.stack, old0 = es0, k.stack
        m = Ctx()
        make_attn_masks(k, c, m)
        lam = k.sb([128, 512], F32, "lam")
        lt = k.sb([128, 256], F32, "lamt")
        ls = k.sb([128, 4], F32, "lams")
        sub_bc = k.sb([128, 256], F32, "subbc")
        k.load(lam[:], I["od_lambda"].rearrange("a d -> (a d)").rearrange("(o n) -> o n", o=1).to_broadcast([128, 512]), w=["lam"])
        k.load(sub_bc[:], bcast_rows(I["od_subln"], 256), w=["subbc"])
        k.ts(sub_bc[:], sub_bc[:], 1.0 - lambda_init, None, ALU.mult, r=["subbc"], w=["subbc"])
        k.tt(lt[:, 0:128], lam[:, 0:128], lam[:, 128:256], ALU.mult, r=["lam"], w=["lamt"])
        k.tt(lt[:, 128:256], lam[:, 256:384], lam[:, 384:512], ALU.mult, r=["lam"], w=["lamt"])
        k.op("dve", lambda q_: q_.reduce_sum(out=ls[:, 0:1], in_=lt[:, 0:128], axis=AX.X), r=["lamt"], w=["lams"])
        k.op("dve", lambda q_: q_.reduce_sum(out=ls[:, 1:2], in_=lt[:, 128:256], axis=AX.X), r=["lamt"], w=["lams"])
        k.act(ls[:, 0:2], ls[:, 0:2], AF.Exp, r=["lams"], w=["lams"])
        k.tt(ls[:, 2:3], ls[:, 1:2], ls[:, 0:1], ALU.subtract, r=["lams"], w=["lams"])
        k.ts(ls[:, 2:3], ls[:, 2:3], -lambda_init, None, ALU.add, r=["lams"], w=["lams"])
        at = Attn(k, 257)
        qT = [k.sb([128, T], BF16, "dqT%d" % i) for i in range(2)]
        kT = [k.sb([128, T], BF16, "dkT%d" % i) for i in range(2)]
        V1 = [k.sb([128, NT, 257], BF16, "dV%d" % i) for i in range(2)]
        o0 = [k.sb([128, 256], F32, "do0%d" % i) for i in range(4)]
        o1 = [k.sb([128, 256], F32, "do1%d" % i) for i in range(2)]
        sq = k.sb([128, 256], F32, "dsq")
        ss = k.sb([128, 2], F32, "dss")
        den = k.sb([128, 2], F32, "dden")
        ob = [k.sb([128, 256], BF16, "dob%d" % i) for i in range(2)]
        oi = 0
        for h in range(4):
            vb = V1[h % 2]
            vk = "dV%d" % (h % 2)
            k.load(vb[:, :, 0:256], S.dv[:, h * 256:(h + 1) * 256].rearrange("(j p) d -> p j d", p=128), w=[vk])
            k.memset(vb[:, :, 256:257], 1.0, w=[vk])
            for mm_ in range(2):
                hh = 2 * h + mm_
                k.load(qT[mm_][:], S.dqT[hh * 128:(hh + 1) * 128, :], w=["dqT%d" % mm_])
                k.load(kT[mm_][:], S.dkT[hh * 128:(hh + 1) * 128, :], w=["dkT%d" % mm_])
            for Q in range(NQB):
                for mm_ in range(2):
                    kts = []
                    for jt in range(4 * Q + 4):
                        b = jt - 4 * Q
                        d_ = dict(kT=kT[mm_][:, jt * 128:(jt + 1) * 128], rk=["dkT%d" % mm_], V=vb[:, jt, :], rv=[vk], subs=(max(b, 0), 4))
                        if b >= 0:
                            d_["mask"] = m.caus[:, b, :]
                            d_["rm"] = ["mcaus"]
                            d_["meng"] = "pool" if b % 2 else "dve"
                        kts.append(d_)
                    at.run(qT[mm_][:, Q * 512:(Q + 1) * 512], ["dqT%d" % mm_], kts)
                    for a in range(4):
                        j = Q * 4 + a
                        k.op("dve", lambda q_, a=a: q_.reciprocal(out=den[:, 0:1], in_=at.acc[a][:, 256:257]), r=["aacc%d" % a], w=["dden"])
                        if mm_ == 0:
                            k.ts(o0[a][:], at.acc[a][:, 0:256], den[:, 0:1], None, ALU.mult, r=["aacc%d" % a, "dden"], w=["do0%d" % a])
                        else:
                            t1 = o1[a % 2]
                            k.ts(den[:, 0:1], den[:, 0:1], ls[:, 2:3], None, ALU.mult, r=["dden", "lams"], w=["dden"])
                            k.stt(t1[:], at.acc[a][:, 0:256], den[:, 0:1], o0[a][:], ALU.mult, ALU.add,
                                  r=["aacc%d" % a, "dden", "do0%d" % a], w=["do1%d" % (a % 2)])
                            k.act(sq[:], t1[:], AF.Square, r=["do1%d" % (a % 2)], w=["dsq", "dss"], accum_out=ss[:, 0:1])
                            rsqrt(k, ss[:, 1:2], ss[:, 0:1], 1.0 / 256, EPS, ["dss"], ["dss2"])
                            o = ob[oi % 2]
                            okk = "dob%d" % (oi % 2)
                            oi += 1
                            k.stt(o[:], t1[:], ss[:, 1:2], sub_bc[:], ALU.mult, ALU.mult, r=["do1%d" % (a % 2), "dss2", "subbc"], w=[okk])
                            k.load(S.o_tm[j * 128:(j + 1) * 128, 1024 + h * 256:1024 + (h + 1) * 256], o[:], r=[okk])
        k.barrier()
        k.stack = old0


IN_SPECS = [
    ("x", None), ("p", None),
    ("norm_mix", (2, 2048)), ("norm_ffn", (2, 2048)), ("norm_ple", (2, 2048)), ("norm_final", (1, 2048)),
    ("ev_w_in", (2048, 12320)), ("ev_conv_w", (4, 4096)), ("ev_conv_b", (1, 4096)), ("ev_dt_bias", (1, 32)),
    ("ev_a_log", (1, 32)), ("ev_d_skip", (1, 32)), ("ev_gate_norm", (1, 2048)), ("ev_sc_w", (3, 2048)),
    ("ev_w_out", (4096, 2048)),
    ("od_w_in", (2048, 5656)), ("od_cmp_pos", (2, 32, 128)), ("od_cmp_w1", (2, 4096, 128)), ("od_cmp_w2", (2, 128, 128)),
    ("od_lambda", (4, 128)), ("od_subln", (1, 256)), ("od_w_out", (2048, 2048)),
    ("moe_w_group", (2, 2048, 4)), ("moe_b_group", (2, 4)), ("moe_w_expert", (2, 2048, 32)), ("moe_b_expert", (2, 32)),
    ("moe_w_gate0", (32, 2048, 1024)), ("moe_w_up0", (32, 2048, 1024)), ("moe_w_down0", (32, 1024, 2048)),
    ("moe_w_gate1", (32, 2048, 1024)), ("moe_w_up1", (32, 2048, 1024)), ("moe_w_down1", (32, 1024, 2048)),
    ("ple_gate", (2, 2048, 2048)), ("ple_proj", (2, 256, 2048)),
    ("rope_cos", None), ("rope_sin", None),
]


def build_program(T, stages, dbg=(), needed=None):
    nc = bass.Bass("TRN2", target_bir_lowering=False)
    I = {}
    for name, shp in IN_SPECS:
        if needed is not None and name not in needed:
            continue
        if name == "x":
            shp = (T, D)
        elif name == "p":
            shp = (2, T, 256)
        elif name in ("rope_cos", "rope_sin"):
            shp = (32, T)
        hnd = nc.dram_tensor(name, list(shp), F32, kind="ExternalInput")
        I[name] = hnd.ap()
        I["_h_" + name] = hnd
    out = nc.dram_tensor("out", [T, D], F32, kind="ExternalOutput").ap()

    def scr(name, shape, dt):
        kind = "ExternalOutput" if name in dbg else "Internal"
        return nc.dram_tensor(name, list(shape), dt, kind=kind).ap()

    S = Ctx()
    S.hn = scr("hn", [T, D], BF16)
    S.z = scr("z", [T, D], BF16)
    S.xbcT = scr("xbcT", [4096, T], BF16)
    S.xbcT2 = scr("xbcT2", [4096, T], BF16)
    S.dt = scr("dt", [T, 32], F32)
    S.scT = scr("scT", [6144, T], BF16)
    S.y_scT = scr("y_scT", [2048, T], BF16)
    S.y_tm = scr("y_tm", [T, D], BF16)
    S.h1 = scr("h1", [T, D], F32)
    S.h2 = scr("h2", [T, D], F32)
    S.h3 = scr("h3", [T, D], F32)
    NSLOT = ((2 * T + 32 * (BLK - 1)) + BLK - 1) // BLK * BLK
    S.xg = scr("xg", [NSLOT, D], BF16)
    S.yslot = scr("yslot", [NSLOT, D], F32)
    S.blk_e = scr("blk_e", [128, 1], I32)
    S.pbf = scr("pbf", [T, 256], BF16)
    S.h4 = scr("h4", [T, D], F32)
    S.h5 = scr("h5", [T, D], F32)
    S.qT = scr("qT", [1024, T], BF16)
    S.kcmpT = scr("kcmpT", [256, T], BF16)
    S.vcmpT = scr("vcmpT", [256, T], BF16)
    S.kselT = scr("kselT", [256, T], BF16)
    S.vsel = scr("vsel", [T, 256], BF16)
    S.kwinT = scr("kwinT", [256, T], BF16)
    S.vwin = scr("vwin", [T, 256], BF16)
    S.gates = scr("gates", [T, 24], F32)
    S.dqT = scr("dqT", [1024, T], BF16)
    S.dkT = scr("dkT", [1024, T], BF16)
    S.dv = scr("dv", [T, 1024], BF16)
    S.kcT = scr("kcT", [256, 256], BF16)
    S.vc = scr("vc", [256, 256], BF16)
    S.ocmp = scr("ocmp", [T, 1024], F32)
    S.o_tm = scr("o_tm", [T, D], BF16)

    k = K(nc)
    c = Ctx()
    make_consts(k, c)
    if "l0mix" in stages:
        norm_to_dram(k, c, I["x"], I["norm_mix"][0:1, :], S.hn, T, "n0")
        linear_stage(k, S.hn, T, I["ev_w_in"], [
            (0, 2048, "tm", S.z, 0, BF16),
            (2048, 4096, "fm", S.xbcT, 0, BF16),
            (6144, 32, "tm", S.dt, 0, F32),
            (6176, 6144, "fm", S.scT, 0, BF16),
        ], "l0in")
        conv_stage(k, c, T, S.xbcT, S.xbcT2, I["ev_conv_w"], I["ev_conv_b"], S.scT, I["ev_sc_w"], S.y_scT)
        ssd_stage(k, c, T, S.xbcT2, S.dt, S.z, S.y_tm, I["ev_dt_bias"], I["ev_a_log"], I["ev_d_skip"], I["ev_gate_norm"])
        outproj_stage(k, c, T, [("tm", S.y_tm), ("fm", S.y_scT)], I["ev_w_out"], I["x"], S.h1, "l0out")
    if "moe0" in stages:
        zero_dram(k, S.xg, NSLOT, D, BF16)
        moe_stage(k, c, T, 0, S.h1, S.h2, I, S)
    if "ple0" in stages:
        ple_stage(k, c, T, 0, S.h2, S.h3, I, S)
    h_l1 = S.h3
    if "l1in_dbg" in stages:
        h_l1 = I["x"]
    if "l1mix" in stages:
        NC_ = T // 16 - 1
        norm_to_dram(k, c, h_l1, I["norm_mix"][1:2, :], S.hn, T, "n1")
        linear_stage(k, S.hn, T, I["od_w_in"], [
            (0, 1024, "fm", S.qT, 0, BF16), (1024, 256, "fm", S.kcmpT, 0, BF16), (1280, 256, "fm", S.vcmpT, 0, BF16),
            (1536, 256, "fm", S.kselT, 0, BF16), (1792, 256, "tm", S.vsel, 0, BF16), (2048, 256, "fm", S.kwinT, 0, BF16),
            (2304, 256, "tm", S.vwin, 0, BF16), (2560, 24, "tm", S.gates, 0, F32), (2584, 1024, "fm", S.dqT, 0, BF16),
            (3608, 1024, "fm", S.dkT, 0, BF16), (4632, 1024, "tm", S.dv, 0, BF16)], "l1in")
        items = [(S.qT, h * 128) for h in range(8)] + [(S.kselT, g * 128) for g in range(2)] + \
                [(S.kwinT, g * 128) for g in range(2)] + [(S.dqT, h * 128) for h in range(8)] + [(S.dkT, h * 128) for h in range(8)]
        rope_stage(k, c, T, items, I["rope_cos"], I["rope_sin"])
        compress_stage(k, c, T, S, I)
        rope_stage(k, c, T, [], I["rope_cos"], I["rope_sin"], cmp_items=[(S.kcT, 0, NC_), (S.kcT, 128, NC_)])
        nsa_stage(k, c, T, S, I)
        diff_stage(k, c, T, S, I, 0.8 - 0.6 * math.exp(-0.3 * 1))
        outproj_stage(k, c, T, [("tm", S.o_tm)], I["od_w_out"], h_l1, S.h4, "l1out")
    if "moe1" in stages:
        if "moe0" not in stages:
            zero_dram(k, S.xg, NSLOT, D, BF16)
        moe_stage(k, c, T, 1, S.h4, S.h5, I, S)
    if "ple1" in stages:
        ple_stage(k, c, T, 1, S.h5, None, I, S, final_g=I["norm_final"], final_out=out)
    if "copy_h1" in stages:
        with ExitStack() as es:
            k.stack, old = es, k.stack
            tl = [k.sb([128, D], F32, "fin%d" % i) for i in range(2)]
            for j in range(T // 128):
                k.load(tl[j % 2][:], S.h1[j * 128:(j + 1) * 128, :], w=["fin%d" % (j % 2)])
                k.load(out[j * 128:(j + 1) * 128, :], tl[j % 2][:], r=["fin%d" % (j % 2)])
            k.barrier()
            k.stack = old
    k.barrier()
    k.stack.close()
    print("instructions:", k.ninst, "sems:", k.nsem)
    return nc


def rope_tables(T):
    half = 16
    inv = (500000.0 ** (-(np.arange(half, dtype=np.float32) / half))).astype(np.float32)
    ang = np.arange(T, dtype=np.float32)[None, :] * np.concatenate([inv, inv])[:, None]
    return np.cos(ang).astype(np.float32), np.sin(ang).astype(np.float32)


ALL_STAGES = ("l0mix", "moe0", "ple0", "l1mix", "moe1", "ple1")
T_FULL = 4096
N_CORES = 4


def _core_inputs(inputs, b):
    m = {}
    for name, shp in IN_SPECS:
        if name in ("rope_cos", "rope_sin"):
            continue
        if name.startswith("moe_w_") and name[-1] in "01" and name[:-1] in ("moe_w_gate", "moe_w_up", "moe_w_down"):
            a = inputs[name[:-1]][int(name[-1])]
        else:
            a = inputs[name]
            if name == "x":
                a = a[b]
            elif name == "p":
                a = a[:, b]
            elif name in ("norm_mix", "norm_ffn", "norm_ple", "moe_w_group", "moe_b_group", "moe_w_expert", "moe_b_expert",
                          "ple_gate", "ple_proj"):
                pass
            elif name == "norm_final":
                a = a.reshape(1, -1)
            else:
                a = a[0]
        a = np.ascontiguousarray(np.asarray(a), dtype=np.float32)
        if shp:
            a = a.reshape(shp)
        m[name] = a
    return m


def kernel(**inputs):
    T = T_FULL
    nc = build_program(T, ALL_STAGES)
    cos, sin = rope_tables(T)
    in_maps = []
    shared = None
    for b in range(N_CORES):
        m = _core_inputs(inputs, b)
        if shared is None:
            shared = m
        else:
            for name in m:
                if name not in ("x", "p"):
                    m[name] = shared[name]
        m["rope_cos"] = cos
        m["rope_sin"] = sin
        in_maps.append(m)
    res = run_bass_kernel_spmd(nc, in_maps, core_ids=list(range(N_CORES)))
    out = np.stack([np.asarray(r["out"], dtype=np.float32) for r in res.results], axis=0)
    return out
```

```python
import math
from contextlib import ExitStack
import numpy as np
import concourse.bass as bass
import concourse.mybir as mybir
from concourse.bass_utils import run_bass_kernel_spmd

F32 = mybir.dt.float32
BF16 = mybir.dt.bfloat16
I32 = mybir.dt.int32
AF = mybir.ActivationFunctionType
ALU = mybir.AluOpType
AX = mybir.AxisListType

D = 2048
NKC = D // 128
EPS = 1e-6
NDS = 48


class Stamp:
    __slots__ = ("sem", "val", "eng", "name")

    def __init__(self, sem, val, eng, name):
        self.sem, self.val, self.eng, self.name = sem, val, eng, name


class K:
    def __init__(self, nc):
        self.nc = nc
        self.stack = ExitStack()
        self.gstack = self.stack
        self.eng = {"pe": nc.tensor, "act": nc.scalar, "dve": nc.vector, "pool": nc.gpsimd, "sp": nc.sync}
        self.nsem = 0
        self.esem = {}
        self.ecnt = {}
        for e in self.eng:
            self._new_esem(e)
        self.waited = {e: {} for e in self.eng}
        self.dsem = [self._sem("d%d" % i) for i in range(NDS)]
        self.dcnt = [0] * NDS
        self.dlast = [None] * NDS
        self.dnext = 0
        self.trk = {}
        self.uid = 0
        self.ninst = 0

    def _sem(self, name):
        self.nsem += 1
        return (self.gstack.enter_context(self.nc.semaphore(name)), name)

    def _new_esem(self, e):
        self.esem[e] = self._sem("e_%s_%d" % (e, self.nsem))
        self.ecnt[e] = 0

    @staticmethod
    def keys(base, n):
        return ["%s_%d" % (base, i) for i in range(n)]

    def name(self, p):
        self.uid += 1
        return "%s_%d" % (p, self.uid)

    def sb(self, shape, dt, name="t"):
        return self.stack.enter_context(self.nc.sbuf_tensor(self.name(name), list(shape), dt))

    def wait(self, e, st):
        if st is None:
            return
        if self.waited[e].get(st.name, 0) >= st.val:
            return
        self.eng[e].wait_ge(st.sem, st.val)
        self.waited[e][st.name] = st.val
        self.ninst += 1

    def _deps(self, e, r, w):
        for k in r:
            t = self.trk.get(k)
            if t is not None and t[0] is not None:
                if not (e == "pe" and t[0].eng == "pe"):
                    self.wait(e, t[0])
        for k in w:
            t = self.trk.get(k)
            if t is not None:
                if t[0] is not None and not (e == "pe" and t[0].eng == "pe"):
                    self.wait(e, t[0])
                for st in t[1].values():
                    if not (e == "pe" and st.eng == "pe"):
                        self.wait(e, st)

    def _mark(self, st, r, w):
        for k in r:
            t = self.trk.setdefault(k, [None, {}])
            t[1][st.name] = st
        for k in w:
            self.trk[k] = [st, {}]

    def op(self, e, fn, r=(), w=()):
        self._deps(e, r, w)
        ins = fn(self.eng[e])
        if self.ecnt[e] >= 30000:
            self._new_esem(e)
        self.ecnt[e] += 1
        sem, name = self.esem[e]
        ins.then_inc(sem, 1)
        st = Stamp(sem, self.ecnt[e], e, name)
        self._mark(st, r, w)
        self.ninst += 1
        return st

    def dma(self, e, fn, r=(), w=()):
        self._deps(e, r, w)
        j = self.dnext
        self.dnext = (self.dnext + 1) % NDS
        self.wait(e, self.dlast[j])
        ins = fn(self.eng[e])
        sem, name = self.dsem[j]
        self.dcnt[j] += 16
        ins.then_inc(sem, 16)
        st = Stamp(sem, self.dcnt[j], "dma", name)
        self.dlast[j] = st
        self._mark(st, r, w)
        self.ninst += 1
        return st

    def barrier(self):
        lasts = []
        for e in self.eng:
            if self.ecnt[e] > 0:
                sem, name = self.esem[e]
                lasts.append(Stamp(sem, self.ecnt[e], e, name))
        for j in range(NDS):
            if self.dlast[j] is not None:
                lasts.append(self.dlast[j])
        for e in self.eng:
            for st in lasts:
                if st.eng == e:
                    continue
                self.wait(e, st)
        self.trk = {}

    def load(self, out, in_, r=(), w=(), e="sp"):
        return self.dma(e, lambda q: q.dma_start(out=out, in_=in_), r=r, w=w)

    def loadT(self, out, in_, r=(), w=(), e="sp"):
        return self.dma(e, lambda q: q.dma_start_transpose(out=out, in_=in_), r=r, w=w)

    def cast_load(self, out, in_, r=(), w=()):
        return self.dma("pool", lambda q: q.dma_start(out=out, in_=in_), r=r, w=w)

    def mm(self, out, lhsT, rhs, start, stop, r=(), w=()):
        return self.op("pe", lambda q: q.matmul(out, lhsT=lhsT, rhs=rhs, start=start, stop=stop), r=r, w=w)

    def act(self, out, in_, func, r=(), w=(), **kw):
        return self.op("act", lambda q: q.activation(out=out, in_=in_, func=func, **kw), r=r, w=w)

    def ts(self, out, in0, s1, s2, op0, op1=None, r=(), w=(), e="dve", **kw):
        if op1 is None:
            return self.op(e, lambda q: q.tensor_scalar(out=out, in0=in0, scalar1=s1, scalar2=None, op0=op0, **kw), r=r, w=w)
        return self.op(e, lambda q: q.tensor_scalar(out=out, in0=in0, scalar1=s1, scalar2=s2, op0=op0, op1=op1, **kw), r=r, w=w)

    def stt(self, out, in0, scalar, in1, op0, op1, r=(), w=(), e="dve"):
        return self.op(e, lambda q: q.scalar_tensor_tensor(out=out, in0=in0, scalar=scalar, in1=in1, op0=op0, op1=op1), r=r, w=w)

    def tt(self, out, in0, in1, op, r=(), w=(), e="dve"):
        return self.op(e, lambda q: q.tensor_tensor(out=out, in0=in0, in1=in1, op=op), r=r, w=w)

    def copy(self, out, in_, r=(), w=(), e="dve"):
        if e == "act":
            return self.op("act", lambda q: q.copy(out=out, in_=in_), r=r, w=w)
        return self.op(e, lambda q: q.tensor_copy(out=out, in_=in_), r=r, w=w)

    def memset(self, ap, v, w=(), e="dve"):
        return self.op(e, lambda q: q.memset(ap, v), w=w)


class Ctx:
    pass


def bcast_rows(ap1d_row, n):
    return ap1d_row.to_broadcast([128, n])


def make_consts(k, c):
    nc = k.nc
    c.ones_f = k.sb([128, 128], F32, "ones_f")
    c.ones_b = k.sb([128, 128], BF16, "ones_b")
    c.tri_incl_f = k.sb([128, 128], F32, "tri_incl")
    c.tri_strict_b = k.sb([128, 128], BF16, "tri_strict")
    c.mask_gt_f = k.sb([128, 128], F32, "mask_gt")
    c.ident_f = k.sb([128, 128], F32, "ident_f")
    c.iota_p = k.sb([128, 1], F32, "iota_p")
    k.memset(c.ones_f[:], 1.0, w=["ones_f"])
    k.memset(c.ones_b[:], 1.0, w=["ones_b"])
    k.op("pool", lambda q: q.affine_select(out=c.tri_incl_f[:], in_=c.ones_f[:], pattern=[[1, 128]],
                                            compare_op=ALU.is_ge, fill=0.0, base=0, channel_multiplier=-1),
         r=["ones_f"], w=["tri_incl"])
    k.op("pool", lambda q: q.affine_select(out=c.tri_strict_b[:], in_=c.ones_b[:], pattern=[[1, 128]],
                                            compare_op=ALU.is_gt, fill=0.0, base=0, channel_multiplier=-1),
         r=["ones_b"], w=["tri_strict"])
    k.op("pool", lambda q: q.affine_select(out=c.mask_gt_f[:], in_=c.ones_f[:], pattern=[[-1, 128]],
                                            compare_op=ALU.is_gt, fill=0.0, base=0, channel_multiplier=1),
         r=["ones_f"], w=["mask_gt"])
    k.op("pool", lambda q: q.affine_select(out=c.ident_f[:], in_=c.ones_f[:], pattern=[[-1, 128]],
                                            compare_op=ALU.is_equal, fill=0.0, base=0, channel_multiplier=1),
         r=["ones_f"], w=["ident_f"])
    k.op("pool", lambda q: q.iota(c.iota_p[:], pattern=[[0, 1]], base=0, channel_multiplier=1,
                                  allow_small_or_imprecise_dtypes=True), w=["iota_p"])


def rsqrt(k, out, in_, scale, eps, r, w):
    k.act(out, in_, AF.Sqrt, r=r, w=w, scale=scale, bias=eps)
    k.op("dve", lambda q: q.reciprocal(out=out, in_=out), r=w, w=w)


def rmsnorm_tile(k, xt, gbc, out, ss, rstd, junk, keys_r, keys_w, tag):
    k.act(junk, xt, AF.Square, r=keys_r, w=[tag + "junk", tag + "ss"], accum_out=ss)
    rsqrt(k, rstd, ss, 1.0 / D, EPS, [tag + "ss"], [tag + "rstd"])
    k.stt(out, xt, rstd, gbc, ALU.mult, ALU.mult, r=list(keys_r) + [tag + "rstd"], w=keys_w)


def norm_to_dram(k, c, src, g_row, dst_bf, T, tag, dst_f32=None):
    with ExitStack() as es:
        k.stack, old = es, k.stack
        gbc = k.sb([128, D], F32, "gbc")
        xt = [k.sb([128, D], F32, "nx%d" % i) for i in range(2)]
        ob = [k.sb([128, D], BF16, "no%d" % i) for i in range(2)]
        of = [k.sb([128, D], F32, "nf%d" % i) for i in range(2)] if dst_f32 is not None else None
        junk = k.sb([128, D], BF16, "njunk")
        ss = k.sb([128, 1], F32, "nss")
        rstd = k.sb([128, 1], F32, "nrstd")
        k.load(gbc[:], bcast_rows(g_row, D), w=[tag + "g"])
        for j in range(T // 128):
            b = j % 2
            k.load(xt[b][:], src[j * 128:(j + 1) * 128, :], w=[tag + "x%d" % b])
            if dst_f32 is not None:
                rmsnorm_tile(k, xt[b][:], gbc[:], of[b][:], ss[:], rstd[:], junk[:],
                             [tag + "x%d" % b, tag + "g"], [tag + "of%d" % b], tag)
                k.copy(ob[b][:], of[b][:], r=[tag + "of%d" % b], w=[tag + "o%d" % b], e="act")
                k.load(dst_f32[j * 128:(j + 1) * 128, :], of[b][:], r=[tag + "of%d" % b])
            else:
                rmsnorm_tile(k, xt[b][:], gbc[:], ob[b][:], ss[:], rstd[:], junk[:],
                             [tag + "x%d" % b, tag + "g"], [tag + "o%d" % b], tag)
            k.load(dst_bf[j * 128:(j + 1) * 128, :], ob[b][:], r=[tag + "o%d" % b])
        k.barrier()
        k.stack = old


def load_actT(k, dst_tile, src_tm, t0, nt, tag, r=()):
    for kc in range(NKC):
        for s0 in range(0, nt, 512):
            n = min(512, nt - s0)
            k.loadT(dst_tile[:, kc, s0:s0 + n], src_tm[t0 + s0:t0 + s0 + n, kc * 128:(kc + 1) * 128],
                    r=r, w=[tag])


def linear_stage(k, aT_src_tm, T, W, col_specs, tag, kdim=D):
    nkc = kdim // 128
    TS = min(T, 2048)
    with ExitStack() as es:
        k.stack, old = es, k.stack
        aT = k.sb([128, nkc, TS], BF16, "aT")
        wb = [k.sb([128, nkc, 512], BF16, "wb%d" % i) for i in range(2)]
        ot = [k.sb([128, 512], F32, "ot%d" % i) for i in range(2)]
        ob = [k.sb([128, 512], BF16, "ob%d" % i) for i in range(2)]
        ofm = [k.sb([128, TS], BF16, "ofm%d" % i) for i in range(2)]
        ps = [k.stack.enter_context(k.nc.psum_tensor(k.name("lps"), [128, 512], F32)) for _ in range(4)]
        blocks = []
        for (c0, ncols, mode, dst, doff, ddt) in col_specs:
            for b0 in range(0, ncols, 512):
                blocks.append((c0 + b0, min(512, ncols - b0), mode, dst, doff + b0, ddt))
        Wv = W.rearrange("(kc p) n -> p kc n", p=128)
        wi = 0
        pi = 0
        oi = 0
        fi = 0
        for st in range(T // TS):
            t0 = st * TS
            for kc in range(nkc):
                for s0 in range(0, TS, 512):
                    k.loadT(aT[:, kc, s0:s0 + 512], aT_src_tm[t0 + s0:t0 + s0 + 512, kc * 128:(kc + 1) * 128],
                            w=[tag + "aT_%d_%d" % (kc, s0 // 512)])
            for (c0, ncols, mode, dst, doff, ddt) in blocks:
                wbuf = wb[wi % 2]
                wkey = tag + "w%d" % (wi % 2)
                wi += 1
                k.cast_load(wbuf[:, :, 0:ncols], Wv[:, :, c0:c0 + ncols], w=[wkey])
                if mode == "tm":
                    for j in range(TS // 128):
                        p = ps[pi % 4]
                        pkey = tag + "ps%d" % (pi % 4)
                        pi += 1
                        for kc in range(nkc):
                            k.mm(p[:, 0:ncols], aT[:, kc, j * 128:(j + 1) * 128], wbuf[:, kc, 0:ncols],
                                 kc == 0, kc == nkc - 1, r=[tag + "aT_%d_%d" % (kc, j // 4), wkey], w=[pkey])
                        if ddt == F32:
                            o = ot[oi % 2]
                            okey = tag + "ot%d" % (oi % 2)
                        else:
                            o = ob[oi % 2]
                            okey = tag + "ob%d" % (oi % 2)
                        oi += 1
                        k.copy(o[:, 0:ncols], p[:, 0:ncols], r=[pkey], w=[okey], e="act")
                        k.load(dst[t0 + j * 128:t0 + (j + 1) * 128, doff:doff + ncols], o[:, 0:ncols], r=[okey])
                else:
                    for m0 in range(0, ncols, 128):
                        o = ofm[fi % 2]
                        okey = tag + "ofm%d" % (fi % 2)
                        fi += 1
                        for n0 in range(0, TS, 512):
                            p = ps[pi % 4]
                            pkey = tag + "ps%d" % (pi % 4)
                            pi += 1
                            for kc in range(nkc):
                                k.mm(p[:, :], wbuf[:, kc, m0:m0 + 128], aT[:, kc, n0:n0 + 512],
                                     kc == 0, kc == nkc - 1, r=[tag + "aT_%d_%d" % (kc, n0 // 512), wkey], w=[pkey])
                            eng = "act" if (n0 // 512) % 2 == 0 else "dve"
                            k.copy(o[:, n0:n0 + 512], p[:, :], r=[pkey], w=[okey], e=eng)
                        k.load(dst[doff + m0:doff + m0 + 128, t0:t0 + TS], o[:, :], r=[okey])
        k.barrier()
        k.stack = old


def conv_stage(k, c, T, xbcT, xbcT2, conv_w, conv_b, scT, sc_w, y_scT):
    with ExitStack() as es:
        k.stack, old = es, k.stack
        cw = k.sb([128, 128], F32, "cw")
        cb = k.sb([128, 32], F32, "cb")
        sw = k.sb([128, 48], F32, "sw")
        craw = k.sb([128, 128], F32, "craw")
        braw = k.sb([32, 128], F32, "braw")
        sraw = k.sb([48, 128], F32, "sraw")
        cps = k.stack.enter_context(k.nc.psum_tensor(k.name("cps"), [128, 512], F32))
        xin = [k.sb([128, 3 + T], BF16, "xin%d" % i) for i in range(2)]
        acc = [k.sb([128, T], F32, "acc%d" % i) for i in range(2)]
        ob = [k.sb([128, T], BF16, "cob%d" % i) for i in range(2)]
        tb = [k.sb([128, T], BF16, "ctb%d" % i) for i in range(2)]
        th = [k.sb([128, T], BF16, "cth%d" % i) for i in range(2)]
        k.load(craw[:], conv_w.rearrange("k (cc p) -> (k cc) p", p=128), w=["craw"])
        k.load(braw[:], conv_b.rearrange("o (cc p) -> (o cc) p", p=128), w=["braw"])
        k.load(sraw[:], sc_w.rearrange("k (cc p) -> (k cc) p", p=128), w=["sraw"])
        k.mm(cps[:, 0:128], craw[:], c.ident_f[:], True, True, r=["craw", "ident_f"], w=["cps"])
        k.mm(cps[:, 128:160], braw[:], c.ident_f[0:32, 0:32], True, True, r=["braw", "ident_f"], w=["cps"])
        k.mm(cps[:, 160:208], sraw[:], c.ident_f[0:48, 0:48], True, True, r=["sraw", "ident_f"], w=["cps"])
        k.copy(cw[:], cps[:, 0:128], r=["cps"], w=["cw"])
        k.copy(cb[:], cps[:, 128:160], r=["cps"], w=["cb"])
        k.copy(sw[:], cps[:, 160:208], r=["cps"], w=["sw"])
        for i in range(2):
            k.memset(xin[i][:, 0:3], 0.0, w=["xin%d" % i])
        for cc in range(32):
            b = cc % 2
            k.load(xin[b][:, 3:3 + T], xbcT[cc * 128:(cc + 1) * 128, :], w=["xin%d" % b])
            k.ts(acc[b][:], xin[b][:, 0:T], cw[:, cc:cc + 1], None, ALU.mult, r=["xin%d" % b, "cw"], w=["acc%d" % b])
            for kk in range(1, 4):
                k.stt(acc[b][:], xin[b][:, kk:kk + T], cw[:, kk * 32 + cc:kk * 32 + cc + 1], acc[b][:], ALU.mult, ALU.add,
                      r=["xin%d" % b, "cw", "acc%d" % b], w=["acc%d" % b])
            k.act(ob[b][:], acc[b][:], AF.Silu, r=["acc%d" % b, "cb"], w=["cob%d" % b], bias=cb[:, cc:cc + 1])
            k.load(xbcT2[cc * 128:(cc + 1) * 128, :], ob[b][:], r=["cob%d" % b])
        for cc in range(16):
            b = cc % 2
            k.load(tb[b][:], scT[cc * 128:(cc + 1) * 128, :], w=["tb%d" % b])
            k.load(xin[b][:, 3:3 + T], scT[2048 + cc * 128:2048 + (cc + 1) * 128, :], w=["xin%d" % b])
            k.load(th[b][:], scT[4096 + cc * 128:4096 + (cc + 1) * 128, :], w=["th%d" % b])
            k.tt(xin[b][:, 3:3 + T], xin[b][:, 3:3 + T], th[b][:], ALU.mult, r=["xin%d" % b, "th%d" % b], w=["xin%d" % b])
            k.ts(acc[b][:], xin[b][:, 1:1 + T], sw[:, cc:cc + 1], None, ALU.mult, r=["xin%d" % b, "sw"], w=["acc%d" % b])
            for kk in range(1, 3):
                k.stt(acc[b][:], xin[b][:, 1 + kk:1 + kk + T], sw[:, kk * 16 + cc:kk * 16 + cc + 1], acc[b][:], ALU.mult, ALU.add,
                      r=["xin%d" % b, "sw", "acc%d" % b], w=["acc%d" % b])
            k.tt(ob[b][:], acc[b][:], tb[b][:], ALU.mult, r=["acc%d" % b, "tb%d" % b], w=["cob%d" % b])
            k.load(y_scT[cc * 128:(cc + 1) * 128, :], ob[b][:], r=["cob%d" % b])
        k.barrier()
        k.stack = old


def ssd_stage(k, c, T, xbcT2, dt_d, z_d, y_tm, dt_bias, a_log, d_skip, gate_norm):
    NCH = T // 128
    with ExitStack() as es:
        k.stack, old = es, k.stack
        nc = k.nc
        sbt = k.sb
        dtb_bc = sbt([128, 32], F32, "dtb")
        a_bc = sbt([128, 32], F32, "abc")
        dsk_bc = sbt([128, 32], F32, "dsk")
        gn_bc = sbt([128, D], F32, "gnbc")
        k.load(dtb_bc[:], bcast_rows(dt_bias, 32), w=["dtb"])
        k.load(a_bc[:], bcast_rows(a_log, 32), w=["abc"])
        k.load(dsk_bc[:], bcast_rows(d_skip, 32), w=["dsk"])
        k.load(gn_bc[:], bcast_rows(gate_norm, D), w=["gnbc"])
        k.act(a_bc[:], a_bc[:], AF.Exp, r=["abc"], w=["abc"])
        k.ts(a_bc[:], a_bc[:], -1.0, None, ALU.mult, r=["abc"], w=["abc"])
        NB = 2
        xs = [sbt([128, D], BF16, "xs%d" % i) for i in range(NB)]
        Btm = [sbt([128, 1024], BF16, "Btm%d" % i) for i in range(NB)]
        BT = [sbt([128, 8, 128], BF16, "BT%d" % i) for i in range(NB)]
        CT = [sbt([128, 8, 128], BF16, "CT%d" % i) for i in range(NB)]
        dtr = [sbt([128, 32], F32, "dtr%d" % i) for i in range(NB)]
        zt = [sbt([128, D], BF16, "zt%d" % i) for i in range(NB)]
        dtp = sbt([128, 32], F32, "dtp")
        dA = sbt([128, 32], F32, "dA")
        cum = sbt([128, 64], F32, "cum")
        ea = sbt([128, 32], F32, "ea")
        cd = sbt([128, 32], F32, "cd")
        wgt = sbt([128, 32], F32, "wgt")
        G = sbt([128, 128], F32, "G")
        L = [sbt([128, 128], F32, "L%d" % i) for i in range(2)]
        E = [sbt([128, 128], F32, "E%d" % i) for i in range(2)]
        MT = [sbt([128, 128], BF16, "MT%d" % i) for i in range(2)]
        xw = [sbt([128, 256], BF16, "xw%d" % i) for i in range(2)]
        ydsb = [sbt([128, 256], F32, "ydsb%d" % i) for i in range(2)]
        tmp = sbt([128, 256], F32, "ytmp")
        yf = sbt([128, D], F32, "yf")
        sz = sbt([128, D], F32, "sz")
        sq = sbt([128, D], F32, "sq")
        ss8 = sbt([128, 8], F32, "ss8")
        yo = [sbt([128, D], BF16, "yo%d" % i) for i in range(2)]
        state_f = sbt([128, 8, 256], F32, "state_f")
        state_b = sbt([128, 8, 256], BF16, "state_b")
        k.memset(state_f[:], 0.0, w=["state_f%d" % g for g in range(8)])
        k.memset(state_b[:], 0.0, w=["state_b%d" % g for g in range(8)])
        P = lambda nm: k.stack.enter_context(nc.psum_tensor(k.name(nm), [128, 512], F32))
        ps_cum = P("ps_cum")
        ps_cb = [P("ps_cb0"), P("ps_cb1")]
        ps_seg = [P("ps_seg0"), P("ps_seg1")]
        ps_y = [P("ps_y0"), P("ps_y1")]
        ps_st = P("ps_st")

        def issue_loads(ch):
            b = ch % NB
            t0 = ch * 128
            for kc in range(16):
                k.loadT(xs[b][:, kc * 128:(kc + 1) * 128], xbcT2[kc * 128:(kc + 1) * 128, t0:t0 + 128], w=["xs%d_%d" % (b, kc)])
            for kc in range(8):
                k.loadT(Btm[b][:, kc * 128:(kc + 1) * 128], xbcT2[2048 + kc * 128:2048 + (kc + 1) * 128, t0:t0 + 128],
                        w=["Btm%d_%d" % (b, kc)])
            k.load(BT[b][:], xbcT2[2048:3072, t0:t0 + 128].rearrange("(g n) t -> n g t", n=128), w=["BT%d" % b])
            k.load(CT[b][:], xbcT2[3072:4096, t0:t0 + 128].rearrange("(g n) t -> n g t", n=128), w=["CT%d" % b])
            k.load(dtr[b][:], dt_d[t0:t0 + 128, :], w=["dtr%d" % b])
            k.load(zt[b][:], z_d[t0:t0 + 128, :], w=["zt%d" % b])

        issue_loads(0)
        hi = 0
        for ch in range(NCH):
            b = ch % NB
            t0 = ch * 128
            if ch + 1 < NCH:
                issue_loads(ch + 1)
            kBT, kCT = "BT%d" % b, "CT%d" % b
            k.tt(dtp[:], dtr[b][:], dtb_bc[:], ALU.add, r=["dtr%d" % b, "dtb"], w=["dtp"])
            k.act(dtp[:], dtp[:], AF.Exp, r=["dtp"], w=["dtp"])
            k.act(dtp[:], dtp[:], AF.Ln, r=["dtp"], w=["dtp"], bias=1.0)
            k.tt(dA[:], dtp[:], a_bc[:], ALU.mult, r=["dtp", "abc"], w=["dA"])
            k.mm(ps_cum[:, 0:32], c.tri_incl_f[:], dA[:], True, True, r=["tri_incl", "dA"], w=["ps_cum"])
            k.mm(ps_cum[:, 32:64], c.ones_f[:], dA[:], True, True, r=["ones_f", "dA"], w=["ps_cum"])
            k.copy(cum[:], ps_cum[:, 0:64], r=["ps_cum"], w=["cum"])
            k.act(ea[:], cum[:, 0:32], AF.Exp, r=["cum"], w=["ea"])
            k.act(cd[:], cum[:, 32:64], AF.Exp, r=["cum"], w=["cd"])
            k.tt(wgt[:], cum[:, 32:64], cum[:, 0:32], ALU.subtract, r=["cum"], w=["wgt"])
            k.act(wgt[:], wgt[:], AF.Exp, r=["wgt"], w=["wgt"])
            k.tt(wgt[:], wgt[:], dtp[:], ALU.mult, r=["wgt", "dtp"], w=["wgt"])
            for g in range(8):
                pcb = ps_cb[g % 2]
                kpcb = "ps_cb%d" % (g % 2)
                py = ps_y[g % 2]
                kpy = "ps_y%d" % (g % 2)
                k.mm(pcb[:, 0:128], BT[b][:, g, :], CT[b][:, g, :], True, True, r=[kBT, kCT], w=[kpcb])
                k.tt(G[:], pcb[:, 0:128], c.tri_incl_f[:], ALU.mult, r=[kpcb, "tri_incl"], w=["G"])
                for rr in range(4):
                    h = 4 * g + rr
                    i2 = hi % 2
                    hi += 1
                    k.ts(L[i2][:], c.mask_gt_f[:], dA[:, h:h + 1], None, ALU.mult, r=["mask_gt", "dA"], w=["L%d" % i2])
                    k.mm(ps_seg[i2][:, 0:128], L[i2][:], c.tri_incl_f[:], True, True, r=["L%d" % i2, "tri_incl"],
                         w=["ps_seg%d" % i2])
                    k.act(E[i2][:], ps_seg[i2][:, 0:128], AF.Exp, r=["ps_seg%d" % i2], w=["E%d" % i2])
                    k.stt(MT[i2][:], E[i2][:], dtp[:, h:h + 1], G[:], ALU.mult, ALU.mult, r=["E%d" % i2, "dtp", "G"],
                          w=["MT%d" % i2])
                    k.mm(py[:, rr * 64:(rr + 1) * 64], MT[i2][:], xs[b][:, h * 64:(h + 1) * 64], True, True,
                         r=["MT%d" % i2, "xs%d_%d" % (b, h // 2)], w=[kpy])
                    k.ts(xw[g % 2][:, rr * 64:(rr + 1) * 64], xs[b][:, h * 64:(h + 1) * 64], wgt[:, h:h + 1], None, ALU.mult,
                         r=["xs%d_%d" % (b, h // 2), "wgt"], w=["xw%d" % (g % 2)])
                k.mm(py[:, 256:512], CT[b][:, g, :], state_b[:, g, :], True, True, r=[kCT, "state_b%d" % g], w=[kpy])
                k.mm(ps_st[:, 0:256], Btm[b][:, g * 128:(g + 1) * 128], xw[g % 2][:], True, True,
                     r=["Btm%d_%d" % (b, g), "xw%d" % (g % 2)], w=["ps_st"])
                k.copy(ydsb[g % 2][:], py[:, 0:256], r=[kpy], w=["ydsb%d" % (g % 2)], e="act")
                for rr in range(4):
                    h = 4 * g + rr
                    k.stt(tmp[:, rr * 64:(rr + 1) * 64], py[:, 256 + rr * 64:256 + (rr + 1) * 64], ea[:, h:h + 1],
                          ydsb[g % 2][:, rr * 64:(rr + 1) * 64], ALU.mult, ALU.add,
                          r=[kpy, "ea", "ydsb%d" % (g % 2)], w=["ytmp"])
                    k.stt(yf[:, h * 64:(h + 1) * 64], xs[b][:, h * 64:(h + 1) * 64], dsk_bc[:, h:h + 1],
                          tmp[:, rr * 64:(rr + 1) * 64], ALU.mult, ALU.add, r=["xs%d_%d" % (b, h // 2), "dsk", "ytmp"], w=["yf"])
                    k.stt(state_f[:, g, rr * 64:(rr + 1) * 64], state_f[:, g, rr * 64:(rr + 1) * 64], cd[:, h:h + 1],
                          ps_st[:, rr * 64:(rr + 1) * 64], ALU.mult, ALU.add,
                          r=["state_f%d" % g, "cd", "ps_st"], w=["state_f%d" % g])
                k.copy(state_b[:, g, :], state_f[:, g, :], r=["state_f%d" % g], w=["state_b%d" % g], e="act")
            k.act(sz[:], zt[b][:], AF.Silu, r=["zt%d" % b], w=["sz"])
            k.tt(yf[:], yf[:], sz[:], ALU.mult, r=["yf", "sz"], w=["yf"])
            k.tt(sq[:], yf[:], yf[:], ALU.mult, r=["yf"], w=["sq"])
            k.op("dve", lambda q: q.tensor_reduce(out=ss8[:], in_=sq[:].rearrange("p (g e) -> p g e", g=8),
                                                  axis=AX.X, op=ALU.add), r=["sq"], w=["ss8"])
            rsqrt(k, ss8[:], ss8[:], 1.0 / 256, EPS, ["ss8"], ["ss8"])
            o = yo[ch % 2]
            for g in range(8):
                k.stt(o[:, g * 256:(g + 1) * 256], yf[:, g * 256:(g + 1) * 256], ss8[:, g:g + 1],
                      gn_bc[:, g * 256:(g + 1) * 256], ALU.mult, ALU.mult, r=["yf", "ss8", "gnbc"], w=["yo%d" % (ch % 2)])
            k.load(y_tm[t0:t0 + 128, :], o[:], r=["yo%d" % (ch % 2)])
        k.barrier()
        k.stack = old


def outproj_stage(k, c, T, kin_specs, W, resid, dst, tag):
    nkc = W.shape[0] // 128
    SP = 512
    with ExitStack() as es:
        k.stack, old = es, k.stack
        wb = [k.sb([128, nkc, 512], BF16, "owb%d" % i) for i in range(2)]
        mt = [k.sb([128, nkc, SP], BF16, "omt%d" % i) for i in range(2)]
        rt = [k.sb([128, 512], F32, "ort%d" % i) for i in range(2)]
        ot = [k.sb([128, 512], F32, "oot%d" % i) for i in range(2)]
        ps = [k.stack.enter_context(k.nc.psum_tensor(k.name("ops"), [128, 512], F32)) for _ in range(2)]
        Wv = W.rearrange("(kc p) n -> p kc n", p=128)
        it = 0
        si = 0
        for cb in range(D // 512):
            wbuf = wb[cb % 2]
            wkey = tag + "w%d" % (cb % 2)
            k.cast_load(wbuf[:], Wv[:, :, cb * 512:(cb + 1) * 512], w=[wkey])
            for sp in range(T // SP):
                sb_ = si % 2
                si += 1
                s0 = sp * SP
                kc = 0
                for (mode, src) in kin_specs:
                    if mode == "tm":
                        for q in range(src.shape[1] // 128):
                            k.loadT(mt[sb_][:, kc, :], src[s0:s0 + SP, q * 128:(q + 1) * 128], w=[tag + "mt%d_%d" % (sb_, kc)])
                            kc += 1
                    else:
                        n = src.shape[0] // 128
                        k.load(mt[sb_][:, kc:kc + n, :], src[:, s0:s0 + SP].rearrange("(q p) t -> p q t", p=128),
                               w=[tag + "mt%d_%d" % (sb_, q2) for q2 in range(kc, kc + n)])
                        kc += n
                for jj in range(SP // 128):
                    b = it % 2
                    it += 1
                    t0 = s0 + jj * 128
                    k.load(rt[b][:], resid[t0:t0 + 128, cb * 512:(cb + 1) * 512], w=[tag + "rt%d" % b])
                    for kc in range(nkc):
                        k.mm(ps[b][:], mt[sb_][:, kc, jj * 128:(jj + 1) * 128], wbuf[:, kc, :], kc == 0, kc == nkc - 1,
                             r=[tag + "mt%d_%d" % (sb_, kc), wkey], w=[tag + "ps%d" % b])
                    k.tt(ot[b][:], ps[b][:], rt[b][:], ALU.add, r=[tag + "ps%d" % b, tag + "rt%d" % b], w=[tag + "ot%d" % b])
                    k.load(dst[t0:t0 + 128, cb * 512:(cb + 1) * 512], ot[b][:], r=[tag + "ot%d" % b])
        k.barrier()
        k.stack = old


BLK = 256
_FREED = {}


def free_pool_tmps(k, n0):
    import re
    nc = k.nc
    freed = _FREED.setdefault(id(nc), set())
    for i in nc.main_func.blocks[-1].instructions[n0:]:
        for nm in set(re.findall(r"(Pool_tmp[A-Za-z0-9_]*|Pool_Pool_[A-Za-z0-9_]*_snap_[0-9]+)", str(i))):
            if nm not in freed:
                freed.add(nm)
                nc.gpsimd.free_register(bass.RegisterHandle(nm, mybir.EngineType.Pool))


def moe_stage(k, c, T, li, h_in, h_out, I, S):
    NT = T // 128
    NSLOT = ((2 * T + 32 * (BLK - 1)) + BLK - 1) // BLK * BLK
    NBLK = NSLOT // BLK
    assert NBLK <= 128
    nc = k.nc
    with ExitStack() as es0:
        k.stack, old0 = es0, k.stack
        R = k.sb([128, NT, 32], F32, "mR")
        OH1 = k.sb([128, NT, 32], F32, "mOH1")
        OH2 = k.sb([128, NT, 32], F32, "mOH2")
        W12 = k.sb([128, NT, 2], F32, "mW12")
        SI = k.sb([128, NT, 2], I32, "mSI")
        cnt = k.sb([128, 32], F32, "mcnt")
        with ExitStack() as es:
            k.stack = es
            gbc = k.sb([128, D], F32, "gbc")
            wr = k.sb([128, NKC, 36], F32, "wr")
            bias = k.sb([128, 36], F32, "rbias")
            xt = [k.sb([128, D], F32, "mx%d" % i) for i in range(2)]
            of = [k.sb([128, D], F32, "mof%d" % i) for i in range(2)]
            ob = [k.sb([128, D], BF16, "mob%d" % i) for i in range(2)]
            hT = [k.sb([128, NKC, 128], F32, "mhT%d" % i) for i in range(2)]
            junk = k.sb([128, D], BF16, "mjunk")
            ss = k.sb([128, 1], F32, "mss")
            rstd = k.sb([128, 1], F32, "mrstd")
            lg = k.sb([128, 36], F32, "mlg")
            gmx = k.sb([128, 4], F32, "mgmx")
            goh = k.sb([128, 4], F32, "mgoh")
            pen = k.sb([128, 4], F32, "mpen")
            ejk = k.sb([128, 4], F32, "mejk")
            elm = k.sb([128, 32], F32, "melm")
            top8 = k.sb([128, 8], F32, "mtop8")
            sc = k.sb([128, 4], F32, "msc")
            A = k.sb([128, 32], BF16, "mA")
            pst = [k.stack.enter_context(nc.psum_tensor(k.name("mpst"), [128, 512], F32)) for _ in range(4)]
            psl = k.stack.enter_context(nc.psum_tensor(k.name("mpsl"), [128, 512], F32))
            psr = k.stack.enter_context(nc.psum_tensor(k.name("mpsr"), [128, 512], F32))
            k.load(gbc[:], bcast_rows(I["norm_ffn"][li:li + 1, :], D), w=["mg"])
            with nc.allow_non_contiguous_dma(reason="router weights are tiny"):
                k.load(wr[:, :, 0:4], I["moe_w_group"][li].rearrange("(kc p) n -> p kc n", p=128), w=["wr"])
                k.load(wr[:, :, 4:36], I["moe_w_expert"][li].rearrange("(kc p) n -> p kc n", p=128), w=["wr"])
            k.load(bias[:, 0:4], bcast_rows(I["moe_b_group"][li:li + 1, :], 4), w=["rbias"])
            k.load(bias[:, 4:36], bcast_rows(I["moe_b_expert"][li:li + 1, :], 32), w=["rbias"])
            k.memset(cnt[:], 0.0, w=["mcnt"])
            for j in range(NT):
                b = j % 2
                k.load(xt[b][:], h_in[j * 128:(j + 1) * 128, :], w=["mx%d" % b])
                rmsnorm_tile(k, xt[b][:], gbc[:], of[b][:], ss[:], rstd[:], junk[:], ["mx%d" % b, "mg"], ["mof%d" % b], "m")
                k.copy(ob[b][:], of[b][:], r=["mof%d" % b], w=["mob%d" % b], e="act")
                k.load(S.hn[j * 128:(j + 1) * 128, :], ob[b][:], r=["mob%d" % b], w=["hn_%d" % j])
                for q in range(4):
                    for u in range(4):
                        kc = q * 4 + u
                        k.mm(pst[q][:, u * 128:(u + 1) * 128], of[b][:, kc * 128:(kc + 1) * 128], c.ident_f[:], True, True,
                             r=["mof%d" % b, "ident_f"], w=["mpst%d" % q])
                    k.copy(hT[b][:, q * 4:(q + 1) * 4, :], pst[q][:, :].rearrange("p (u t) -> p u t", u=4),
                           r=["mpst%d" % q], w=["mhT%d_%d" % (b, q)], e=("act" if q % 2 == 0 else "dve"))
                for kc in range(NKC):
                    k.mm(psl[:, 0:36], hT[b][:, kc, :], wr[:, kc, :], kc == 0, kc == NKC - 1,
                         r=["mhT%d_%d" % (b, kc // 4), "wr"], w=["mpsl"])
                k.tt(lg[:], psl[:, 0:36], bias[:], ALU.add, r=["mpsl", "rbias"], w=["mlg"])
                k.op("dve", lambda q_: q_.reduce_max(out=gmx[:, 0:1], in_=lg[:, 0:4], axis=AX.X), r=["mlg"], w=["mgmx"])
                k.ts(goh[:], lg[:, 0:4], gmx[:, 0:1], None, ALU.is_equal, r=["mlg", "mgmx"], w=["mgoh"])
                k.ts(gmx[:, 1:2], gmx[:, 0:1], -1.0, None, ALU.mult, r=["mgmx"], w=["mgmx"])
                k.act(ejk[:], lg[:, 0:4], AF.Exp, r=["mlg", "mgmx"], w=["mejk", "mgsum"], bias=gmx[:, 1:2], accum_out=gmx[:, 2:3])
                k.op("dve", lambda q_: q_.reciprocal(out=gmx[:, 3:4], in_=gmx[:, 2:3]), r=["mgsum"], w=["mgprob"])
                k.ts(pen[:], goh[:], 1e30, -1e30, ALU.mult, ALU.add, r=["mgoh"], w=["mpen"])
                for g in range(4):
                    k.ts(elm[:, g * 8:(g + 1) * 8], lg[:, 4 + g * 8:4 + (g + 1) * 8], pen[:, g:g + 1], None, ALU.add,
                         r=["mlg", "mpen"], w=["melm"])
                k.op("dve", lambda q_: q_.max(out=top8[:], in_=elm[:]), r=["melm"], w=["mtop8"])
                k.ts(OH1[:, j, :], elm[:], top8[:, 0:1], None, ALU.is_equal, r=["melm", "mtop8"], w=["mOH1_%d" % j])
                k.ts(OH2[:, j, :], elm[:], top8[:, 1:2], None, ALU.is_equal, r=["melm", "mtop8"], w=["mOH2_%d" % j])
                k.tt(sc[:, 0:1], top8[:, 1:2], top8[:, 0:1], ALU.subtract, r=["mtop8"], w=["msc"])
                k.act(sc[:, 1:2], sc[:, 0:1], AF.Exp, r=["msc"], w=["msc"])
                k.ts(sc[:, 1:2], sc[:, 1:2], 1.0, None, ALU.add, r=["msc"], w=["msc"])
                k.op("dve", lambda q_: q_.reciprocal(out=sc[:, 2:3], in_=sc[:, 1:2]), r=["msc"], w=["msc"])
                k.tt(W12[:, j, 0:1], gmx[:, 3:4], sc[:, 2:3], ALU.mult, r=["mgprob", "msc"], w=["mW12_%d" % j])
                k.tt(W12[:, j, 1:2], gmx[:, 3:4], W12[:, j, 0:1], ALU.subtract, r=["mgprob", "mW12_%d" % j], w=["mW12_%d" % j])
                k.tt(A[:], OH1[:, j, :], OH2[:, j, :], ALU.add, r=["mOH1_%d" % j, "mOH2_%d" % j], w=["mA"])
                k.mm(psr[:, 0:32], c.tri_strict_b[:], A[:], True, True, r=["tri_strict", "mA"], w=["mpsr"])
                k.mm(psr[:, 32:64], c.ones_b[:], A[:], True, True, r=["ones_b", "mA"], w=["mpsr"])
                k.tt(R[:, j, :], psr[:, 0:32], cnt[:], ALU.add, r=["mpsr", "mcnt"], w=["mR_%d" % j])
                k.tt(cnt[:], psr[:, 32:64], cnt[:], ALU.add, r=["mpsr", "mcnt"], w=["mcnt"])
            nb = k.sb([128, 32], F32, "mnb")
            cs = [k.sb([128, 32], F32, "mcs%d" % i) for i in range(2)]
            pstart = k.sb([128, 32], F32, "mpstart")
            tmp = k.sb([128, 32], F32, "mtmp")
            SF = k.sb([128, NT, 2], F32, "mSF")
            be = k.sb([128, 4], F32, "mbe")
            bei = k.sb([128, 1], I32, "mbei")
            k.memset(nb[:], 0.0, w=["mnb"])
            for m in range((T + BLK - 1) // BLK):
                k.stt(nb[:], cnt[:], float(m * BLK), nb[:], ALU.is_gt, ALU.add, r=["mcnt", "mnb"], w=["mnb"])
            k.ts(cs[0][:], nb[:], float(BLK), None, ALU.mult, r=["mnb"], w=["mcs0"])
            k.copy(nb[:], cs[0][:], r=["mcs0"], w=["mnb"])
            cur = 0
            for sh in (1, 2, 4, 8, 16):
                nx = 1 - cur
                k.copy(cs[nx][:, 0:sh], cs[cur][:, 0:sh], r=["mcs%d" % cur], w=["mcs%d" % nx])
                k.tt(cs[nx][:, sh:32], cs[cur][:, sh:32], cs[cur][:, 0:32 - sh], ALU.add, r=["mcs%d" % cur], w=["mcs%d" % nx])
                cur = nx
            pend = cs[cur]
            k.tt(pstart[:], pend[:], nb[:], ALU.subtract, r=["mcs%d" % cur, "mnb"], w=["mpstart"])
            for j in range(NT):
                k.tt(tmp[:], R[:, j, :], pstart[:], ALU.add, r=["mR_%d" % j, "mpstart"], w=["mtmp"])
                k.tt(elm[:], tmp[:], OH1[:, j, :], ALU.mult, r=["mtmp", "mOH1_%d" % j], w=["melm"])
                k.op("dve", lambda q_: q_.reduce_sum(out=SF[:, j, 0:1], in_=elm[:], axis=AX.X), r=["melm"], w=["mSF"])
                k.tt(elm[:], tmp[:], OH2[:, j, :], ALU.mult, r=["mtmp", "mOH2_%d" % j], w=["melm"])
                k.op("dve", lambda q_: q_.reduce_sum(out=SF[:, j, 1:2], in_=elm[:], axis=AX.X), r=["melm"], w=["mSF"])
            k.copy(SI[:], SF[:], r=["mSF"], w=["mSI"])
            k.ts(be[:, 0:1], c.iota_p[:], float(BLK), None, ALU.mult, r=["iota_p"], w=["mbe"])
            k.ts(elm[:], pend[:], be[:, 0:1], None, ALU.is_le, r=["mcs%d" % cur, "mbe"], w=["melm"])
            k.op("dve", lambda q_: q_.reduce_sum(out=be[:, 1:2], in_=elm[:], axis=AX.X), r=["melm"], w=["mbe"])
            k.ts(be[:, 1:2], be[:, 1:2], 31.0, None, ALU.min, r=["mbe"], w=["mbe"])
            k.copy(bei[:], be[:, 1:2], r=["mbe"], w=["mbei"])
            k.load(S.blk_e[:, :], bei[:], r=["mbei"])
            for j in range(NT):
                b = j % 2
                k.load(ob[b][:], S.hn[j * 128:(j + 1) * 128, :], r=["hn_%d" % j], w=["mob%d" % b])
                for kk in range(2):
                    k.dma("pool", lambda q_, j=j, kk=kk, b=b: q_.indirect_dma_start(
                        out=S.xg, out_offset=bass.IndirectOffsetOnAxis(ap=SI[:, j, kk:kk + 1], axis=0),
                        in_=ob[b][:], in_offset=None), r=["mob%d" % b, "mSI"])
            k.barrier()
            k.stack = es0
        mlim = SUBLIM.get("moe", 99)
        with ExitStack() as es:
            if mlim < 2:
                NBLK = 0
            k.stack = es
            xgT = [k.sb([128, NKC, BLK], BF16, "xgT%d" % i) for i in range(2)]
            wgu = [k.sb([128, NKC, 512], BF16, "wgu%d" % i) for i in range(4)]
            wdn = [k.sb([128, 8, 512], BF16, "wdn%d" % i) for i in range(3)]
            hTt = [k.sb([128, 8, BLK], BF16, "hTt%d" % i) for i in range(2)]
            sg = [k.sb([128, BLK], F32, "sg%d" % i) for i in range(2)]
            yt = [k.sb([128, D], F32, "yt%d" % i) for i in range(4)]
            psg = [k.stack.enter_context(nc.psum_tensor(k.name("psg"), [128, 512], F32)) for _ in range(2)]
            psu = [k.stack.enter_context(nc.psum_tensor(k.name("psu"), [128, 512], F32)) for _ in range(2)]
            psy = [k.stack.enter_context(nc.psum_tensor(k.name("psy"), [128, 512], F32)) for _ in range(2)]
            ereg = nc.gpsimd.alloc_register(k.name("ereg"))
            obase = nc.gpsimd.alloc_register(k.name("obase"))
            oreg = [nc.gpsimd.alloc_register(k.name("oreg")) for _ in range(8)]
            Hg, Hu, Hd = I["_h_moe_w_gate%d" % li], I["_h_moe_w_up%d" % li], I["_h_moe_w_down%d" % li]
            PAT_GU = [[1024, 128], [128 * 1024, NKC], [1, 512]]
            PAT_D = [[2048, 128], [128 * 2048, 8], [1, 512]]

            def load_xg(bk):
                bb = bk % 2
                for kc in range(NKC):
                    k.loadT(xgT[bb][:, kc, :], S.xg[bk * BLK:(bk + 1) * BLK, kc * 128:(kc + 1) * 128], w=["xgT%d_%d" % (bb, kc)])

            if NBLK:
                load_xg(0)
            gi = 0
            di = 0
            mi = 0
            yi = 0
            for bk in range(NBLK):
                bb = bk % 2
                if bk + 1 < NBLK:
                    load_xg(bk + 1)
                n_ins0 = len(nc.main_func.blocks[-1].instructions)
                nc.gpsimd.reg_load(ereg, S.blk_e[bk:bk + 1, 0:1])
                nc.gpsimd.reg_mul(obase, ereg, 2048 * 1024)
                ori = 0
                for hq in range(2):
                    gb, ub = wgu[gi % 4], wgu[(gi + 1) % 4]
                    gk, uk = "wgu%d" % (gi % 4), "wgu%d" % ((gi + 1) % 4)
                    gi += 2
                    nc.gpsimd.reg_add(oreg[ori], obase, hq * 512)
                    k.cast_load(gb[:], bass.AP(Hg, oreg[ori], PAT_GU), w=[gk])
                    k.cast_load(ub[:], bass.AP(Hu, oreg[ori], PAT_GU), w=[uk])
                    ori += 1
                    for m in range(4):
                        ffc = hq * 4 + m
                        pi = mi % 2
                        mi += 1
                        for kc in range(NKC):
                            k.mm(psg[pi][:, 0:BLK], gb[:, kc, m * 128:(m + 1) * 128], xgT[bb][:, kc, :], kc == 0, kc == NKC - 1,
                                 r=[gk, "xgT%d_%d" % (bb, kc)], w=["psg%d" % pi])
                        for kc in range(NKC):
                            k.mm(psu[pi][:, 0:BLK], ub[:, kc, m * 128:(m + 1) * 128], xgT[bb][:, kc, :], kc == 0, kc == NKC - 1,
                                 r=[uk, "xgT%d_%d" % (bb, kc)], w=["psu%d" % pi])
                        k.act(sg[pi][:], psg[pi][:, 0:BLK], AF.Silu, r=["psg%d" % pi], w=["sg%d" % pi])
                        k.tt(hTt[bb][:, ffc, :], sg[pi][:], psu[pi][:, 0:BLK], ALU.mult, r=["sg%d" % pi, "psu%d" % pi],
                             w=["hTt%d_%d" % (bb, ffc)])
                yts = [yt[(yi + s_) % 4] for s_ in range(BLK // 128)]
                ytk = ["yt%d" % ((yi + s_) % 4) for s_ in range(BLK // 128)]
                yi += BLK // 128
                for cb in range(4):
                    db = wdn[di % 3]
                    dk = "wdn%d" % (di % 3)
                    di += 1
                    nc.gpsimd.reg_add(oreg[ori], obase, cb * 512)
                    k.cast_load(db[:], bass.AP(Hd, oreg[ori], PAT_D), w=[dk])
                    ori += 1
                    for s_ in range(BLK // 128):
                        pi = mi % 2
                        mi += 1
                        for ffc in range(8):
                            k.mm(psy[pi][:], hTt[bb][:, ffc, s_ * 128:(s_ + 1) * 128], db[:, ffc, :], ffc == 0, ffc == 7,
                                 r=["hTt%d_%d" % (bb, ffc), dk], w=["psy%d" % pi])
                        k.copy(yts[s_][:, cb * 512:(cb + 1) * 512], psy[pi][:], r=["psy%d" % pi], w=[ytk[s_]],
                               e=("act" if s_ % 2 == 0 else "dve"))
                for s_ in range(BLK // 128):
                    k.load(S.yslot[bk * BLK + s_ * 128:bk * BLK + (s_ + 1) * 128, :], yts[s_][:], r=[ytk[s_]])
                free_pool_tmps(k, n_ins0)
            k.barrier()
            k.stack = es0
        with ExitStack() as es:
            k.stack = es
            ht = [k.sb([128, D], F32, "ch%d" % i) for i in range(2)]
            y1 = [k.sb([128, D], F32, "cy1%d" % i) for i in range(2)]
            y2 = [k.sb([128, D], F32, "cy2%d" % i) for i in range(2)]
            for j in range(NT if mlim >= 3 else 0):
                b = j % 2
                k.load(ht[b][:], h_in[j * 128:(j + 1) * 128, :], w=["ch%d" % b])
                k.dma("pool", lambda q_, j=j, b=b: q_.indirect_dma_start(
                    out=y1[b][:], out_offset=None, in_=S.yslot,
                    in_offset=bass.IndirectOffsetOnAxis(ap=SI[:, j, 0:1], axis=0)), w=["cy1%d" % b])
                k.dma("pool", lambda q_, j=j, b=b: q_.indirect_dma_start(
                    out=y2[b][:], out_offset=None, in_=S.yslot,
                    in_offset=bass.IndirectOffsetOnAxis(ap=SI[:, j, 1:2], axis=0)), w=["cy2%d" % b])
                k.stt(ht[b][:], y1[b][:], W12[:, j, 0:1], ht[b][:], ALU.mult, ALU.add, r=["cy1%d" % b, "ch%d" % b], w=["ch%d" % b])
                k.stt(ht[b][:], y2[b][:], W12[:, j, 1:2], ht[b][:], ALU.mult, ALU.add, r=["cy2%d" % b, "ch%d" % b], w=["ch%d" % b])
                k.load(h_out[j * 128:(j + 1) * 128, :], ht[b][:], r=["ch%d" % b])
            k.barrier()
            k.stack = es0
        k.stack = old0


def zero_dram(k, dst, rows, cols, dt):
    with ExitStack() as es:
        k.stack, old = es, k.stack
        z = k.sb([128, cols], dt, "zero")
        k.memset(z[:], 0.0, w=["zero"])
        for r0 in range(0, rows, 128):
            k.load(dst[r0:r0 + 128, :], z[:], r=["zero"])
        k.barrier()
        k.stack = old


def ple_stage(k, c, T, li, h_in, h_out, I, S, final_g=None, final_out=None):
    nc = k.nc
    norm_to_dram(k, c, h_in, I["norm_ple"][li:li + 1, :], S.hn, T, "pl")
    with ExitStack() as es:
        k.stack, old = es, k.stack
        wg = k.sb([128, NKC, D], BF16, "plwg")
        wp = k.sb([128, 2, D], BF16, "plwp")
        aT = [k.sb([128, NKC, 512], BF16, "plaT%d" % i) for i in range(2)]
        pf = [k.sb([128, 256], F32, "plpf%d" % i) for i in range(2)]
        pb = [k.sb([128, 256], BF16, "plpb%d" % i) for i in range(2)]
        pT = [k.sb([128, 2, 512], BF16, "plpT%d" % i) for i in range(2)]
        ht = [k.sb([128, D], F32, "plh%d" % i) for i in range(2)]
        gt = [k.sb([128, 512], F32, "plg%d" % i) for i in range(2)]
        psa = [k.stack.enter_context(nc.psum_tensor(k.name("plpsa"), [128, 512], F32)) for _ in range(2)]
        psb = [k.stack.enter_context(nc.psum_tensor(k.name("plpsb"), [128, 512], F32)) for _ in range(2)]
        if final_g is not None:
            fg = k.sb([128, D], F32, "plfg")
            fo = [k.sb([128, D], F32, "plfo%d" % i) for i in range(2)]
            junk = k.sb([128, D], BF16, "pljunk")
            ss = k.sb([128, 1], F32, "plss")
            rstd = k.sb([128, 1], F32, "plrstd")
            k.load(fg[:], bcast_rows(final_g, D), w=["plfg"])
        k.cast_load(wg[:], I["ple_gate"][li].rearrange("(kc p) n -> p kc n", p=128), w=["plwg"])
        k.cast_load(wp[:], I["ple_proj"][li].rearrange("(kc p) n -> p kc n", p=128), w=["plwp"])
        for j in range(T // 128):
            b = j % 2
            k.load(pf[b][:], I["p"][li, j * 128:(j + 1) * 128, :], w=["plpf%d" % b])
            k.copy(pb[b][:], pf[b][:], r=["plpf%d" % b], w=["plpb%d" % b])
            k.load(S.pbf[j * 128:(j + 1) * 128, :], pb[b][:], r=["plpb%d" % b], w=["pbf_%d" % j])
        mi = 0
        SP = 512
        for sp in range(T // SP):
            sb_ = sp % 2
            s0 = sp * SP
            for kc in range(NKC):
                k.loadT(aT[sb_][:, kc, :], S.hn[s0:s0 + SP, kc * 128:(kc + 1) * 128], w=["plaT%d_%d" % (sb_, kc)])
            for q in range(2):
                k.loadT(pT[sb_][:, q, :], S.pbf[s0:s0 + SP, q * 128:(q + 1) * 128],
                        r=["pbf_%d" % jq for jq in range(s0 // 128, (s0 + SP) // 128)], w=["plpT%d_%d" % (sb_, q)])
            for jj in range(SP // 128):
                j = (s0 // 128) + jj
                b = j % 2
                t0 = j * 128
                k.load(ht[b][:], h_in[t0:t0 + 128, :], w=["plh%d" % b])
                for cb in range(4):
                    pi = mi % 2
                    mi += 1
                    for kc in range(NKC):
                        k.mm(psa[pi][:], aT[sb_][:, kc, jj * 128:(jj + 1) * 128], wg[:, kc, cb * 512:(cb + 1) * 512], kc == 0, kc == NKC - 1,
                             r=["plaT%d_%d" % (sb_, kc), "plwg"], w=["plpsa%d" % pi])
                    for q in range(2):
                        k.mm(psb[pi][:], pT[sb_][:, q, jj * 128:(jj + 1) * 128], wp[:, q, cb * 512:(cb + 1) * 512], q == 0, q == 1,
                             r=["plpT%d_%d" % (sb_, q), "plwp"], w=["plpsb%d" % pi])
                    k.act(gt[pi][:], psa[pi][:], AF.Sigmoid, r=["plpsa%d" % pi], w=["plg%d" % pi])
                    k.tt(gt[pi][:], gt[pi][:], psb[pi][:], ALU.mult, r=["plg%d" % pi, "plpsb%d" % pi], w=["plg%d" % pi])
                    k.tt(ht[b][:, cb * 512:(cb + 1) * 512], ht[b][:, cb * 512:(cb + 1) * 512], gt[pi][:], ALU.add,
                         r=["plg%d" % pi, "plh%d" % b], w=["plh%d" % b])
                if final_g is None:
                    k.load(h_out[t0:t0 + 128, :], ht[b][:], r=["plh%d" % b])
                else:
                    rmsnorm_tile(k, ht[b][:], fg[:], fo[b][:], ss[:], rstd[:], junk[:], ["plh%d" % b, "plfg"], ["plfo%d" % b], "plf")
                    k.load(final_out[t0:t0 + 128, :], fo[b][:], r=["plfo%d" % b])
        k.barrier()
        k.stack = old


SCALE = 128.0 ** -0.5
NEGB = -30000.0


def rope_stage(k, c, T, items, ropeC, ropeS, cmp_items=()):
    nc = k.nc
    with ExitStack() as es:
        k.stack, old = es, k.stack
        Cs = k.sb([32, T], F32, "ropeC")
        Ss = k.sb([32, T], F32, "ropeS")
        pa = k.sb([32, 32], F32, "rpa")
        pb = k.sb([32, 32], F32, "rpb")
        Pm = k.sb([32, 32], BF16, "rPm")
        xt = [k.sb([32, T], BF16, "rx%d" % i) for i in range(2)]
        t1 = [k.sb([32, 512], F32, "rt1%d" % i) for i in range(2)]
        t2 = [k.sb([32, 512], F32, "rt2%d" % i) for i in range(2)]
        ot = [k.sb([32, T], BF16, "ro%d" % i) for i in range(2)]
        ps = [k.stack.enter_context(nc.psum_tensor(k.name("rps"), [128, 512], F32)) for _ in range(2)]
        k.load(Cs[:], ropeC, w=["ropeC"])
        k.load(Ss[:], ropeS, w=["ropeS"])
        k.op("pool", lambda q: q.affine_select(out=pa[:], in_=c.ones_f[0:32, 0:32], pattern=[[1, 32]], compare_op=ALU.is_equal,
                                               fill=0.0, base=-16, channel_multiplier=-1), r=["ones_f"], w=["rpa"])
        k.op("pool", lambda q: q.affine_select(out=pb[:], in_=c.ones_f[0:32, 0:32], pattern=[[-1, 32]], compare_op=ALU.is_equal,
                                               fill=0.0, base=-16, channel_multiplier=1), r=["ones_f"], w=["rpb"])
        k.tt(Pm[:], pa[:], pb[:], ALU.subtract, r=["rpa", "rpb"], w=["rPm"])
        it = 0
        pi = 0
        for (ten, row0, Tn, cstep, c0) in [(a, b, T, 1, 0) for (a, b) in items] + [(a, b, n, 16, 31) for (a, b, n) in cmp_items]:
            b = it % 2
            it += 1
            k.load(xt[b][:, 0:Tn], ten[row0:row0 + 32, 0:Tn], w=["rx%d" % b])
            for n0 in range(0, Tn, 512):
                n = min(512, Tn - n0)
                p = pi % 2
                pi += 1
                k.mm(ps[p][0:32, 0:n], Pm[:], xt[b][:, n0:n0 + n], True, True, r=["rPm", "rx%d" % b], w=["rps%d" % p])
                if cstep == 1:
                    cc, sc_ = Cs[:, n0:n0 + n], Ss[:, n0:n0 + n]
                else:
                    cc = Cs[:, c0 + cstep * n0:c0 + cstep * (n0 + n - 1) + 1:cstep]
                    sc_ = Ss[:, c0 + cstep * n0:c0 + cstep * (n0 + n - 1) + 1:cstep]
                k.tt(t1[p][:, 0:n], xt[b][:, n0:n0 + n], cc, ALU.mult, r=["rx%d" % b, "ropeC"], w=["rt1%d" % p])
                k.tt(t2[p][:, 0:n], ps[p][0:32, 0:n], sc_, ALU.mult, r=["rps%d" % p, "ropeS"], w=["rt2%d" % p])
                k.tt(ot[b][:, n0:n0 + n], t1[p][:, 0:n], t2[p][:, 0:n], ALU.add, r=["rt1%d" % p, "rt2%d" % p], w=["ro%d" % b])
            k.load(ten[row0:row0 + 32, 0:Tn], ot[b][:, 0:Tn], r=["ro%d" % b])
        k.barrier()
        k.stack = old


def compress_stage(k, c, T, S, I):
    nc = k.nc
    NC = T // 16 - 1
    with ExitStack() as es:
        k.stack, old = es, k.stack
        w1 = k.sb([128, 32, 128], BF16, "cw1")
        w2 = k.sb([128, 128], BF16, "cw2")
        posr = k.sb([32, 128], F32, "cposr")
        posT = k.sb([128, 32], BF16, "cposT")
        cb = k.sb([128, 1], F32, "ccb")
        xT = k.sb([128, T], BF16, "cxT")
        h1 = k.sb([128, 256], BF16, "ch1")
        okc = k.sb([128, 256], BF16, "cokc")
        ovc = k.sb([128, 2, 128], BF16, "covc")
        ps1 = k.stack.enter_context(nc.psum_tensor(k.name("cps1"), [128, 512], F32))
        ps2 = k.stack.enter_context(nc.psum_tensor(k.name("cps2"), [128, 512], F32))
        ps3 = k.stack.enter_context(nc.psum_tensor(k.name("cps3"), [128, 512], F32))
        k.memset(okc[:], 0.0, w=["cokc"])
        for kv in range(2):
            k.cast_load(w1[:], I["od_cmp_w1"][kv].rearrange("(j d) o -> d j o", d=128), w=["cw1"])
            k.cast_load(w2[:], I["od_cmp_w2"][kv], w=["cw2"])
            k.load(posr[:], I["od_cmp_pos"][kv], w=["cposr"])
            k.mm(ps3[:, 0:32], posr[:], c.ident_f[0:32, 0:32], True, True, r=["cposr", "ident_f"], w=["cps3"])
            k.copy(posT[:], ps3[:, 0:32], r=["cps3"], w=["cposT"])
            for j in range(32):
                k.mm(ps3[:, 64:65], w1[:, j, :], posT[:, j:j + 1], j == 0, j == 31, r=["cw1", "cposT"], w=["cps3"])
            k.copy(cb[:], ps3[:, 64:65], r=["cps3"], w=["ccb"])
            src = S.kcmpT if kv == 0 else S.vcmpT
            for g in range(2):
                k.load(xT[:], src[g * 128:(g + 1) * 128, :], w=["cxT"])
                for j in range(32):
                    k.mm(ps1[:, 0:NC], w1[:, j, :], xT[:, j:j + 16 * (NC - 1) + 1:16], j == 0, j == 31, r=["cw1", "cxT"], w=["cps1"])
                k.act(h1[:, 0:NC], ps1[:, 0:NC], AF.Silu, r=["cps1", "ccb"], w=["ch1"], bias=cb[:, 0:1])
                if kv == 0:
                    k.mm(ps2[:, 0:NC], w2[:], h1[:, 0:NC], True, True, r=["cw2", "ch1"], w=["cps2"])
                    k.copy(okc[:, 0:NC], ps2[:, 0:NC], r=["cps2"], w=["cokc"])
                    k.load(S.kcT[g * 128:(g + 1) * 128, :], okc[:], r=["cokc"])
                else:
                    for h in range(2):
                        n = min(128, NC - h * 128)
                        if n <= 0:
                            continue
                        k.mm(ps2[0:n, h * 128:(h + 1) * 128], h1[:, h * 128:h * 128 + n], w2[:], True, True, r=["cw2", "ch1"], w=["cps2"])
                    k.memset(ovc[:], 0.0, w=["covc"])
                    for h in range(2):
                        n = min(128, NC - h * 128)
                        if n <= 0:
                            continue
                        k.copy(ovc[0:n, h, :], ps2[0:n, h * 128:(h + 1) * 128], r=["cps2"], w=["covc"])
                    k.load(S.vc[:, g * 128:(g + 1) * 128].rearrange("(h p) d -> p h d", p=128), ovc[:], r=["covc"])
        k.barrier()
        k.stack = old


class Attn:
    def __init__(self, k, NV):
        nc = k.nc
        self.k = k
        self.NV = NV
        self.pst = [k.stack.enter_context(nc.psum_tensor(k.name("aps"), [128, 512], F32)) for _ in range(2)]
        self.acc = [k.stack.enter_context(nc.psum_tensor(k.name("aacc"), [128, 512], F32)) for _ in range(4)]
        self.pT = [k.sb([128, 512], BF16, "apT%d" % i) for i in range(3)]
        self.si = 0
        self.pi = 0

    def run(self, qT_ap, rq, ktiles):
        k = self.k
        NV = self.NV
        first = [None] * 4
        last = [None] * 4
        for ti, t in enumerate(ktiles):
            for a in range(t["subs"][0], t["subs"][1]):
                if first[a] is None:
                    first[a] = ti
                last[a] = ti
        slots = []

        def qk(ti):
            t = ktiles[ti]
            a0, a1 = t["subs"]
            ps = self.pst[self.si % 2]
            pk = "aps%d" % (self.si % 2)
            self.si += 1
            c0, c1 = a0 * 128, a1 * 128
            k.mm(ps[:, c0:c1], t["kT"], qT_ap[:, c0:c1], True, t.get("bias") is None, r=list(t["rk"]) + list(rq), w=[pk])
            if t.get("bias") is not None:
                bl, br, bkeys = t["bias"]
                k.mm(ps[:, c0:c1], bl, br[:, c0:c1], False, True, r=list(bkeys), w=[pk])
            slots.append((ps, pk))

        if ktiles:
            qk(0)
        for ti, t in enumerate(ktiles):
            a0, a1 = t["subs"]
            ps, pk = slots[ti]
            if ti + 1 < len(ktiles):
                qk(ti + 1)
            pT = self.pT[self.pi % 3]
            tk = "apT%d" % (self.pi % 3)
            self.pi += 1
            c0, c1 = a0 * 128, a1 * 128
            k.act(pT[:, c0:c1], ps[:, c0:c1], AF.Exp, r=[pk], w=[tk], scale=SCALE)
            if t.get("mask") is not None:
                k.tt(pT[:, c0:c1], pT[:, c0:c1], t["mask"][:, c0:c1], ALU.mult, r=[tk] + list(t["rm"]), w=[tk],
                     e=t.get("meng", "dve"))
            for a in range(a0, a1):
                k.mm(self.acc[a][:, 0:NV], pT[:, a * 128:(a + 1) * 128], t["V"], first[a] == ti, last[a] == ti,
                     r=[tk] + list(t["rv"]), w=["aacc%d" % a])


def make_attn_masks(k, c, m):
    m.caus = k.sb([128, 4, 512], BF16, "mcaus")
    m.win = k.sb([128, 8, 512], BF16, "mwin")
    ones = k.sb([128, 512], BF16, "mones")
    tmp = k.sb([128, 512], BF16, "mtmp")
    k.memset(ones[:], 1.0, w=["mones"])
    m.ones = ones
    for b in range(4):
        k.op("pool", lambda q, b=b: q.affine_select(out=m.caus[:, b, :], in_=ones[:], pattern=[[1, 512]], compare_op=ALU.is_ge,
                                                    fill=0.0, base=-128 * b, channel_multiplier=-1), r=["mones"], w=["mcaus"])
    for cc in range(8):
        k.op("pool", lambda q, cc=cc: q.affine_select(out=tmp[:], in_=ones[:], pattern=[[1, 512]], compare_op=ALU.is_ge,
                                                      fill=0.0, base=-128 * (cc - 4), channel_multiplier=-1), r=["mones"], w=["mtmp"])
        k.op("pool", lambda q, cc=cc: q.affine_select(out=m.win[:, cc, :], in_=tmp[:], pattern=[[-1, 512]], compare_op=ALU.is_ge,
                                                      fill=0.0, base=128 * (cc - 4) + 511, channel_multiplier=1), r=["mtmp"], w=["mwin"])


def nsa_stage(k, c, T, S, I):
    nc = k.nc
    NT = T // 128
    NQB = T // 512
    NC = T // 16 - 1
    NSEL = T // 64
    NTOP = min(16, NSEL)
    with ExitStack() as es0:
        k.stack, old0 = es0, k.stack
        m = Ctx()
        make_attn_masks(k, c, m)
        Eb = k.sb([128, NT, 128], BF16, "Eb")
        onesE = k.sb([128, NT, 128], BF16, "onesE")
        k.memset(onesE[:], 1.0, w=["onesE"])
        tmpE = k.sb([128, NT, 128], BF16, "tmpE")
        k.op("pool", lambda q: q.affine_select(out=tmpE[:], in_=onesE[:], pattern=[[128, NT], [1, 128]], compare_op=ALU.is_ge,
                                               fill=0.0, base=0, channel_multiplier=-64), r=["onesE"], w=["tmpE"])
        k.op("pool", lambda q: q.affine_select(out=Eb[:], in_=tmpE[:], pattern=[[-128, NT], [-1, 128]], compare_op=ALU.is_ge,
                                               fill=0.0, base=63, channel_multiplier=64), r=["tmpE"], w=["Eb"])
        Am = k.sb([128, 2, 64], BF16, "Am")
        tmpA = k.sb([128, 2, 64], BF16, "tmpA")
        k.op("pool", lambda q: q.affine_select(out=tmpA[:], in_=onesE[:, 0, :].rearrange("p (h b) -> p h b", h=2), pattern=[[128, 2], [-4, 64]],
                                               compare_op=ALU.is_ge, fill=0.0, base=1, channel_multiplier=1), r=["onesE"], w=["tmpA"])
        k.op("pool", lambda q: q.affine_select(out=Am[:], in_=tmpA[:], pattern=[[-128, 2], [4, 64]],
                                               compare_op=ALU.is_ge, fill=0.0, base=3, channel_multiplier=-1), r=["tmpA"], w=["Am"])
        selbT = k.sb([64, T], BF16, "selbT")
        gates = k.sb([128, NT, 24], F32, "gates")
        k.load(gates[:], S.gates.rearrange("(j p) n -> p j n", p=128), w=["gates"])
        k.act(gates[:], gates[:], AF.Sigmoid, r=["gates"], w=["gates"])
        for g in range(2):
            with ExitStack() as es:
                k.stack = es
                at = Attn(k, 193)
                kcT = k.sb([128, 256], BF16, "kcT")
                V1 = k.sb([128, 2, 193], BF16, "cV1")
                qT = [k.sb([128, T], BF16, "cqT%d" % i) for i in range(2)]
                cmask = [k.sb([128, 512], BF16, "cmask%d" % i) for i in range(4)]
                psel = k.sb([128, NT, 64], F32, "psel")
                oc = [k.sb([128, 4, 128], F32, "coc%d" % i) for i in range(2)]
                den = k.sb([128, 4], F32, "cden")
                usb = k.sb([128, 64], F32, "cusb")
                k.load(kcT[:], S.kcT[g * 128:(g + 1) * 128, :], w=["kcT"])
                k.memset(V1[:], 0.0, w=["cV1"])
                k.load(V1[:, :, 0:128], S.vc[:, g * 128:(g + 1) * 128].rearrange("(h p) d -> p h d", p=128), w=["cV1"])
                k.memset(V1[:, :, 128:129], 1.0, w=["cV1"])
                k.copy(V1[:, :, 129:193], Am[:], r=["Am", "cV1"], w=["cV1"])
                k.memset(psel[:], 0.0, w=["psel"])
                ci = 0
                for r_ in range(4):
                    h = 4 * g + r_
                    qb = qT[r_ % 2]
                    qk = "cqT%d" % (r_ % 2)
                    k.load(qb[:], S.qT[h * 128:(h + 1) * 128, :], w=[qk])
                    for Q in range(NQB):
                        kts = []
                        for nt in range(2):
                            if 16 * (nt * 128) + 31 > Q * 512 + 511:
                                continue
                            cm = cmask[ci % 4]
                            ck = "cmask%d" % (ci % 4)
                            ci += 1
                            k.op("pool", lambda q, Q=Q, nt=nt, cm=cm: q.affine_select(
                                out=cm[:], in_=m.ones[:], pattern=[[1, 512]], compare_op=ALU.is_ge, fill=0.0,
                                base=512 * Q - 16 * 128 * nt - 31, channel_multiplier=-16), r=["mones"], w=[ck])
                            kts.append(dict(kT=kcT[:, nt * 128:(nt + 1) * 128], rk=["kcT"], V=V1[:, nt, :], rv=["cV1"],
                                            subs=(0, 4), mask=cm, rm=[ck]))
                        at.run(qb[:, Q * 512:(Q + 1) * 512], [qk], kts)
                        for a in range(4):
                            j = Q * 4 + a
                            ob = oc[j % 2]
                            okk = "coc%d" % (j % 2)
                            k.ts(den[:, 0:1], at.acc[a][:, 128:129], 1e-30, None, ALU.max, r=["aacc%d" % a], w=["cden"])
                            k.op("dve", lambda q_: q_.reciprocal(out=den[:, 1:2], in_=den[:, 0:1]), r=["cden"], w=["cden"])
                            k.ts(ob[:, r_, :], at.acc[a][:, 0:128], den[:, 1:2], None, ALU.mult, r=["aacc%d" % a, "cden"], w=[okk])
                            k.stt(psel[:, j, :], at.acc[a][:, 129:193], den[:, 1:2], psel[:, j, :], ALU.mult, ALU.add,
                                  r=["aacc%d" % a, "cden", "psel"], w=["psel"])
                            k.load(S.ocmp[j * 128:(j + 1) * 128, h * 128:(h + 1) * 128], ob[:, r_, :], r=[okk])
                vm = k.sb([128, 64], F32, "svm")
                fm_ = k.sb([128, 64], F32, "sfm")
                f2 = k.sb([128, 64], F32, "sf2")
                sc = k.sb([128, 64], F32, "ssc")
                sc2 = k.sb([128, 64], F32, "ssc2")
                t8 = k.sb([128, 16], F32, "st8")
                selb = k.sb([128, 64], F32, "sselb")
                pss = k.stack.enter_context(nc.psum_tensor(k.name("spss"), [128, 512], F32)) if False else at.pst[0]
                for j in range(NT):
                    q0 = j * 128
                    k.op("pool", lambda q, q0=q0: q.affine_select(out=vm[:, 0:NSEL], in_=c.ones_f[:, 0:NSEL], pattern=[[-64, NSEL]], compare_op=ALU.is_ge,
                                                                  fill=0.0, base=q0, channel_multiplier=1), r=["ones_f"], w=["svm"])
                    k.op("pool", lambda q, q0=q0: q.affine_select(out=f2[:, 0:NSEL], in_=vm[:, 0:NSEL], pattern=[[64, NSEL]], compare_op=ALU.is_ge,
                                                                  fill=0.0, base=127 - q0, channel_multiplier=-1), r=["svm"], w=["sf2"])
                    k.memset(f2[:, 0:1], 1.0, w=["sf2"], e="pool")
                    k.tt(sc[:, 0:NSEL], psel[:, j, 0:NSEL], vm[:, 0:NSEL], ALU.mult, r=["psel", "svm"], w=["ssc"])
                    k.ts(fm_[:, 0:NSEL], vm[:, 0:NSEL], 1e30, -1e30, ALU.mult, ALU.add, r=["svm"], w=["sfm"])
                    k.tt(sc[:, 0:NSEL], sc[:, 0:NSEL], fm_[:, 0:NSEL], ALU.add, r=["ssc", "sfm"], w=["ssc"])
                    k.stt(sc[:, 0:NSEL], f2[:, 0:NSEL], 1e4, sc[:, 0:NSEL], ALU.mult, ALU.max, r=["sf2", "ssc"], w=["ssc"])
                    if NSEL > NTOP:
                        k.op("dve", lambda q_: q_.max(out=t8[:, 0:8], in_=sc[:, 0:NSEL]), r=["ssc"], w=["st8"])
                        k.op("dve", lambda q_: q_.match_replace(out=sc2[:, 0:NSEL], in_to_replace=t8[:, 0:8], in_values=sc[:, 0:NSEL],
                                                                imm_value=-3e38), r=["ssc", "st8"], w=["ssc2"])
                        k.op("dve", lambda q_: q_.max(out=t8[:, 8:16], in_=sc2[:, 0:NSEL]), r=["ssc2"], w=["st8"])
                        k.ts(selb[:, 0:NSEL], sc[:, 0:NSEL], t8[:, 15:16], None, ALU.is_ge, r=["ssc", "st8"], w=["sselb"])
                        k.ts(selb[:, 0:NSEL], selb[:, 0:NSEL], -NEGB, NEGB, ALU.mult, ALU.add, r=["sselb"], w=["sselb"])
                    else:
                        k.memset(selb[:, 0:NSEL], 0.0, w=["sselb"])
                    k.mm(pss[0:NSEL, 0:128], selb[:, 0:NSEL], c.ident_f[:], True, True, r=["sselb", "ident_f"], w=["aps0"])
                    k.copy(selbT[0:NSEL, q0:q0 + 128], pss[0:NSEL, 0:128], r=["aps0"], w=["selbT"])
                k.barrier()
                k.stack = es0
            with ExitStack() as es:
                k.stack = es
                at = Attn(k, 129)
                ksT = k.sb([128, T], BF16, "ksT")
                kwT = k.sb([128, T], BF16, "kwT")
                Vs = k.sb([128, NT, 129], BF16, "Vs")
                Vw = k.sb([128, NT, 129], BF16, "Vw")
                qT = [k.sb([128, T], BF16, "sqT%d" % i) for i in range(2)]
                osel = [k.sb([128, 128], F32, "osel%d" % i) for i in range(4)]
                ocm = [k.sb([128, 128], F32, "ocm%d" % i) for i in range(2)]
                oo = [k.sb([128, 128], BF16, "oo%d" % i) for i in range(2)]
                den = k.sb([128, 4], F32, "sden")
                k.load(ksT[:], S.kselT[g * 128:(g + 1) * 128, :], w=["ksT"])
                k.load(kwT[:], S.kwinT[g * 128:(g + 1) * 128, :], w=["kwT"])
                k.load(Vs[:, :, 0:128], S.vsel[:, g * 128:(g + 1) * 128].rearrange("(j p) d -> p j d", p=128), w=["Vs"])
                k.load(Vw[:, :, 0:128], S.vwin[:, g * 128:(g + 1) * 128].rearrange("(j p) d -> p j d", p=128), w=["Vw"])
                k.memset(Vs[:, :, 128:129], 1.0, w=["Vs"])
                k.memset(Vw[:, :, 128:129], 1.0, w=["Vw"])
                oi = 0
                for r_ in range(4):
                    h = 4 * g + r_
                    qb = qT[r_ % 2]
                    qk = "sqT%d" % (r_ % 2)
                    k.load(qb[:], S.qT[h * 128:(h + 1) * 128, :], w=[qk])
                    for Q in range(NQB):
                        kts = []
                        for jt in range(4 * Q + 4):
                            b = jt - 4 * Q
                            d_ = dict(kT=ksT[:, jt * 128:(jt + 1) * 128], rk=["ksT"], V=Vs[:, jt, :], rv=["Vs"],
                                      subs=(max(b, 0), 4), bias=(Eb[0:NSEL, jt, :], selbT[0:NSEL, Q * 512:(Q + 1) * 512], ["Eb", "selbT"]))
                            if b >= 0:
                                d_["mask"] = m.caus[:, b, :]
                                d_["rm"] = ["mcaus"]
                            kts.append(d_)
                        at.run(qb[:, Q * 512:(Q + 1) * 512], [qk], kts)
                        res = []
                        for a in range(4):
                            ob = osel[a]
                            okk = "osel%d" % a
                            j = Q * 4 + a
                            gi = (4 * g + r_) * 3
                            k.op("dve", lambda q_, a=a: q_.reciprocal(out=den[:, 0:1], in_=at.acc[a][:, 128:129]), r=["aacc%d" % a], w=["sden"])
                            k.tt(den[:, 0:1], den[:, 0:1], gates[:, j, gi + 1:gi + 2], ALU.mult, r=["sden", "gates"], w=["sden"])
                            k.ts(ob[:], at.acc[a][:, 0:128], den[:, 0:1], None, ALU.mult, r=["aacc%d" % a, "sden"], w=[okk])
                            res.append((ob, okk))
                        kts = []
                        for cc in range(8):
                            jt = 4 * Q - 4 + cc
                            if jt < 0:
                                continue
                            a0 = max(cc - 4, 0)
                            a1 = min(cc + 1, 4)
                            kts.append(dict(kT=kwT[:, jt * 128:(jt + 1) * 128], rk=["kwT"], V=Vw[:, jt, :], rv=["Vw"], subs=(a0, a1),
                                            mask=m.win[:, cc, :], rm=["mwin"]))
                        at.run(qb[:, Q * 512:(Q + 1) * 512], [qk], kts)
                        for a in range(4):
                            j = Q * 4 + a
                            gi = (4 * g + r_) * 3
                            ob, okk = res[a]
                            cmb = ocm[a % 2]
                            ckk = "ocm%d" % (a % 2)
                            k.load(cmb[:], S.ocmp[j * 128:(j + 1) * 128, h * 128:(h + 1) * 128], w=[ckk])
                            k.op("dve", lambda q_, a=a: q_.reciprocal(out=den[:, 1:2], in_=at.acc[a][:, 128:129]), r=["aacc%d" % a], w=["sden"])
                            k.tt(den[:, 1:2], den[:, 1:2], gates[:, j, gi + 2:gi + 3], ALU.mult, r=["sden", "gates"], w=["sden"])
                            k.stt(ob[:], at.acc[a][:, 0:128], den[:, 1:2], ob[:], ALU.mult, ALU.add, r=["aacc%d" % a, "sden", okk], w=[okk])
                            k.stt(oo[a % 2][:], cmb[:], gates[:, j, gi:gi + 1], ob[:], ALU.mult, ALU.add, r=[ckk, "gates", okk], w=["oo%d" % (a % 2)])
                            k.load(S.o_tm[j * 128:(j + 1) * 128, h * 128:(h + 1) * 128], oo[a % 2][:], r=["oo%d" % (a % 2)])
                k.barrier()
                k.stack = es0
        k.stack = old0


def diff_stage(k, c, T, S, I, lambda_init):
    nc = k.nc
    NT = T // 128
    NQB = T // 512
    with ExitStack() as es0:
        k.stack, old0 = es0, k.stack
        m = Ctx()
        make_attn_masks(k, c, m)
        lam = k.sb([128, 512], F32, "lam")
        lt = k.sb([128, 256], F32, "lamt")
        ls = k.sb([128, 4], F32, "lams")
        sub_bc = k.sb([128, 256], F32, "subbc")
        k.load(lam[:], I["od_lambda"].rearrange("a d -> (a d)").rearrange("(o n) -> o n", o=1).to_broadcast([128, 512]), w=["lam"])
        k.load(sub_bc[:], bcast_rows(I["od_subln"], 256), w=["subbc"])
        k.ts(sub_bc[:], sub_bc[:], 1.0 - lambda_init, None, ALU.mult, r=["subbc"], w=["subbc"])
        k.tt(lt[:, 0:128], lam[:, 0:128], lam[:, 128:256], ALU.mult, r=["lam"], w=["lamt"])
        k.tt(lt[:, 128:256], lam[:, 256:384], lam[:, 384:512], ALU.mult, r=["lam"], w=["lamt"])
        k.op("dve", lambda q_: q_.reduce_sum(out=ls[:, 0:1], in_=lt[:, 0:128], axis=AX.X), r=["lamt"], w=["lams"])
        k.op("dve", lambda q_: q_.reduce_sum(out=ls[:, 1:2], in_=lt[:, 128:256], axis=AX.X), r=["lamt"], w=["lams"])
        k.act(ls[:, 0:2], ls[:, 0:2], AF.Exp, r=["lams"], w=["lams"])
        k.tt(ls[:, 2:3], ls[:, 1:2], ls[:, 0:1], ALU.subtract, r=["lams"], w=["lams"])
        k.ts(ls[:, 2:3], ls[:, 2:3], -lambda_init, None, ALU.add, r=["lams"], w=["lams"])
        at = Attn(k, 257)
        qT = [k.sb([128, T], BF16, "dqT%d" % i) for i in range(2)]
        kT = [k.sb([128, T], BF16, "dkT%d" % i) for i in range(2)]
        V1 = [k.sb([128, NT, 257], BF16, "dV%d" % i) for i in range(2)]
        o0 = [k.sb([128, 256], F32, "do0%d" % i) for i in range(4)]
        o1 = [k.sb([128, 256], F32, "do1%d" % i) for i in range(2)]
        sq = k.sb([128, 256], F32, "dsq")
        ss = k.sb([128, 2], F32, "dss")
        den = k.sb([128, 2], F32, "dden")
        ob = [k.sb([128, 256], BF16, "dob%d" % i) for i in range(2)]
        oi = 0
        for h in range(4):
            vb = V1[h % 2]
            vk = "dV%d" % (h % 2)
            k.load(vb[:, :, 0:256], S.dv[:, h * 256:(h + 1) * 256].rearrange("(j p) d -> p j d", p=128), w=[vk])
            k.memset(vb[:, :, 256:257], 1.0, w=[vk])
            for mm_ in range(2):
                hh = 2 * h + mm_
                k.load(qT[mm_][:], S.dqT[hh * 128:(hh + 1) * 128, :], w=["dqT%d" % mm_])
                k.load(kT[mm_][:], S.dkT[hh * 128:(hh + 1) * 128, :], w=["dkT%d" % mm_])
            for Q in range(NQB):
                for mm_ in range(2):
                    kts = []
                    for jt in range(4 * Q + 4):
                        b = jt - 4 * Q
                        d_ = dict(kT=kT[mm_][:, jt * 128:(jt + 1) * 128], rk=["dkT%d" % mm_], V=vb[:, jt, :], rv=[vk], subs=(max(b, 0), 4))
                        if b >= 0:
                            d_["mask"] = m.caus[:, b, :]
                            d_["rm"] = ["mcaus"]
                            d_["meng"] = "pool" if b % 2 else "dve"
                        kts.append(d_)
                    at.run(qT[mm_][:, Q * 512:(Q + 1) * 512], ["dqT%d" % mm_], kts)
                    for a in range(4):
                        j = Q * 4 + a
                        k.op("dve", lambda q_, a=a: q_.reciprocal(out=den[:, 0:1], in_=at.acc[a][:, 256:257]), r=["aacc%d" % a], w=["dden"])
                        if mm_ == 0:
                            k.ts(o0[a][:], at.acc[a][:, 0:256], den[:, 0:1], None, ALU.mult, r=["aacc%d" % a, "dden"], w=["do0%d" % a])
                        else:
                            t1 = o1[a % 2]
                            k.ts(den[:, 0:1], den[:, 0:1], ls[:, 2:3], None, ALU.mult, r=["dden", "lams"], w=["dden"])
                            k.stt(t1[:], at.acc[a][:, 0:256], den[:, 0:1], o0[a][:], ALU.mult, ALU.add,
                                  r=["aacc%d" % a, "dden", "do0%d" % a], w=["do1%d" % (a % 2)])
                            k.act(sq[:], t1[:], AF.Square, r=["do1%d" % (a % 2)], w=["dsq", "dss"], accum_out=ss[:, 0:1])
                            rsqrt(k, ss[:, 1:2], ss[:, 0:1], 1.0 / 256, EPS, ["dss"], ["dss2"])
                            o = ob[oi % 2]
                            okk = "dob%d" % (oi % 2)
                            oi += 1
                            k.stt(o[:], t1[:], ss[:, 1:2], sub_bc[:], ALU.mult, ALU.mult, r=["do1%d" % (a % 2), "dss2", "subbc"], w=[okk])
                            k.load(S.o_tm[j * 128:(j + 1) * 128, 1024 + h * 256:1024 + (h + 1) * 256], o[:], r=[okk])
        k.barrier()
        k.stack = old0


IN_SPECS = [
    ("x", None), ("p", None),
    ("norm_mix", (2, 2048)), ("norm_ffn", (2, 2048)), ("norm_ple", (2, 2048)), ("norm_final", (1, 2048)),
    ("ev_w_in", (2048, 12320)), ("ev_conv_w", (4, 4096)), ("ev_conv_b", (1, 4096)), ("ev_dt_bias", (1, 32)),
    ("ev_a_log", (1, 32)), ("ev_d_skip", (1, 32)), ("ev_gate_norm", (1, 2048)), ("ev_sc_w", (3, 2048)),
    ("ev_w_out", (4096, 2048)),
    ("od_w_in", (2048, 5656)), ("od_cmp_pos", (2, 32, 128)), ("od_cmp_w1", (2, 4096, 128)), ("od_cmp_w2", (2, 128, 128)),
    ("od_lambda", (4, 128)), ("od_subln", (1, 256)), ("od_w_out", (2048, 2048)),
    ("moe_w_group", (2, 2048, 4)), ("moe_b_group", (2, 4)), ("moe_w_expert", (2, 2048, 32)), ("moe_b_expert", (2, 32)),
    ("moe_w_gate0", (32, 2048, 1024)), ("moe_w_up0", (32, 2048, 1024)), ("moe_w_down0", (32, 1024, 2048)),
    ("moe_w_gate1", (32, 2048, 1024)), ("moe_w_up1", (32, 2048, 1024)), ("moe_w_down1", (32, 1024, 2048)),
    ("ple_gate", (2, 2048, 2048)), ("ple_proj", (2, 256, 2048)),
    ("rope_cos", None), ("rope_sin", None),
]


SUBLIM = {}


def build_program(T, stages, dbg=(), needed=None):
    nc = bass.Bass("TRN2", target_bir_lowering=False)
    I = {}
    for name, shp in IN_SPECS:
        if needed is not None and name not in needed:
            continue
        if name == "x":
            shp = (T, D)
        elif name == "p":
            shp = (2, T, 256)
        elif name in ("rope_cos", "rope_sin"):
            shp = (32, T)
        hnd = nc.dram_tensor(name, list(shp), F32, kind="ExternalInput")
        I[name] = hnd.ap()
        I["_h_" + name] = hnd
    out = nc.dram_tensor("out", [T, D], F32, kind="ExternalOutput").ap()

    def scr(name, shape, dt):
        kind = "ExternalOutput" if name in dbg else "Internal"
        return nc.dram_tensor(name, list(shape), dt, kind=kind).ap()

    S = Ctx()
    S.hn = scr("hn", [T, D], BF16)
    S.z = scr("z", [T, D], BF16)
    S.xbcT = scr("xbcT", [4096, T], BF16)
    S.xbcT2 = scr("xbcT2", [4096, T], BF16)
    S.dt = scr("dt", [T, 32], F32)
    S.scT = scr("scT", [6144, T], BF16)
    S.y_scT = scr("y_scT", [2048, T], BF16)
    S.y_tm = scr("y_tm", [T, D], BF16)
    S.h1 = scr("h1", [T, D], F32)
    S.h2 = scr("h2", [T, D], F32)
    S.h3 = scr("h3", [T, D], F32)
    NSLOT = ((2 * T + 32 * (BLK - 1)) + BLK - 1) // BLK * BLK
    S.xg = scr("xg", [NSLOT, D], BF16)
    S.yslot = scr("yslot", [NSLOT, D], F32)
    S.blk_e = scr("blk_e", [128, 1], I32)
    S.pbf = scr("pbf", [T, 256], BF16)
    S.h4 = scr("h4", [T, D], F32)
    S.h5 = scr("h5", [T, D], F32)
    S.qT = scr("qT", [1024, T], BF16)
    S.kcmpT = scr("kcmpT", [256, T], BF16)
    S.vcmpT = scr("vcmpT", [256, T], BF16)
    S.kselT = scr("kselT", [256, T], BF16)
    S.vsel = scr("vsel", [T, 256], BF16)
    S.kwinT = scr("kwinT", [256, T], BF16)
    S.vwin = scr("vwin", [T, 256], BF16)
    S.gates = scr("gates", [T, 24], F32)
    S.dqT = scr("dqT", [1024, T], BF16)
    S.dkT = scr("dkT", [1024, T], BF16)
    S.dv = scr("dv", [T, 1024], BF16)
    S.kcT = scr("kcT", [256, 256], BF16)
    S.vc = scr("vc", [256, 256], BF16)
    S.ocmp = scr("ocmp", [T, 1024], F32)
    S.o_tm = scr("o_tm", [T, D], BF16)

    k = K(nc)
    c = Ctx()
    make_consts(k, c)
    if "l0mix" in stages:
        lim = SUBLIM.get("l0mix", 99)
        norm_to_dram(k, c, I["x"], I["norm_mix"][0:1, :], S.hn, T, "n0")
        if lim >= 2:
          linear_stage(k, S.hn, T, I["ev_w_in"], [
            (0, 2048, "tm", S.z, 0, BF16),
            (2048, 4096, "fm", S.xbcT, 0, BF16),
            (6144, 32, "tm", S.dt, 0, F32),
            (6176, 6144, "fm", S.scT, 0, BF16),
        ], "l0in")
        if lim >= 3:
            conv_stage(k, c, T, S.xbcT, S.xbcT2, I["ev_conv_w"], I["ev_conv_b"], S.scT, I["ev_sc_w"], S.y_scT)
        if lim >= 4:
            ssd_stage(k, c, T, S.xbcT2, S.dt, S.z, S.y_tm, I["ev_dt_bias"], I["ev_a_log"], I["ev_d_skip"], I["ev_gate_norm"])
        if lim >= 5:
            outproj_stage(k, c, T, [("tm", S.y_tm), ("fm", S.y_scT)], I["ev_w_out"], I["x"], S.h1, "l0out")
    if "moe0" in stages:
        zero_dram(k, S.xg, NSLOT, D, BF16)
        moe_stage(k, c, T, 0, S.h1, S.h2, I, S)
    if "ple0" in stages:
        ple_stage(k, c, T, 0, S.h2, S.h3, I, S)
    h_l1 = S.h3
    if "l1in_dbg" in stages:
        h_l1 = I["x"]
    if "l1mix" in stages:
        NC_ = T // 16 - 1
        lim = SUBLIM.get("l1mix", 99)
        norm_to_dram(k, c, h_l1, I["norm_mix"][1:2, :], S.hn, T, "n1")
        if lim >= 2:
          linear_stage(k, S.hn, T, I["od_w_in"], [
            (0, 1024, "fm", S.qT, 0, BF16), (1024, 256, "fm", S.kcmpT, 0, BF16), (1280, 256, "fm", S.vcmpT, 0, BF16),
            (1536, 256, "fm", S.kselT, 0, BF16), (1792, 256, "tm", S.vsel, 0, BF16), (2048, 256, "fm", S.kwinT, 0, BF16),
            (2304, 256, "tm", S.vwin, 0, BF16), (2560, 24, "tm", S.gates, 0, F32), (2584, 1024, "fm", S.dqT, 0, BF16),
            (3608, 1024, "fm", S.dkT, 0, BF16), (4632, 1024, "tm", S.dv, 0, BF16)], "l1in")
        items = [(S.qT, h * 128) for h in range(8)] + [(S.kselT, g * 128) for g in range(2)] + \
                [(S.kwinT, g * 128) for g in range(2)] + [(S.dqT, h * 128) for h in range(8)] + [(S.dkT, h * 128) for h in range(8)]
        if lim >= 3:
            rope_stage(k, c, T, items, I["rope_cos"], I["rope_sin"])
            compress_stage(k, c, T, S, I)
            rope_stage(k, c, T, [], I["rope_cos"], I["rope_sin"], cmp_items=[(S.kcT, 0, NC_), (S.kcT, 128, NC_)])
        if lim >= 4:
            nsa_stage(k, c, T, S, I)
        if lim >= 5:
            diff_stage(k, c, T, S, I, 0.8 - 0.6 * math.exp(-0.3 * 1))
        if lim >= 6:
            outproj_stage(k, c, T, [("tm", S.o_tm)], I["od_w_out"], h_l1, S.h4, "l1out")
    if "moe1" in stages:
        if "moe0" not in stages:
            zero_dram(k, S.xg, NSLOT, D, BF16)
        moe_stage(k, c, T, 1, S.h4, S.h5, I, S)
    if "ple1" in stages:
        ple_stage(k, c, T, 1, S.h5, None, I, S, final_g=I["norm_final"], final_out=out)
    if "copy_h1" in stages:
        with ExitStack() as es:
            k.stack, old = es, k.stack
            tl = [k.sb([128, D], F32, "fin%d" % i) for i in range(2)]
            for j in range(T // 128):
                k.load(tl[j % 2][:], S.h1[j * 128:(j + 1) * 128, :], w=["fin%d" % (j % 2)])
                k.load(out[j * 128:(j + 1) * 128, :], tl[j % 2][:], r=["fin%d" % (j % 2)])
            k.barrier()
            k.stack = old
    k.barrier()
    k.stack.close()
    print("instructions:", k.ninst, "sems:", k.nsem)
    return nc


def rope_tables(T):
    half = 16
    inv = (500000.0 ** (-(np.arange(half, dtype=np.float32) / half))).astype(np.float32)
    ang = np.arange(T, dtype=np.float32)[None, :] * np.concatenate([inv, inv])[:, None]
    return np.cos(ang).astype(np.float32), np.sin(ang).astype(np.float32)


ALL_STAGES = ("l0mix", "moe0", "ple0", "l1mix", "moe1", "ple1")
T_FULL = 4096
N_CORES = 4


def _core_inputs(inputs, b):
    m = {}
    for name, shp in IN_SPECS:
        if name in ("rope_cos", "rope_sin"):
            continue
        if name.startswith("moe_w_") and name[-1] in "01" and name[:-1] in ("moe_w_gate", "moe_w_up", "moe_w_down"):
            a = inputs[name[:-1]][int(name[-1])]
        else:
            a = inputs[name]
            if name == "x":
                a = a[b]
            elif name == "p":
                a = a[:, b]
            elif name in ("norm_mix", "norm_ffn", "norm_ple", "moe_w_group", "moe_b_group", "moe_w_expert", "moe_b_expert",
                          "ple_gate", "ple_proj"):
                pass
            elif name == "norm_final":
                a = a.reshape(1, -1)
            else:
                a = a[0]
        a = np.ascontiguousarray(np.asarray(a), dtype=np.float32)
        if shp:
            a = a.reshape(shp)
        m[name] = a
    return m


def kernel(**inputs):
    T = T_FULL
    nc = build_program(T, ALL_STAGES)
    cos, sin = rope_tables(T)
    in_maps = []
    shared = None
    for b in range(N_CORES):
        m = _core_inputs(inputs, b)
        if shared is None:
            shared = m
        else:
            for name in m:
                if name not in ("x", "p"):
                    m[name] = shared[name]
        m["rope_cos"] = cos
        m["rope_sin"] = sin
        in_maps.append(m)
    res = run_bass_kernel_spmd(nc, in_maps, core_ids=list(range(N_CORES)))
    out = np.stack([np.asarray(r["out"], dtype=np.float32) for r in res.results], axis=0)
    return out
```

```python
import math
from contextlib import ExitStack
import numpy as np
import concourse.bass as bass
import concourse.mybir as mybir
from concourse.bass_utils import run_bass_kernel_spmd

F32 = mybir.dt.float32
BF16 = mybir.dt.bfloat16
I32 = mybir.dt.int32
AF = mybir.ActivationFunctionType
ALU = mybir.AluOpType
AX = mybir.AxisListType

D = 2048
NKC = D // 128
EPS = 1e-6
NDS = 48


class Stamp:
    __slots__ = ("sem", "val", "eng", "name")

    def __init__(self, sem, val, eng, name):
        self.sem, self.val, self.eng, self.name = sem, val, eng, name


class K:
    def __init__(self, nc):
        self.nc = nc
        self.stack = ExitStack()
        self.gstack = self.stack
        self.eng = {"pe": nc.tensor, "act": nc.scalar, "dve": nc.vector, "pool": nc.gpsimd, "sp": nc.sync}
        self.nsem = 0
        self.esem = {}
        self.ecnt = {}
        for e in self.eng:
            self._new_esem(e)
        self.waited = {e: {} for e in self.eng}
        self.dsem = [self._sem("d%d" % i) for i in range(NDS)]
        self.dcnt = [0] * NDS
        self.dlast = [None] * NDS
        self.dnext = 0
        self.trk = {}
        self.uid = 0
        self.ninst = 0

    def _sem(self, name):
        self.nsem += 1
        return (self.gstack.enter_context(self.nc.semaphore(name)), name)

    def _new_esem(self, e):
        self.esem[e] = self._sem("e_%s_%d" % (e, self.nsem))
        self.ecnt[e] = 0

    @staticmethod
    def keys(base, n):
        return ["%s_%d" % (base, i) for i in range(n)]

    def name(self, p):
        self.uid += 1
        return "%s_%d" % (p, self.uid)

    def sb(self, shape, dt, name="t"):
        return self.stack.enter_context(self.nc.sbuf_tensor(self.name(name), list(shape), dt))

    def wait(self, e, st):
        if st is None:
            return
        if self.waited[e].get(st.name, 0) >= st.val:
            return
        self.eng[e].wait_ge(st.sem, st.val)
        self.waited[e][st.name] = st.val
        self.ninst += 1

    def _deps(self, e, r, w):
        for k in r:
            t = self.trk.get(k)
            if t is not None and t[0] is not None:
                if not (e == "pe" and t[0].eng == "pe"):
                    self.wait(e, t[0])
        for k in w:
            t = self.trk.get(k)
            if t is not None:
                if t[0] is not None and not (e == "pe" and t[0].eng == "pe"):
                    self.wait(e, t[0])
                for st in t[1].values():
                    if not (e == "pe" and st.eng == "pe"):
                        self.wait(e, st)

    def _mark(self, st, r, w):
        for k in r:
            t = self.trk.setdefault(k, [None, {}])
            t[1][st.name] = st
        for k in w:
            self.trk[k] = [st, {}]

    def op(self, e, fn, r=(), w=()):
        self._deps(e, r, w)
        ins = fn(self.eng[e])
        if self.ecnt[e] >= 30000:
            self._new_esem(e)
        self.ecnt[e] += 1
        sem, name = self.esem[e]
        ins.then_inc(sem, 1)
        st = Stamp(sem, self.ecnt[e], e, name)
        self._mark(st, r, w)
        self.ninst += 1
        return st

    def dma(self, e, fn, r=(), w=()):
        self._deps(e, r, w)
        j = self.dnext
        self.dnext = (self.dnext + 1) % NDS
        self.wait(e, self.dlast[j])
        ins = fn(self.eng[e])
        sem, name = self.dsem[j]
        self.dcnt[j] += 16
        ins.then_inc(sem, 16)
        st = Stamp(sem, self.dcnt[j], "dma", name)
        self.dlast[j] = st
        self._mark(st, r, w)
        self.ninst += 1
        return st

    def barrier(self):
        lasts = []
        for e in self.eng:
            if self.ecnt[e] > 0:
                sem, name = self.esem[e]
                lasts.append(Stamp(sem, self.ecnt[e], e, name))
        for j in range(NDS):
            if self.dlast[j] is not None:
                lasts.append(self.dlast[j])
        for e in self.eng:
            for st in lasts:
                if st.eng == e:
                    continue
                self.wait(e, st)
        self.trk = {}

    def load(self, out, in_, r=(), w=(), e="sp"):
        return self.dma(e, lambda q: q.dma_start(out=out, in_=in_), r=r, w=w)

    def loadT(self, out, in_, r=(), w=(), e="sp"):
        return self.dma(e, lambda q: q.dma_start_transpose(out=out, in_=in_), r=r, w=w)

    def cast_load(self, out, in_, r=(), w=()):
        return self.dma("pool", lambda q: q.dma_start(out=out, in_=in_), r=r, w=w)

    def mm(self, out, lhsT, rhs, start, stop, r=(), w=()):
        return self.op("pe", lambda q: q.matmul(out, lhsT=lhsT, rhs=rhs, start=start, stop=stop), r=r, w=w)

    def act(self, out, in_, func, r=(), w=(), **kw):
        return self.op("act", lambda q: q.activation(out=out, in_=in_, func=func, **kw), r=r, w=w)

    def ts(self, out, in0, s1, s2, op0, op1=None, r=(), w=(), e="dve", **kw):
        if op1 is None:
            return self.op(e, lambda q: q.tensor_scalar(out=out, in0=in0, scalar1=s1, scalar2=None, op0=op0, **kw), r=r, w=w)
        return self.op(e, lambda q: q.tensor_scalar(out=out, in0=in0, scalar1=s1, scalar2=s2, op0=op0, op1=op1, **kw), r=r, w=w)

    def stt(self, out, in0, scalar, in1, op0, op1, r=(), w=(), e="dve"):
        return self.op(e, lambda q: q.scalar_tensor_tensor(out=out, in0=in0, scalar=scalar, in1=in1, op0=op0, op1=op1), r=r, w=w)

    def tt(self, out, in0, in1, op, r=(), w=(), e="dve"):
        return self.op(e, lambda q: q.tensor_tensor(out=out, in0=in0, in1=in1, op=op), r=r, w=w)

    def copy(self, out, in_, r=(), w=(), e="dve"):
        if e == "act":
            return self.op("act", lambda q: q.copy(out=out, in_=in_), r=r, w=w)
        return self.op(e, lambda q: q.tensor_copy(out=out, in_=in_), r=r, w=w)

    def memset(self, ap, v, w=(), e="dve"):
        return self.op(e, lambda q: q.memset(ap, v), w=w)


class Ctx:
    pass


def bcast_rows(ap1d_row, n):
    return ap1d_row.to_broadcast([128, n])


def make_consts(k, c):
    nc = k.nc
    c.ones_f = k.sb([128, 128], F32, "ones_f")
    c.ones_b = k.sb([128, 128], BF16, "ones_b")
    c.tri_incl_f = k.sb([128, 128], F32, "tri_incl")
    c.tri_strict_b = k.sb([128, 128], BF16, "tri_strict")
    c.mask_gt_f = k.sb([128, 128], F32, "mask_gt")
    c.ident_f = k.sb([128, 128], F32, "ident_f")
    c.iota_p = k.sb([128, 1], F32, "iota_p")
    k.memset(c.ones_f[:], 1.0, w=["ones_f"])
    k.memset(c.ones_b[:], 1.0, w=["ones_b"])
    k.op("pool", lambda q: q.affine_select(out=c.tri_incl_f[:], in_=c.ones_f[:], pattern=[[1, 128]],
                                            compare_op=ALU.is_ge, fill=0.0, base=0, channel_multiplier=-1),
         r=["ones_f"], w=["tri_incl"])
    k.op("pool", lambda q: q.affine_select(out=c.tri_strict_b[:], in_=c.ones_b[:], pattern=[[1, 128]],
                                            compare_op=ALU.is_gt, fill=0.0, base=0, channel_multiplier=-1),
         r=["ones_b"], w=["tri_strict"])
    k.op("pool", lambda q: q.affine_select(out=c.mask_gt_f[:], in_=c.ones_f[:], pattern=[[-1, 128]],
                                            compare_op=ALU.is_gt, fill=0.0, base=0, channel_multiplier=1),
         r=["ones_f"], w=["mask_gt"])
    k.op("pool", lambda q: q.affine_select(out=c.ident_f[:], in_=c.ones_f[:], pattern=[[-1, 128]],
                                            compare_op=ALU.is_equal, fill=0.0, base=0, channel_multiplier=1),
         r=["ones_f"], w=["ident_f"])
    c.ident_b = k.sb([128, 128], BF16, "ident_b")
    k.copy(c.ident_b[:], c.ident_f[:], r=["ident_f"], w=["ident_b"])
    k.op("pool", lambda q: q.iota(c.iota_p[:], pattern=[[0, 1]], base=0, channel_multiplier=1,
                                  allow_small_or_imprecise_dtypes=True), w=["iota_p"])


def rsqrt(k, out, in_, scale, eps, r, w):
    k.act(out, in_, AF.Sqrt, r=r, w=w, scale=scale, bias=eps)
    k.op("dve", lambda q: q.reciprocal(out=out, in_=out), r=w, w=w)


def rmsnorm_tile(k, xt, gbc, out, ss, rstd, junk, keys_r, keys_w, tag):
    k.act(junk, xt, AF.Square, r=keys_r, w=[tag + "junk", tag + "ss"], accum_out=ss)
    rsqrt(k, rstd, ss, 1.0 / D, EPS, [tag + "ss"], [tag + "rstd"])
    k.stt(out, xt, rstd, gbc, ALU.mult, ALU.mult, r=list(keys_r) + [tag + "rstd"], w=keys_w)


def norm_to_dram(k, c, src, g_row, dst_bf, T, tag, dst_f32=None):
    with ExitStack() as es:
        k.stack, old = es, k.stack
        gbc = k.sb([128, D], F32, "gbc")
        xt = [k.sb([128, D], F32, "nx%d" % i) for i in range(2)]
        ob = [k.sb([128, D], BF16, "no%d" % i) for i in range(2)]
        of = [k.sb([128, D], F32, "nf%d" % i) for i in range(2)] if dst_f32 is not None else None
        junk = k.sb([128, D], BF16, "njunk")
        ss = k.sb([128, 1], F32, "nss")
        rstd = k.sb([128, 1], F32, "nrstd")
        k.load(gbc[:], bcast_rows(g_row, D), w=[tag + "g"])
        for j in range(T // 128):
            b = j % 2
            k.load(xt[b][:], src[j * 128:(j + 1) * 128, :], w=[tag + "x%d" % b])
            if dst_f32 is not None:
                rmsnorm_tile(k, xt[b][:], gbc[:], of[b][:], ss[:], rstd[:], junk[:],
                             [tag + "x%d" % b, tag + "g"], [tag + "of%d" % b], tag)
                k.copy(ob[b][:], of[b][:], r=[tag + "of%d" % b], w=[tag + "o%d" % b], e="act")
                k.load(dst_f32[j * 128:(j + 1) * 128, :], of[b][:], r=[tag + "of%d" % b])
            else:
                rmsnorm_tile(k, xt[b][:], gbc[:], ob[b][:], ss[:], rstd[:], junk[:],
                             [tag + "x%d" % b, tag + "g"], [tag + "o%d" % b], tag)
            k.load(dst_bf[j * 128:(j + 1) * 128, :], ob[b][:], r=[tag + "o%d" % b])
        k.barrier()
        k.stack = old


def load_actT(k, dst_tile, src_tm, t0, nt, tag, r=()):
    for kc in range(NKC):
        for s0 in range(0, nt, 512):
            n = min(512, nt - s0)
            k.loadT(dst_tile[:, kc, s0:s0 + n], src_tm[t0 + s0:t0 + s0 + n, kc * 128:(kc + 1) * 128],
                    r=r, w=[tag])


def linear_stage(k, aT_src_tm, T, W, col_specs, tag, kdim=D):
    nkc = kdim // 128
    TS = min(T, 2048)
    with ExitStack() as es:
        k.stack, old = es, k.stack
        aT = k.sb([128, nkc, TS], BF16, "aT")
        wb = [k.sb([128, nkc, 512], BF16, "wb%d" % i) for i in range(2)]
        ot = [k.sb([128, 512], F32, "ot%d" % i) for i in range(2)]
        ob = [k.sb([128, 512], BF16, "ob%d" % i) for i in range(2)]
        ofm = [k.sb([128, TS], BF16, "ofm%d" % i) for i in range(2)]
        ps = [k.stack.enter_context(k.nc.psum_tensor(k.name("lps"), [128, 512], F32)) for _ in range(4)]
        blocks = []
        for (c0, ncols, mode, dst, doff, ddt) in col_specs:
            for b0 in range(0, ncols, 512):
                blocks.append((c0 + b0, min(512, ncols - b0), mode, dst, doff + b0, ddt))
        Wv = W.rearrange("(kc p) n -> p kc n", p=128)
        wi = 0
        pi = 0
        oi = 0
        fi = 0
        for st in range(T // TS):
            t0 = st * TS
            for kc in range(nkc):
                for s0 in range(0, TS, 512):
                    k.loadT(aT[:, kc, s0:s0 + 512], aT_src_tm[t0 + s0:t0 + s0 + 512, kc * 128:(kc + 1) * 128],
                            w=[tag + "aT_%d_%d" % (kc, s0 // 512)])
            for (c0, ncols, mode, dst, doff, ddt) in blocks:
                wbuf = wb[wi % 2]
                wkey = tag + "w%d" % (wi % 2)
                wi += 1
                k.cast_load(wbuf[:, :, 0:ncols], Wv[:, :, c0:c0 + ncols], w=[wkey])
                if mode == "tm":
                    for j in range(TS // 128):
                        p = ps[pi % 4]
                        pkey = tag + "ps%d" % (pi % 4)
                        pi += 1
                        for kc in range(nkc):
                            k.mm(p[:, 0:ncols], aT[:, kc, j * 128:(j + 1) * 128], wbuf[:, kc, 0:ncols],
                                 kc == 0, kc == nkc - 1, r=[tag + "aT_%d_%d" % (kc, j // 4), wkey], w=[pkey])
                        if ddt == F32:
                            o = ot[oi % 2]
                            okey = tag + "ot%d" % (oi % 2)
                        else:
                            o = ob[oi % 2]
                            okey = tag + "ob%d" % (oi % 2)
                        oi += 1
                        k.copy(o[:, 0:ncols], p[:, 0:ncols], r=[pkey], w=[okey], e="act")
                        k.load(dst[t0 + j * 128:t0 + (j + 1) * 128, doff:doff + ncols], o[:, 0:ncols], r=[okey])
                else:
                    for m0 in range(0, ncols, 128):
                        o = ofm[fi % 2]
                        okey = tag + "ofm%d" % (fi % 2)
                        fi += 1
                        for n0 in range(0, TS, 512):
                            p = ps[pi % 4]
                            pkey = tag + "ps%d" % (pi % 4)
                            pi += 1
                            for kc in range(nkc):
                                k.mm(p[:, :], wbuf[:, kc, m0:m0 + 128], aT[:, kc, n0:n0 + 512],
                                     kc == 0, kc == nkc - 1, r=[tag + "aT_%d_%d" % (kc, n0 // 512), wkey], w=[pkey])
                            eng = "act" if (n0 // 512) % 2 == 0 else "dve"
                            k.copy(o[:, n0:n0 + 512], p[:, :], r=[pkey], w=[okey], e=eng)
                        k.load(dst[doff + m0:doff + m0 + 128, t0:t0 + TS], o[:, :], r=[okey])
        k.barrier()
        k.stack = old


def conv_stage(k, c, T, xbcT, xbcT2, conv_w, conv_b, scT, sc_w, y_scT):
    with ExitStack() as es:
        k.stack, old = es, k.stack
        cw = k.sb([128, 128], F32, "cw")
        cb = k.sb([128, 32], F32, "cb")
        sw = k.sb([128, 48], F32, "sw")
        craw = k.sb([128, 128], F32, "craw")
        braw = k.sb([32, 128], F32, "braw")
        sraw = k.sb([48, 128], F32, "sraw")
        cps = k.stack.enter_context(k.nc.psum_tensor(k.name("cps"), [128, 512], F32))
        xin = [k.sb([128, 3 + T], BF16, "xin%d" % i) for i in range(3)]
        acc = [k.sb([128, T], F32, "acc%d" % i) for i in range(3)]
        ob = [k.sb([128, T], BF16, "cob%d" % i) for i in range(3)]
        tb = [k.sb([128, T], BF16, "ctb%d" % i) for i in range(2)]
        th = [k.sb([128, T], BF16, "cth%d" % i) for i in range(2)]
        k.load(craw[:], conv_w.rearrange("k (cc p) -> (k cc) p", p=128), w=["craw"])
        k.load(braw[:], conv_b.rearrange("o (cc p) -> (o cc) p", p=128), w=["braw"])
        k.load(sraw[:], sc_w.rearrange("k (cc p) -> (k cc) p", p=128), w=["sraw"])
        k.mm(cps[:, 0:128], craw[:], c.ident_f[:], True, True, r=["craw", "ident_f"], w=["cps"])
        k.mm(cps[:, 128:160], braw[:], c.ident_f[0:32, 0:32], True, True, r=["braw", "ident_f"], w=["cps"])
        k.mm(cps[:, 160:208], sraw[:], c.ident_f[0:48, 0:48], True, True, r=["sraw", "ident_f"], w=["cps"])
        k.copy(cw[:], cps[:, 0:128], r=["cps"], w=["cw"])
        k.copy(cb[:], cps[:, 128:160], r=["cps"], w=["cb"])
        k.copy(sw[:], cps[:, 160:208], r=["cps"], w=["sw"])
        for i in range(3):
            k.memset(xin[i][:, 0:3], 0.0, w=["xin%d" % i])
        for cc in range(32):
            b = cc % 3
            k.load(xin[b][:, 3:3 + T], xbcT[cc * 128:(cc + 1) * 128, :], w=["xin%d" % b])
            ce = "dve"
            k.ts(acc[b][:], xin[b][:, 0:T], cw[:, cc:cc + 1], None, ALU.mult, r=["xin%d" % b, "cw"], w=["acc%d" % b], e=ce)
            for kk in range(1, 4):
                k.stt(acc[b][:], xin[b][:, kk:kk + T], cw[:, kk * 32 + cc:kk * 32 + cc + 1], acc[b][:], ALU.mult, ALU.add,
                      r=["xin%d" % b, "cw", "acc%d" % b], w=["acc%d" % b], e=ce)
            k.act(ob[b][:], acc[b][:], AF.Silu, r=["acc%d" % b, "cb"], w=["cob%d" % b], bias=cb[:, cc:cc + 1])
            k.load(xbcT2[cc * 128:(cc + 1) * 128, :], ob[b][:], r=["cob%d" % b])
        for cc in range(16):
            b = cc % 2
            k.load(tb[b][:], scT[cc * 128:(cc + 1) * 128, :], w=["tb%d" % b])
            k.load(xin[b][:, 3:3 + T], scT[2048 + cc * 128:2048 + (cc + 1) * 128, :], w=["xin%d" % b])
            k.load(th[b][:], scT[4096 + cc * 128:4096 + (cc + 1) * 128, :], w=["th%d" % b])
            k.tt(xin[b][:, 3:3 + T], xin[b][:, 3:3 + T], th[b][:], ALU.mult, r=["xin%d" % b, "th%d" % b], w=["xin%d" % b])
            k.ts(acc[b][:], xin[b][:, 1:1 + T], sw[:, cc:cc + 1], None, ALU.mult, r=["xin%d" % b, "sw"], w=["acc%d" % b])
            for kk in range(1, 3):
                k.stt(acc[b][:], xin[b][:, 1 + kk:1 + kk + T], sw[:, kk * 16 + cc:kk * 16 + cc + 1], acc[b][:], ALU.mult, ALU.add,
                      r=["xin%d" % b, "sw", "acc%d" % b], w=["acc%d" % b])
            k.tt(ob[b][:], acc[b][:], tb[b][:], ALU.mult, r=["acc%d" % b, "tb%d" % b], w=["cob%d" % b])
            k.load(y_scT[cc * 128:(cc + 1) * 128, :], ob[b][:], r=["cob%d" % b])
        k.barrier()
        k.stack = old


def ssd_stage(k, c, T, xbcT2, dt_d, z_d, y_tm, dt_bias, a_log, d_skip, gate_norm):
    NCH = T // 128
    with ExitStack() as es:
        k.stack, old = es, k.stack
        nc = k.nc
        sbt = k.sb
        dtb_bc = sbt([128, 32], F32, "dtb")
        a_bc = sbt([128, 32], F32, "abc")
        dsk_bc = sbt([128, 32], F32, "dsk")
        gn_bc = sbt([128, D], F32, "gnbc")
        k.load(dtb_bc[:], bcast_rows(dt_bias, 32), w=["dtb"])
        k.load(a_bc[:], bcast_rows(a_log, 32), w=["abc"])
        k.load(dsk_bc[:], bcast_rows(d_skip, 32), w=["dsk"])
        k.load(gn_bc[:], bcast_rows(gate_norm, D), w=["gnbc"])
        k.act(a_bc[:], a_bc[:], AF.Exp, r=["abc"], w=["abc"])
        k.ts(a_bc[:], a_bc[:], -1.0, None, ALU.mult, r=["abc"], w=["abc"])
        NB = 2
        xs = [sbt([128, D], BF16, "xs%d" % i) for i in range(NB)]
        Btm = [sbt([128, 1024], BF16, "Btm%d" % i) for i in range(NB)]
        BT = [sbt([128, 8, 128], BF16, "BT%d" % i) for i in range(NB)]
        CT = [sbt([128, 8, 128], BF16, "CT%d" % i) for i in range(NB)]
        dtr = [sbt([128, 32], F32, "dtr%d" % i) for i in range(NB)]
        zt = [sbt([128, D], BF16, "zt%d" % i) for i in range(NB)]
        dtp = sbt([128, 32], F32, "dtp")
        dA = sbt([128, 32], F32, "dA")
        cum = sbt([128, 64], F32, "cum")
        ea = sbt([128, 32], F32, "ea")
        cd = sbt([128, 32], F32, "cd")
        wgt = sbt([128, 32], F32, "wgt")
        G = sbt([128, 128], F32, "G")
        L = [sbt([128, 4, 128], F32, "L%d" % i) for i in range(2)]
        E = [sbt([128, 4, 128], F32, "E%d" % i) for i in range(2)]
        MT = [sbt([128, 4, 128], BF16, "MT%d" % i) for i in range(2)]
        xw = [sbt([128, D], BF16, "xw%d" % i) for i in range(2)]
        xd = sbt([128, D], F32, "xd")
        dskfull = sbt([128, D], F32, "dskfull")
        tg = [sbt([128, 256], F32, "tg%d" % i) for i in range(2)]
        yf = sbt([128, D], F32, "yf")
        sz = sbt([128, D], F32, "sz")
        sq = sbt([128, D], F32, "sq")
        ss8 = sbt([128, 8], F32, "ss8")
        yo = [sbt([128, D], BF16, "yo%d" % i) for i in range(2)]
        state_f = sbt([128, 8, 256], F32, "state_f")
        state_b = sbt([128, 8, 256], BF16, "state_b")
        k.memset(state_f[:], 0.0, w=["state_f%d" % g for g in range(8)])
        k.memset(state_b[:], 0.0, w=["state_b%d" % g for g in range(8)])
        k.copy(dskfull[:].rearrange("p (h e) -> p h e", h=32), dsk_bc[:].unsqueeze(2).to_broadcast([128, 32, 64]),
               r=["dsk"], w=["dskfull"])
        P = lambda nm: k.stack.enter_context(nc.psum_tensor(k.name(nm), [128, 512], F32))
        ps_cum = P("ps_cum")
        ps_cb = [P("ps_cb0"), P("ps_cb1")]
        ps_seg = [P("ps_seg0"), P("ps_seg1")]
        ps_y = [P("ps_y0"), P("ps_y1")]
        ps_st = P("ps_st")

        def issue_loads(ch):
            b = ch % NB
            t0 = ch * 128
            for q4 in range(4):
                k.loadT(xs[b][:, q4 * 512:(q4 + 1) * 512], xbcT2[q4 * 512:(q4 + 1) * 512, t0:t0 + 128],
                        w=["xs%d_%d" % (b, kc) for kc in range(q4 * 4, q4 * 4 + 4)])
            for q4 in range(2):
                k.loadT(Btm[b][:, q4 * 512:(q4 + 1) * 512], xbcT2[2048 + q4 * 512:2048 + (q4 + 1) * 512, t0:t0 + 128],
                        w=["Btm%d_%d" % (b, kc) for kc in range(q4 * 4, q4 * 4 + 4)])
            k.load(BT[b][:], xbcT2[2048:3072, t0:t0 + 128].rearrange("(g n) t -> n g t", n=128), w=["BT%d" % b])
            k.load(CT[b][:], xbcT2[3072:4096, t0:t0 + 128].rearrange("(g n) t -> n g t", n=128), w=["CT%d" % b])
            k.load(dtr[b][:], dt_d[t0:t0 + 128, :], w=["dtr%d" % b])
            k.load(zt[b][:], z_d[t0:t0 + 128, :], w=["zt%d" % b])

        issue_loads(0)
        for ch in range(NCH):
            b = ch % NB
            t0 = ch * 128
            if ch + 1 < NCH:
                issue_loads(ch + 1)
            kBT, kCT = "BT%d" % b, "CT%d" % b
            xkeys = ["xs%d_%d" % (b, kc) for kc in range(16)]
            k.tt(dtp[:], dtr[b][:], dtb_bc[:], ALU.add, r=["dtr%d" % b, "dtb"], w=["dtp"])
            k.act(dtp[:], dtp[:], AF.Exp, r=["dtp"], w=["dtp"])
            k.act(dtp[:], dtp[:], AF.Ln, r=["dtp"], w=["dtp"], bias=1.0)
            k.tt(dA[:], dtp[:], a_bc[:], ALU.mult, r=["dtp", "abc"], w=["dA"])
            k.mm(ps_cum[:, 0:32], c.tri_incl_f[:], dA[:], True, True, r=["tri_incl", "dA"], w=["ps_cum"])
            k.mm(ps_cum[:, 32:64], c.ones_f[:], dA[:], True, True, r=["ones_f", "dA"], w=["ps_cum"])
            k.copy(cum[:], ps_cum[:, 0:64], r=["ps_cum"], w=["cum"])
            k.act(ea[:], cum[:, 0:32], AF.Exp, r=["cum"], w=["ea"])
            k.act(cd[:], cum[:, 32:64], AF.Exp, r=["cum"], w=["cd"])
            k.tt(wgt[:], cum[:, 32:64], cum[:, 0:32], ALU.subtract, r=["cum"], w=["wgt"])
            k.act(wgt[:], wgt[:], AF.Exp, r=["wgt"], w=["wgt"])
            k.tt(wgt[:], wgt[:], dtp[:], ALU.mult, r=["wgt", "dtp"], w=["wgt"])
            xwb = xw[ch % 2]
            xwk = "xw%d" % (ch % 2)
            k.tt(xwb[:].rearrange("p (h e) -> p h e", h=32), xs[b][:].rearrange("p (h e) -> p h e", h=32),
                 wgt[:].unsqueeze(2).to_broadcast([128, 32, 64]), ALU.mult, r=xkeys + ["wgt"], w=[xwk])
            k.tt(xd[:], xs[b][:], dskfull[:], ALU.mult, r=xkeys + ["dskfull"], w=["xd"], e="pool")
            for g in range(8):
                i2 = g % 2
                pcb = ps_cb[i2]
                kpcb = "ps_cb%d" % i2
                py = ps_y[i2]
                kpy = "ps_y%d" % i2
                h0 = 4 * g
                k.mm(pcb[:, 0:128], BT[b][:, g, :], CT[b][:, g, :], True, True, r=[kBT, kCT], w=[kpcb])
                k.tt(G[:], pcb[:, 0:128], c.tri_incl_f[:], ALU.mult, r=[kpcb, "tri_incl"], w=["G"])
                k.tt(L[i2][:], c.mask_gt_f[:].unsqueeze(1).to_broadcast([128, 4, 128]),
                     dA[:, h0:h0 + 4].unsqueeze(2).to_broadcast([128, 4, 128]), ALU.mult, r=["mask_gt", "dA"], w=["L%d" % i2])
                for rr in range(4):
                    k.mm(ps_seg[i2][:, rr * 128:(rr + 1) * 128], L[i2][:, rr, :], c.tri_incl_f[:], True, True,
                         r=["L%d" % i2, "tri_incl"], w=["ps_seg%d" % i2])
                k.act(E[i2][:].rearrange("p h t -> p (h t)"), ps_seg[i2][:, :], AF.Exp, r=["ps_seg%d" % i2], w=["E%d" % i2])
                k.tt(E[i2][:], E[i2][:], G[:].unsqueeze(1).to_broadcast([128, 4, 128]), ALU.mult, r=["E%d" % i2, "G"], w=["E%d" % i2])
                k.tt(MT[i2][:], E[i2][:], dtp[:, h0:h0 + 4].unsqueeze(2).to_broadcast([128, 4, 128]), ALU.mult,
                     r=["E%d" % i2, "dtp"], w=["MT%d" % i2])
                for rr in range(4):
                    h = h0 + rr
                    k.mm(py[:, rr * 64:(rr + 1) * 64], MT[i2][:, rr, :], xs[b][:, h * 64:(h + 1) * 64], True, True,
                         r=["MT%d" % i2, "xs%d_%d" % (b, h // 2)], w=[kpy])
                k.mm(py[:, 256:512], CT[b][:, g, :], state_b[:, g, :], True, True, r=[kCT, "state_b%d" % g], w=[kpy])
                k.mm(ps_st[:, 0:256], Btm[b][:, g * 128:(g + 1) * 128], xwb[:, g * 256:(g + 1) * 256], True, True,
                     r=["Btm%d_%d" % (b, g), xwk], w=["ps_st"])
                k.tt(tg[i2][:].rearrange("p (h e) -> p h e", h=4), py[:, 256:512].rearrange("p (h e) -> p h e", h=4),
                     ea[:, h0:h0 + 4].unsqueeze(2).to_broadcast([128, 4, 64]), ALU.mult, r=[kpy, "ea"], w=["tg%d" % i2])
                k.tt(yf[:, g * 256:(g + 1) * 256], tg[i2][:], py[:, 0:256], ALU.add, r=["tg%d" % i2, kpy], w=["yf"])
                k.tt(state_f[:, g, :].rearrange("p (h e) -> p h e", h=4), state_f[:, g, :].rearrange("p (h e) -> p h e", h=4),
                     cd[:, h0:h0 + 4].unsqueeze(2).to_broadcast([128, 4, 64]), ALU.mult,
                     r=["state_f%d" % g, "cd"], w=["state_f%d" % g], e="pool")
                k.tt(state_f[:, g, :], state_f[:, g, :], ps_st[:, 0:256], ALU.add, r=["state_f%d" % g, "ps_st"], w=["state_f%d" % g])
                k.copy(state_b[:, g, :], state_f[:, g, :], r=["state_f%d" % g], w=["state_b%d" % g], e="act")
            k.tt(yf[:], yf[:], xd[:], ALU.add, r=["yf", "xd"], w=["yf"])
            k.act(sz[:], zt[b][:], AF.Silu, r=["zt%d" % b], w=["sz"])
            k.tt(yf[:], yf[:], sz[:], ALU.mult, r=["yf", "sz"], w=["yf"])
            k.tt(sq[:], yf[:], yf[:], ALU.mult, r=["yf"], w=["sq"])
            k.op("dve", lambda q: q.tensor_reduce(out=ss8[:], in_=sq[:].rearrange("p (g e) -> p g e", g=8),
                                                  axis=AX.X, op=ALU.add), r=["sq"], w=["ss8"])
            rsqrt(k, ss8[:], ss8[:], 1.0 / 256, EPS, ["ss8"], ["ss8"])
            o = yo[ch % 2]
            for g in range(8):
                k.stt(o[:, g * 256:(g + 1) * 256], yf[:, g * 256:(g + 1) * 256], ss8[:, g:g + 1],
                      gn_bc[:, g * 256:(g + 1) * 256], ALU.mult, ALU.mult, r=["yf", "ss8", "gnbc"], w=["yo%d" % (ch % 2)])
            k.load(y_tm[t0:t0 + 128, :], o[:], r=["yo%d" % (ch % 2)])
        k.barrier()
        k.stack = old


def outproj_stage(k, c, T, kin_specs, W, resid, dst, tag):
    nkc = W.shape[0] // 128
    SP = 512
    resident = nkc * D * 2 <= 65536
    with ExitStack() as es:
        k.stack, old = es, k.stack
        if resident:
            wfull = k.sb([128, nkc, D], BF16, "owf")
        else:
            wb = [k.sb([128, nkc, 512], BF16, "owb%d" % i) for i in range(2)]
        mt = [k.sb([128, nkc, SP], BF16, "omt%d" % i) for i in range(2)]
        rt = [k.sb([128, 512], F32, "ort%d" % i) for i in range(2)]
        ot = [k.sb([128, 512], F32, "oot%d" % i) for i in range(2)]
        ps = [k.stack.enter_context(k.nc.psum_tensor(k.name("ops"), [128, 512], F32)) for _ in range(2)]
        Wv = W.rearrange("(kc p) n -> p kc n", p=128)
        it = 0
        si = 0

        def load_span(sp):
            nonlocal si
            sb_ = si % 2
            si += 1
            s0 = sp * SP
            kc = 0
            for (mode, src) in kin_specs:
                if mode == "tm":
                    for q in range(src.shape[1] // 128):
                        k.loadT(mt[sb_][:, kc, :], src[s0:s0 + SP, q * 128:(q + 1) * 128], w=[tag + "mt%d_%d" % (sb_, kc)])
                        kc += 1
                else:
                    n = src.shape[0] // 128
                    k.load(mt[sb_][:, kc:kc + n, :], src[:, s0:s0 + SP].rearrange("(q p) t -> p q t", p=128),
                           w=[tag + "mt%d_%d" % (sb_, q2) for q2 in range(kc, kc + n)])
                    kc += n
            return sb_

        def tile_block(sb_, s0, jj, cb, wtile, wkey):
            nonlocal it
            b = it % 2
            it += 1
            t0 = s0 + jj * 128
            k.load(rt[b][:], resid[t0:t0 + 128, cb * 512:(cb + 1) * 512], w=[tag + "rt%d" % b])
            for kc in range(nkc):
                k.mm(ps[b][:], mt[sb_][:, kc, jj * 128:(jj + 1) * 128], wtile(kc), kc == 0, kc == nkc - 1,
                     r=[tag + "mt%d_%d" % (sb_, kc), wkey], w=[tag + "ps%d" % b])
            k.tt(ot[b][:], ps[b][:], rt[b][:], ALU.add, r=[tag + "ps%d" % b, tag + "rt%d" % b], w=[tag + "ot%d" % b])
            k.load(dst[t0:t0 + 128, cb * 512:(cb + 1) * 512], ot[b][:], r=[tag + "ot%d" % b])

        if resident:
            for cb in range(D // 512):
                k.cast_load(wfull[:, :, cb * 512:(cb + 1) * 512], Wv[:, :, cb * 512:(cb + 1) * 512], w=[tag + "wf%d" % cb])
            for sp in range(T // SP):
                sb_ = load_span(sp)
                for jj in range(SP // 128):
                    for cb in range(D // 512):
                        tile_block(sb_, sp * SP, jj, cb, lambda kc, cb=cb: wfull[:, kc, cb * 512:(cb + 1) * 512], tag + "wf%d" % cb)
        else:
            for cb in range(D // 512):
                wbuf = wb[cb % 2]
                wkey = tag + "w%d" % (cb % 2)
                k.cast_load(wbuf[:], Wv[:, :, cb * 512:(cb + 1) * 512], w=[wkey])
                for sp in range(T // SP):
                    sb_ = load_span(sp)
                    for jj in range(SP // 128):
                        tile_block(sb_, sp * SP, jj, cb, lambda kc, wbuf=wbuf: wbuf[:, kc, :], wkey)
        k.barrier()
        k.stack = old


BLK = 128
_FREED = {}


def free_pool_tmps(k, n0):
    import re
    nc = k.nc
    freed = _FREED.setdefault(id(nc), set())
    for i in nc.main_func.blocks[-1].instructions[n0:]:
        for nm in set(re.findall(r"(Pool_tmp[A-Za-z0-9_]*|Pool_Pool_[A-Za-z0-9_]*_snap_[0-9]+)", str(i))):
            if nm not in freed:
                freed.add(nm)
                nc.gpsimd.free_register(bass.RegisterHandle(nm, mybir.EngineType.Pool))


def moe_stage(k, c, T, li, h_in, h_out, I, S):
    NT = T // 128
    NSLOT = ((2 * T + 32 * (BLK - 1)) + BLK - 1) // BLK * BLK
    NBLK = NSLOT // BLK
    assert NBLK <= 128
    nc = k.nc
    with ExitStack() as es0:
        k.stack, old0 = es0, k.stack
        R = k.sb([128, NT, 32], F32, "mR")
        OH1 = k.sb([128, NT, 32], F32, "mOH1")
        OH2 = k.sb([128, NT, 32], F32, "mOH2")
        W12 = k.sb([128, NT, 2], F32, "mW12")
        SI = k.sb([128, NT, 2], I32, "mSI")
        cnt = k.sb([128, 32], F32, "mcnt")
        with ExitStack() as es:
            k.stack = es
            gbc = k.sb([128, D], F32, "gbc")
            wr = k.sb([128, NKC, 36], F32, "wr")
            bias = k.sb([128, 36], F32, "rbias")
            xt = [k.sb([128, D], F32, "mx%d" % i) for i in range(2)]
            of = [k.sb([128, D], F32, "mof%d" % i) for i in range(2)]
            ob = [k.sb([128, D], BF16, "mob%d" % i) for i in range(2)]
            hT = [k.sb([128, NKC, 128], F32, "mhT%d" % i) for i in range(2)]
            junk = k.sb([128, D], BF16, "mjunk")
            ss = k.sb([128, 1], F32, "mss")
            rstd = k.sb([128, 1], F32, "mrstd")
            lg = k.sb([128, 36], F32, "mlg")
            gmx = k.sb([128, 4], F32, "mgmx")
            goh = k.sb([128, 4], F32, "mgoh")
            pen = k.sb([128, 4], F32, "mpen")
            ejk = k.sb([128, 4], F32, "mejk")
            elm = k.sb([128, 32], F32, "melm")
            top8 = k.sb([128, 8], F32, "mtop8")
            sc = k.sb([128, 4], F32, "msc")
            A = k.sb([128, 32], BF16, "mA")
            pst = [k.stack.enter_context(nc.psum_tensor(k.name("mpst"), [128, 512], F32)) for _ in range(4)]
            psl = k.stack.enter_context(nc.psum_tensor(k.name("mpsl"), [128, 512], F32))
            psr = k.stack.enter_context(nc.psum_tensor(k.name("mpsr"), [128, 512], F32))
            k.load(gbc[:], bcast_rows(I["norm_ffn"][li:li + 1, :], D), w=["mg"])
            with nc.allow_non_contiguous_dma(reason="router weights are tiny"):
                k.load(wr[:, :, 0:4], I["moe_w_group"][li].rearrange("(kc p) n -> p kc n", p=128), w=["wr"])
                k.load(wr[:, :, 4:36], I["moe_w_expert"][li].rearrange("(kc p) n -> p kc n", p=128), w=["wr"])
            k.load(bias[:, 0:4], bcast_rows(I["moe_b_group"][li:li + 1, :], 4), w=["rbias"])
            k.load(bias[:, 4:36], bcast_rows(I["moe_b_expert"][li:li + 1, :], 32), w=["rbias"])
            k.memset(cnt[:], 0.0, w=["mcnt"])
            for j in range(NT):
                b = j % 2
                k.load(xt[b][:], h_in[j * 128:(j + 1) * 128, :], w=["mx%d" % b])
                rmsnorm_tile(k, xt[b][:], gbc[:], of[b][:], ss[:], rstd[:], junk[:], ["mx%d" % b, "mg"], ["mof%d" % b], "m")
                k.copy(ob[b][:], of[b][:], r=["mof%d" % b], w=["mob%d" % b], e="act")
                k.load(S.hn[j * 128:(j + 1) * 128, :], ob[b][:], r=["mob%d" % b], w=["hn_%d" % j])
                for q in range(4):
                    for u in range(4):
                        kc = q * 4 + u
                        k.mm(pst[q][:, u * 128:(u + 1) * 128], of[b][:, kc * 128:(kc + 1) * 128], c.ident_f[:], True, True,
                             r=["mof%d" % b, "ident_f"], w=["mpst%d" % q])
                    k.copy(hT[b][:, q * 4:(q + 1) * 4, :], pst[q][:, :].rearrange("p (u t) -> p u t", u=4),
                           r=["mpst%d" % q], w=["mhT%d_%d" % (b, q)], e=("act" if q % 2 == 0 else "dve"))
                for kc in range(NKC):
                    k.mm(psl[:, 0:36], hT[b][:, kc, :], wr[:, kc, :], kc == 0, kc == NKC - 1,
                         r=["mhT%d_%d" % (b, kc // 4), "wr"], w=["mpsl"])
                k.tt(lg[:], psl[:, 0:36], bias[:], ALU.add, r=["mpsl", "rbias"], w=["mlg"])
                k.op("dve", lambda q_: q_.reduce_max(out=gmx[:, 0:1], in_=lg[:, 0:4], axis=AX.X), r=["mlg"], w=["mgmx"])
                k.ts(goh[:], lg[:, 0:4], gmx[:, 0:1], None, ALU.is_equal, r=["mlg", "mgmx"], w=["mgoh"])
                k.ts(gmx[:, 1:2], gmx[:, 0:1], -1.0, None, ALU.mult, r=["mgmx"], w=["mgmx"])
                k.act(ejk[:], lg[:, 0:4], AF.Exp, r=["mlg", "mgmx"], w=["mejk", "mgsum"], bias=gmx[:, 1:2], accum_out=gmx[:, 2:3])
                k.op("dve", lambda q_: q_.reciprocal(out=gmx[:, 3:4], in_=gmx[:, 2:3]), r=["mgsum"], w=["mgprob"])
                k.ts(pen[:], goh[:], 1e30, -1e30, ALU.mult, ALU.add, r=["mgoh"], w=["mpen"])
                for g in range(4):
                    k.ts(elm[:, g * 8:(g + 1) * 8], lg[:, 4 + g * 8:4 + (g + 1) * 8], pen[:, g:g + 1], None, ALU.add,
                         r=["mlg", "mpen"], w=["melm"])
                k.op("dve", lambda q_: q_.max(out=top8[:], in_=elm[:]), r=["melm"], w=["mtop8"])
                k.ts(OH1[:, j, :], elm[:], top8[:, 0:1], None, ALU.is_equal, r=["melm", "mtop8"], w=["mOH1_%d" % j])
                k.ts(OH2[:, j, :], elm[:], top8[:, 1:2], None, ALU.is_equal, r=["melm", "mtop8"], w=["mOH2_%d" % j])
                k.tt(sc[:, 0:1], top8[:, 1:2], top8[:, 0:1], ALU.subtract, r=["mtop8"], w=["msc"])
                k.act(sc[:, 1:2], sc[:, 0:1], AF.Exp, r=["msc"], w=["msc"])
                k.ts(sc[:, 1:2], sc[:, 1:2], 1.0, None, ALU.add, r=["msc"], w=["msc"])
                k.op("dve", lambda q_: q_.reciprocal(out=sc[:, 2:3], in_=sc[:, 1:2]), r=["msc"], w=["msc"])
                k.tt(W12[:, j, 0:1], gmx[:, 3:4], sc[:, 2:3], ALU.mult, r=["mgprob", "msc"], w=["mW12_%d" % j])
                k.tt(W12[:, j, 1:2], gmx[:, 3:4], W12[:, j, 0:1], ALU.subtract, r=["mgprob", "mW12_%d" % j], w=["mW12_%d" % j])
                k.tt(A[:], OH1[:, j, :], OH2[:, j, :], ALU.add, r=["mOH1_%d" % j, "mOH2_%d" % j], w=["mA"])
                k.mm(psr[:, 0:32], c.tri_strict_b[:], A[:], True, True, r=["tri_strict", "mA"], w=["mpsr"])
                k.mm(psr[:, 32:64], c.ones_b[:], A[:], True, True, r=["ones_b", "mA"], w=["mpsr"])
                k.tt(R[:, j, :], psr[:, 0:32], cnt[:], ALU.add, r=["mpsr", "mcnt"], w=["mR_%d" % j])
                k.tt(cnt[:], psr[:, 32:64], cnt[:], ALU.add, r=["mpsr", "mcnt"], w=["mcnt"])
            nb = k.sb([128, 32], F32, "mnb")
            cs = [k.sb([128, 32], F32, "mcs%d" % i) for i in range(2)]
            pstart = k.sb([128, 32], F32, "mpstart")
            tmp = k.sb([128, 32], F32, "mtmp")
            SF = k.sb([128, NT, 2], F32, "mSF")
            be = k.sb([128, 4], F32, "mbe")
            bei = k.sb([128, 1], I32, "mbei")
            k.memset(nb[:], 0.0, w=["mnb"])
            for m in range((T + BLK - 1) // BLK):
                k.stt(nb[:], cnt[:], float(m * BLK), nb[:], ALU.is_gt, ALU.add, r=["mcnt", "mnb"], w=["mnb"])
            k.ts(cs[0][:], nb[:], float(BLK), None, ALU.mult, r=["mnb"], w=["mcs0"])
            k.copy(nb[:], cs[0][:], r=["mcs0"], w=["mnb"])
            cur = 0
            for sh in (1, 2, 4, 8, 16):
                nx = 1 - cur
                k.copy(cs[nx][:, 0:sh], cs[cur][:, 0:sh], r=["mcs%d" % cur], w=["mcs%d" % nx])
                k.tt(cs[nx][:, sh:32], cs[cur][:, sh:32], cs[cur][:, 0:32 - sh], ALU.add, r=["mcs%d" % cur], w=["mcs%d" % nx])
                cur = nx
            pend = cs[cur]
            k.tt(pstart[:], pend[:], nb[:], ALU.subtract, r=["mcs%d" % cur, "mnb"], w=["mpstart"])
            for j in range(NT):
                k.tt(tmp[:], R[:, j, :], pstart[:], ALU.add, r=["mR_%d" % j, "mpstart"], w=["mtmp"])
                k.tt(elm[:], tmp[:], OH1[:, j, :], ALU.mult, r=["mtmp", "mOH1_%d" % j], w=["melm"])
                k.op("dve", lambda q_: q_.reduce_sum(out=SF[:, j, 0:1], in_=elm[:], axis=AX.X), r=["melm"], w=["mSF"])
                k.tt(elm[:], tmp[:], OH2[:, j, :], ALU.mult, r=["mtmp", "mOH2_%d" % j], w=["melm"])
                k.op("dve", lambda q_: q_.reduce_sum(out=SF[:, j, 1:2], in_=elm[:], axis=AX.X), r=["melm"], w=["mSF"])
            k.copy(SI[:], SF[:], r=["mSF"], w=["mSI"])
            k.ts(be[:, 0:1], c.iota_p[:], float(BLK), None, ALU.mult, r=["iota_p"], w=["mbe"])
            k.ts(elm[:], pend[:], be[:, 0:1], None, ALU.is_le, r=["mcs%d" % cur, "mbe"], w=["melm"])
            k.op("dve", lambda q_: q_.reduce_sum(out=be[:, 1:2], in_=elm[:], axis=AX.X), r=["melm"], w=["mbe"])
            k.ts(be[:, 1:2], be[:, 1:2], 31.0, None, ALU.min, r=["mbe"], w=["mbe"])
            k.copy(bei[:], be[:, 1:2], r=["mbe"], w=["mbei"])
            k.load(S.blk_e[:, :], bei[:], r=["mbei"])
            k.ts(be[:, 2:3], c.iota_p[:], float(BLK), float(-BLK), ALU.mult, ALU.add, r=["iota_p"], w=["mbe2"])
            k.ts(elm[:], pend[:], be[:, 2:3], None, ALU.is_le, r=["mcs%d" % cur, "mbe2"], w=["melm"])
            k.op("dve", lambda q_: q_.reduce_sum(out=be[:, 3:4], in_=elm[:], axis=AX.X), r=["melm"], w=["mbe3"])
            k.ts(be[:, 3:4], be[:, 3:4], 31.0, None, ALU.min, r=["mbe3"], w=["mbe3"])
            k.tt(be[:, 3:4], be[:, 3:4], be[:, 1:2], ALU.is_equal, r=["mbe3", "mbe"], w=["mbe3"])
            k.stt(be[:, 3:4], c.iota_p[:], 1.0, be[:, 3:4], ALU.min, ALU.mult, r=["iota_p", "mbe3"], w=["mbe3"])
            bsi = k.sb([128, 1], I32, "mbsi")
            k.copy(bsi[:], be[:, 3:4], r=["mbe3"], w=["mbsi"])
            k.load(S.blk_same[:, :], bsi[:], r=["mbsi"])
            for j in range(NT):
                b = j % 2
                k.load(ob[b][:], S.hn[j * 128:(j + 1) * 128, :], r=["hn_%d" % j], w=["mob%d" % b])
                for kk in range(2):
                    k.dma("pool", lambda q_, j=j, kk=kk, b=b: q_.indirect_dma_start(
                        out=S.xg, out_offset=bass.IndirectOffsetOnAxis(ap=SI[:, j, kk:kk + 1], axis=0),
                        in_=ob[b][:], in_offset=None), r=["mob%d" % b, "mSI"])
            k.barrier()
            k.stack = es0
        mlim = SUBLIM.get("moe", 99)
        with ExitStack() as es:
            if mlim < 2:
                NBLK = 0
            k.stack = es
            xgT = [k.sb([128, NKC, BLK], BF16, "xgT%d" % i) for i in range(2)]
            wg = [k.sb([128, NKC, 512], BF16, "wg%d" % i) for i in range(2)]
            wu = [k.sb([128, NKC, 512], BF16, "wu%d" % i) for i in range(2)]
            wdn = [k.sb([128, 8, 512], BF16, "wdn%d" % i) for i in range(4)]
            hTt = [k.sb([128, 8, BLK], BF16, "hTt%d" % i) for i in range(2)]
            sgt = [k.sb([128, 512], F32, "sgt%d" % i) for i in range(2)]
            hb = [k.sb([128, 512], BF16, "hb%d" % i) for i in range(2)]
            yt = [k.sb([128, D], BF16, "yt%d" % i) for i in range(4)]
            pst = [k.stack.enter_context(nc.psum_tensor(k.name("pst"), [128, 512], F32)) for _ in range(2)]
            psg = [k.stack.enter_context(nc.psum_tensor(k.name("psg"), [128, 512], F32)) for _ in range(2)]
            psu = [k.stack.enter_context(nc.psum_tensor(k.name("psu"), [128, 512], F32)) for _ in range(2)]
            psy = [k.stack.enter_context(nc.psum_tensor(k.name("psy"), [128, 512], F32)) for _ in range(2)]
            if not hasattr(k, "moe_regs"):
                k.moe_regs = [nc.gpsimd.alloc_register(k.name("mreg")) for _ in range(4)]
            ereg, creg, obase, oreg0 = k.moe_regs
            Hg, Hu, Hd = I["_h_moe_w_gate%d" % li], I["_h_moe_w_up%d" % li], I["_h_moe_w_down%d" % li]
            PAT_GU = [[1024, 128], [128 * 1024, NKC], [1, 512]]
            PAT_D = [[2048, 128], [128 * 2048, 8], [1, 512]]

            xgtm = [k.sb([128, BLK // 128, D], BF16, "xgtm%d" % i) for i in range(2)]

            def load_xg(bk):
                bb = bk % 2
                k.load(xgtm[bb][:], S.xg[bk * BLK:(bk + 1) * BLK, :].rearrange("(s p) d -> p s d", p=128), w=["xgtm%d" % bb])

            def transpose_xg(bk):
                nonlocal mi
                bb = bk % 2
                for s_ in range(BLK // 128):
                    for q in range(4):
                        pi = mi % 2
                        mi += 1
                        for u in range(4):
                            kc = q * 4 + u
                            k.mm(pst[pi][:, u * 128:(u + 1) * 128], xgtm[bb][:, s_, kc * 128:(kc + 1) * 128], c.ident_b[:], True, True,
                                 r=["xgtm%d" % bb, "ident_b"], w=["pst%d" % pi])
                        k.copy(xgT[bb][:, q * 4:(q + 1) * 4, s_ * 128:(s_ + 1) * 128], pst[pi][:, :].rearrange("p (u t) -> p u t", u=4),
                               r=["pst%d" % pi], w=["xgT%d_%d" % (bb, kc2) for kc2 in range(q * 4, q * 4 + 4)],
                               e=("act" if q % 2 == 0 else "dve"))

            def wload(buf, key, hnd, pat, off):
                n0_ = len(nc.main_func.blocks[-1].instructions)
                nc.gpsimd.reg_add(oreg0, obase, off)
                same = nc.gpsimd.snap(creg, min_val=0, max_val=1)
                st_ = k.dma("pool", lambda q_: q_.dma_start(out=buf, in_=bass.AP(hnd, oreg0, pat), cond=same < 1,
                                                             bounds_check="skip_entire_dma"), w=[key])
                free_pool_tmps(k, n0_)
                return st_

            mi = 0
            yi = 0
            if NBLK:
                load_xg(0)
                transpose_xg(0)
            for bk in range(NBLK):
                bb = bk % 2
                if bk + 1 < NBLK:
                    load_xg(bk + 1)
                n_ins0 = len(nc.main_func.blocks[-1].instructions)
                nc.gpsimd.reg_load(ereg, S.blk_e[bk:bk + 1, 0:1])
                nc.gpsimd.reg_load(creg, S.blk_same[bk:bk + 1, 0:1])
                nc.gpsimd.reg_mul(obase, ereg, 2048 * 1024)
                free_pool_tmps(k, n_ins0)
                for hq in range(2):
                    wload(wg[hq][:], "wg%d" % hq, Hg, PAT_GU, hq * 512)
                    wload(wu[hq][:], "wu%d" % hq, Hu, PAT_GU, hq * 512)
                for cb in range(4):
                    wload(wdn[cb][:], "wdn%d" % cb, Hd, PAT_D, cb * 512)
                for s_ in range(BLK // 128):
                    for hq in range(2):
                        gb, ub = wg[hq], wu[hq]
                        gk, uk = "wg%d" % hq, "wu%d" % hq
                        pi = mi % 2
                        mi += 1
                        for kc in range(NKC):
                            k.mm(psg[pi][:, :], xgT[bb][:, kc, s_ * 128:(s_ + 1) * 128], gb[:, kc, :], kc == 0, kc == NKC - 1,
                                 r=[gk, "xgT%d_%d" % (bb, kc)], w=["psg%d" % pi])
                        for kc in range(NKC):
                            k.mm(psu[pi][:, :], xgT[bb][:, kc, s_ * 128:(s_ + 1) * 128], ub[:, kc, :], kc == 0, kc == NKC - 1,
                                 r=[uk, "xgT%d_%d" % (bb, kc)], w=["psu%d" % pi])
                        k.act(sgt[pi][:], psg[pi][:, :], AF.Silu, r=["psg%d" % pi], w=["sgt%d" % pi])
                        k.tt(hb[pi][:], sgt[pi][:], psu[pi][:, :], ALU.mult, r=["sgt%d" % pi, "psu%d" % pi], w=["hb%d" % pi])
                        for m in range(4):
                            k.mm(pst[pi][:, m * 128:(m + 1) * 128], hb[pi][:, m * 128:(m + 1) * 128], c.ident_b[:], True, True,
                                 r=["hb%d" % pi, "ident_b"], w=["pst%d" % pi])
                        k.copy(hTt[bb][:, hq * 4:(hq + 1) * 4, s_ * 128:(s_ + 1) * 128],
                               pst[pi][:, :].rearrange("p (m t) -> p m t", m=4), r=["pst%d" % pi],
                               w=["hTt%d_%d" % (bb, ffc) for ffc in range(hq * 4, hq * 4 + 4)], e=("act" if hq == 0 else "dve"))
                if bk + 1 < NBLK:
                    transpose_xg(bk + 1)
                yts = [yt[(yi + s_) % 4] for s_ in range(BLK // 128)]
                ytk = ["yt%d" % ((yi + s_) % 4) for s_ in range(BLK // 128)]
                yi += BLK // 128
                for cb in range(4):
                    db = wdn[cb]
                    dk = "wdn%d" % cb
                    for s_ in range(BLK // 128):
                        pi = mi % 2
                        mi += 1
                        for ffc in range(8):
                            k.mm(psy[pi][:], hTt[bb][:, ffc, s_ * 128:(s_ + 1) * 128], db[:, ffc, :], ffc == 0, ffc == 7,
                                 r=["hTt%d_%d" % (bb, ffc), dk], w=["psy%d" % pi])
                        k.copy(yts[s_][:, cb * 512:(cb + 1) * 512], psy[pi][:], r=["psy%d" % pi], w=[ytk[s_]],
                               e=("act" if s_ % 2 == 0 else "dve"))
                for s_ in range(BLK // 128):
                    k.load(S.yslot[bk * BLK + s_ * 128:bk * BLK + (s_ + 1) * 128, :], yts[s_][:], r=[ytk[s_]])
                free_pool_tmps(k, n_ins0)
            k.barrier()
            k.stack = es0
        with ExitStack() as es:
            k.stack = es
            ht = [k.sb([128, D], F32, "ch%d" % i) for i in range(2)]
            y1 = [k.sb([128, D], BF16, "cy1%d" % i) for i in range(2)]
            y2 = [k.sb([128, D], BF16, "cy2%d" % i) for i in range(2)]
            for j in range(NT if mlim >= 3 else 0):
                b = j % 2
                k.load(ht[b][:], h_in[j * 128:(j + 1) * 128, :], w=["ch%d" % b])
                k.dma("pool", lambda q_, j=j, b=b: q_.indirect_dma_start(
                    out=y1[b][:], out_offset=None, in_=S.yslot,
                    in_offset=bass.IndirectOffsetOnAxis(ap=SI[:, j, 0:1], axis=0)), w=["cy1%d" % b])
                k.dma("pool", lambda q_, j=j, b=b: q_.indirect_dma_start(
                    out=y2[b][:], out_offset=None, in_=S.yslot,
                    in_offset=bass.IndirectOffsetOnAxis(ap=SI[:, j, 1:2], axis=0)), w=["cy2%d" % b])
                k.stt(ht[b][:], y1[b][:], W12[:, j, 0:1], ht[b][:], ALU.mult, ALU.add, r=["cy1%d" % b, "ch%d" % b], w=["ch%d" % b])
                k.stt(ht[b][:], y2[b][:], W12[:, j, 1:2], ht[b][:], ALU.mult, ALU.add, r=["cy2%d" % b, "ch%d" % b], w=["ch%d" % b])
                k.load(h_out[j * 128:(j + 1) * 128, :], ht[b][:], r=["ch%d" % b])
            k.barrier()
            k.stack = es0
        k.stack = old0


def zero_dram(k, dst, rows, cols, dt):
    with ExitStack() as es:
        k.stack, old = es, k.stack
        z = k.sb([128, cols], dt, "zero")
        k.memset(z[:], 0.0, w=["zero"])
        for r0 in range(0, rows, 128):
            k.load(dst[r0:r0 + 128, :], z[:], r=["zero"])
        k.barrier()
        k.stack = old


def ple_stage(k, c, T, li, h_in, h_out, I, S, final_g=None, final_out=None):
    nc = k.nc
    norm_to_dram(k, c, h_in, I["norm_ple"][li:li + 1, :], S.hn, T, "pl")
    with ExitStack() as es:
        k.stack, old = es, k.stack
        wg = k.sb([128, NKC, D], BF16, "plwg")
        wp = k.sb([128, 2, D], BF16, "plwp")
        aT = [k.sb([128, NKC, 512], BF16, "plaT%d" % i) for i in range(2)]
        pf = [k.sb([128, 256], F32, "plpf%d" % i) for i in range(2)]
        pb = [k.sb([128, 256], BF16, "plpb%d" % i) for i in range(2)]
        pT = [k.sb([128, 2, 512], BF16, "plpT%d" % i) for i in range(2)]
        ht = [k.sb([128, D], F32, "plh%d" % i) for i in range(2)]
        gt = [k.sb([128, 512], F32, "plg%d" % i) for i in range(2)]
        psa = [k.stack.enter_context(nc.psum_tensor(k.name("plpsa"), [128, 512], F32)) for _ in range(2)]
        psb = [k.stack.enter_context(nc.psum_tensor(k.name("plpsb"), [128, 512], F32)) for _ in range(2)]
        if final_g is not None:
            fg = k.sb([128, D], F32, "plfg")
            fo = [k.sb([128, D], F32, "plfo%d" % i) for i in range(2)]
            junk = k.sb([128, D], BF16, "pljunk")
            ss = k.sb([128, 1], F32, "plss")
            rstd = k.sb([128, 1], F32, "plrstd")
            k.load(fg[:], bcast_rows(final_g, D), w=["plfg"])
        k.cast_load(wg[:], I["ple_gate"][li].rearrange("(kc p) n -> p kc n", p=128), w=["plwg"])
        k.cast_load(wp[:], I["ple_proj"][li].rearrange("(kc p) n -> p kc n", p=128), w=["plwp"])
        for j in range(T // 128):
            b = j % 2
            k.load(pf[b][:], I["p"][li, j * 128:(j + 1) * 128, :], w=["plpf%d" % b])
            k.copy(pb[b][:], pf[b][:], r=["plpf%d" % b], w=["plpb%d" % b])
            k.load(S.pbf[j * 128:(j + 1) * 128, :], pb[b][:], r=["plpb%d" % b], w=["pbf_%d" % j])
        mi = 0
        SP = 512
        for sp in range(T // SP):
            sb_ = sp % 2
            s0 = sp * SP
            for kc in range(NKC):
                k.loadT(aT[sb_][:, kc, :], S.hn[s0:s0 + SP, kc * 128:(kc + 1) * 128], w=["plaT%d_%d" % (sb_, kc)])
            for q in range(2):
                k.loadT(pT[sb_][:, q, :], S.pbf[s0:s0 + SP, q * 128:(q + 1) * 128],
                        r=["pbf_%d" % jq for jq in range(s0 // 128, (s0 + SP) // 128)], w=["plpT%d_%d" % (sb_, q)])
            for jj in range(SP // 128):
                j = (s0 // 128) + jj
                b = j % 2
                t0 = j * 128
                k.load(ht[b][:], h_in[t0:t0 + 128, :], w=["plh%d" % b])
                for cb in range(4):
                    pi = mi % 2
                    mi += 1
                    for kc in range(NKC):
                        k.mm(psa[pi][:], aT[sb_][:, kc, jj * 128:(jj + 1) * 128], wg[:, kc, cb * 512:(cb + 1) * 512], kc == 0, kc == NKC - 1,
                             r=["plaT%d_%d" % (sb_, kc), "plwg"], w=["plpsa%d" % pi])
                    for q in range(2):
                        k.mm(psb[pi][:], pT[sb_][:, q, jj * 128:(jj + 1) * 128], wp[:, q, cb * 512:(cb + 1) * 512], q == 0, q == 1,
                             r=["plpT%d_%d" % (sb_, q), "plwp"], w=["plpsb%d" % pi])
                    k.act(gt[pi][:], psa[pi][:], AF.Sigmoid, r=["plpsa%d" % pi], w=["plg%d" % pi])
                    k.tt(gt[pi][:], gt[pi][:], psb[pi][:], ALU.mult, r=["plg%d" % pi, "plpsb%d" % pi], w=["plg%d" % pi])
                    k.tt(ht[b][:, cb * 512:(cb + 1) * 512], ht[b][:, cb * 512:(cb + 1) * 512], gt[pi][:], ALU.add,
                         r=["plg%d" % pi, "plh%d" % b], w=["plh%d" % b])
                if final_g is None:
                    k.load(h_out[t0:t0 + 128, :], ht[b][:], r=["plh%d" % b])
                else:
                    rmsnorm_tile(k, ht[b][:], fg[:], fo[b][:], ss[:], rstd[:], junk[:], ["plh%d" % b, "plfg"], ["plfo%d" % b], "plf")
                    k.load(final_out[t0:t0 + 128, :], fo[b][:], r=["plfo%d" % b])
        k.barrier()
        k.stack = old


SCALE = 128.0 ** -0.5
NEGB = -30000.0


def rope_stage(k, c, T, items, ropeC, ropeS, cmp_items=()):
    nc = k.nc
    with ExitStack() as es:
        k.stack, old = es, k.stack
        Cs = k.sb([32, T], F32, "ropeC")
        Ss = k.sb([32, T], F32, "ropeS")
        pa = k.sb([32, 32], F32, "rpa")
        pb = k.sb([32, 32], F32, "rpb")
        Pm = k.sb([32, 32], BF16, "rPm")
        xt = [k.sb([32, T], BF16, "rx%d" % i) for i in range(2)]
        t1 = [k.sb([32, 512], F32, "rt1%d" % i) for i in range(2)]
        t2 = [k.sb([32, 512], F32, "rt2%d" % i) for i in range(2)]
        ot = [k.sb([32, T], BF16, "ro%d" % i) for i in range(2)]
        ps = [k.stack.enter_context(nc.psum_tensor(k.name("rps"), [128, 512], F32)) for _ in range(2)]
        k.load(Cs[:], ropeC, w=["ropeC"])
        k.load(Ss[:], ropeS, w=["ropeS"])
        k.op("pool", lambda q: q.affine_select(out=pa[:], in_=c.ones_f[0:32, 0:32], pattern=[[1, 32]], compare_op=ALU.is_equal,
                                               fill=0.0, base=-16, channel_multiplier=-1), r=["ones_f"], w=["rpa"])
        k.op("pool", lambda q: q.affine_select(out=pb[:], in_=c.ones_f[0:32, 0:32], pattern=[[-1, 32]], compare_op=ALU.is_equal,
                                               fill=0.0, base=-16, channel_multiplier=1), r=["ones_f"], w=["rpb"])
        k.tt(Pm[:], pa[:], pb[:], ALU.subtract, r=["rpa", "rpb"], w=["rPm"])
        it = 0
        pi = 0
        for (ten, row0, Tn, cstep, c0) in [(a, b, T, 1, 0) for (a, b) in items] + [(a, b, n, 16, 31) for (a, b, n) in cmp_items]:
            b = it % 2
            it += 1
            k.load(xt[b][:, 0:Tn], ten[row0:row0 + 32, 0:Tn], w=["rx%d" % b])
            for n0 in range(0, Tn, 512):
                n = min(512, Tn - n0)
                p = pi % 2
                pi += 1
                k.mm(ps[p][0:32, 0:n], Pm[:], xt[b][:, n0:n0 + n], True, True, r=["rPm", "rx%d" % b], w=["rps%d" % p])
                if cstep == 1:
                    cc, sc_ = Cs[:, n0:n0 + n], Ss[:, n0:n0 + n]
                else:
                    cc = Cs[:, c0 + cstep * n0:c0 + cstep * (n0 + n - 1) + 1:cstep]
                    sc_ = Ss[:, c0 + cstep * n0:c0 + cstep * (n0 + n - 1) + 1:cstep]
                k.tt(t1[p][:, 0:n], xt[b][:, n0:n0 + n], cc, ALU.mult, r=["rx%d" % b, "ropeC"], w=["rt1%d" % p])
                k.tt(t2[p][:, 0:n], ps[p][0:32, 0:n], sc_, ALU.mult, r=["rps%d" % p, "ropeS"], w=["rt2%d" % p])
                k.tt(ot[b][:, n0:n0 + n], t1[p][:, 0:n], t2[p][:, 0:n], ALU.add, r=["rt1%d" % p, "rt2%d" % p], w=["ro%d" % b])
            k.load(ten[row0:row0 + 32, 0:Tn], ot[b][:, 0:Tn], r=["ro%d" % b])
        k.barrier()
        k.stack = old


def compress_stage(k, c, T, S, I):
    nc = k.nc
    NC = T // 16 - 1
    with ExitStack() as es:
        k.stack, old = es, k.stack
        w1 = k.sb([128, 32, 128], BF16, "cw1")
        w2 = k.sb([128, 128], BF16, "cw2")
        posr = k.sb([32, 128], F32, "cposr")
        posT = k.sb([128, 32], BF16, "cposT")
        cb = k.sb([128, 1], F32, "ccb")
        xT = k.sb([128, T], BF16, "cxT")
        h1 = k.sb([128, 256], BF16, "ch1")
        okc = k.sb([128, 256], BF16, "cokc")
        ovc = k.sb([128, 2, 128], BF16, "covc")
        ps1 = k.stack.enter_context(nc.psum_tensor(k.name("cps1"), [128, 512], F32))
        ps2 = k.stack.enter_context(nc.psum_tensor(k.name("cps2"), [128, 512], F32))
        ps3 = k.stack.enter_context(nc.psum_tensor(k.name("cps3"), [128, 512], F32))
        k.memset(okc[:], 0.0, w=["cokc"])
        for kv in range(2):
            k.cast_load(w1[:], I["od_cmp_w1"][kv].rearrange("(j d) o -> d j o", d=128), w=["cw1"])
            k.cast_load(w2[:], I["od_cmp_w2"][kv], w=["cw2"])
            k.load(posr[:], I["od_cmp_pos"][kv], w=["cposr"])
            k.mm(ps3[:, 0:32], posr[:], c.ident_f[0:32, 0:32], True, True, r=["cposr", "ident_f"], w=["cps3"])
            k.copy(posT[:], ps3[:, 0:32], r=["cps3"], w=["cposT"])
            for j in range(32):
                k.mm(ps3[:, 64:65], w1[:, j, :], posT[:, j:j + 1], j == 0, j == 31, r=["cw1", "cposT"], w=["cps3"])
            k.copy(cb[:], ps3[:, 64:65], r=["cps3"], w=["ccb"])
            src = S.kcmpT if kv == 0 else S.vcmpT
            for g in range(2):
                k.load(xT[:], src[g * 128:(g + 1) * 128, :], w=["cxT"])
                for j in range(32):
                    k.mm(ps1[:, 0:NC], w1[:, j, :], xT[:, j:j + 16 * (NC - 1) + 1:16], j == 0, j == 31, r=["cw1", "cxT"], w=["cps1"])
                k.act(h1[:, 0:NC], ps1[:, 0:NC], AF.Silu, r=["cps1", "ccb"], w=["ch1"], bias=cb[:, 0:1])
                if kv == 0:
                    k.mm(ps2[:, 0:NC], w2[:], h1[:, 0:NC], True, True, r=["cw2", "ch1"], w=["cps2"])
                    k.copy(okc[:, 0:NC], ps2[:, 0:NC], r=["cps2"], w=["cokc"])
                    k.load(S.kcT[g * 128:(g + 1) * 128, :], okc[:], r=["cokc"])
                else:
                    for h in range(2):
                        n = min(128, NC - h * 128)
                        if n <= 0:
                            continue
                        k.mm(ps2[0:n, h * 128:(h + 1) * 128], h1[:, h * 128:h * 128 + n], w2[:], True, True, r=["cw2", "ch1"], w=["cps2"])
                    k.memset(ovc[:], 0.0, w=["covc"])
                    for h in range(2):
                        n = min(128, NC - h * 128)
                        if n <= 0:
                            continue
                        k.copy(ovc[0:n, h, :], ps2[0:n, h * 128:(h + 1) * 128], r=["cps2"], w=["covc"])
                    k.load(S.vc[:, g * 128:(g + 1) * 128].rearrange("(h p) d -> p h d", p=128), ovc[:], r=["covc"])
        k.barrier()
        k.stack = old


class Attn:
    def __init__(self, k, NV):
        nc = k.nc
        self.k = k
        self.NV = NV
        self.pst = [k.stack.enter_context(nc.psum_tensor(k.name("aps"), [128, 512], F32)) for _ in range(2)]
        self.acc = [k.stack.enter_context(nc.psum_tensor(k.name("aacc"), [128, 512], F32)) for _ in range(4)]
        self.pT = [k.sb([128, 512], BF16, "apT%d" % i) for i in range(3)]
        self.accs = [k.sb([128, NV], F32, "accs%d" % i) for i in range(8)]
        self.ai = 0
        self.si = 0
        self.pi = 0

    def drain(self):
        k = self.k
        out = []
        for a in range(4):
            i = self.ai % 8
            self.ai += 1
            k.copy(self.accs[i][:], self.acc[a][:, 0:self.NV], r=["aacc%d" % a], w=["accs%d" % i], e="act")
            out.append((self.accs[i], "accs%d" % i))
        return out

    def run(self, qT_ap, rq, ktiles):
        k = self.k
        NV = self.NV
        first = [None] * 4
        last = [None] * 4
        for ti, t in enumerate(ktiles):
            for a in range(t["subs"][0], t["subs"][1]):
                if first[a] is None:
                    first[a] = ti
                last[a] = ti
        slots = []

        def qk(ti):
            t = ktiles[ti]
            a0, a1 = t["subs"]
            ps = self.pst[self.si % 2]
            pk = "aps%d" % (self.si % 2)
            self.si += 1
            c0, c1 = a0 * 128, a1 * 128
            k.mm(ps[:, c0:c1], t["kT"], qT_ap[:, c0:c1], True, t.get("bias") is None, r=list(t["rk"]) + list(rq), w=[pk])
            if t.get("bias") is not None:
                bl, br, bkeys = t["bias"]
                k.mm(ps[:, c0:c1], bl, br[:, c0:c1], False, True, r=list(bkeys), w=[pk])
            slots.append((ps, pk))

        if ktiles:
            qk(0)
        for ti, t in enumerate(ktiles):
            a0, a1 = t["subs"]
            ps, pk = slots[ti]
            if ti + 1 < len(ktiles):
                qk(ti + 1)
            pT = self.pT[self.pi % 3]
            tk = "apT%d" % (self.pi % 3)
            self.pi += 1
            c0, c1 = a0 * 128, a1 * 128
            k.act(pT[:, c0:c1], ps[:, c0:c1], AF.Exp, r=[pk], w=[tk], scale=SCALE)
            if t.get("mask") is not None:
                k.tt(pT[:, c0:c1], pT[:, c0:c1], t["mask"][:, c0:c1], ALU.mult, r=[tk] + list(t["rm"]), w=[tk],
                     e=t.get("meng", "dve"))
            for a in range(a0, a1):
                k.mm(self.acc[a][:, 0:NV], pT[:, a * 128:(a + 1) * 128], t["V"], first[a] == ti, last[a] == ti,
                     r=[tk] + list(t["rv"]), w=["aacc%d" % a])


def make_attn_masks(k, c, m):
    m.caus = k.sb([128, 4, 512], BF16, "mcaus")
    m.win = k.sb([128, 8, 512], BF16, "mwin")
    ones = k.sb([128, 512], BF16, "mones")
    tmp = k.sb([128, 512], BF16, "mtmp")
    k.memset(ones[:], 1.0, w=["mones"])
    m.ones = ones
    for b in range(4):
        k.op("pool", lambda q, b=b: q.affine_select(out=m.caus[:, b, :], in_=ones[:], pattern=[[1, 512]], compare_op=ALU.is_ge,
                                                    fill=0.0, base=-128 * b, channel_multiplier=-1), r=["mones"], w=["mcaus"])
    for cc in range(8):
        k.op("pool", lambda q, cc=cc: q.affine_select(out=tmp[:], in_=ones[:], pattern=[[1, 512]], compare_op=ALU.is_ge,
                                                      fill=0.0, base=-128 * (cc - 4), channel_multiplier=-1), r=["mones"], w=["mtmp"])
        k.op("pool", lambda q, cc=cc: q.affine_select(out=m.win[:, cc, :], in_=tmp[:], pattern=[[-1, 512]], compare_op=ALU.is_ge,
                                                      fill=0.0, base=128 * (cc - 4) + 511, channel_multiplier=1), r=["mtmp"], w=["mwin"])


def nsa_stage(k, c, T, S, I):
    nc = k.nc
    NT = T // 128
    NQB = T // 512
    NC = T // 16 - 1
    NSEL = T // 64
    NTOP = min(16, NSEL)
    with ExitStack() as es0:
        k.stack, old0 = es0, k.stack
        m = Ctx()
        make_attn_masks(k, c, m)
        Eb = k.sb([128, NT, 128], BF16, "Eb")
        onesE = k.sb([128, NT, 128], BF16, "onesE")
        k.memset(onesE[:], 1.0, w=["onesE"])
        tmpE = k.sb([128, NT, 128], BF16, "tmpE")
        k.op("pool", lambda q: q.affine_select(out=tmpE[:], in_=onesE[:], pattern=[[128, NT], [1, 128]], compare_op=ALU.is_ge,
                                               fill=0.0, base=0, channel_multiplier=-64), r=["onesE"], w=["tmpE"])
        k.op("pool", lambda q: q.affine_select(out=Eb[:], in_=tmpE[:], pattern=[[-128, NT], [-1, 128]], compare_op=ALU.is_ge,
                                               fill=0.0, base=63, channel_multiplier=64), r=["tmpE"], w=["Eb"])
        Am = k.sb([128, 2, 64], BF16, "Am")
        tmpA = k.sb([128, 2, 64], BF16, "tmpA")
        k.op("pool", lambda q: q.affine_select(out=tmpA[:], in_=onesE[:, 0, :].rearrange("p (h b) -> p h b", h=2), pattern=[[128, 2], [-4, 64]],
                                               compare_op=ALU.is_ge, fill=0.0, base=1, channel_multiplier=1), r=["onesE"], w=["tmpA"])
        k.op("pool", lambda q: q.affine_select(out=Am[:], in_=tmpA[:], pattern=[[-128, 2], [4, 64]],
                                               compare_op=ALU.is_ge, fill=0.0, base=3, channel_multiplier=-1), r=["tmpA"], w=["Am"])
        selbT = k.sb([64, T], BF16, "selbT")
        gates = k.sb([128, NT, 24], F32, "gates")
        k.load(gates[:], S.gates.rearrange("(j p) n -> p j n", p=128), w=["gates"])
        k.act(gates[:], gates[:], AF.Sigmoid, r=["gates"], w=["gates"])
        for g in range(2):
            with ExitStack() as es:
                k.stack = es
                at = Attn(k, 193)
                kcT = k.sb([128, 256], BF16, "kcT")
                V1 = k.sb([128, 2, 193], BF16, "cV1")
                qT = [k.sb([128, T], BF16, "cqT%d" % i) for i in range(2)]
                cmask = [k.sb([128, 512], BF16, "cmask%d" % i) for i in range(4)]
                psel = k.sb([128, NT, 64], F32, "psel")
                oc = [k.sb([128, 4, 128], F32, "coc%d" % i) for i in range(2)]
                den = k.sb([128, 4], F32, "cden")
                usb = k.sb([128, 64], F32, "cusb")
                k.load(kcT[:], S.kcT[g * 128:(g + 1) * 128, :], w=["kcT"])
                k.memset(V1[:], 0.0, w=["cV1"])
                k.load(V1[:, :, 0:128], S.vc[:, g * 128:(g + 1) * 128].rearrange("(h p) d -> p h d", p=128), w=["cV1"])
                k.memset(V1[:, :, 128:129], 1.0, w=["cV1"])
                k.copy(V1[:, :, 129:193], Am[:], r=["Am", "cV1"], w=["cV1"])
                k.memset(psel[:], 0.0, w=["psel"])
                ci = 0
                for r_ in range(4):
                    h = 4 * g + r_
                    qb = qT[r_ % 2]
                    qk = "cqT%d" % (r_ % 2)
                    k.load(qb[:], S.qT[h * 128:(h + 1) * 128, :], w=[qk])
                    for Q in range(NQB):
                        kts = []
                        for nt in range(2):
                            if 16 * (nt * 128) + 31 > Q * 512 + 511:
                                continue
                            cm = cmask[ci % 4]
                            ck = "cmask%d" % (ci % 4)
                            ci += 1
                            k.op("pool", lambda q, Q=Q, nt=nt, cm=cm: q.affine_select(
                                out=cm[:], in_=m.ones[:], pattern=[[1, 512]], compare_op=ALU.is_ge, fill=0.0,
                                base=512 * Q - 16 * 128 * nt - 31, channel_multiplier=-16), r=["mones"], w=[ck])
                            kts.append(dict(kT=kcT[:, nt * 128:(nt + 1) * 128], rk=["kcT"], V=V1[:, nt, :], rv=["cV1"],
                                            subs=(0, 4), mask=cm, rm=[ck]))
                        at.run(qb[:, Q * 512:(Q + 1) * 512], [qk], kts)
                        dr = at.drain()
                        for a in range(4):
                            j = Q * 4 + a
                            ob = oc[j % 2]
                            okk = "coc%d" % (j % 2)
                            ac, ak = dr[a]
                            k.ts(den[:, 0:1], ac[:, 128:129], 1e-30, None, ALU.max, r=[ak], w=["cden"])
                            k.op("dve", lambda q_: q_.reciprocal(out=den[:, 1:2], in_=den[:, 0:1]), r=["cden"], w=["cden"])
                            k.ts(ob[:, r_, :], ac[:, 0:128], den[:, 1:2], None, ALU.mult, r=[ak, "cden"], w=[okk])
                            k.stt(psel[:, j, :], ac[:, 129:193], den[:, 1:2], psel[:, j, :], ALU.mult, ALU.add,
                                  r=[ak, "cden", "psel"], w=["psel"])
                            k.load(S.ocmp[j * 128:(j + 1) * 128, h * 128:(h + 1) * 128], ob[:, r_, :], r=[okk])
                vm = k.sb([128, 64], F32, "svm")
                fm_ = k.sb([128, 64], F32, "sfm")
                f2 = k.sb([128, 64], F32, "sf2")
                sc = k.sb([128, 64], F32, "ssc")
                sc2 = k.sb([128, 64], F32, "ssc2")
                t8 = k.sb([128, 16], F32, "st8")
                selb = k.sb([128, 64], F32, "sselb")
                pss = k.stack.enter_context(nc.psum_tensor(k.name("spss"), [128, 512], F32)) if False else at.pst[0]
                for j in range(NT):
                    q0 = j * 128
                    k.op("pool", lambda q, q0=q0: q.affine_select(out=vm[:, 0:NSEL], in_=c.ones_f[:, 0:NSEL], pattern=[[-64, NSEL]], compare_op=ALU.is_ge,
                                                                  fill=0.0, base=q0, channel_multiplier=1), r=["ones_f"], w=["svm"])
                    k.op("pool", lambda q, q0=q0: q.affine_select(out=f2[:, 0:NSEL], in_=vm[:, 0:NSEL], pattern=[[64, NSEL]], compare_op=ALU.is_ge,
                                                                  fill=0.0, base=127 - q0, channel_multiplier=-1), r=["svm"], w=["sf2"])
                    k.memset(f2[:, 0:1], 1.0, w=["sf2"], e="pool")
                    k.tt(sc[:, 0:NSEL], psel[:, j, 0:NSEL], vm[:, 0:NSEL], ALU.mult, r=["psel", "svm"], w=["ssc"])
                    k.ts(fm_[:, 0:NSEL], vm[:, 0:NSEL], 1e30, -1e30, ALU.mult, ALU.add, r=["svm"], w=["sfm"])
                    k.tt(sc[:, 0:NSEL], sc[:, 0:NSEL], fm_[:, 0:NSEL], ALU.add, r=["ssc", "sfm"], w=["ssc"])
                    k.stt(sc[:, 0:NSEL], f2[:, 0:NSEL], 1e4, sc[:, 0:NSEL], ALU.mult, ALU.max, r=["sf2", "ssc"], w=["ssc"])
                    if NSEL > NTOP:
                        k.op("dve", lambda q_: q_.max(out=t8[:, 0:8], in_=sc[:, 0:NSEL]), r=["ssc"], w=["st8"])
                        k.op("dve", lambda q_: q_.match_replace(out=sc2[:, 0:NSEL], in_to_replace=t8[:, 0:8], in_values=sc[:, 0:NSEL],
                                                                imm_value=-3e38), r=["ssc", "st8"], w=["ssc2"])
                        k.op("dve", lambda q_: q_.max(out=t8[:, 8:16], in_=sc2[:, 0:NSEL]), r=["ssc2"], w=["st8"])
                        k.ts(selb[:, 0:NSEL], sc[:, 0:NSEL], t8[:, 15:16], None, ALU.is_ge, r=["ssc", "st8"], w=["sselb"])
                        k.ts(selb[:, 0:NSEL], selb[:, 0:NSEL], -NEGB, NEGB, ALU.mult, ALU.add, r=["sselb"], w=["sselb"])
                    else:
                        k.memset(selb[:, 0:NSEL], 0.0, w=["sselb"])
                    k.mm(pss[0:NSEL, 0:128], selb[:, 0:NSEL], c.ident_f[:], True, True, r=["sselb", "ident_f"], w=["aps0"])
                    k.copy(selbT[0:NSEL, q0:q0 + 128], pss[0:NSEL, 0:128], r=["aps0"], w=["selbT"])
                k.barrier()
                k.stack = es0
            with ExitStack() as es:
                k.stack = es
                at = Attn(k, 129)
                ksT = k.sb([128, T], BF16, "ksT")
                kwT = k.sb([128, T], BF16, "kwT")
                Vs = k.sb([128, NT, 129], BF16, "Vs")
                Vw = k.sb([128, NT, 129], BF16, "Vw")
                qT = [k.sb([128, T], BF16, "sqT%d" % i) for i in range(2)]
                osel = [k.sb([128, 128], F32, "osel%d" % i) for i in range(4)]
                ocm = [k.sb([128, 128], F32, "ocm%d" % i) for i in range(2)]
                oo = [k.sb([128, 128], BF16, "oo%d" % i) for i in range(2)]
                den = k.sb([128, 4], F32, "sden")
                k.load(ksT[:], S.kselT[g * 128:(g + 1) * 128, :], w=["ksT"])
                k.load(kwT[:], S.kwinT[g * 128:(g + 1) * 128, :], w=["kwT"])
                k.load(Vs[:, :, 0:128], S.vsel[:, g * 128:(g + 1) * 128].rearrange("(j p) d -> p j d", p=128), w=["Vs"])
                k.load(Vw[:, :, 0:128], S.vwin[:, g * 128:(g + 1) * 128].rearrange("(j p) d -> p j d", p=128), w=["Vw"])
                k.memset(Vs[:, :, 128:129], 1.0, w=["Vs"])
                k.memset(Vw[:, :, 128:129], 1.0, w=["Vw"])
                oi = 0
                for r_ in range(4):
                    h = 4 * g + r_
                    qb = qT[r_ % 2]
                    qk = "sqT%d" % (r_ % 2)
                    k.load(qb[:], S.qT[h * 128:(h + 1) * 128, :], w=[qk])
                    for Q in range(NQB):
                        kts = []
                        for jt in range(4 * Q + 4):
                            b = jt - 4 * Q
                            d_ = dict(kT=ksT[:, jt * 128:(jt + 1) * 128], rk=["ksT"], V=Vs[:, jt, :], rv=["Vs"],
                                      subs=(max(b, 0), 4), bias=(Eb[0:NSEL, jt, :], selbT[0:NSEL, Q * 512:(Q + 1) * 512], ["Eb", "selbT"]))
                            if b >= 0:
                                d_["mask"] = m.caus[:, b, :]
                                d_["rm"] = ["mcaus"]
                            kts.append(d_)
                        at.run(qb[:, Q * 512:(Q + 1) * 512], [qk], kts)
                        dr = at.drain()
                        res = []
                        for a in range(4):
                            ob = osel[a]
                            okk = "osel%d" % a
                            j = Q * 4 + a
                            gi = (4 * g + r_) * 3
                            ac, ak = dr[a]
                            k.op("dve", lambda q_, ac=ac: q_.reciprocal(out=den[:, 0:1], in_=ac[:, 128:129]), r=[ak], w=["sden"])
                            k.tt(den[:, 0:1], den[:, 0:1], gates[:, j, gi + 1:gi + 2], ALU.mult, r=["sden", "gates"], w=["sden"])
                            k.ts(ob[:], ac[:, 0:128], den[:, 0:1], None, ALU.mult, r=[ak, "sden"], w=[okk])
                            res.append((ob, okk))
                        kts = []
                        for cc in range(8):
                            jt = 4 * Q - 4 + cc
                            if jt < 0:
                                continue
                            a0 = max(cc - 4, 0)
                            a1 = min(cc + 1, 4)
                            kts.append(dict(kT=kwT[:, jt * 128:(jt + 1) * 128], rk=["kwT"], V=Vw[:, jt, :], rv=["Vw"], subs=(a0, a1),
                                            mask=m.win[:, cc, :], rm=["mwin"]))
                        at.run(qb[:, Q * 512:(Q + 1) * 512], [qk], kts)
                        dr = at.drain()
                        for a in range(4):
                            j = Q * 4 + a
                            gi = (4 * g + r_) * 3
                            ac, ak = dr[a]
                            ob, okk = res[a]
                            cmb = ocm[a % 2]
                            ckk = "ocm%d" % (a % 2)
                            k.load(cmb[:], S.ocmp[j * 128:(j + 1) * 128, h * 128:(h + 1) * 128], w=[ckk])
                            k.op("dve", lambda q_, ac=ac: q_.reciprocal(out=den[:, 1:2], in_=ac[:, 128:129]), r=[ak], w=["sden"])
                            k.tt(den[:, 1:2], den[:, 1:2], gates[:, j, gi + 2:gi + 3], ALU.mult, r=["sden", "gates"], w=["sden"])
                            k.stt(ob[:], ac[:, 0:128], den[:, 1:2], ob[:], ALU.mult, ALU.add, r=[ak, "sden", okk], w=[okk])
                            k.stt(oo[a % 2][:], cmb[:], gates[:, j, gi:gi + 1], ob[:], ALU.mult, ALU.add, r=[ckk, "gates", okk], w=["oo%d" % (a % 2)])
                            k.load(S.o_tm[j * 128:(j + 1) * 128, h * 128:(h + 1) * 128], oo[a % 2][:], r=["oo%d" % (a % 2)])
                k.barrier()
                k.stack = es0
        k.stack = old0


def diff_stage(k, c, T, S, I, lambda_init):
    nc = k.nc
    NT = T // 128
    NQB = T // 512
    with ExitStack() as es0:
        k.stack, old0 = es0, k.stack
        m = Ctx()
        make_attn_masks(k, c, m)
        lam = k.sb([128, 512], F32, "lam")
        lt = k.sb([128, 256], F32, "lamt")
        ls = k.sb([128, 4], F32, "lams")
        sub_bc = k.sb([128, 256], F32, "subbc")
        k.load(lam[:], I["od_lambda"].rearrange("a d -> (a d)").rearrange("(o n) -> o n", o=1).to_broadcast([128, 512]), w=["lam"])
        k.load(sub_bc[:], bcast_rows(I["od_subln"], 256), w=["subbc"])
        k.ts(sub_bc[:], sub_bc[:], 1.0 - lambda_init, None, ALU.mult, r=["subbc"], w=["subbc"])
        k.tt(lt[:, 0:128], lam[:, 0:128], lam[:, 128:256], ALU.mult, r=["lam"], w=["lamt"])
        k.tt(lt[:, 128:256], lam[:, 256:384], lam[:, 384:512], ALU.mult, r=["lam"], w=["lamt"])
        k.op("dve", lambda q_: q_.reduce_sum(out=ls[:, 0:1], in_=lt[:, 0:128], axis=AX.X), r=["lamt"], w=["lams"])
        k.op("dve", lambda q_: q_.reduce_sum(out=ls[:, 1:2], in_=lt[:, 128:256], axis=AX.X), r=["lamt"], w=["lams"])
        k.act(ls[:, 0:2], ls[:, 0:2], AF.Exp, r=["lams"], w=["lams"])
        k.tt(ls[:, 2:3], ls[:, 1:2], ls[:, 0:1], ALU.subtract, r=["lams"], w=["lams"])
        k.ts(ls[:, 2:3], ls[:, 2:3], -lambda_init, None, ALU.add, r=["lams"], w=["lams"])
        at = Attn(k, 257)
        qT = [k.sb([128, T], BF16, "dqT%d" % i) for i in range(2)]
        kT = [k.sb([128, T], BF16, "dkT%d" % i) for i in range(2)]
        V1 = [k.sb([128, NT, 257], BF16, "dV%d" % i) for i in range(2)]
        o0 = [k.sb([128, 256], F32, "do0%d" % i) for i in range(4)]
        o1 = [k.sb([128, 256], F32, "do1%d" % i) for i in range(2)]
        sq = k.sb([128, 256], F32, "dsq")
        ss = k.sb([128, 2], F32, "dss")
        den = k.sb([128, 2], F32, "dden")
        ob = [k.sb([128, 256], BF16, "dob%d" % i) for i in range(2)]
        oi = 0
        for h in range(4):
            vb = V1[h % 2]
            vk = "dV%d" % (h % 2)
            k.load(vb[:, :, 0:256], S.dv[:, h * 256:(h + 1) * 256].rearrange("(j p) d -> p j d", p=128), w=[vk])
            k.memset(vb[:, :, 256:257], 1.0, w=[vk])
            for mm_ in range(2):
                hh = 2 * h + mm_
                k.load(qT[mm_][:], S.dqT[hh * 128:(hh + 1) * 128, :], w=["dqT%d" % mm_])
                k.load(kT[mm_][:], S.dkT[hh * 128:(hh + 1) * 128, :], w=["dkT%d" % mm_])
            for Q in range(NQB):
                for mm_ in range(2):
                    kts = []
                    for jt in range(4 * Q + 4):
                        b = jt - 4 * Q
                        d_ = dict(kT=kT[mm_][:, jt * 128:(jt + 1) * 128], rk=["dkT%d" % mm_], V=vb[:, jt, :], rv=[vk], subs=(max(b, 0), 4))
                        if b >= 0:
                            d_["mask"] = m.caus[:, b, :]
                            d_["rm"] = ["mcaus"]
                            d_["meng"] = "pool" if b % 2 else "dve"
                        kts.append(d_)
                    at.run(qT[mm_][:, Q * 512:(Q + 1) * 512], ["dqT%d" % mm_], kts)
                    dr = at.drain()
                    for a in range(4):
                        j = Q * 4 + a
                        ac, ak = dr[a]
                        k.op("dve", lambda q_, ac=ac: q_.reciprocal(out=den[:, 0:1], in_=ac[:, 256:257]), r=[ak], w=["dden"])
                        if mm_ == 0:
                            k.ts(o0[a][:], ac[:, 0:256], den[:, 0:1], None, ALU.mult, r=[ak, "dden"], w=["do0%d" % a])
                        else:
                            t1 = o1[a % 2]
                            k.ts(den[:, 0:1], den[:, 0:1], ls[:, 2:3], None, ALU.mult, r=["dden", "lams"], w=["dden"])
                            k.stt(t1[:], ac[:, 0:256], den[:, 0:1], o0[a][:], ALU.mult, ALU.add,
                                  r=[ak, "dden", "do0%d" % a], w=["do1%d" % (a % 2)])
                            k.act(sq[:], t1[:], AF.Square, r=["do1%d" % (a % 2)], w=["dsq", "dss"], accum_out=ss[:, 0:1])
                            rsqrt(k, ss[:, 1:2], ss[:, 0:1], 1.0 / 256, EPS, ["dss"], ["dss2"])
                            o = ob[oi % 2]
                            okk = "dob%d" % (oi % 2)
                            oi += 1
                            k.stt(o[:], t1[:], ss[:, 1:2], sub_bc[:], ALU.mult, ALU.mult, r=["do1%d" % (a % 2), "dss2", "subbc"], w=[okk])
                            k.load(S.o_tm[j * 128:(j + 1) * 128, 1024 + h * 256:1024 + (h + 1) * 256], o[:], r=[okk])
        k.barrier()
        k.stack = old0


IN_SPECS = [
    ("x", None), ("p", None),
    ("norm_mix", (2, 2048)), ("norm_ffn", (2, 2048)), ("norm_ple", (2, 2048)), ("norm_final", (1, 2048)),
    ("ev_w_in", (2048, 12320)), ("ev_conv_w", (4, 4096)), ("ev_conv_b", (1, 4096)), ("ev_dt_bias", (1, 32)),
    ("ev_a_log", (1, 32)), ("ev_d_skip", (1, 32)), ("ev_gate_norm", (1, 2048)), ("ev_sc_w", (3, 2048)),
    ("ev_w_out", (4096, 2048)),
    ("od_w_in", (2048, 5656)), ("od_cmp_pos", (2, 32, 128)), ("od_cmp_w1", (2, 4096, 128)), ("od_cmp_w2", (2, 128, 128)),
    ("od_lambda", (4, 128)), ("od_subln", (1, 256)), ("od_w_out", (2048, 2048)),
    ("moe_w_group", (2, 2048, 4)), ("moe_b_group", (2, 4)), ("moe_w_expert", (2, 2048, 32)), ("moe_b_expert", (2, 32)),
    ("moe_w_gate0", (32, 2048, 1024)), ("moe_w_up0", (32, 2048, 1024)), ("moe_w_down0", (32, 1024, 2048)),
    ("moe_w_gate1", (32, 2048, 1024)), ("moe_w_up1", (32, 2048, 1024)), ("moe_w_down1", (32, 1024, 2048)),
    ("ple_gate", (2, 2048, 2048)), ("ple_proj", (2, 256, 2048)),
    ("rope_cos", None), ("rope_sin", None),
]


SUBLIM = {}


def build_program(T, stages, dbg=(), needed=None):
    nc = bass.Bass("TRN2", target_bir_lowering=False)
    I = {}
    for name, shp in IN_SPECS:
        if needed is not None and name not in needed:
            continue
        if name == "x":
            shp = (T, D)
        elif name == "p":
            shp = (2, T, 256)
        elif name in ("rope_cos", "rope_sin"):
            shp = (32, T)
        hnd = nc.dram_tensor(name, list(shp), F32, kind="ExternalInput")
        I[name] = hnd.ap()
        I["_h_" + name] = hnd
    out = nc.dram_tensor("out", [T, D], F32, kind="ExternalOutput").ap()

    def scr(name, shape, dt):
        kind = "ExternalOutput" if name in dbg else "Internal"
        return nc.dram_tensor(name, list(shape), dt, kind=kind).ap()

    S = Ctx()
    S.hn = scr("hn", [T, D], BF16)
    S.z = scr("z", [T, D], BF16)
    S.xbcT = scr("xbcT", [4096, T], BF16)
    S.xbcT2 = scr("xbcT2", [4096, T], BF16)
    S.dt = scr("dt", [T, 32], F32)
    S.scT = scr("scT", [6144, T], BF16)
    S.y_scT = scr("y_scT", [2048, T], BF16)
    S.y_tm = scr("y_tm", [T, D], BF16)
    S.h1 = scr("h1", [T, D], F32)
    S.h2 = scr("h2", [T, D], F32)
    S.h3 = scr("h3", [T, D], F32)
    NSLOT = ((2 * T + 32 * (BLK - 1)) + BLK - 1) // BLK * BLK
    S.xg = scr("xg", [NSLOT, D], BF16)
    S.yslot = scr("yslot", [NSLOT, D], BF16)
    S.blk_e = scr("blk_e", [128, 1], I32)
    S.blk_same = scr("blk_same", [128, 1], I32)
    S.pbf = scr("pbf", [T, 256], BF16)
    S.h4 = scr("h4", [T, D], F32)
    S.h5 = scr("h5", [T, D], F32)
    S.qT = scr("qT", [1024, T], BF16)
    S.kcmpT = scr("kcmpT", [256, T], BF16)
    S.vcmpT = scr("vcmpT", [256, T], BF16)
    S.kselT = scr("kselT", [256, T], BF16)
    S.vsel = scr("vsel", [T, 256], BF16)
    S.kwinT = scr("kwinT", [256, T], BF16)
    S.vwin = scr("vwin", [T, 256], BF16)
    S.gates = scr("gates", [T, 24], F32)
    S.dqT = scr("dqT", [1024, T], BF16)
    S.dkT = scr("dkT", [1024, T], BF16)
    S.dv = scr("dv", [T, 1024], BF16)
    S.kcT = scr("kcT", [256, 256], BF16)
    S.vc = scr("vc", [256, 256], BF16)
    S.ocmp = scr("ocmp", [T, 1024], F32)
    S.o_tm = scr("o_tm", [T, D], BF16)

    k = K(nc)
    c = Ctx()
    make_consts(k, c)
    if "l0mix" in stages:
        lim = SUBLIM.get("l0mix", 99)
        norm_to_dram(k, c, I["x"], I["norm_mix"][0:1, :], S.hn, T, "n0")
        if lim >= 2:
          linear_stage(k, S.hn, T, I["ev_w_in"], [
            (0, 2048, "tm", S.z, 0, BF16),
            (2048, 4096, "fm", S.xbcT, 0, BF16),
            (6144, 32, "tm", S.dt, 0, F32),
            (6176, 6144, "fm", S.scT, 0, BF16),
        ], "l0in")
        if lim >= 3:
            conv_stage(k, c, T, S.xbcT, S.xbcT2, I["ev_conv_w"], I["ev_conv_b"], S.scT, I["ev_sc_w"], S.y_scT)
        if lim >= 4:
            ssd_stage(k, c, T, S.xbcT2, S.dt, S.z, S.y_tm, I["ev_dt_bias"], I["ev_a_log"], I["ev_d_skip"], I["ev_gate_norm"])
        if lim >= 5:
            outproj_stage(k, c, T, [("tm", S.y_tm), ("fm", S.y_scT)], I["ev_w_out"], I["x"], S.h1, "l0out")
    if "moe0" in stages:
        zero_dram(k, S.xg, NSLOT, D, BF16)
        moe_stage(k, c, T, 0, S.h1, S.h2, I, S)
    if "ple0" in stages:
        ple_stage(k, c, T, 0, S.h2, S.h3, I, S)
    h_l1 = S.h3
    if "l1in_dbg" in stages:
        h_l1 = I["x"]
    if "l1mix" in stages:
        NC_ = T // 16 - 1
        lim = SUBLIM.get("l1mix", 99)
        norm_to_dram(k, c, h_l1, I["norm_mix"][1:2, :], S.hn, T, "n1")
        if lim >= 2:
          linear_stage(k, S.hn, T, I["od_w_in"], [
            (0, 1024, "fm", S.qT, 0, BF16), (1024, 256, "fm", S.kcmpT, 0, BF16), (1280, 256, "fm", S.vcmpT, 0, BF16),
            (1536, 256, "fm", S.kselT, 0, BF16), (1792, 256, "tm", S.vsel, 0, BF16), (2048, 256, "fm", S.kwinT, 0, BF16),
            (2304, 256, "tm", S.vwin, 0, BF16), (2560, 24, "tm", S.gates, 0, F32), (2584, 1024, "fm", S.dqT, 0, BF16),
            (3608, 1024, "fm", S.dkT, 0, BF16), (4632, 1024, "tm", S.dv, 0, BF16)], "l1in")
        items = [(S.qT, h * 128) for h in range(8)] + [(S.kselT, g * 128) for g in range(2)] + \
                [(S.kwinT, g * 128) for g in range(2)] + [(S.dqT, h * 128) for h in range(8)] + [(S.dkT, h * 128) for h in range(8)]
        if lim >= 3:
            rope_stage(k, c, T, items, I["rope_cos"], I["rope_sin"])
            compress_stage(k, c, T, S, I)
            rope_stage(k, c, T, [], I["rope_cos"], I["rope_sin"], cmp_items=[(S.kcT, 0, NC_), (S.kcT, 128, NC_)])
        if lim >= 4:
            nsa_stage(k, c, T, S, I)
        if lim >= 5:
            diff_stage(k, c, T, S, I, 0.8 - 0.6 * math.exp(-0.3 * 1))
        if lim >= 6:
            outproj_stage(k, c, T, [("tm", S.o_tm)], I["od_w_out"], h_l1, S.h4, "l1out")
    if "moe1" in stages:
        if "moe0" not in stages:
            zero_dram(k, S.xg, NSLOT, D, BF16)
        moe_stage(k, c, T, 1, S.h4, S.h5, I, S)
    if "ple1" in stages:
        ple_stage(k, c, T, 1, S.h5, None, I, S, final_g=I["norm_final"], final_out=out)
    if "copy_h1" in stages:
        with ExitStack() as es:
            k.stack, old = es, k.stack
            tl = [k.sb([128, D], F32, "fin%d" % i) for i in range(2)]
            for j in range(T // 128):
                k.load(tl[j % 2][:], S.h1[j * 128:(j + 1) * 128, :], w=["fin%d" % (j % 2)])
                k.load(out[j * 128:(j + 1) * 128, :], tl[j % 2][:], r=["fin%d" % (j % 2)])
            k.barrier()
            k.stack = old
    k.barrier()
    k.stack.close()
    print("instructions:", k.ninst, "sems:", k.nsem)
    return nc


def rope_tables(T):
    half = 16
    inv = (500000.0 ** (-(np.arange(half, dtype=np.float32) / half))).astype(np.float32)
    ang = np.arange(T, dtype=np.float32)[None, :] * np.concatenate([inv, inv])[:, None]
    return np.cos(ang).astype(np.float32), np.sin(ang).astype(np.float32)


ALL_STAGES = ("l0mix", "moe0", "ple0", "l1mix", "moe1", "ple1")
T_FULL = 4096
N_CORES = 4


def _core_inputs(inputs, b):
    m = {}
    for name, shp in IN_SPECS:
        if name in ("rope_cos", "rope_sin"):
            continue
        if name.startswith("moe_w_") and name[-1] in "01" and name[:-1] in ("moe_w_gate", "moe_w_up", "moe_w_down"):
            a = inputs[name[:-1]][int(name[-1])]
        else:
            a = inputs[name]
            if name == "x":
                a = a[b]
            elif name == "p":
                a = a[:, b]
            elif name in ("norm_mix", "norm_ffn", "norm_ple", "moe_w_group", "moe_b_group", "moe_w_expert", "moe_b_expert",
                          "ple_gate", "ple_proj"):
                pass
            elif name == "norm_final":
                a = a.reshape(1, -1)
            else:
                a = a[0]
        a = np.ascontiguousarray(np.asarray(a), dtype=np.float32)
        if shp:
            a = a.reshape(shp)
        m[name] = a
    return m


def kernel(**inputs):
    T = T_FULL
    nc = build_program(T, ALL_STAGES)
    cos, sin = rope_tables(T)
    in_maps = []
    shared = None
    for b in range(N_CORES):
        m = _core_inputs(inputs, b)
        if shared is None:
            shared = m
        else:
            for name in m:
                if name not in ("x", "p"):
                    m[name] = shared[name]
        m["rope_cos"] = cos
        m["rope_sin"] = sin
        in_maps.append(m)
    res = run_bass_kernel_spmd(nc, in_maps, core_ids=list(range(N_CORES)))
    out = np.stack([np.asarray(r["out"], dtype=np.float32) for r in res.results], axis=0)
    return out
```

```python
import math
from contextlib import ExitStack
import numpy as np
import concourse.bass as bass
import concourse.mybir as mybir
from concourse.bass_utils import run_bass_kernel_spmd

F32 = mybir.dt.float32
BF16 = mybir.dt.bfloat16
I32 = mybir.dt.int32
AF = mybir.ActivationFunctionType
ALU = mybir.AluOpType
AX = mybir.AxisListType

D = 2048
NKC = D // 128
EPS = 1e-6
NDS = 48


class Stamp:
    __slots__ = ("sem", "val", "eng", "name")

    def __init__(self, sem, val, eng, name):
        self.sem, self.val, self.eng, self.name = sem, val, eng, name


class K:
    def __init__(self, nc):
        self.nc = nc
        self.stack = ExitStack()
        self.gstack = self.stack
        self.eng = {"pe": nc.tensor, "act": nc.scalar, "dve": nc.vector, "pool": nc.gpsimd, "sp": nc.sync}
        self.nsem = 0
        self.esem = {}
        self.ecnt = {}
        for e in self.eng:
            self._new_esem(e)
        self.waited = {e: {} for e in self.eng}
        self.dsem = [self._sem("d%d" % i) for i in range(NDS)]
        self.dcnt = [0] * NDS
        self.dlast = [None] * NDS
        self.dnext = 0
        self.trk = {}
        self.uid = 0
        self.ninst = 0

    def _sem(self, name):
        self.nsem += 1
        return (self.gstack.enter_context(self.nc.semaphore(name)), name)

    def _new_esem(self, e):
        self.esem[e] = self._sem("e_%s_%d" % (e, self.nsem))
        self.ecnt[e] = 0

    @staticmethod
    def keys(base, n):
        return ["%s_%d" % (base, i) for i in range(n)]

    def name(self, p):
        self.uid += 1
        return "%s_%d" % (p, self.uid)

    def sb(self, shape, dt, name="t"):
        return self.stack.enter_context(self.nc.sbuf_tensor(self.name(name), list(shape), dt))

    def wait(self, e, st):
        if st is None:
            return
        if self.waited[e].get(st.name, 0) >= st.val:
            return
        self.eng[e].wait_ge(st.sem, st.val)
        self.waited[e][st.name] = st.val
        self.ninst += 1

    def _deps(self, e, r, w):
        for k in r:
            t = self.trk.get(k)
            if t is not None and t[0] is not None:
                if not (e == "pe" and t[0].eng == "pe"):
                    self.wait(e, t[0])
        for k in w:
            t = self.trk.get(k)
            if t is not None:
                if t[0] is not None and not (e == "pe" and t[0].eng == "pe"):
                    self.wait(e, t[0])
                for st in t[1].values():
                    if not (e == "pe" and st.eng == "pe"):
                        self.wait(e, st)

    def _mark(self, st, r, w):
        for k in r:
            t = self.trk.setdefault(k, [None, {}])
            t[1][st.name] = st
        for k in w:
            self.trk[k] = [st, {}]

    def op(self, e, fn, r=(), w=()):
        self._deps(e, r, w)
        ins = fn(self.eng[e])
        if self.ecnt[e] >= 30000:
            self._new_esem(e)
        self.ecnt[e] += 1
        sem, name = self.esem[e]
        ins.then_inc(sem, 1)
        st = Stamp(sem, self.ecnt[e], e, name)
        self._mark(st, r, w)
        self.ninst += 1
        return st

    def dma(self, e, fn, r=(), w=()):
        self._deps(e, r, w)
        j = self.dnext
        self.dnext = (self.dnext + 1) % NDS
        self.wait(e, self.dlast[j])
        ins = fn(self.eng[e])
        sem, name = self.dsem[j]
        self.dcnt[j] += 16
        ins.then_inc(sem, 16)
        st = Stamp(sem, self.dcnt[j], "dma", name)
        self.dlast[j] = st
        self._mark(st, r, w)
        self.ninst += 1
        return st

    def barrier(self):
        lasts = []
        for e in self.eng:
            if self.ecnt[e] > 0:
                sem, name = self.esem[e]
                lasts.append(Stamp(sem, self.ecnt[e], e, name))
        for j in range(NDS):
            if self.dlast[j] is not None:
                lasts.append(self.dlast[j])
        for e in self.eng:
            for st in lasts:
                if st.eng == e:
                    continue
                self.wait(e, st)
        self.trk = {}

    def load(self, out, in_, r=(), w=(), e="sp"):
        return self.dma(e, lambda q: q.dma_start(out=out, in_=in_), r=r, w=w)

    def loadT(self, out, in_, r=(), w=(), e="sp"):
        return self.dma(e, lambda q: q.dma_start_transpose(out=out, in_=in_), r=r, w=w)

    def cast_load(self, out, in_, r=(), w=()):
        return self.dma("pool", lambda q: q.dma_start(out=out, in_=in_), r=r, w=w)

    def mm(self, out, lhsT, rhs, start, stop, r=(), w=()):
        return self.op("pe", lambda q: q.matmul(out, lhsT=lhsT, rhs=rhs, start=start, stop=stop), r=r, w=w)

    def act(self, out, in_, func, r=(), w=(), **kw):
        return self.op("act", lambda q: q.activation(out=out, in_=in_, func=func, **kw), r=r, w=w)

    def ts(self, out, in0, s1, s2, op0, op1=None, r=(), w=(), e="dve", **kw):
        if op1 is None:
            return self.op(e, lambda q: q.tensor_scalar(out=out, in0=in0, scalar1=s1, scalar2=None, op0=op0, **kw), r=r, w=w)
        return self.op(e, lambda q: q.tensor_scalar(out=out, in0=in0, scalar1=s1, scalar2=s2, op0=op0, op1=op1, **kw), r=r, w=w)

    def stt(self, out, in0, scalar, in1, op0, op1, r=(), w=(), e="dve"):
        return self.op(e, lambda q: q.scalar_tensor_tensor(out=out, in0=in0, scalar=scalar, in1=in1, op0=op0, op1=op1), r=r, w=w)

    def tt(self, out, in0, in1, op, r=(), w=(), e="dve"):
        return self.op(e, lambda q: q.tensor_tensor(out=out, in0=in0, in1=in1, op=op), r=r, w=w)

    def copy(self, out, in_, r=(), w=(), e="dve"):
        if e == "act":
            return self.op("act", lambda q: q.copy(out=out, in_=in_), r=r, w=w)
        return self.op(e, lambda q: q.tensor_copy(out=out, in_=in_), r=r, w=w)

    def memset(self, ap, v, w=(), e="dve"):
        return self.op(e, lambda q: q.memset(ap, v), w=w)


class Ctx:
    pass


def bcast_rows(ap1d_row, n):
    return ap1d_row.to_broadcast([128, n])


def make_consts(k, c):
    nc = k.nc
    c.ones_f = k.sb([128, 128], F32, "ones_f")
    c.ones_b = k.sb([128, 128], BF16, "ones_b")
    c.tri_incl_f = k.sb([128, 128], F32, "tri_incl")
    c.tri_strict_b = k.sb([128, 128], BF16, "tri_strict")
    c.mask_gt_f = k.sb([128, 128], F32, "mask_gt")
    c.ident_f = k.sb([128, 128], F32, "ident_f")
    c.iota_p = k.sb([128, 1], F32, "iota_p")
    k.memset(c.ones_f[:], 1.0, w=["ones_f"])
    k.memset(c.ones_b[:], 1.0, w=["ones_b"])
    k.op("pool", lambda q: q.affine_select(out=c.tri_incl_f[:], in_=c.ones_f[:], pattern=[[1, 128]],
                                            compare_op=ALU.is_ge, fill=0.0, base=0, channel_multiplier=-1),
         r=["ones_f"], w=["tri_incl"])
    k.op("pool", lambda q: q.affine_select(out=c.tri_strict_b[:], in_=c.ones_b[:], pattern=[[1, 128]],
                                            compare_op=ALU.is_gt, fill=0.0, base=0, channel_multiplier=-1),
         r=["ones_b"], w=["tri_strict"])
    k.op("pool", lambda q: q.affine_select(out=c.mask_gt_f[:], in_=c.ones_f[:], pattern=[[-1, 128]],
                                            compare_op=ALU.is_gt, fill=0.0, base=0, channel_multiplier=1),
         r=["ones_f"], w=["mask_gt"])
    k.op("pool", lambda q: q.affine_select(out=c.ident_f[:], in_=c.ones_f[:], pattern=[[-1, 128]],
                                            compare_op=ALU.is_equal, fill=0.0, base=0, channel_multiplier=1),
         r=["ones_f"], w=["ident_f"])
    c.ident_b = k.sb([128, 128], BF16, "ident_b")
    k.copy(c.ident_b[:], c.ident_f[:], r=["ident_f"], w=["ident_b"])
    k.op("pool", lambda q: q.iota(c.iota_p[:], pattern=[[0, 1]], base=0, channel_multiplier=1,
                                  allow_small_or_imprecise_dtypes=True), w=["iota_p"])


def rsqrt(k, out, in_, scale, eps, r, w):
    k.act(out, in_, AF.Sqrt, r=r, w=w, scale=scale, bias=eps)
    k.op("dve", lambda q: q.reciprocal(out=out, in_=out), r=w, w=w)


def rmsnorm_tile(k, xt, gbc, out, ss, rstd, junk, keys_r, keys_w, tag):
    k.act(junk, xt, AF.Square, r=keys_r, w=[tag + "junk", tag + "ss"], accum_out=ss)
    rsqrt(k, rstd, ss, 1.0 / D, EPS, [tag + "ss"], [tag + "rstd"])
    k.stt(out, xt, rstd, gbc, ALU.mult, ALU.mult, r=list(keys_r) + [tag + "rstd"], w=keys_w)


def norm_to_dram(k, c, src, g_row, dst_bf, T, tag, dst_f32=None):
    with ExitStack() as es:
        k.stack, old = es, k.stack
        gbc = k.sb([128, D], F32, "gbc")
        xt = [k.sb([128, D], F32, "nx%d" % i) for i in range(2)]
        ob = [k.sb([128, D], BF16, "no%d" % i) for i in range(2)]
        of = [k.sb([128, D], F32, "nf%d" % i) for i in range(2)] if dst_f32 is not None else None
        junk = k.sb([128, D], BF16, "njunk")
        ss = k.sb([128, 1], F32, "nss")
        rstd = k.sb([128, 1], F32, "nrstd")
        k.load(gbc[:], bcast_rows(g_row, D), w=[tag + "g"])
        for j in range(T // 128):
            b = j % 2
            k.load(xt[b][:], src[j * 128:(j + 1) * 128, :], w=[tag + "x%d" % b])
            if dst_f32 is not None:
                rmsnorm_tile(k, xt[b][:], gbc[:], of[b][:], ss[:], rstd[:], junk[:],
                             [tag + "x%d" % b, tag + "g"], [tag + "of%d" % b], tag)
                k.copy(ob[b][:], of[b][:], r=[tag + "of%d" % b], w=[tag + "o%d" % b], e="act")
                k.load(dst_f32[j * 128:(j + 1) * 128, :], of[b][:], r=[tag + "of%d" % b])
            else:
                rmsnorm_tile(k, xt[b][:], gbc[:], ob[b][:], ss[:], rstd[:], junk[:],
                             [tag + "x%d" % b, tag + "g"], [tag + "o%d" % b], tag)
            k.load(dst_bf[j * 128:(j + 1) * 128, :], ob[b][:], r=[tag + "o%d" % b])
        k.barrier()
        k.stack = old


def load_actT(k, dst_tile, src_tm, t0, nt, tag, r=()):
    for kc in range(NKC):
        for s0 in range(0, nt, 512):
            n = min(512, nt - s0)
            k.loadT(dst_tile[:, kc, s0:s0 + n], src_tm[t0 + s0:t0 + s0 + n, kc * 128:(kc + 1) * 128],
                    r=r, w=[tag])


def linear_stage(k, aT_src_tm, T, W, col_specs, tag, kdim=D):
    nkc = kdim // 128
    TS = min(T, 2048)
    with ExitStack() as es:
        k.stack, old = es, k.stack
        aT = k.sb([128, nkc, TS], BF16, "aT")
        wb = [k.sb([128, nkc, 512], BF16, "wb%d" % i) for i in range(2)]
        ot = [k.sb([128, 512], F32, "ot%d" % i) for i in range(2)]
        ob = [k.sb([128, 512], BF16, "ob%d" % i) for i in range(2)]
        ofm = [k.sb([128, TS], BF16, "ofm%d" % i) for i in range(2)]
        ps = [k.stack.enter_context(k.nc.psum_tensor(k.name("lps"), [128, 512], F32)) for _ in range(4)]
        blocks = []
        for (c0, ncols, mode, dst, doff, ddt) in col_specs:
            for b0 in range(0, ncols, 512):
                blocks.append((c0 + b0, min(512, ncols - b0), mode, dst, doff + b0, ddt))
        Wv = W.rearrange("(kc p) n -> p kc n", p=128)
        wi = 0
        pi = 0
        oi = 0
        fi = 0
        for st in range(T // TS):
            t0 = st * TS
            for kc in range(nkc):
                for s0 in range(0, TS, 512):
                    k.loadT(aT[:, kc, s0:s0 + 512], aT_src_tm[t0 + s0:t0 + s0 + 512, kc * 128:(kc + 1) * 128],
                            w=[tag + "aT_%d_%d" % (kc, s0 // 512)])
            for (c0, ncols, mode, dst, doff, ddt) in blocks:
                wbuf = wb[wi % 2]
                wkey = tag + "w%d" % (wi % 2)
                wi += 1
                k.cast_load(wbuf[:, :, 0:ncols], Wv[:, :, c0:c0 + ncols], w=[wkey])
                if mode == "tm":
                    for j in range(TS // 128):
                        p = ps[pi % 4]
                        pkey = tag + "ps%d" % (pi % 4)
                        pi += 1
                        for kc in range(nkc):
                            k.mm(p[:, 0:ncols], aT[:, kc, j * 128:(j + 1) * 128], wbuf[:, kc, 0:ncols],
                                 kc == 0, kc == nkc - 1, r=[tag + "aT_%d_%d" % (kc, j // 4), wkey], w=[pkey])
                        if ddt == F32:
                            o = ot[oi % 2]
                            okey = tag + "ot%d" % (oi % 2)
                        else:
                            o = ob[oi % 2]
                            okey = tag + "ob%d" % (oi % 2)
                        oi += 1
                        k.copy(o[:, 0:ncols], p[:, 0:ncols], r=[pkey], w=[okey], e="act")
                        k.load(dst[t0 + j * 128:t0 + (j + 1) * 128, doff:doff + ncols], o[:, 0:ncols], r=[okey])
                else:
                    for m0 in range(0, ncols, 128):
                        o = ofm[fi % 2]
                        okey = tag + "ofm%d" % (fi % 2)
                        fi += 1
                        for n0 in range(0, TS, 512):
                            p = ps[pi % 4]
                            pkey = tag + "ps%d" % (pi % 4)
                            pi += 1
                            for kc in range(nkc):
                                k.mm(p[:, :], wbuf[:, kc, m0:m0 + 128], aT[:, kc, n0:n0 + 512],
                                     kc == 0, kc == nkc - 1, r=[tag + "aT_%d_%d" % (kc, n0 // 512), wkey], w=[pkey])
                            eng = "act" if (n0 // 512) % 2 == 0 else "dve"
                            k.copy(o[:, n0:n0 + 512], p[:, :], r=[pkey], w=[okey], e=eng)
                        k.load(dst[doff + m0:doff + m0 + 128, t0:t0 + TS], o[:, :], r=[okey])
        k.barrier()
        k.stack = old


def conv_stage(k, c, T, xbcT, xbcT2, conv_w, conv_b, scT, sc_w, y_scT):
    with ExitStack() as es:
        k.stack, old = es, k.stack
        cw = k.sb([128, 128], F32, "cw")
        cb = k.sb([128, 32], F32, "cb")
        sw = k.sb([128, 48], F32, "sw")
        craw = k.sb([128, 128], F32, "craw")
        braw = k.sb([32, 128], F32, "braw")
        sraw = k.sb([48, 128], F32, "sraw")
        cps = k.stack.enter_context(k.nc.psum_tensor(k.name("cps"), [128, 512], F32))
        xin = [k.sb([128, 3 + T], BF16, "xin%d" % i) for i in range(3)]
        acc = [k.sb([128, T], F32, "acc%d" % i) for i in range(3)]
        ob = [k.sb([128, T], BF16, "cob%d" % i) for i in range(3)]
        tb = [k.sb([128, T], BF16, "ctb%d" % i) for i in range(2)]
        th = [k.sb([128, T], BF16, "cth%d" % i) for i in range(2)]
        k.load(craw[:], conv_w.rearrange("k (cc p) -> (k cc) p", p=128), w=["craw"])
        k.load(braw[:], conv_b.rearrange("o (cc p) -> (o cc) p", p=128), w=["braw"])
        k.load(sraw[:], sc_w.rearrange("k (cc p) -> (k cc) p", p=128), w=["sraw"])
        k.mm(cps[:, 0:128], craw[:], c.ident_f[:], True, True, r=["craw", "ident_f"], w=["cps"])
        k.mm(cps[:, 128:160], braw[:], c.ident_f[0:32, 0:32], True, True, r=["braw", "ident_f"], w=["cps"])
        k.mm(cps[:, 160:208], sraw[:], c.ident_f[0:48, 0:48], True, True, r=["sraw", "ident_f"], w=["cps"])
        k.copy(cw[:], cps[:, 0:128], r=["cps"], w=["cw"])
        k.copy(cb[:], cps[:, 128:160], r=["cps"], w=["cb"])
        k.copy(sw[:], cps[:, 160:208], r=["cps"], w=["sw"])
        for i in range(3):
            k.memset(xin[i][:, 0:3], 0.0, w=["xin%d" % i])
        for cc in range(32):
            b = cc % 3
            k.load(xin[b][:, 3:3 + T], xbcT[cc * 128:(cc + 1) * 128, :], w=["xin%d" % b])
            ce = "dve"
            k.ts(acc[b][:], xin[b][:, 0:T], cw[:, cc:cc + 1], None, ALU.mult, r=["xin%d" % b, "cw"], w=["acc%d" % b], e=ce)
            for kk in range(1, 4):
                k.stt(acc[b][:], xin[b][:, kk:kk + T], cw[:, kk * 32 + cc:kk * 32 + cc + 1], acc[b][:], ALU.mult, ALU.add,
                      r=["xin%d" % b, "cw", "acc%d" % b], w=["acc%d" % b], e=ce)
            k.act(ob[b][:], acc[b][:], AF.Silu, r=["acc%d" % b, "cb"], w=["cob%d" % b], bias=cb[:, cc:cc + 1])
            k.load(xbcT2[cc * 128:(cc + 1) * 128, :], ob[b][:], r=["cob%d" % b])
        for cc in range(16):
            b = cc % 2
            k.load(tb[b][:], scT[cc * 128:(cc + 1) * 128, :], w=["tb%d" % b])
            k.load(xin[b][:, 3:3 + T], scT[2048 + cc * 128:2048 + (cc + 1) * 128, :], w=["xin%d" % b])
            k.load(th[b][:], scT[4096 + cc * 128:4096 + (cc + 1) * 128, :], w=["th%d" % b])
            k.tt(xin[b][:, 3:3 + T], xin[b][:, 3:3 + T], th[b][:], ALU.mult, r=["xin%d" % b, "th%d" % b], w=["xin%d" % b])
            k.ts(acc[b][:], xin[b][:, 1:1 + T], sw[:, cc:cc + 1], None, ALU.mult, r=["xin%d" % b, "sw"], w=["acc%d" % b])
            for kk in range(1, 3):
                k.stt(acc[b][:], xin[b][:, 1 + kk:1 + kk + T], sw[:, kk * 16 + cc:kk * 16 + cc + 1], acc[b][:], ALU.mult, ALU.add,
                      r=["xin%d" % b, "sw", "acc%d" % b], w=["acc%d" % b])
            k.tt(ob[b][:], acc[b][:], tb[b][:], ALU.mult, r=["acc%d" % b, "tb%d" % b], w=["cob%d" % b])
            k.load(y_scT[cc * 128:(cc + 1) * 128, :], ob[b][:], r=["cob%d" % b])
        k.barrier()
        k.stack = old


def ssd_stage(k, c, T, xbcT2, dt_d, z_d, y_tm, dt_bias, a_log, d_skip, gate_norm):
    NCH = T // 128
    with ExitStack() as es:
        k.stack, old = es, k.stack
        nc = k.nc
        sbt = k.sb
        dtb_bc = sbt([128, 32], F32, "dtb")
        a_bc = sbt([128, 32], F32, "abc")
        dsk_bc = sbt([128, 32], F32, "dsk")
        gn_bc = sbt([128, D], F32, "gnbc")
        k.load(dtb_bc[:], bcast_rows(dt_bias, 32), w=["dtb"])
        k.load(a_bc[:], bcast_rows(a_log, 32), w=["abc"])
        k.load(dsk_bc[:], bcast_rows(d_skip, 32), w=["dsk"])
        k.load(gn_bc[:], bcast_rows(gate_norm, D), w=["gnbc"])
        k.act(a_bc[:], a_bc[:], AF.Exp, r=["abc"], w=["abc"])
        k.ts(a_bc[:], a_bc[:], -1.0, None, ALU.mult, r=["abc"], w=["abc"])
        NB = 2
        xs = [sbt([128, D], BF16, "xs%d" % i) for i in range(NB)]
        Btm = [sbt([128, 1024], BF16, "Btm%d" % i) for i in range(NB)]
        BT = [sbt([128, 8, 128], BF16, "BT%d" % i) for i in range(NB)]
        CT = [sbt([128, 8, 128], BF16, "CT%d" % i) for i in range(NB)]
        dtr = [sbt([128, 32], F32, "dtr%d" % i) for i in range(NB)]
        zt = [sbt([128, D], BF16, "zt%d" % i) for i in range(NB)]
        dtp = sbt([128, 32], F32, "dtp")
        dA = sbt([128, 32], F32, "dA")
        cum = sbt([128, 64], F32, "cum")
        ea = sbt([128, 32], F32, "ea")
        cd = sbt([128, 32], F32, "cd")
        wgt = sbt([128, 32], F32, "wgt")
        G = sbt([128, 128], F32, "G")
        L = [sbt([128, 4, 128], F32, "L%d" % i) for i in range(2)]
        E = [sbt([128, 4, 128], F32, "E%d" % i) for i in range(2)]
        MT = [sbt([128, 4, 128], BF16, "MT%d" % i) for i in range(2)]
        xw = [sbt([128, D], BF16, "xw%d" % i) for i in range(2)]
        xd = sbt([128, D], F32, "xd")
        dskfull = sbt([128, D], F32, "dskfull")
        tg = [sbt([128, 256], F32, "tg%d" % i) for i in range(2)]
        yf = sbt([128, D], F32, "yf")
        sz = sbt([128, D], F32, "sz")
        sq = sbt([128, D], F32, "sq")
        ss8 = sbt([128, 8], F32, "ss8")
        yo = [sbt([128, D], BF16, "yo%d" % i) for i in range(2)]
        state_f = sbt([128, 8, 256], F32, "state_f")
        state_b = sbt([128, 8, 256], BF16, "state_b")
        k.memset(state_f[:], 0.0, w=["state_f%d" % g for g in range(8)])
        k.memset(state_b[:], 0.0, w=["state_b%d" % g for g in range(8)])
        k.copy(dskfull[:].rearrange("p (h e) -> p h e", h=32), dsk_bc[:].unsqueeze(2).to_broadcast([128, 32, 64]),
               r=["dsk"], w=["dskfull"])
        P = lambda nm: k.stack.enter_context(nc.psum_tensor(k.name(nm), [128, 512], F32))
        ps_cum = P("ps_cum")
        ps_cb = [P("ps_cb0"), P("ps_cb1")]
        ps_seg = [P("ps_seg0"), P("ps_seg1")]
        ps_y = [P("ps_y0"), P("ps_y1")]
        ps_st = P("ps_st")

        def issue_loads(ch):
            b = ch % NB
            t0 = ch * 128
            for q4 in range(4):
                k.loadT(xs[b][:, q4 * 512:(q4 + 1) * 512], xbcT2[q4 * 512:(q4 + 1) * 512, t0:t0 + 128],
                        w=["xs%d_%d" % (b, kc) for kc in range(q4 * 4, q4 * 4 + 4)])
            for q4 in range(2):
                k.loadT(Btm[b][:, q4 * 512:(q4 + 1) * 512], xbcT2[2048 + q4 * 512:2048 + (q4 + 1) * 512, t0:t0 + 128],
                        w=["Btm%d_%d" % (b, kc) for kc in range(q4 * 4, q4 * 4 + 4)])
            k.load(BT[b][:], xbcT2[2048:3072, t0:t0 + 128].rearrange("(g n) t -> n g t", n=128), w=["BT%d" % b])
            k.load(CT[b][:], xbcT2[3072:4096, t0:t0 + 128].rearrange("(g n) t -> n g t", n=128), w=["CT%d" % b])
            k.load(dtr[b][:], dt_d[t0:t0 + 128, :], w=["dtr%d" % b])
            k.load(zt[b][:], z_d[t0:t0 + 128, :], w=["zt%d" % b])

        issue_loads(0)
        for ch in range(NCH):
            b = ch % NB
            t0 = ch * 128
            if ch + 1 < NCH:
                issue_loads(ch + 1)
            kBT, kCT = "BT%d" % b, "CT%d" % b
            xkeys = ["xs%d_%d" % (b, kc) for kc in range(16)]
            k.tt(dtp[:], dtr[b][:], dtb_bc[:], ALU.add, r=["dtr%d" % b, "dtb"], w=["dtp"])
            k.act(dtp[:], dtp[:], AF.Exp, r=["dtp"], w=["dtp"])
            k.act(dtp[:], dtp[:], AF.Ln, r=["dtp"], w=["dtp"], bias=1.0)
            k.tt(dA[:], dtp[:], a_bc[:], ALU.mult, r=["dtp", "abc"], w=["dA"])
            k.mm(ps_cum[:, 0:32], c.tri_incl_f[:], dA[:], True, True, r=["tri_incl", "dA"], w=["ps_cum"])
            k.mm(ps_cum[:, 32:64], c.ones_f[:], dA[:], True, True, r=["ones_f", "dA"], w=["ps_cum"])
            k.copy(cum[:], ps_cum[:, 0:64], r=["ps_cum"], w=["cum"])
            k.act(ea[:], cum[:, 0:32], AF.Exp, r=["cum"], w=["ea"])
            k.act(cd[:], cum[:, 32:64], AF.Exp, r=["cum"], w=["cd"])
            k.tt(wgt[:], cum[:, 32:64], cum[:, 0:32], ALU.subtract, r=["cum"], w=["wgt"])
            k.act(wgt[:], wgt[:], AF.Exp, r=["wgt"], w=["wgt"])
            k.tt(wgt[:], wgt[:], dtp[:], ALU.mult, r=["wgt", "dtp"], w=["wgt"])
            xwb = xw[ch % 2]
            xwk = "xw%d" % (ch % 2)
            k.tt(xwb[:].rearrange("p (h e) -> p h e", h=32), xs[b][:].rearrange("p (h e) -> p h e", h=32),
                 wgt[:].unsqueeze(2).to_broadcast([128, 32, 64]), ALU.mult, r=xkeys + ["wgt"], w=[xwk])
            k.tt(xd[:], xs[b][:], dskfull[:], ALU.mult, r=xkeys + ["dskfull"], w=["xd"], e="pool")
            for g in range(8):
                i2 = g % 2
                pcb = ps_cb[i2]
                kpcb = "ps_cb%d" % i2
                py = ps_y[i2]
                kpy = "ps_y%d" % i2
                h0 = 4 * g
                k.mm(pcb[:, 0:128], BT[b][:, g, :], CT[b][:, g, :], True, True, r=[kBT, kCT], w=[kpcb])
                k.tt(G[:], pcb[:, 0:128], c.tri_incl_f[:], ALU.mult, r=[kpcb, "tri_incl"], w=["G"])
                k.tt(L[i2][:], c.mask_gt_f[:].unsqueeze(1).to_broadcast([128, 4, 128]),
                     dA[:, h0:h0 + 4].unsqueeze(2).to_broadcast([128, 4, 128]), ALU.mult, r=["mask_gt", "dA"], w=["L%d" % i2])
                for rr in range(4):
                    k.mm(ps_seg[i2][:, rr * 128:(rr + 1) * 128], L[i2][:, rr, :], c.tri_incl_f[:], True, True,
                         r=["L%d" % i2, "tri_incl"], w=["ps_seg%d" % i2])
                k.act(E[i2][:].rearrange("p h t -> p (h t)"), ps_seg[i2][:, :], AF.Exp, r=["ps_seg%d" % i2], w=["E%d" % i2])
                k.tt(E[i2][:], E[i2][:], G[:].unsqueeze(1).to_broadcast([128, 4, 128]), ALU.mult, r=["E%d" % i2, "G"], w=["E%d" % i2])
                k.tt(MT[i2][:], E[i2][:], dtp[:, h0:h0 + 4].unsqueeze(2).to_broadcast([128, 4, 128]), ALU.mult,
                     r=["E%d" % i2, "dtp"], w=["MT%d" % i2])
                for rr in range(4):
                    h = h0 + rr
                    k.mm(py[:, rr * 64:(rr + 1) * 64], MT[i2][:, rr, :], xs[b][:, h * 64:(h + 1) * 64], True, True,
                         r=["MT%d" % i2, "xs%d_%d" % (b, h // 2)], w=[kpy])
                k.mm(py[:, 256:512], CT[b][:, g, :], state_b[:, g, :], True, True, r=[kCT, "state_b%d" % g], w=[kpy])
                k.mm(ps_st[:, 0:256], Btm[b][:, g * 128:(g + 1) * 128], xwb[:, g * 256:(g + 1) * 256], True, True,
                     r=["Btm%d_%d" % (b, g), xwk], w=["ps_st"])
                k.tt(tg[i2][:].rearrange("p (h e) -> p h e", h=4), py[:, 256:512].rearrange("p (h e) -> p h e", h=4),
                     ea[:, h0:h0 + 4].unsqueeze(2).to_broadcast([128, 4, 64]), ALU.mult, r=[kpy, "ea"], w=["tg%d" % i2])
                k.tt(yf[:, g * 256:(g + 1) * 256], tg[i2][:], py[:, 0:256], ALU.add, r=["tg%d" % i2, kpy], w=["yf"])
                k.tt(state_f[:, g, :].rearrange("p (h e) -> p h e", h=4), state_f[:, g, :].rearrange("p (h e) -> p h e", h=4),
                     cd[:, h0:h0 + 4].unsqueeze(2).to_broadcast([128, 4, 64]), ALU.mult,
                     r=["state_f%d" % g, "cd"], w=["state_f%d" % g], e="pool")
                k.tt(state_f[:, g, :], state_f[:, g, :], ps_st[:, 0:256], ALU.add, r=["state_f%d" % g, "ps_st"], w=["state_f%d" % g])
                k.copy(state_b[:, g, :], state_f[:, g, :], r=["state_f%d" % g], w=["state_b%d" % g], e="act")
            k.tt(yf[:], yf[:], xd[:], ALU.add, r=["yf", "xd"], w=["yf"])
            k.act(sz[:], zt[b][:], AF.Silu, r=["zt%d" % b], w=["sz"])
            k.tt(yf[:], yf[:], sz[:], ALU.mult, r=["yf", "sz"], w=["yf"])
            k.tt(sq[:], yf[:], yf[:], ALU.mult, r=["yf"], w=["sq"])
            k.op("dve", lambda q: q.tensor_reduce(out=ss8[:], in_=sq[:].rearrange("p (g e) -> p g e", g=8),
                                                  axis=AX.X, op=ALU.add), r=["sq"], w=["ss8"])
            rsqrt(k, ss8[:], ss8[:], 1.0 / 256, EPS, ["ss8"], ["ss8"])
            o = yo[ch % 2]
            for g in range(8):
                k.stt(o[:, g * 256:(g + 1) * 256], yf[:, g * 256:(g + 1) * 256], ss8[:, g:g + 1],
                      gn_bc[:, g * 256:(g + 1) * 256], ALU.mult, ALU.mult, r=["yf", "ss8", "gnbc"], w=["yo%d" % (ch % 2)])
            k.load(y_tm[t0:t0 + 128, :], o[:], r=["yo%d" % (ch % 2)])
        k.barrier()
        k.stack = old


def outproj_stage(k, c, T, kin_specs, W, resid, dst, tag):
    nkc = W.shape[0] // 128
    SP = 512
    resident = nkc * D * 2 <= 65536
    with ExitStack() as es:
        k.stack, old = es, k.stack
        if resident:
            wfull = k.sb([128, nkc, D], BF16, "owf")
        else:
            wb = [k.sb([128, nkc, 512], BF16, "owb%d" % i) for i in range(2)]
        mt = [k.sb([128, nkc, SP], BF16, "omt%d" % i) for i in range(2)]
        rt = [k.sb([128, 512], F32, "ort%d" % i) for i in range(2)]
        ot = [k.sb([128, 512], F32, "oot%d" % i) for i in range(2)]
        ps = [k.stack.enter_context(k.nc.psum_tensor(k.name("ops"), [128, 512], F32)) for _ in range(2)]
        Wv = W.rearrange("(kc p) n -> p kc n", p=128)
        it = 0
        si = 0

        def load_span(sp):
            nonlocal si
            sb_ = si % 2
            si += 1
            s0 = sp * SP
            kc = 0
            for (mode, src) in kin_specs:
                if mode == "tm":
                    for q in range(src.shape[1] // 128):
                        k.loadT(mt[sb_][:, kc, :], src[s0:s0 + SP, q * 128:(q + 1) * 128], w=[tag + "mt%d_%d" % (sb_, kc)])
                        kc += 1
                else:
                    n = src.shape[0] // 128
                    k.load(mt[sb_][:, kc:kc + n, :], src[:, s0:s0 + SP].rearrange("(q p) t -> p q t", p=128),
                           w=[tag + "mt%d_%d" % (sb_, q2) for q2 in range(kc, kc + n)])
                    kc += n
            return sb_

        def tile_block(sb_, s0, jj, cb, wtile, wkey):
            nonlocal it
            b = it % 2
            it += 1
            t0 = s0 + jj * 128
            k.load(rt[b][:], resid[t0:t0 + 128, cb * 512:(cb + 1) * 512], w=[tag + "rt%d" % b])
            for kc in range(nkc):
                k.mm(ps[b][:], mt[sb_][:, kc, jj * 128:(jj + 1) * 128], wtile(kc), kc == 0, kc == nkc - 1,
                     r=[tag + "mt%d_%d" % (sb_, kc), wkey], w=[tag + "ps%d" % b])
            k.tt(ot[b][:], ps[b][:], rt[b][:], ALU.add, r=[tag + "ps%d" % b, tag + "rt%d" % b], w=[tag + "ot%d" % b])
            k.load(dst[t0:t0 + 128, cb * 512:(cb + 1) * 512], ot[b][:], r=[tag + "ot%d" % b])

        if resident:
            for cb in range(D // 512):
                k.cast_load(wfull[:, :, cb * 512:(cb + 1) * 512], Wv[:, :, cb * 512:(cb + 1) * 512], w=[tag + "wf%d" % cb])
            for sp in range(T // SP):
                sb_ = load_span(sp)
                for jj in range(SP // 128):
                    for cb in range(D // 512):
                        tile_block(sb_, sp * SP, jj, cb, lambda kc, cb=cb: wfull[:, kc, cb * 512:(cb + 1) * 512], tag + "wf%d" % cb)
        else:
            wi = 0
            for sp in range(T // SP):
                sb_ = load_span(sp)
                for cb in range(D // 512):
                    wbuf = wb[wi % 2]
                    wkey = tag + "w%d" % (wi % 2)
                    wi += 1
                    k.cast_load(wbuf[:], Wv[:, :, cb * 512:(cb + 1) * 512], w=[wkey])
                    for jj in range(SP // 128):
                        tile_block(sb_, sp * SP, jj, cb, lambda kc, wbuf=wbuf: wbuf[:, kc, :], wkey)
        k.barrier()
        k.stack = old


BLK = 128
_FREED = {}


def free_pool_tmps(k, n0):
    import re
    nc = k.nc
    freed = _FREED.setdefault(id(nc), set())
    for i in nc.main_func.blocks[-1].instructions[n0:]:
        for nm in set(re.findall(r"(Pool_tmp[A-Za-z0-9_]*|Pool_Pool_[A-Za-z0-9_]*_snap_[0-9]+)", str(i))):
            if nm not in freed:
                freed.add(nm)
                nc.gpsimd.free_register(bass.RegisterHandle(nm, mybir.EngineType.Pool))


def moe_stage(k, c, T, li, h_in, h_out, I, S):
    NT = T // 128
    NSLOT = ((2 * T + 32 * (BLK - 1)) + BLK - 1) // BLK * BLK
    NBLK = NSLOT // BLK
    assert NBLK <= 128
    nc = k.nc
    with ExitStack() as es0:
        k.stack, old0 = es0, k.stack
        R = k.sb([128, NT, 32], F32, "mR")
        OH1 = k.sb([128, NT, 32], F32, "mOH1")
        OH2 = k.sb([128, NT, 32], F32, "mOH2")
        W12 = k.sb([128, NT, 2], F32, "mW12")
        SI = k.sb([128, NT, 2], I32, "mSI")
        cnt = k.sb([128, 32], F32, "mcnt")
        with ExitStack() as es:
            k.stack = es
            gbc = k.sb([128, D], F32, "gbc")
            wr = k.sb([128, NKC, 36], F32, "wr")
            bias = k.sb([128, 36], F32, "rbias")
            xt = [k.sb([128, D], F32, "mx%d" % i) for i in range(2)]
            of = [k.sb([128, D], F32, "mof%d" % i) for i in range(2)]
            ob = [k.sb([128, D], BF16, "mob%d" % i) for i in range(2)]
            hT = [k.sb([128, NKC, 128], F32, "mhT%d" % i) for i in range(2)]
            junk = k.sb([128, D], BF16, "mjunk")
            ss = k.sb([128, 1], F32, "mss")
            rstd = k.sb([128, 1], F32, "mrstd")
            lg = k.sb([128, 36], F32, "mlg")
            gmx = k.sb([128, 4], F32, "mgmx")
            goh = k.sb([128, 4], F32, "mgoh")
            pen = k.sb([128, 4], F32, "mpen")
            ejk = k.sb([128, 4], F32, "mejk")
            elm = k.sb([128, 32], F32, "melm")
            top8 = k.sb([128, 8], F32, "mtop8")
            sc = k.sb([128, 4], F32, "msc")
            A = k.sb([128, 32], BF16, "mA")
            pst = [k.stack.enter_context(nc.psum_tensor(k.name("mpst"), [128, 512], F32)) for _ in range(4)]
            psl = k.stack.enter_context(nc.psum_tensor(k.name("mpsl"), [128, 512], F32))
            psr = k.stack.enter_context(nc.psum_tensor(k.name("mpsr"), [128, 512], F32))
            k.load(gbc[:], bcast_rows(I["norm_ffn"][li:li + 1, :], D), w=["mg"])
            with nc.allow_non_contiguous_dma(reason="router weights are tiny"):
                k.load(wr[:, :, 0:4], I["moe_w_group"][li].rearrange("(kc p) n -> p kc n", p=128), w=["wr"])
                k.load(wr[:, :, 4:36], I["moe_w_expert"][li].rearrange("(kc p) n -> p kc n", p=128), w=["wr"])
            k.load(bias[:, 0:4], bcast_rows(I["moe_b_group"][li:li + 1, :], 4), w=["rbias"])
            k.load(bias[:, 4:36], bcast_rows(I["moe_b_expert"][li:li + 1, :], 32), w=["rbias"])
            k.memset(cnt[:], 0.0, w=["mcnt"])
            for j in range(NT):
                b = j % 2
                k.load(xt[b][:], h_in[j * 128:(j + 1) * 128, :], w=["mx%d" % b])
                rmsnorm_tile(k, xt[b][:], gbc[:], of[b][:], ss[:], rstd[:], junk[:], ["mx%d" % b, "mg"], ["mof%d" % b], "m")
                k.copy(ob[b][:], of[b][:], r=["mof%d" % b], w=["mob%d" % b], e="act")
                k.load(S.hn[j * 128:(j + 1) * 128, :], ob[b][:], r=["mob%d" % b], w=["hn_%d" % j])
                for q in range(4):
                    for u in range(4):
                        kc = q * 4 + u
                        k.mm(pst[q][:, u * 128:(u + 1) * 128], of[b][:, kc * 128:(kc + 1) * 128], c.ident_f[:], True, True,
                             r=["mof%d" % b, "ident_f"], w=["mpst%d" % q])
                    k.copy(hT[b][:, q * 4:(q + 1) * 4, :], pst[q][:, :].rearrange("p (u t) -> p u t", u=4),
                           r=["mpst%d" % q], w=["mhT%d_%d" % (b, q)], e=("act" if q % 2 == 0 else "dve"))
                for kc in range(NKC):
                    k.mm(psl[:, 0:36], hT[b][:, kc, :], wr[:, kc, :], kc == 0, kc == NKC - 1,
                         r=["mhT%d_%d" % (b, kc // 4), "wr"], w=["mpsl"])
                k.tt(lg[:], psl[:, 0:36], bias[:], ALU.add, r=["mpsl", "rbias"], w=["mlg"])
                k.op("dve", lambda q_: q_.reduce_max(out=gmx[:, 0:1], in_=lg[:, 0:4], axis=AX.X), r=["mlg"], w=["mgmx"])
                k.ts(goh[:], lg[:, 0:4], gmx[:, 0:1], None, ALU.is_equal, r=["mlg", "mgmx"], w=["mgoh"])
                k.ts(gmx[:, 1:2], gmx[:, 0:1], -1.0, None, ALU.mult, r=["mgmx"], w=["mgmx"])
                k.act(ejk[:], lg[:, 0:4], AF.Exp, r=["mlg", "mgmx"], w=["mejk", "mgsum"], bias=gmx[:, 1:2], accum_out=gmx[:, 2:3])
                k.op("dve", lambda q_: q_.reciprocal(out=gmx[:, 3:4], in_=gmx[:, 2:3]), r=["mgsum"], w=["mgprob"])
                k.ts(pen[:], goh[:], 1e30, -1e30, ALU.mult, ALU.add, r=["mgoh"], w=["mpen"])
                for g in range(4):
                    k.ts(elm[:, g * 8:(g + 1) * 8], lg[:, 4 + g * 8:4 + (g + 1) * 8], pen[:, g:g + 1], None, ALU.add,
                         r=["mlg", "mpen"], w=["melm"])
                k.op("dve", lambda q_: q_.max(out=top8[:], in_=elm[:]), r=["melm"], w=["mtop8"])
                k.ts(OH1[:, j, :], elm[:], top8[:, 0:1], None, ALU.is_equal, r=["melm", "mtop8"], w=["mOH1_%d" % j])
                k.ts(OH2[:, j, :], elm[:], top8[:, 1:2], None, ALU.is_equal, r=["melm", "mtop8"], w=["mOH2_%d" % j])
                k.tt(sc[:, 0:1], top8[:, 1:2], top8[:, 0:1], ALU.subtract, r=["mtop8"], w=["msc"])
                k.act(sc[:, 1:2], sc[:, 0:1], AF.Exp, r=["msc"], w=["msc"])
                k.ts(sc[:, 1:2], sc[:, 1:2], 1.0, None, ALU.add, r=["msc"], w=["msc"])
                k.op("dve", lambda q_: q_.reciprocal(out=sc[:, 2:3], in_=sc[:, 1:2]), r=["msc"], w=["msc"])
                k.tt(W12[:, j, 0:1], gmx[:, 3:4], sc[:, 2:3], ALU.mult, r=["mgprob", "msc"], w=["mW12_%d" % j])
                k.tt(W12[:, j, 1:2], gmx[:, 3:4], W12[:, j, 0:1], ALU.subtract, r=["mgprob", "mW12_%d" % j], w=["mW12_%d" % j])
                k.tt(A[:], OH1[:, j, :], OH2[:, j, :], ALU.add, r=["mOH1_%d" % j, "mOH2_%d" % j], w=["mA"])
                k.mm(psr[:, 0:32], c.tri_strict_b[:], A[:], True, True, r=["tri_strict", "mA"], w=["mpsr"])
                k.mm(psr[:, 32:64], c.ones_b[:], A[:], True, True, r=["ones_b", "mA"], w=["mpsr"])
                k.tt(R[:, j, :], psr[:, 0:32], cnt[:], ALU.add, r=["mpsr", "mcnt"], w=["mR_%d" % j])
                k.tt(cnt[:], psr[:, 32:64], cnt[:], ALU.add, r=["mpsr", "mcnt"], w=["mcnt"])
            nb = k.sb([128, 32], F32, "mnb")
            cs = [k.sb([128, 32], F32, "mcs%d" % i) for i in range(2)]
            pstart = k.sb([128, 32], F32, "mpstart")
            tmp = k.sb([128, 32], F32, "mtmp")
            SF = k.sb([128, NT, 2], F32, "mSF")
            be = k.sb([128, 4], F32, "mbe")
            bei = k.sb([128, 1], I32, "mbei")
            k.memset(nb[:], 0.0, w=["mnb"])
            for m in range((T + BLK - 1) // BLK):
                k.stt(nb[:], cnt[:], float(m * BLK), nb[:], ALU.is_gt, ALU.add, r=["mcnt", "mnb"], w=["mnb"])
            k.ts(cs[0][:], nb[:], float(BLK), None, ALU.mult, r=["mnb"], w=["mcs0"])
            k.copy(nb[:], cs[0][:], r=["mcs0"], w=["mnb"])
            cur = 0
            for sh in (1, 2, 4, 8, 16):
                nx = 1 - cur
                k.copy(cs[nx][:, 0:sh], cs[cur][:, 0:sh], r=["mcs%d" % cur], w=["mcs%d" % nx])
                k.tt(cs[nx][:, sh:32], cs[cur][:, sh:32], cs[cur][:, 0:32 - sh], ALU.add, r=["mcs%d" % cur], w=["mcs%d" % nx])
                cur = nx
            pend = cs[cur]
            k.tt(pstart[:], pend[:], nb[:], ALU.subtract, r=["mcs%d" % cur, "mnb"], w=["mpstart"])
            for j in range(NT):
                k.tt(tmp[:], R[:, j, :], pstart[:], ALU.add, r=["mR_%d" % j, "mpstart"], w=["mtmp"])
                k.tt(elm[:], tmp[:], OH1[:, j, :], ALU.mult, r=["mtmp", "mOH1_%d" % j], w=["melm"])
                k.op("dve", lambda q_: q_.reduce_sum(out=SF[:, j, 0:1], in_=elm[:], axis=AX.X), r=["melm"], w=["mSF"])
                k.tt(elm[:], tmp[:], OH2[:, j, :], ALU.mult, r=["mtmp", "mOH2_%d" % j], w=["melm"])
                k.op("dve", lambda q_: q_.reduce_sum(out=SF[:, j, 1:2], in_=elm[:], axis=AX.X), r=["melm"], w=["mSF"])
            k.copy(SI[:], SF[:], r=["mSF"], w=["mSI"])
            k.ts(be[:, 0:1], c.iota_p[:], float(BLK), None, ALU.mult, r=["iota_p"], w=["mbe"])
            k.ts(elm[:], pend[:], be[:, 0:1], None, ALU.is_le, r=["mcs%d" % cur, "mbe"], w=["melm"])
            k.op("dve", lambda q_: q_.reduce_sum(out=be[:, 1:2], in_=elm[:], axis=AX.X), r=["melm"], w=["mbe"])
            k.ts(be[:, 1:2], be[:, 1:2], 31.0, None, ALU.min, r=["mbe"], w=["mbe"])
            k.copy(bei[:], be[:, 1:2], r=["mbe"], w=["mbei"])
            k.load(S.blk_e[:, :], bei[:], r=["mbei"])
            k.ts(be[:, 2:3], c.iota_p[:], float(BLK), float(-BLK), ALU.mult, ALU.add, r=["iota_p"], w=["mbe2"])
            k.ts(elm[:], pend[:], be[:, 2:3], None, ALU.is_le, r=["mcs%d" % cur, "mbe2"], w=["melm"])
            k.op("dve", lambda q_: q_.reduce_sum(out=be[:, 3:4], in_=elm[:], axis=AX.X), r=["melm"], w=["mbe3"])
            k.ts(be[:, 3:4], be[:, 3:4], 31.0, None, ALU.min, r=["mbe3"], w=["mbe3"])
            k.tt(be[:, 3:4], be[:, 3:4], be[:, 1:2], ALU.is_equal, r=["mbe3", "mbe"], w=["mbe3"])
            k.stt(be[:, 3:4], c.iota_p[:], 1.0, be[:, 3:4], ALU.min, ALU.mult, r=["iota_p", "mbe3"], w=["mbe3"])
            bsi = k.sb([128, 1], I32, "mbsi")
            k.copy(bsi[:], be[:, 3:4], r=["mbe3"], w=["mbsi"])
            k.load(S.blk_same[:, :], bsi[:], r=["mbsi"])
            for j in range(NT):
                b = j % 2
                k.load(ob[b][:], S.hn[j * 128:(j + 1) * 128, :], r=["hn_%d" % j], w=["mob%d" % b])
                for kk in range(2):
                    k.dma("pool", lambda q_, j=j, kk=kk, b=b: q_.indirect_dma_start(
                        out=S.xg, out_offset=bass.IndirectOffsetOnAxis(ap=SI[:, j, kk:kk + 1], axis=0),
                        in_=ob[b][:], in_offset=None), r=["mob%d" % b, "mSI"])
            k.barrier()
            k.stack = es0
        mlim = SUBLIM.get("moe", 99)
        with ExitStack() as es:
            if mlim < 2:
                NBLK = 0
            k.stack = es
            xgT = [k.sb([128, NKC, BLK], BF16, "xgT%d" % i) for i in range(2)]
            wg = [k.sb([128, NKC, 512], BF16, "wg%d" % i) for i in range(2)]
            wu = [k.sb([128, NKC, 512], BF16, "wu%d" % i) for i in range(2)]
            wdn = [k.sb([128, 8, 512], BF16, "wdn%d" % i) for i in range(4)]
            hTt = [k.sb([128, 8, BLK], BF16, "hTt%d" % i) for i in range(2)]
            sgt = [k.sb([128, 512], F32, "sgt%d" % i) for i in range(2)]
            hb = [k.sb([128, 512], BF16, "hb%d" % i) for i in range(2)]
            yt = [k.sb([128, D], BF16, "yt%d" % i) for i in range(4)]
            pst = [k.stack.enter_context(nc.psum_tensor(k.name("pst"), [128, 512], F32)) for _ in range(2)]
            psg = [k.stack.enter_context(nc.psum_tensor(k.name("psg"), [128, 512], F32)) for _ in range(2)]
            psu = [k.stack.enter_context(nc.psum_tensor(k.name("psu"), [128, 512], F32)) for _ in range(2)]
            psy = [k.stack.enter_context(nc.psum_tensor(k.name("psy"), [128, 512], F32)) for _ in range(2)]
            if not hasattr(k, "moe_regs"):
                k.moe_regs = [nc.gpsimd.alloc_register(k.name("mreg")) for _ in range(4)]
            ereg, creg, obase, oreg0 = k.moe_regs
            Hg, Hu, Hd = I["_h_moe_w_gate%d" % li], I["_h_moe_w_up%d" % li], I["_h_moe_w_down%d" % li]
            PAT_GU = [[1024, 128], [128 * 1024, NKC], [1, 512]]
            PAT_D = [[2048, 128], [128 * 2048, 8], [1, 512]]

            xgtm = [k.sb([128, BLK // 128, D], BF16, "xgtm%d" % i) for i in range(2)]

            def load_xg(bk):
                bb = bk % 2
                k.load(xgtm[bb][:], S.xg[bk * BLK:(bk + 1) * BLK, :].rearrange("(s p) d -> p s d", p=128), w=["xgtm%d" % bb])

            def transpose_xg(bk):
                nonlocal mi
                bb = bk % 2
                for s_ in range(BLK // 128):
                    for q in range(4):
                        pi = mi % 2
                        mi += 1
                        for u in range(4):
                            kc = q * 4 + u
                            k.mm(pst[pi][:, u * 128:(u + 1) * 128], xgtm[bb][:, s_, kc * 128:(kc + 1) * 128], c.ident_b[:], True, True,
                                 r=["xgtm%d" % bb, "ident_b"], w=["pst%d" % pi])
                        k.copy(xgT[bb][:, q * 4:(q + 1) * 4, s_ * 128:(s_ + 1) * 128], pst[pi][:, :].rearrange("p (u t) -> p u t", u=4),
                               r=["pst%d" % pi], w=["xgT%d_%d" % (bb, kc2) for kc2 in range(q * 4, q * 4 + 4)],
                               e=("act" if q % 2 == 0 else "dve"))

            def wload(buf, key, hnd, pat, off):
                n0_ = len(nc.main_func.blocks[-1].instructions)
                nc.gpsimd.reg_add(oreg0, obase, off)
                same = nc.gpsimd.snap(creg, min_val=0, max_val=1)
                st_ = k.dma("pool", lambda q_: q_.dma_start(out=buf, in_=bass.AP(hnd, oreg0, pat), cond=same < 1,
                                                             bounds_check="skip_entire_dma"), w=[key])
                free_pool_tmps(k, n0_)
                return st_

            mi = 0
            yi = 0
            if NBLK:
                load_xg(0)
                transpose_xg(0)
            for bk in range(NBLK):
                bb = bk % 2
                if bk + 1 < NBLK:
                    load_xg(bk + 1)
                n_ins0 = len(nc.main_func.blocks[-1].instructions)
                nc.gpsimd.reg_load(ereg, S.blk_e[bk:bk + 1, 0:1])
                nc.gpsimd.reg_load(creg, S.blk_same[bk:bk + 1, 0:1])
                nc.gpsimd.reg_mul(obase, ereg, 2048 * 1024)
                free_pool_tmps(k, n_ins0)
                for hq in range(2):
                    wload(wg[hq][:], "wg%d" % hq, Hg, PAT_GU, hq * 512)
                    wload(wu[hq][:], "wu%d" % hq, Hu, PAT_GU, hq * 512)
                for cb in range(4):
                    wload(wdn[cb][:], "wdn%d" % cb, Hd, PAT_D, cb * 512)
                for s_ in range(BLK // 128):
                    for hq in range(2):
                        gb, ub = wg[hq], wu[hq]
                        gk, uk = "wg%d" % hq, "wu%d" % hq
                        pi = mi % 2
                        mi += 1
                        for kc in range(NKC):
                            k.mm(psg[pi][:, :], xgT[bb][:, kc, s_ * 128:(s_ + 1) * 128], gb[:, kc, :], kc == 0, kc == NKC - 1,
                                 r=[gk, "xgT%d_%d" % (bb, kc)], w=["psg%d" % pi])
                        for kc in range(NKC):
                            k.mm(psu[pi][:, :], xgT[bb][:, kc, s_ * 128:(s_ + 1) * 128], ub[:, kc, :], kc == 0, kc == NKC - 1,
                                 r=[uk, "xgT%d_%d" % (bb, kc)], w=["psu%d" % pi])
                        k.act(sgt[pi][:], psg[pi][:, :], AF.Silu, r=["psg%d" % pi], w=["sgt%d" % pi])
                        k.tt(hb[pi][:], sgt[pi][:], psu[pi][:, :], ALU.mult, r=["sgt%d" % pi, "psu%d" % pi], w=["hb%d" % pi])
                        for m in range(4):
                            k.mm(pst[pi][:, m * 128:(m + 1) * 128], hb[pi][:, m * 128:(m + 1) * 128], c.ident_b[:], True, True,
                                 r=["hb%d" % pi, "ident_b"], w=["pst%d" % pi])
                        k.copy(hTt[bb][:, hq * 4:(hq + 1) * 4, s_ * 128:(s_ + 1) * 128],
                               pst[pi][:, :].rearrange("p (m t) -> p m t", m=4), r=["pst%d" % pi],
                               w=["hTt%d_%d" % (bb, ffc) for ffc in range(hq * 4, hq * 4 + 4)], e=("act" if hq == 0 else "dve"))
                if bk + 1 < NBLK:
                    transpose_xg(bk + 1)
                yts = [yt[(yi + s_) % 4] for s_ in range(BLK // 128)]
                ytk = ["yt%d" % ((yi + s_) % 4) for s_ in range(BLK // 128)]
                yi += BLK // 128
                for cb in range(4):
                    db = wdn[cb]
                    dk = "wdn%d" % cb
                    for s_ in range(BLK // 128):
                        pi = mi % 2
                        mi += 1
                        for ffc in range(8):
                            k.mm(psy[pi][:], hTt[bb][:, ffc, s_ * 128:(s_ + 1) * 128], db[:, ffc, :], ffc == 0, ffc == 7,
                                 r=["hTt%d_%d" % (bb, ffc), dk], w=["psy%d" % pi])
                        k.copy(yts[s_][:, cb * 512:(cb + 1) * 512], psy[pi][:], r=["psy%d" % pi], w=[ytk[s_]],
                               e=("act" if s_ % 2 == 0 else "dve"))
                for s_ in range(BLK // 128):
                    k.load(S.yslot[bk * BLK + s_ * 128:bk * BLK + (s_ + 1) * 128, :], yts[s_][:], r=[ytk[s_]])
                free_pool_tmps(k, n_ins0)
            k.barrier()
            k.stack = es0
        with ExitStack() as es:
            k.stack = es
            ht = [k.sb([128, D], F32, "ch%d" % i) for i in range(2)]
            y1 = [k.sb([128, D], BF16, "cy1%d" % i) for i in range(2)]
            y2 = [k.sb([128, D], BF16, "cy2%d" % i) for i in range(2)]
            for j in range(NT if mlim >= 3 else 0):
                b = j % 2
                k.load(ht[b][:], h_in[j * 128:(j + 1) * 128, :], w=["ch%d" % b])
                k.dma("pool", lambda q_, j=j, b=b: q_.indirect_dma_start(
                    out=y1[b][:], out_offset=None, in_=S.yslot,
                    in_offset=bass.IndirectOffsetOnAxis(ap=SI[:, j, 0:1], axis=0)), w=["cy1%d" % b])
                k.dma("pool", lambda q_, j=j, b=b: q_.indirect_dma_start(
                    out=y2[b][:], out_offset=None, in_=S.yslot,
                    in_offset=bass.IndirectOffsetOnAxis(ap=SI[:, j, 1:2], axis=0)), w=["cy2%d" % b])
                k.stt(ht[b][:], y1[b][:], W12[:, j, 0:1], ht[b][:], ALU.mult, ALU.add, r=["cy1%d" % b, "ch%d" % b], w=["ch%d" % b])
                k.stt(ht[b][:], y2[b][:], W12[:, j, 1:2], ht[b][:], ALU.mult, ALU.add, r=["cy2%d" % b, "ch%d" % b], w=["ch%d" % b])
                k.load(h_out[j * 128:(j + 1) * 128, :], ht[b][:], r=["ch%d" % b])
            k.barrier()
            k.stack = es0
        k.stack = old0


def zero_dram(k, dst, rows, cols, dt):
    with ExitStack() as es:
        k.stack, old = es, k.stack
        z = k.sb([128, cols], dt, "zero")
        k.memset(z[:], 0.0, w=["zero"])
        for r0 in range(0, rows, 128):
            k.load(dst[r0:r0 + 128, :], z[:], r=["zero"])
        k.barrier()
        k.stack = old


def ple_stage(k, c, T, li, h_in, h_out, I, S, final_g=None, final_out=None):
    nc = k.nc
    norm_to_dram(k, c, h_in, I["norm_ple"][li:li + 1, :], S.hn, T, "pl")
    with ExitStack() as es:
        k.stack, old = es, k.stack
        wg = k.sb([128, NKC, D], BF16, "plwg")
        wp = k.sb([128, 2, D], BF16, "plwp")
        aT = [k.sb([128, NKC, 512], BF16, "plaT%d" % i) for i in range(2)]
        pf = [k.sb([128, 256], F32, "plpf%d" % i) for i in range(2)]
        pb = [k.sb([128, 256], BF16, "plpb%d" % i) for i in range(2)]
        pT = [k.sb([128, 2, 512], BF16, "plpT%d" % i) for i in range(2)]
        ht = [k.sb([128, D], F32, "plh%d" % i) for i in range(2)]
        gt = [k.sb([128, 512], F32, "plg%d" % i) for i in range(2)]
        psa = [k.stack.enter_context(nc.psum_tensor(k.name("plpsa"), [128, 512], F32)) for _ in range(2)]
        psb = [k.stack.enter_context(nc.psum_tensor(k.name("plpsb"), [128, 512], F32)) for _ in range(2)]
        if final_g is not None:
            fg = k.sb([128, D], F32, "plfg")
            fo = [k.sb([128, D], F32, "plfo%d" % i) for i in range(2)]
            junk = k.sb([128, D], BF16, "pljunk")
            ss = k.sb([128, 1], F32, "plss")
            rstd = k.sb([128, 1], F32, "plrstd")
            k.load(fg[:], bcast_rows(final_g, D), w=["plfg"])
        k.cast_load(wg[:], I["ple_gate"][li].rearrange("(kc p) n -> p kc n", p=128), w=["plwg"])
        k.cast_load(wp[:], I["ple_proj"][li].rearrange("(kc p) n -> p kc n", p=128), w=["plwp"])
        for j in range(T // 128):
            b = j % 2
            k.load(pf[b][:], I["p"][li, j * 128:(j + 1) * 128, :], w=["plpf%d" % b])
            k.copy(pb[b][:], pf[b][:], r=["plpf%d" % b], w=["plpb%d" % b])
            k.load(S.pbf[j * 128:(j + 1) * 128, :], pb[b][:], r=["plpb%d" % b], w=["pbf_%d" % j])
        mi = 0
        SP = 512
        for sp in range(T // SP):
            sb_ = sp % 2
            s0 = sp * SP
            for kc in range(NKC):
                k.loadT(aT[sb_][:, kc, :], S.hn[s0:s0 + SP, kc * 128:(kc + 1) * 128], w=["plaT%d_%d" % (sb_, kc)])
            for q in range(2):
                k.loadT(pT[sb_][:, q, :], S.pbf[s0:s0 + SP, q * 128:(q + 1) * 128],
                        r=["pbf_%d" % jq for jq in range(s0 // 128, (s0 + SP) // 128)], w=["plpT%d_%d" % (sb_, q)])
            for jj in range(SP // 128):
                j = (s0 // 128) + jj
                b = j % 2
                t0 = j * 128
                k.load(ht[b][:], h_in[t0:t0 + 128, :], w=["plh%d" % b])
                for cb in range(4):
                    pi = mi % 2
                    mi += 1
                    for kc in range(NKC):
                        k.mm(psa[pi][:], aT[sb_][:, kc, jj * 128:(jj + 1) * 128], wg[:, kc, cb * 512:(cb + 1) * 512], kc == 0, kc == NKC - 1,
                             r=["plaT%d_%d" % (sb_, kc), "plwg"], w=["plpsa%d" % pi])
                    for q in range(2):
                        k.mm(psb[pi][:], pT[sb_][:, q, jj * 128:(jj + 1) * 128], wp[:, q, cb * 512:(cb + 1) * 512], q == 0, q == 1,
                             r=["plpT%d_%d" % (sb_, q), "plwp"], w=["plpsb%d" % pi])
                    k.act(gt[pi][:], psa[pi][:], AF.Sigmoid, r=["plpsa%d" % pi], w=["plg%d" % pi])
                    k.tt(gt[pi][:], gt[pi][:], psb[pi][:], ALU.mult, r=["plg%d" % pi, "plpsb%d" % pi], w=["plg%d" % pi])
                    k.tt(ht[b][:, cb * 512:(cb + 1) * 512], ht[b][:, cb * 512:(cb + 1) * 512], gt[pi][:], ALU.add,
                         r=["plg%d" % pi, "plh%d" % b], w=["plh%d" % b])
                if final_g is None:
                    k.load(h_out[t0:t0 + 128, :], ht[b][:], r=["plh%d" % b])
                else:
                    rmsnorm_tile(k, ht[b][:], fg[:], fo[b][:], ss[:], rstd[:], junk[:], ["plh%d" % b, "plfg"], ["plfo%d" % b], "plf")
                    k.load(final_out[t0:t0 + 128, :], fo[b][:], r=["plfo%d" % b])
        k.barrier()
        k.stack = old


SCALE = 128.0 ** -0.5
NEGB = -30000.0


def rope_stage(k, c, T, items, ropeC, ropeS, cmp_items=()):
    nc = k.nc
    with ExitStack() as es:
        k.stack, old = es, k.stack
        Cs = k.sb([32, T], F32, "ropeC")
        Ss = k.sb([32, T], F32, "ropeS")
        pa = k.sb([32, 32], F32, "rpa")
        pb = k.sb([32, 32], F32, "rpb")
        Pm = k.sb([32, 32], BF16, "rPm")
        xt = [k.sb([32, T], BF16, "rx%d" % i) for i in range(2)]
        t1 = [k.sb([32, 512], F32, "rt1%d" % i) for i in range(2)]
        t2 = [k.sb([32, 512], F32, "rt2%d" % i) for i in range(2)]
        ot = [k.sb([32, T], BF16, "ro%d" % i) for i in range(2)]
        ps = [k.stack.enter_context(nc.psum_tensor(k.name("rps"), [128, 512], F32)) for _ in range(2)]
        k.load(Cs[:], ropeC, w=["ropeC"])
        k.load(Ss[:], ropeS, w=["ropeS"])
        k.op("pool", lambda q: q.affine_select(out=pa[:], in_=c.ones_f[0:32, 0:32], pattern=[[1, 32]], compare_op=ALU.is_equal,
                                               fill=0.0, base=-16, channel_multiplier=-1), r=["ones_f"], w=["rpa"])
        k.op("pool", lambda q: q.affine_select(out=pb[:], in_=c.ones_f[0:32, 0:32], pattern=[[-1, 32]], compare_op=ALU.is_equal,
                                               fill=0.0, base=-16, channel_multiplier=1), r=["ones_f"], w=["rpb"])
        k.tt(Pm[:], pa[:], pb[:], ALU.subtract, r=["rpa", "rpb"], w=["rPm"])
        it = 0
        pi = 0
        for (ten, row0, Tn, cstep, c0) in [(a, b, T, 1, 0) for (a, b) in items] + [(a, b, n, 16, 31) for (a, b, n) in cmp_items]:
            b = it % 2
            it += 1
            k.load(xt[b][:, 0:Tn], ten[row0:row0 + 32, 0:Tn], w=["rx%d" % b])
            for n0 in range(0, Tn, 512):
                n = min(512, Tn - n0)
                p = pi % 2
                pi += 1
                k.mm(ps[p][0:32, 0:n], Pm[:], xt[b][:, n0:n0 + n], True, True, r=["rPm", "rx%d" % b], w=["rps%d" % p])
                if cstep == 1:
                    cc, sc_ = Cs[:, n0:n0 + n], Ss[:, n0:n0 + n]
                else:
                    cc = Cs[:, c0 + cstep * n0:c0 + cstep * (n0 + n - 1) + 1:cstep]
                    sc_ = Ss[:, c0 + cstep * n0:c0 + cstep * (n0 + n - 1) + 1:cstep]
                k.tt(t1[p][:, 0:n], xt[b][:, n0:n0 + n], cc, ALU.mult, r=["rx%d" % b, "ropeC"], w=["rt1%d" % p])
                k.tt(t2[p][:, 0:n], ps[p][0:32, 0:n], sc_, ALU.mult, r=["rps%d" % p, "ropeS"], w=["rt2%d" % p])
                k.tt(ot[b][:, n0:n0 + n], t1[p][:, 0:n], t2[p][:, 0:n], ALU.add, r=["rt1%d" % p, "rt2%d" % p], w=["ro%d" % b])
            k.load(ten[row0:row0 + 32, 0:Tn], ot[b][:, 0:Tn], r=["ro%d" % b])
        k.barrier()
        k.stack = old


def compress_stage(k, c, T, S, I):
    nc = k.nc
    NC = T // 16 - 1
    with ExitStack() as es:
        k.stack, old = es, k.stack
        w1 = k.sb([128, 32, 128], BF16, "cw1")
        w2 = k.sb([128, 128], BF16, "cw2")
        posr = k.sb([32, 128], F32, "cposr")
        posT = k.sb([128, 32], BF16, "cposT")
        cb = k.sb([128, 1], F32, "ccb")
        xT = k.sb([128, T], BF16, "cxT")
        h1 = k.sb([128, 256], BF16, "ch1")
        okc = k.sb([128, 256], BF16, "cokc")
        ovc = k.sb([128, 2, 128], BF16, "covc")
        ps1 = k.stack.enter_context(nc.psum_tensor(k.name("cps1"), [128, 512], F32))
        ps2 = k.stack.enter_context(nc.psum_tensor(k.name("cps2"), [128, 512], F32))
        ps3 = k.stack.enter_context(nc.psum_tensor(k.name("cps3"), [128, 512], F32))
        k.memset(okc[:], 0.0, w=["cokc"])
        for kv in range(2):
            k.cast_load(w1[:], I["od_cmp_w1"][kv].rearrange("(j d) o -> d j o", d=128), w=["cw1"])
            k.cast_load(w2[:], I["od_cmp_w2"][kv], w=["cw2"])
            k.load(posr[:], I["od_cmp_pos"][kv], w=["cposr"])
            k.mm(ps3[:, 0:32], posr[:], c.ident_f[0:32, 0:32], True, True, r=["cposr", "ident_f"], w=["cps3"])
            k.copy(posT[:], ps3[:, 0:32], r=["cps3"], w=["cposT"])
            for j in range(32):
                k.mm(ps3[:, 64:65], w1[:, j, :], posT[:, j:j + 1], j == 0, j == 31, r=["cw1", "cposT"], w=["cps3"])
            k.copy(cb[:], ps3[:, 64:65], r=["cps3"], w=["ccb"])
            src = S.kcmpT if kv == 0 else S.vcmpT
            for g in range(2):
                k.load(xT[:], src[g * 128:(g + 1) * 128, :], w=["cxT"])
                for j in range(32):
                    k.mm(ps1[:, 0:NC], w1[:, j, :], xT[:, j:j + 16 * (NC - 1) + 1:16], j == 0, j == 31, r=["cw1", "cxT"], w=["cps1"])
                k.act(h1[:, 0:NC], ps1[:, 0:NC], AF.Silu, r=["cps1", "ccb"], w=["ch1"], bias=cb[:, 0:1])
                if kv == 0:
                    k.mm(ps2[:, 0:NC], w2[:], h1[:, 0:NC], True, True, r=["cw2", "ch1"], w=["cps2"])
                    k.copy(okc[:, 0:NC], ps2[:, 0:NC], r=["cps2"], w=["cokc"])
                    k.load(S.kcT[g * 128:(g + 1) * 128, :], okc[:], r=["cokc"])
                else:
                    for h in range(2):
                        n = min(128, NC - h * 128)
                        if n <= 0:
                            continue
                        k.mm(ps2[0:n, h * 128:(h + 1) * 128], h1[:, h * 128:h * 128 + n], w2[:], True, True, r=["cw2", "ch1"], w=["cps2"])
                    k.memset(ovc[:], 0.0, w=["covc"])
                    for h in range(2):
                        n = min(128, NC - h * 128)
                        if n <= 0:
                            continue
                        k.copy(ovc[0:n, h, :], ps2[0:n, h * 128:(h + 1) * 128], r=["cps2"], w=["covc"])
                    k.load(S.vc[:, g * 128:(g + 1) * 128].rearrange("(h p) d -> p h d", p=128), ovc[:], r=["covc"])
        k.barrier()
        k.stack = old


class Attn:
    def __init__(self, k, NV):
        nc = k.nc
        self.k = k
        self.NV = NV
        self.pst = [k.stack.enter_context(nc.psum_tensor(k.name("aps"), [128, 512], F32)) for _ in range(3)]
        self.acc = [k.stack.enter_context(nc.psum_tensor(k.name("aacc"), [128, 512], F32)) for _ in range(4)]
        self.pT = [k.sb([128, 512], BF16, "apT%d" % i) for i in range(4)]
        self.accs = [k.sb([128, NV], F32, "accs%d" % i) for i in range(8)]
        self.ai = 0
        self.si = 0
        self.pi = 0

    def drain(self):
        k = self.k
        out = []
        for a in range(4):
            i = self.ai % 8
            self.ai += 1
            k.copy(self.accs[i][:], self.acc[a][:, 0:self.NV], r=["aacc%d" % a], w=["accs%d" % i], e="act")
            out.append((self.accs[i], "accs%d" % i))
        return out

    def run(self, qT_ap, rq, ktiles):
        k = self.k
        NV = self.NV
        first = [None] * 4
        last = [None] * 4
        for ti, t in enumerate(ktiles):
            for a in range(t["subs"][0], t["subs"][1]):
                if first[a] is None:
                    first[a] = ti
                last[a] = ti
        slots = []

        def qk(ti):
            t = ktiles[ti]
            a0, a1 = t["subs"]
            ps = self.pst[self.si % 3]
            pk = "aps%d" % (self.si % 3)
            self.si += 1
            c0, c1 = a0 * 128, a1 * 128
            k.mm(ps[:, c0:c1], t["kT"], qT_ap[:, c0:c1], True, t.get("bias") is None, r=list(t["rk"]) + list(rq), w=[pk])
            if t.get("bias") is not None:
                bl, br, bkeys = t["bias"]
                k.mm(ps[:, c0:c1], bl, br[:, c0:c1], False, True, r=list(bkeys), w=[pk])
            slots.append((ps, pk))

        for t0_ in range(min(2, len(ktiles))):
            qk(t0_)
        for ti, t in enumerate(ktiles):
            a0, a1 = t["subs"]
            ps, pk = slots[ti]
            if ti + 2 < len(ktiles):
                qk(ti + 2)
            pT = self.pT[self.pi % 4]
            tk = "apT%d" % (self.pi % 4)
            self.pi += 1
            c0, c1 = a0 * 128, a1 * 128
            k.act(pT[:, c0:c1], ps[:, c0:c1], AF.Exp, r=[pk], w=[tk], scale=SCALE)
            if t.get("mask") is not None:
                k.tt(pT[:, c0:c1], pT[:, c0:c1], t["mask"][:, c0:c1], ALU.mult, r=[tk] + list(t["rm"]), w=[tk],
                     e=t.get("meng", "dve"))
            for a in range(a0, a1):
                k.mm(self.acc[a][:, 0:NV], pT[:, a * 128:(a + 1) * 128], t["V"], first[a] == ti, last[a] == ti,
                     r=[tk] + list(t["rv"]), w=["aacc%d" % a])


def make_attn_masks(k, c, m):
    m.caus = k.sb([128, 4, 512], BF16, "mcaus")
    m.win = k.sb([128, 8, 512], BF16, "mwin")
    ones = k.sb([128, 512], BF16, "mones")
    tmp = k.sb([128, 512], BF16, "mtmp")
    k.memset(ones[:], 1.0, w=["mones"])
    m.ones = ones
    for b in range(4):
        k.op("pool", lambda q, b=b: q.affine_select(out=m.caus[:, b, :], in_=ones[:], pattern=[[1, 512]], compare_op=ALU.is_ge,
                                                    fill=0.0, base=-128 * b, channel_multiplier=-1), r=["mones"], w=["mcaus"])
    for cc in range(8):
        k.op("pool", lambda q, cc=cc: q.affine_select(out=tmp[:], in_=ones[:], pattern=[[1, 512]], compare_op=ALU.is_ge,
                                                      fill=0.0, base=-128 * (cc - 4), channel_multiplier=-1), r=["mones"], w=["mtmp"])
        k.op("pool", lambda q, cc=cc: q.affine_select(out=m.win[:, cc, :], in_=tmp[:], pattern=[[-1, 512]], compare_op=ALU.is_ge,
                                                      fill=0.0, base=128 * (cc - 4) + 511, channel_multiplier=1), r=["mtmp"], w=["mwin"])


def nsa_stage(k, c, T, S, I):
    nc = k.nc
    NT = T // 128
    NQB = T // 512
    NC = T // 16 - 1
    NSEL = T // 64
    NTOP = min(16, NSEL)
    with ExitStack() as es0:
        k.stack, old0 = es0, k.stack
        m = Ctx()
        make_attn_masks(k, c, m)
        Eb = k.sb([128, NT, 128], BF16, "Eb")
        onesE = k.sb([128, NT, 128], BF16, "onesE")
        k.memset(onesE[:], 1.0, w=["onesE"])
        tmpE = k.sb([128, NT, 128], BF16, "tmpE")
        k.op("pool", lambda q: q.affine_select(out=tmpE[:], in_=onesE[:], pattern=[[128, NT], [1, 128]], compare_op=ALU.is_ge,
                                               fill=0.0, base=0, channel_multiplier=-64), r=["onesE"], w=["tmpE"])
        k.op("pool", lambda q: q.affine_select(out=Eb[:], in_=tmpE[:], pattern=[[-128, NT], [-1, 128]], compare_op=ALU.is_ge,
                                               fill=0.0, base=63, channel_multiplier=64), r=["tmpE"], w=["Eb"])
        Am = k.sb([128, 2, 64], BF16, "Am")
        tmpA = k.sb([128, 2, 64], BF16, "tmpA")
        k.op("pool", lambda q: q.affine_select(out=tmpA[:], in_=onesE[:, 0, :].rearrange("p (h b) -> p h b", h=2), pattern=[[128, 2], [-4, 64]],
                                               compare_op=ALU.is_ge, fill=0.0, base=1, channel_multiplier=1), r=["onesE"], w=["tmpA"])
        k.op("pool", lambda q: q.affine_select(out=Am[:], in_=tmpA[:], pattern=[[-128, 2], [4, 64]],
                                               compare_op=ALU.is_ge, fill=0.0, base=3, channel_multiplier=-1), r=["tmpA"], w=["Am"])
        selbT = k.sb([64, T], BF16, "selbT")
        gates = k.sb([128, NT, 24], F32, "gates")
        k.load(gates[:], S.gates.rearrange("(j p) n -> p j n", p=128), w=["gates"])
        k.act(gates[:], gates[:], AF.Sigmoid, r=["gates"], w=["gates"])
        for g in range(2):
            with ExitStack() as es:
                k.stack = es
                at = Attn(k, 193)
                kcT = k.sb([128, 256], BF16, "kcT")
                V1 = k.sb([128, 2, 193], BF16, "cV1")
                qT = [k.sb([128, T], BF16, "cqT%d" % i) for i in range(2)]
                cmask = [k.sb([128, 512], BF16, "cmask%d" % i) for i in range(4)]
                psel = k.sb([128, NT, 64], F32, "psel")
                oc = [k.sb([128, 4, 128], F32, "coc%d" % i) for i in range(2)]
                den = k.sb([128, 4], F32, "cden")
                usb = k.sb([128, 64], F32, "cusb")
                k.load(kcT[:], S.kcT[g * 128:(g + 1) * 128, :], w=["kcT"])
                k.memset(V1[:], 0.0, w=["cV1"])
                k.load(V1[:, :, 0:128], S.vc[:, g * 128:(g + 1) * 128].rearrange("(h p) d -> p h d", p=128), w=["cV1"])
                k.memset(V1[:, :, 128:129], 1.0, w=["cV1"])
                k.copy(V1[:, :, 129:193], Am[:], r=["Am", "cV1"], w=["cV1"])
                k.memset(psel[:], 0.0, w=["psel"])
                ci = 0
                for r_ in range(4):
                    h = 4 * g + r_
                    qb = qT[r_ % 2]
                    qk = "cqT%d" % (r_ % 2)
                    k.load(qb[:], S.qT[h * 128:(h + 1) * 128, :], w=[qk])
                    for Q in range(NQB):
                        kts = []
                        for nt in range(2):
                            if 16 * (nt * 128) + 31 > Q * 512 + 511:
                                continue
                            cm = cmask[ci % 4]
                            ck = "cmask%d" % (ci % 4)
                            ci += 1
                            k.op("pool", lambda q, Q=Q, nt=nt, cm=cm: q.affine_select(
                                out=cm[:], in_=m.ones[:], pattern=[[1, 512]], compare_op=ALU.is_ge, fill=0.0,
                                base=512 * Q - 16 * 128 * nt - 31, channel_multiplier=-16), r=["mones"], w=[ck])
                            kts.append(dict(kT=kcT[:, nt * 128:(nt + 1) * 128], rk=["kcT"], V=V1[:, nt, :], rv=["cV1"],
                                            subs=(0, 4), mask=cm, rm=[ck]))
                        at.run(qb[:, Q * 512:(Q + 1) * 512], [qk], kts)
                        dr = at.drain()
                        for a in range(4):
                            j = Q * 4 + a
                            ob = oc[j % 2]
                            okk = "coc%d" % (j % 2)
                            ac, ak = dr[a]
                            k.ts(den[:, 0:1], ac[:, 128:129], 1e-30, None, ALU.max, r=[ak], w=["cden"])
                            k.op("dve", lambda q_: q_.reciprocal(out=den[:, 1:2], in_=den[:, 0:1]), r=["cden"], w=["cden"])
                            k.ts(ob[:, r_, :], ac[:, 0:128], den[:, 1:2], None, ALU.mult, r=[ak, "cden"], w=[okk])
                            k.stt(psel[:, j, :], ac[:, 129:193], den[:, 1:2], psel[:, j, :], ALU.mult, ALU.add,
                                  r=[ak, "cden", "psel"], w=["psel"])
                            k.load(S.ocmp[j * 128:(j + 1) * 128, h * 128:(h + 1) * 128], ob[:, r_, :], r=[okk])
                vm = k.sb([128, 64], F32, "svm")
                fm_ = k.sb([128, 64], F32, "sfm")
                f2 = k.sb([128, 64], F32, "sf2")
                sc = k.sb([128, 64], F32, "ssc")
                sc2 = k.sb([128, 64], F32, "ssc2")
                t8 = k.sb([128, 16], F32, "st8")
                selb = k.sb([128, 64], F32, "sselb")
                pss = k.stack.enter_context(nc.psum_tensor(k.name("spss"), [128, 512], F32)) if False else at.pst[0]
                for j in range(NT):
                    q0 = j * 128
                    k.op("pool", lambda q, q0=q0: q.affine_select(out=vm[:, 0:NSEL], in_=c.ones_f[:, 0:NSEL], pattern=[[-64, NSEL]], compare_op=ALU.is_ge,
                                                                  fill=0.0, base=q0, channel_multiplier=1), r=["ones_f"], w=["svm"])
                    k.op("pool", lambda q, q0=q0: q.affine_select(out=f2[:, 0:NSEL], in_=vm[:, 0:NSEL], pattern=[[64, NSEL]], compare_op=ALU.is_ge,
                                                                  fill=0.0, base=127 - q0, channel_multiplier=-1), r=["svm"], w=["sf2"])
                    k.memset(f2[:, 0:1], 1.0, w=["sf2"], e="pool")
                    k.tt(sc[:, 0:NSEL], psel[:, j, 0:NSEL], vm[:, 0:NSEL], ALU.mult, r=["psel", "svm"], w=["ssc"])
                    k.ts(fm_[:, 0:NSEL], vm[:, 0:NSEL], 1e30, -1e30, ALU.mult, ALU.add, r=["svm"], w=["sfm"])
                    k.tt(sc[:, 0:NSEL], sc[:, 0:NSEL], fm_[:, 0:NSEL], ALU.add, r=["ssc", "sfm"], w=["ssc"])
                    k.stt(sc[:, 0:NSEL], f2[:, 0:NSEL], 1e4, sc[:, 0:NSEL], ALU.mult, ALU.max, r=["sf2", "ssc"], w=["ssc"])
                    if NSEL > NTOP:
                        k.op("dve", lambda q_: q_.max(out=t8[:, 0:8], in_=sc[:, 0:NSEL]), r=["ssc"], w=["st8"])
                        k.op("dve", lambda q_: q_.match_replace(out=sc2[:, 0:NSEL], in_to_replace=t8[:, 0:8], in_values=sc[:, 0:NSEL],
                                                                imm_value=-3e38), r=["ssc", "st8"], w=["ssc2"])
                        k.op("dve", lambda q_: q_.max(out=t8[:, 8:16], in_=sc2[:, 0:NSEL]), r=["ssc2"], w=["st8"])
                        k.ts(selb[:, 0:NSEL], sc[:, 0:NSEL], t8[:, 15:16], None, ALU.is_ge, r=["ssc", "st8"], w=["sselb"])
                        k.ts(selb[:, 0:NSEL], selb[:, 0:NSEL], -NEGB, NEGB, ALU.mult, ALU.add, r=["sselb"], w=["sselb"])
                    else:
                        k.memset(selb[:, 0:NSEL], 0.0, w=["sselb"])
                    k.mm(pss[0:NSEL, 0:128], selb[:, 0:NSEL], c.ident_f[:], True, True, r=["sselb", "ident_f"], w=["aps0"])
                    k.copy(selbT[0:NSEL, q0:q0 + 128], pss[0:NSEL, 0:128], r=["aps0"], w=["selbT"])
                k.barrier()
                k.stack = es0
            with ExitStack() as es:
                k.stack = es
                at = Attn(k, 129)
                ksT = k.sb([128, T], BF16, "ksT")
                kwT = k.sb([128, T], BF16, "kwT")
                Vs = k.sb([128, NT, 129], BF16, "Vs")
                Vw = k.sb([128, NT, 129], BF16, "Vw")
                qT = [k.sb([128, T], BF16, "sqT%d" % i) for i in range(2)]
                osel = [k.sb([128, 128], F32, "osel%d" % i) for i in range(4)]
                ocm = [k.sb([128, 128], F32, "ocm%d" % i) for i in range(2)]
                oo = [k.sb([128, 128], BF16, "oo%d" % i) for i in range(2)]
                den = k.sb([128, 4], F32, "sden")
                k.load(ksT[:], S.kselT[g * 128:(g + 1) * 128, :], w=["ksT"])
                k.load(kwT[:], S.kwinT[g * 128:(g + 1) * 128, :], w=["kwT"])
                k.load(Vs[:, :, 0:128], S.vsel[:, g * 128:(g + 1) * 128].rearrange("(j p) d -> p j d", p=128), w=["Vs"])
                k.load(Vw[:, :, 0:128], S.vwin[:, g * 128:(g + 1) * 128].rearrange("(j p) d -> p j d", p=128), w=["Vw"])
                k.memset(Vs[:, :, 128:129], 1.0, w=["Vs"])
                k.memset(Vw[:, :, 128:129], 1.0, w=["Vw"])
                oi = 0
                for r_ in range(4):
                    h = 4 * g + r_
                    qb = qT[r_ % 2]
                    qk = "sqT%d" % (r_ % 2)
                    k.load(qb[:], S.qT[h * 128:(h + 1) * 128, :], w=[qk])
                    for Q in range(NQB):
                        kts = []
                        for jt in range(4 * Q + 4):
                            b = jt - 4 * Q
                            d_ = dict(kT=ksT[:, jt * 128:(jt + 1) * 128], rk=["ksT"], V=Vs[:, jt, :], rv=["Vs"],
                                      subs=(max(b, 0), 4), bias=(Eb[0:NSEL, jt, :], selbT[0:NSEL, Q * 512:(Q + 1) * 512], ["Eb", "selbT"]))
                            if b >= 0:
                                d_["mask"] = m.caus[:, b, :]
                                d_["rm"] = ["mcaus"]
                            kts.append(d_)
                        at.run(qb[:, Q * 512:(Q + 1) * 512], [qk], kts)
                        dr = at.drain()
                        res = []
                        for a in range(4):
                            ob = osel[a]
                            okk = "osel%d" % a
                            j = Q * 4 + a
                            gi = (4 * g + r_) * 3
                            ac, ak = dr[a]
                            k.op("dve", lambda q_, ac=ac: q_.reciprocal(out=den[:, 0:1], in_=ac[:, 128:129]), r=[ak], w=["sden"])
                            k.tt(den[:, 0:1], den[:, 0:1], gates[:, j, gi + 1:gi + 2], ALU.mult, r=["sden", "gates"], w=["sden"])
                            k.ts(ob[:], ac[:, 0:128], den[:, 0:1], None, ALU.mult, r=[ak, "sden"], w=[okk])
                            res.append((ob, okk))
                        kts = []
                        for cc in range(8):
                            jt = 4 * Q - 4 + cc
                            if jt < 0:
                                continue
                            a0 = max(cc - 4, 0)
                            a1 = min(cc + 1, 4)
                            kts.append(dict(kT=kwT[:, jt * 128:(jt + 1) * 128], rk=["kwT"], V=Vw[:, jt, :], rv=["Vw"], subs=(a0, a1),
                                            mask=m.win[:, cc, :], rm=["mwin"]))
                        at.run(qb[:, Q * 512:(Q + 1) * 512], [qk], kts)
                        dr = at.drain()
                        for a in range(4):
                            j = Q * 4 + a
                            gi = (4 * g + r_) * 3
                            ac, ak = dr[a]
                            ob, okk = res[a]
                            cmb = ocm[a % 2]
                            ckk = "ocm%d" % (a % 2)
                            k.load(cmb[:], S.ocmp[j * 128:(j + 1) * 128, h * 128:(h + 1) * 128], w=[ckk])
                            k.op("dve", lambda q_, ac=ac: q_.reciprocal(out=den[:, 1:2], in_=ac[:, 128:129]), r=[ak], w=["sden"])
                            k.tt(den[:, 1:2], den[:, 1:2], gates[:, j, gi + 2:gi + 3], ALU.mult, r=["sden", "gates"], w=["sden"])
                            k.stt(ob[:], ac[:, 0:128], den[:, 1:2], ob[:], ALU.mult, ALU.add, r=[ak, "sden", okk], w=[okk])
                            k.stt(oo[a % 2][:], cmb[:], gates[:, j, gi:gi + 1], ob[:], ALU.mult, ALU.add, r=[ckk, "gates", okk], w=["oo%d" % (a % 2)])
                            k.load(S.o_tm[j * 128:(j + 1) * 128, h * 128:(h + 1) * 128], oo[a % 2][:], r=["oo%d" % (a % 2)])
                k.barrier()
                k.stack = es0
        k.stack = old0


def diff_stage(k, c, T, S, I, lambda_init):
    nc = k.nc
    NT = T // 128
    NQB = T // 512
    with ExitStack() as es0:
        k.stack, old0 = es0, k.stack
        m = Ctx()
        make_attn_masks(k, c, m)
        lam = k.sb([128, 512], F32, "lam")
        lt = k.sb([128, 256], F32, "lamt")
        ls = k.sb([128, 4], F32, "lams")
        sub_bc = k.sb([128, 256], F32, "subbc")
        k.load(lam[:], I["od_lambda"].rearrange("a d -> (a d)").rearrange("(o n) -> o n", o=1).to_broadcast([128, 512]), w=["lam"])
        k.load(sub_bc[:], bcast_rows(I["od_subln"], 256), w=["subbc"])
        k.ts(sub_bc[:], sub_bc[:], 1.0 - lambda_init, None, ALU.mult, r=["subbc"], w=["subbc"])
        k.tt(lt[:, 0:128], lam[:, 0:128], lam[:, 128:256], ALU.mult, r=["lam"], w=["lamt"])
        k.tt(lt[:, 128:256], lam[:, 256:384], lam[:, 384:512], ALU.mult, r=["lam"], w=["lamt"])
        k.op("dve", lambda q_: q_.reduce_sum(out=ls[:, 0:1], in_=lt[:, 0:128], axis=AX.X), r=["lamt"], w=["lams"])
        k.op("dve", lambda q_: q_.reduce_sum(out=ls[:, 1:2], in_=lt[:, 128:256], axis=AX.X), r=["lamt"], w=["lams"])
        k.act(ls[:, 0:2], ls[:, 0:2], AF.Exp, r=["lams"], w=["lams"])
        k.tt(ls[:, 2:3], ls[:, 1:2], ls[:, 0:1], ALU.subtract, r=["lams"], w=["lams"])
        k.ts(ls[:, 2:3], ls[:, 2:3], -lambda_init, None, ALU.add, r=["lams"], w=["lams"])
        at = Attn(k, 257)
        qT = [k.sb([128, T], BF16, "dqT%d" % i) for i in range(2)]
        kT = [k.sb([128, T], BF16, "dkT%d" % i) for i in range(2)]
        V1 = [k.sb([128, NT, 257], BF16, "dV%d" % i) for i in range(2)]
        o0 = [k.sb([128, 256], F32, "do0%d" % i) for i in range(4)]
        o1 = [k.sb([128, 256], F32, "do1%d" % i) for i in range(2)]
        sq = k.sb([128, 256], F32, "dsq")
        ss = k.sb([128, 2], F32, "dss")
        den = k.sb([128, 2], F32, "dden")
        ob = [k.sb([128, 256], BF16, "dob%d" % i) for i in range(2)]
        oi = 0
        for h in range(4):
            vb = V1[h % 2]
            vk = "dV%d" % (h % 2)
            k.load(vb[:, :, 0:256], S.dv[:, h * 256:(h + 1) * 256].rearrange("(j p) d -> p j d", p=128), w=[vk])
            k.memset(vb[:, :, 256:257], 1.0, w=[vk])
            for mm_ in range(2):
                hh = 2 * h + mm_
                k.load(qT[mm_][:], S.dqT[hh * 128:(hh + 1) * 128, :], w=["dqT%d" % mm_])
                k.load(kT[mm_][:], S.dkT[hh * 128:(hh + 1) * 128, :], w=["dkT%d" % mm_])
            for Q in range(NQB):
                for mm_ in range(2):
                    kts = []
                    for jt in range(4 * Q + 4):
                        b = jt - 4 * Q
                        d_ = dict(kT=kT[mm_][:, jt * 128:(jt + 1) * 128], rk=["dkT%d" % mm_], V=vb[:, jt, :], rv=[vk], subs=(max(b, 0), 4))
                        if b >= 0:
                            d_["mask"] = m.caus[:, b, :]
                            d_["rm"] = ["mcaus"]
                            d_["meng"] = "pool" if b % 2 else "dve"
                        kts.append(d_)
                    at.run(qT[mm_][:, Q * 512:(Q + 1) * 512], ["dqT%d" % mm_], kts)
                    dr = at.drain()
                    for a in range(4):
                        j = Q * 4 + a
                        ac, ak = dr[a]
                        k.op("dve", lambda q_, ac=ac: q_.reciprocal(out=den[:, 0:1], in_=ac[:, 256:257]), r=[ak], w=["dden"])
                        if mm_ == 0:
                            k.ts(o0[a][:], ac[:, 0:256], den[:, 0:1], None, ALU.mult, r=[ak, "dden"], w=["do0%d" % a])
                        else:
                            t1 = o1[a % 2]
                            k.ts(den[:, 0:1], den[:, 0:1], ls[:, 2:3], None, ALU.mult, r=["dden", "lams"], w=["dden"])
                            k.stt(t1[:], ac[:, 0:256], den[:, 0:1], o0[a][:], ALU.mult, ALU.add,
                                  r=[ak, "dden", "do0%d" % a], w=["do1%d" % (a % 2)])
                            k.act(sq[:], t1[:], AF.Square, r=["do1%d" % (a % 2)], w=["dsq", "dss"], accum_out=ss[:, 0:1])
                            rsqrt(k, ss[:, 1:2], ss[:, 0:1], 1.0 / 256, EPS, ["dss"], ["dss2"])
                            o = ob[oi % 2]
                            okk = "dob%d" % (oi % 2)
                            oi += 1
                            k.stt(o[:], t1[:], ss[:, 1:2], sub_bc[:], ALU.mult, ALU.mult, r=["do1%d" % (a % 2), "dss2", "subbc"], w=[okk])
                            k.load(S.o_tm[j * 128:(j + 1) * 128, 1024 + h * 256:1024 + (h + 1) * 256], o[:], r=[okk])
        k.barrier()
        k.stack = old0


IN_SPECS = [
    ("x", None), ("p", None),
    ("norm_mix", (2, 2048)), ("norm_ffn", (2, 2048)), ("norm_ple", (2, 2048)), ("norm_final", (1, 2048)),
    ("ev_w_in", (2048, 12320)), ("ev_conv_w", (4, 4096)), ("ev_conv_b", (1, 4096)), ("ev_dt_bias", (1, 32)),
    ("ev_a_log", (1, 32)), ("ev_d_skip", (1, 32)), ("ev_gate_norm", (1, 2048)), ("ev_sc_w", (3, 2048)),
    ("ev_w_out", (4096, 2048)),
    ("od_w_in", (2048, 5656)), ("od_cmp_pos", (2, 32, 128)), ("od_cmp_w1", (2, 4096, 128)), ("od_cmp_w2", (2, 128, 128)),
    ("od_lambda", (4, 128)), ("od_subln", (1, 256)), ("od_w_out", (2048, 2048)),
    ("moe_w_group", (2, 2048, 4)), ("moe_b_group", (2, 4)), ("moe_w_expert", (2, 2048, 32)), ("moe_b_expert", (2, 32)),
    ("moe_w_gate0", (32, 2048, 1024)), ("moe_w_up0", (32, 2048, 1024)), ("moe_w_down0", (32, 1024, 2048)),
    ("moe_w_gate1", (32, 2048, 1024)), ("moe_w_up1", (32, 2048, 1024)), ("moe_w_down1", (32, 1024, 2048)),
    ("ple_gate", (2, 2048, 2048)), ("ple_proj", (2, 256, 2048)),
    ("rope_cos", None), ("rope_sin", None),
]


SUBLIM = {}


def build_program(T, stages, dbg=(), needed=None):
    nc = bass.Bass("TRN2", target_bir_lowering=False)
    I = {}
    for name, shp in IN_SPECS:
        if needed is not None and name not in needed:
            continue
        if name == "x":
            shp = (T, D)
        elif name == "p":
            shp = (2, T, 256)
        elif name in ("rope_cos", "rope_sin"):
            shp = (32, T)
        hnd = nc.dram_tensor(name, list(shp), F32, kind="ExternalInput")
        I[name] = hnd.ap()
        I["_h_" + name] = hnd
    out = nc.dram_tensor("out", [T, D], F32, kind="ExternalOutput").ap()

    def scr(name, shape, dt):
        kind = "ExternalOutput" if name in dbg else "Internal"
        return nc.dram_tensor(name, list(shape), dt, kind=kind).ap()

    S = Ctx()
    S.hn = scr("hn", [T, D], BF16)
    S.z = scr("z", [T, D], BF16)
    S.xbcT = scr("xbcT", [4096, T], BF16)
    S.xbcT2 = scr("xbcT2", [4096, T], BF16)
    S.dt = scr("dt", [T, 32], F32)
    S.scT = scr("scT", [6144, T], BF16)
    S.y_scT = scr("y_scT", [2048, T], BF16)
    S.y_tm = scr("y_tm", [T, D], BF16)
    S.h1 = scr("h1", [T, D], F32)
    S.h2 = scr("h2", [T, D], F32)
    S.h3 = scr("h3", [T, D], F32)
    NSLOT = ((2 * T + 32 * (BLK - 1)) + BLK - 1) // BLK * BLK
    S.xg = scr("xg", [NSLOT, D], BF16)
    S.yslot = scr("yslot", [NSLOT, D], BF16)
    S.blk_e = scr("blk_e", [128, 1], I32)
    S.blk_same = scr("blk_same", [128, 1], I32)
    S.pbf = scr("pbf", [T, 256], BF16)
    S.h4 = scr("h4", [T, D], F32)
    S.h5 = scr("h5", [T, D], F32)
    S.qT = scr("qT", [1024, T], BF16)
    S.kcmpT = scr("kcmpT", [256, T], BF16)
    S.vcmpT = scr("vcmpT", [256, T], BF16)
    S.kselT = scr("kselT", [256, T], BF16)
    S.vsel = scr("vsel", [T, 256], BF16)
    S.kwinT = scr("kwinT", [256, T], BF16)
    S.vwin = scr("vwin", [T, 256], BF16)
    S.gates = scr("gates", [T, 24], F32)
    S.dqT = scr("dqT", [1024, T], BF16)
    S.dkT = scr("dkT", [1024, T], BF16)
    S.dv = scr("dv", [T, 1024], BF16)
    S.kcT = scr("kcT", [256, 256], BF16)
    S.vc = scr("vc", [256, 256], BF16)
    S.ocmp = scr("ocmp", [T, 1024], F32)
    S.o_tm = scr("o_tm", [T, D], BF16)

    k = K(nc)
    c = Ctx()
    make_consts(k, c)
    if "l0mix" in stages:
        lim = SUBLIM.get("l0mix", 99)
        norm_to_dram(k, c, I["x"], I["norm_mix"][0:1, :], S.hn, T, "n0")
        if lim >= 2:
          linear_stage(k, S.hn, T, I["ev_w_in"], [
            (0, 2048, "tm", S.z, 0, BF16),
            (2048, 4096, "fm", S.xbcT, 0, BF16),
            (6144, 32, "tm", S.dt, 0, F32),
            (6176, 6144, "fm", S.scT, 0, BF16),
        ], "l0in")
        if lim >= 3:
            conv_stage(k, c, T, S.xbcT, S.xbcT2, I["ev_conv_w"], I["ev_conv_b"], S.scT, I["ev_sc_w"], S.y_scT)
        if lim >= 4:
            ssd_stage(k, c, T, S.xbcT2, S.dt, S.z, S.y_tm, I["ev_dt_bias"], I["ev_a_log"], I["ev_d_skip"], I["ev_gate_norm"])
        if lim >= 5:
            outproj_stage(k, c, T, [("tm", S.y_tm), ("fm", S.y_scT)], I["ev_w_out"], I["x"], S.h1, "l0out")
    if "moe0" in stages:
        zero_dram(k, S.xg, NSLOT, D, BF16)
        moe_stage(k, c, T, 0, S.h1, S.h2, I, S)
    if "ple0" in stages:
        ple_stage(k, c, T, 0, S.h2, S.h3, I, S)
    h_l1 = S.h3
    if "l1in_dbg" in stages:
        h_l1 = I["x"]
    if "l1mix" in stages:
        NC_ = T // 16 - 1
        lim = SUBLIM.get("l1mix", 99)
        norm_to_dram(k, c, h_l1, I["norm_mix"][1:2, :], S.hn, T, "n1")
        if lim >= 2:
          linear_stage(k, S.hn, T, I["od_w_in"], [
            (0, 1024, "fm", S.qT, 0, BF16), (1024, 256, "fm", S.kcmpT, 0, BF16), (1280, 256, "fm", S.vcmpT, 0, BF16),
            (1536, 256, "fm", S.kselT, 0, BF16), (1792, 256, "tm", S.vsel, 0, BF16), (2048, 256, "fm", S.kwinT, 0, BF16),
            (2304, 256, "tm", S.vwin, 0, BF16), (2560, 24, "tm", S.gates, 0, F32), (2584, 1024, "fm", S.dqT, 0, BF16),
            (3608, 1024, "fm", S.dkT, 0, BF16), (4632, 1024, "tm", S.dv, 0, BF16)], "l1in")
        items = [(S.qT, h * 128) for h in range(8)] + [(S.kselT, g * 128) for g in range(2)] + \
                [(S.kwinT, g * 128) for g in range(2)] + [(S.dqT, h * 128) for h in range(8)] + [(S.dkT, h * 128) for h in range(8)]
        if lim >= 3:
            rope_stage(k, c, T, items, I["rope_cos"], I["rope_sin"])
            compress_stage(k, c, T, S, I)
            rope_stage(k, c, T, [], I["rope_cos"], I["rope_sin"], cmp_items=[(S.kcT, 0, NC_), (S.kcT, 128, NC_)])
        if lim >= 4:
            nsa_stage(k, c, T, S, I)
        if lim >= 5:
            diff_stage(k, c, T, S, I, 0.8 - 0.6 * math.exp(-0.3 * 1))
        if lim >= 6:
            outproj_stage(k, c, T, [("tm", S.o_tm)], I["od_w_out"], h_l1, S.h4, "l1out")
    if "moe1" in stages:
        if "moe0" not in stages:
            zero_dram(k, S.xg, NSLOT, D, BF16)
        moe_stage(k, c, T, 1, S.h4, S.h5, I, S)
    if "ple1" in stages:
        ple_stage(k, c, T, 1, S.h5, None, I, S, final_g=I["norm_final"], final_out=out)
    if "copy_h1" in stages:
        with ExitStack() as es:
            k.stack, old = es, k.stack
            tl = [k.sb([128, D], F32, "fin%d" % i) for i in range(2)]
            for j in range(T // 128):
                k.load(tl[j % 2][:], S.h1[j * 128:(j + 1) * 128, :], w=["fin%d" % (j % 2)])
                k.load(out[j * 128:(j + 1) * 128, :], tl[j % 2][:], r=["fin%d" % (j % 2)])
            k.barrier()
            k.stack = old
    k.barrier()
    k.stack.close()
    print("instructions:", k.ninst, "sems:", k.nsem)
    return nc


def rope_tables(T):
    half = 16
    inv = (500000.0 ** (-(np.arange(half, dtype=np.float32) / half))).astype(np.float32)
    ang = np.arange(T, dtype=np.float32)[None, :] * np.concatenate([inv, inv])[:, None]
    return np.cos(ang).astype(np.float32), np.sin(ang).astype(np.float32)


ALL_STAGES = ("l0mix", "moe0", "ple0", "l1mix", "moe1", "ple1")
T_FULL = 4096
N_CORES = 4


def _core_inputs(inputs, b):
    m = {}
    for name, shp in IN_SPECS:
        if name in ("rope_cos", "rope_sin"):
            continue
        if name.startswith("moe_w_") and name[-1] in "01" and name[:-1] in ("moe_w_gate", "moe_w_up", "moe_w_down"):
            a = inputs[name[:-1]][int(name[-1])]
        else:
            a = inputs[name]
            if name == "x":
                a = a[b]
            elif name == "p":
                a = a[:, b]
            elif name in ("norm_mix", "norm_ffn", "norm_ple", "moe_w_group", "moe_b_group", "moe_w_expert", "moe_b_expert",
                          "ple_gate", "ple_proj"):
                pass
            elif name == "norm_final":
                a = a.reshape(1, -1)
            else:
                a = a[0]
        a = np.ascontiguousarray(np.asarray(a), dtype=np.float32)
        if shp:
            a = a.reshape(shp)
        m[name] = a
    return m


def kernel(**inputs):
    T = T_FULL
    nc = build_program(T, ALL_STAGES)
    cos, sin = rope_tables(T)
    in_maps = []
    shared = None
    for b in range(N_CORES):
        m = _core_inputs(inputs, b)
        if shared is None:
            shared = m
        else:
            for name in m:
                if name not in ("x", "p"):
                    m[name] = shared[name]
        m["rope_cos"] = cos
        m["rope_sin"] = sin
        in_maps.append(m)
    res = run_bass_kernel_spmd(nc, in_maps, core_ids=list(range(N_CORES)))
    out = np.stack([np.asarray(r["out"], dtype=np.float32) for r in res.results], axis=0)
    return out
```
